# Optimizing a Trainium2 kernel written in Bass

```python
import jax, jax.numpy as jnp
from jax import lax
import numpy as np

D_MODEL = 1024
BATCH = 8
SEQ = 4096
DEPTH = 1
DEC_BATCH = 32
DEC_SEQ = 4
PAST_LEN = 16384
PAGE_SIZE = 128

A_HEADS = 8
A_HD = 64
A_WIDTH = A_HEADS * A_HD
DILATED_BRANCHES = ((128, 1), (512, 4), (2048, 16))
A_MAX_WINDOW = 2048
B_HEADS = 4
B_DK = 128
B_DV = 128
B_WIDTH = B_HEADS * B_DV
B_CHUNK = 64
MIX_IN = 3 * A_WIDTH + 2 * B_HEADS * B_DK + 2 * B_WIDTH
MIX_WIDTH = A_WIDTH + B_WIDTH
N_MEM = 256
X_HEADS = 4
X_HD = 128
X_WIDTH = X_HEADS * X_HD
PEER_HEADS = 8
PEER_NKEYS = 128
PEER_N = PEER_NKEYS * PEER_NKEYS
PEER_QDIM = 256
PEER_HALF = PEER_QDIM // 2
PEER_TOPK = 16
PEER_BLOCK = 256
EPS = 1e-6

kernel_name = 'hymba_dilated_hgrn2_peer_step'


def rms_norm(x, g):
    xf = x.astype(jnp.float32)
    y = xf * lax.rsqrt(jnp.mean(xf * xf, axis=-1, keepdims=True) + EPS)
    return (y * g.astype(jnp.float32)).astype(x.dtype)


def _band_attention(q, k, v, span):
    N, n, H, hd = q.shape
    blk = span
    nb = -(-n // blk)
    pad = nb * blk - n

    def blocks(a):
        return jnp.pad(a, ((0, 0), (0, pad), (0, 0), (0, 0))).reshape(N, nb, blk, H, hd)

    def with_prev(a):
        prev = jnp.concatenate([jnp.zeros_like(a[:, :1]), a[:, :-1]], axis=1)
        return jnp.concatenate([prev, a], axis=2)

    qb = blocks(q)
    kk = with_prev(blocks(k))
    vv = with_prev(blocks(v))
    s = jnp.einsum('nbqhd,nbkhd->nbhqk', qb, kk, preferred_element_type=jnp.float32) * (hd ** -0.5)
    qi = np.arange(blk)[:, None]
    ki = np.arange(2 * blk)[None, :]
    dist = blk + qi - ki
    keypos = (np.arange(nb)[:, None, None] - 1) * blk + ki[None]
    mask = (dist >= 0) & (dist <= span) & (keypos >= 0)
    s = jnp.where(mask[None, :, None], s, -jnp.inf)
    lse = jax.nn.logsumexp(s, axis=-1)
    p = jnp.exp(s - lse[..., None]).astype(v.dtype)
    o = jnp.einsum('nbhqk,nbkhd->nbqhd', p, vv).reshape(N, nb * blk, H, hd)[:, :n]
    lse = lse.transpose(0, 1, 3, 2).reshape(N, nb * blk, H)[:, :n]
    return o, lse


def _combine_branches(outs, lses):
    w = jax.nn.softmax(jnp.stack(lses, axis=0), axis=0)
    return jnp.sum(w[..., None] * jnp.stack(outs, axis=0).astype(jnp.float32), axis=0)


def dilated_attention_prompt(q, k, v):
    B, L, H, hd = q.shape
    outs, lses = [], []
    for window, dil in DILATED_BRANCHES:
        n = L // dil

        def by_residue(a):
            return a.reshape(B, n, dil, H, hd).transpose(0, 2, 1, 3, 4).reshape(B * dil, n, H, hd)

        o, lse = _band_attention(by_residue(q), by_residue(k), by_residue(v), window // dil)
        outs.append(o.reshape(B, dil, n, H, hd).transpose(0, 2, 1, 3, 4).reshape(B, L, H, hd))
        lses.append(lse.reshape(B, dil, n, H).transpose(0, 2, 1, 3).reshape(B, L, H))
    return _combine_branches(outs, lses)


def dilated_attention_sample(q, k_new, v_new, k_buf, v_buf):
    W = k_buf.shape[1]
    T = q.shape[1]
    hd = q.shape[-1]
    k_all = jnp.concatenate([k_buf, k_new], axis=1)
    v_all = jnp.concatenate([v_buf, v_new], axis=1)
    outs, lses = [], []
    for window, dil in DILATED_BRANCHES:
        span = window // dil
        idx = (W + np.arange(T))[:, None] - dil * np.arange(span + 1)[None, :]
        valid = idx >= 0
        idx = np.maximum(idx, 0)
        kg = k_all[:, idx]
        vg = v_all[:, idx]
        s = jnp.einsum('bthd,btshd->bhts', q, kg, preferred_element_type=jnp.float32) * (hd ** -0.5)
        s = jnp.where(valid[None, None], s, -jnp.inf)
        lse = jax.nn.logsumexp(s, axis=-1)
        p = jnp.exp(s - lse[..., None]).astype(v_all.dtype)
        outs.append(jnp.einsum('bhts,btshd->bthd', p, vg))
        lses.append(lse.transpose(0, 2, 1))
    return _combine_branches(outs, lses)


def hgrn2_chunked(q, logf, k, i, s0):
    B, L, H, DK = q.shape
    C = min(B_CHUNK, L)
    nc = -(-L // C)
    pad = nc * C - L

    def chunks(a):
        a = jnp.pad(a, ((0, 0), (0, pad), (0, 0), (0, 0)))
        return jnp.moveaxis(a.reshape(B, nc, C, H, a.shape[-1]), 1, 0)

    tril = np.tril(np.ones((C, C), dtype=bool))

    def step(S, inp):
        qc, lfc, kc, ic = inp
        c = jnp.cumsum(lfc, axis=1)
        o_inter = jnp.einsum('bchk,bhkv->bchv', qc * jnp.exp(c), S)
        diff = c[:, :, None] - c[:, None, :]
        decay = jnp.exp(jnp.where(tril[None, :, :, None, None], diff, -jnp.inf))
        A = jnp.einsum('btshk,bshk->bhts', qc[:, :, None] * decay, kc)
        o_intra = jnp.einsum('bhts,bshv->bthv', A, ic)
        c_last = c[:, -1]
        S_new = jnp.exp(c_last)[..., None] * S + jnp.einsum(
            'bshk,bshv->bhkv', kc * jnp.exp(c_last[:, None] - c), ic)
        return S_new, o_inter + o_intra

    S_fin, o = lax.scan(step, s0.astype(jnp.float32), (chunks(q), chunks(logf), chunks(k), chunks(i)))
    o = jnp.moveaxis(o, 0, 1).reshape(B, nc * C, H, i.shape[-1])[:, :L]
    return o, S_fin


def _mix_proj(h, w_in, lb):
    B, L, _ = h.shape
    splits = np.cumsum([A_WIDTH, A_WIDTH, A_WIDTH, B_HEADS * B_DK, B_HEADS * B_DK, B_WIDTH])
    qa, ka, va, qb, fb, ib, gb = jnp.split(h @ w_in, splits, axis=-1)

    def heads_a(t):
        return t.reshape(B, L, A_HEADS, A_HD)

    f = lb + (1.0 - lb) * jax.nn.sigmoid(fb.astype(jnp.float32))
    logf = jnp.log(f).reshape(B, L, B_HEADS, B_DK)
    kb = (1.0 - f).reshape(B, L, B_HEADS, B_DK)
    qb = jax.nn.silu(qb.astype(jnp.float32)).reshape(B, L, B_HEADS, B_DK)
    ib = ib.astype(jnp.float32).reshape(B, L, B_HEADS, B_DV)
    return heads_a(qa), heads_a(ka), heads_a(va), qb, logf, kb, ib, gb


def _mix_out(oa, ob, gb, beta_a, gnorm_b, w_out):
    B, L = oa.shape[:2]
    oa = rms_norm(oa.reshape(B, L, A_WIDTH), beta_a)
    ob = rms_norm(ob, gnorm_b) * jax.nn.silu(gb.reshape(B, L, B_HEADS, B_DV))
    return jnp.concatenate([oa, ob.reshape(B, L, B_WIDTH)], axis=-1) @ w_out


def mixing(h, lb, w_in, beta_a, gnorm_b, w_out, s0, win_k=None, win_v=None):
    qa, ka, va, qb, logf, kb, ib, gb = _mix_proj(h, w_in, lb)
    if win_k is None:
        oa = dilated_attention_prompt(qa, ka, va)
        keep = min(A_MAX_WINDOW, h.shape[1])
        rows_k, rows_v = ka[:, -keep:], va[:, -keep:]
    else:
        oa = dilated_attention_sample(qa, ka, va, win_k, win_v)
        rows_k, rows_v = ka, va
    ob, s_new = hgrn2_chunked(qb, logf, kb, ib, s0)
    y = _mix_out(oa.astype(h.dtype), ob.astype(h.dtype), gb, beta_a, gnorm_b, w_out)
    return y, rows_k, rows_v, s_new.astype(h.dtype)


def memory_kv(mem, g_mem, w_mk, w_mv):
    B = mem.shape[0]
    m = rms_norm(mem, g_mem)
    return (m @ w_mk).reshape(B, N_MEM, X_HEADS, X_HD), (m @ w_mv).reshape(B, N_MEM, X_HEADS, X_HD)


def cross_attention(h, mk, mv, w_cq, w_co):
    B, L, _ = h.shape
    q = (h @ w_cq).reshape(B, L, X_HEADS, X_HD)
    s = jnp.einsum('blhd,bmhd->bhlm', q, mk, preferred_element_type=jnp.float32) * (X_HD ** -0.5)
    p = jax.nn.softmax(s, axis=-1).astype(mv.dtype)
    o = jnp.einsum('bhlm,bmhd->blhd', p, mv).reshape(B, L, X_WIDTH)
    return o @ w_co


def peer_ffn(h, w_pq, peer_k1, peer_k2, peer_u, peer_v):
    shp = h.shape
    x = h.reshape(-1, D_MODEL)
    T = x.shape[0]
    blk = min(PEER_BLOCK, T)
    nb = -(-T // blk)
    xb = jnp.pad(x, ((0, nb * blk - T), (0, 0))).reshape(nb, blk, D_MODEL)

    def one_block(xc):
        q = (xc @ w_pq).reshape(blk, PEER_HEADS, PEER_QDIM)
        s1 = jnp.einsum('thd,hnd->thn', q[..., :PEER_HALF], peer_k1, preferred_element_type=jnp.float32)
        s2 = jnp.einsum('thd,hnd->thn', q[..., PEER_HALF:], peer_k2, preferred_element_type=jnp.float32)
        v1, i1 = lax.top_k(s1, PEER_TOPK)
        v2, i2 = lax.top_k(s2, PEER_TOPK)
        cand = (v1[..., :, None] + v2[..., None, :]).reshape(blk, PEER_HEADS, PEER_TOPK * PEER_TOPK)
        sc, ci = lax.top_k(cand, PEER_TOPK)
        e = (jnp.take_along_axis(i1, ci // PEER_TOPK, axis=-1) * PEER_NKEYS
             + jnp.take_along_axis(i2, ci % PEER_TOPK, axis=-1))
        g = jax.nn.softmax(sc, axis=-1)
        u = peer_u[e]
        a = jax.nn.gelu(jnp.einsum('td,thkd->thk', xc, u, preferred_element_type=jnp.float32), approximate=False)
        return jnp.einsum('thk,thkd->td', (g * a).astype(xc.dtype), peer_v[e])

    y = lax.map(one_block, xb).reshape(nb * blk, D_MODEL)[:T]
    return y.reshape(shp)


def setup_inputs(seed: int = 0) -> dict:
    key = jax.random.key(seed)
    ks = iter(jax.random.split(key, 40))

    def nrm(shape, scale):
        return jax.random.normal(next(ks), shape, jnp.float32) * scale

    def gain(shape):
        return 1.0 + 0.05 * jax.random.normal(next(ks), shape, jnp.float32)

    w_buf = min(A_MAX_WINDOW, PAST_LEN)
    return {
        'x_prompt': nrm((BATCH, SEQ, D_MODEL), 1.0),
        'x_sample': nrm((DEC_BATCH, DEC_SEQ, D_MODEL), 1.0),
        'cache_swa_k': nrm((DEPTH, DEC_BATCH, w_buf, A_HEADS, A_HD), 1.0),
        'cache_swa_v': nrm((DEPTH, DEC_BATCH, w_buf, A_HEADS, A_HD), 1.0),
        'state_hgrn': nrm((DEPTH, DEC_BATCH, B_HEADS, B_DK, B_DV), 0.5),
        'cache_mem_k': nrm((DEPTH, DEC_BATCH, N_MEM, X_HEADS, X_HD), 1.0),
        'cache_mem_v': nrm((DEPTH, DEC_BATCH, N_MEM, X_HEADS, X_HD), 1.0),
        'mem_prompt': nrm((BATCH, N_MEM, D_MODEL), 1.0),
        'norm_mix': gain((DEPTH, D_MODEL)),
        'w_in': nrm((DEPTH, D_MODEL, MIX_IN), D_MODEL ** -0.5),
        'lb_logits': nrm((DEPTH + 1, B_HEADS * B_DK), 0.5),
        'beta_a': gain((DEPTH, A_WIDTH)),
        'gnorm_b': gain((DEPTH, B_HEADS, B_DV)),
        'w_out': nrm((DEPTH, MIX_WIDTH, D_MODEL), MIX_WIDTH ** -0.5),
        'norm_cross': gain((DEPTH, D_MODEL)),
        'norm_mem': gain((DEPTH, D_MODEL)),
        'w_cq': nrm((DEPTH, D_MODEL, X_WIDTH), D_MODEL ** -0.5),
        'w_mk': nrm((DEPTH, D_MODEL, X_WIDTH), D_MODEL ** -0.5),
        'w_mv': nrm((DEPTH, D_MODEL, X_WIDTH), D_MODEL ** -0.5),
        'w_co': nrm((DEPTH, X_WIDTH, D_MODEL), X_WIDTH ** -0.5),
        'norm_ffn': gain((DEPTH, D_MODEL)),
        'w_pq': nrm((DEPTH, D_MODEL, PEER_HEADS * PEER_QDIM), D_MODEL ** -0.5),
        'peer_k1': nrm((DEPTH, PEER_HEADS, PEER_NKEYS, PEER_HALF), PEER_HALF ** -0.5),
        'peer_k2': nrm((DEPTH, PEER_HEADS, PEER_NKEYS, PEER_HALF), PEER_HALF ** -0.5),
        'peer_u': nrm((DEPTH, PEER_N, D_MODEL), D_MODEL ** -0.5),
        'peer_v': nrm((DEPTH, PEER_N, D_MODEL), PEER_HEADS ** -0.5),
        'norm_final': gain((D_MODEL,)),
    }


def reference(x_prompt, x_sample, cache_swa_k, cache_swa_v, state_hgrn, cache_mem_k, cache_mem_v,
              mem_prompt, norm_mix, w_in, lb_logits, beta_a, gnorm_b, w_out, norm_cross, norm_mem,
              w_cq, w_mk, w_mv, w_co, norm_ffn, w_pq, peer_k1, peer_k2, peer_u, peer_v, norm_final):
    lb_all = jnp.cumsum(jax.nn.softmax(lb_logits.astype(jnp.float32), axis=0), axis=0)
    xp, xs = x_prompt, x_sample
    p_k, p_v, p_s, p_mk, p_mv, s_k, s_v, s_s = [], [], [], [], [], [], [], []
    for l in range(DEPTH):
        s0 = jnp.zeros((xp.shape[0], B_HEADS, B_DK, B_DV), jnp.float32)
        y, rk, rv, st = mixing(rms_norm(xp, norm_mix[l]), lb_all[l], w_in[l], beta_a[l], gnorm_b[l], w_out[l], s0)
        xp = xp + y
        p_k.append(rk)
        p_v.append(rv)
        p_s.append(st)
        y, rk, rv, st = mixing(rms_norm(xs, norm_mix[l]), lb_all[l], w_in[l], beta_a[l], gnorm_b[l], w_out[l],
                               state_hgrn[l], cache_swa_k[l], cache_swa_v[l])
        xs = xs + y
        s_k.append(rk)
        s_v.append(rv)
        s_s.append(st)
        mk, mv = memory_kv(mem_prompt, norm_mem[l], w_mk[l], w_mv[l])
        p_mk.append(mk)
        p_mv.append(mv)
        xp = xp + cross_attention(rms_norm(xp, norm_cross[l]), mk, mv, w_cq[l], w_co[l])
        xs = xs + cross_attention(rms_norm(xs, norm_cross[l]), cache_mem_k[l], cache_mem_v[l], w_cq[l], w_co[l])
        xp = xp + peer_ffn(rms_norm(xp, norm_ffn[l]), w_pq[l], peer_k1[l], peer_k2[l], peer_u[l], peer_v[l])
        xs = xs + peer_ffn(rms_norm(xs, norm_ffn[l]), w_pq[l], peer_k1[l], peer_k2[l], peer_u[l], peer_v[l])
    y_prompt = rms_norm(xp, norm_final)
    y_sample = rms_norm(xs, norm_final)
    return (y_prompt, y_sample, jnp.stack(p_k), jnp.stack(p_v), jnp.stack(p_s), jnp.stack(p_mk),
            jnp.stack(p_mv), jnp.stack(s_k), jnp.stack(s_v), jnp.stack(s_s))
```

```python
import contextlib
import numpy as np
import concourse.bass as bass
import concourse.mybir as mybir
from concourse.bass_utils import run_bass_kernel_spmd

F32 = mybir.dt.float32
BF16 = mybir.dt.bfloat16
U32 = mybir.dt.uint32
AF = mybir.ActivationFunctionType
ALU = mybir.AluOpType
AX = mybir.AxisListType

NCORES = 8
_ATT_LVL = 9
_ATT_MAXB = 10 ** 9
_BRANCHES = (1, 4, 16)
T = 4096
NT = 32
NTT = 33
D = 1024
MIXIN = 3584
EPS = 1e-6


class Sync:
    def __init__(self, nc, es):
        self.nc = nc
        self.eng = {'pe': nc.tensor, 'dve': nc.vector, 'act': nc.scalar, 'pool': nc.gpsimd, 'sp': nc.sync}
        self.sem = {}
        self.cnt = {}
        for e in self.eng:
            self.sem[e] = es.enter_context(nc.semaphore('c_' + e))
            self.cnt[e] = 0
        self.R = 8
        for q in ('sp', 'pool'):
            for r in range(self.R):
                k = ('d', q, r)
                self.sem[k] = es.enter_context(nc.semaphore('d_%s%d' % (q, r)))
                self.cnt[k] = 0
        self.dnext = {'sp': 0, 'pool': 0}
        self.waited = {}
        self.last_w = {}
        self.readers = {}

    def _wait(self, eng, dep):
        k, v = dep
        if k == 'pe' and eng == 'pe':
            return
        if self.waited.get((eng, k), 0) >= v:
            return
        self.eng[eng].wait_ge(self.sem[k], v)
        self.waited[(eng, k)] = v

    def _deps(self, eng, reads, writes):
        deps = {}
        def add(d):
            if d is None:
                return
            if deps.get(d[0], 0) < d[1]:
                deps[d[0]] = d[1]
        for r in reads:
            add(self.last_w.get(r))
        for w in writes:
            add(self.last_w.get(w))
            for d in self.readers.get(w, ()):
                add(d)
        for k, v in deps.items():
            self._wait(eng, (k, v))

    def _record(self, me, reads, writes):
        for r in reads:
            self.readers.setdefault(r, []).append(me)
        for w in writes:
            self.last_w[w] = me
            self.readers[w] = []

    def op(self, eng, inst_fn, reads=(), writes=()):
        reads = [getattr(r, 'k', r) for r in reads]
        writes = [getattr(w, 'k', w) for w in writes]
        self._deps(eng, reads, writes)
        inst = inst_fn(self.eng[eng])
        self.cnt[eng] += 1
        inst.then_inc(self.sem[eng], 1)
        self._record((eng, self.cnt[eng]), reads, writes)

    def dma(self, q, out, in_, reads=(), writes=(), **kw):
        reads = [getattr(r, 'k', r) for r in reads]
        writes = [getattr(w, 'k', w) for w in writes]
        self._deps(q, reads, writes)
        r = self.dnext[q]
        self.dnext[q] = (r + 1) % self.R
        k = ('d', q, r)
        inst = self.eng[q].dma_start(out=out, in_=in_, **kw)
        self.cnt[k] += 16
        inst.then_inc(self.sem[k], 16)
        self._record((k, self.cnt[k]), reads, writes)

    def barrier(self):
        for e in self.eng:
            for k, v in self.cnt.items():
                if v > 0 and k != e:
                    self._wait(e, (k, v))

    def finish(self):
        for k, v in self.cnt.items():
            if v > 0 and k != 'sp':
                self._wait('sp', (k, v))


class Tl:
    def __init__(self, t, k):
        self.t, self.k = t, k

    def __getitem__(self, idx):
        return self.t[idx]


def build_program(debug=False):
    nc = bass.Bass("TRN2", target_bir_lowering=False)

    def din(name, shape, dt=F32):
        return nc.dram_tensor(name, list(shape), dt, kind="ExternalInput").ap()

    def dout(name, shape, dt=F32):
        return nc.dram_tensor(name, list(shape), dt, kind="ExternalOutput").ap()

    def dscr(name, shape, dt=F32):
        return nc.dram_tensor(name, list(shape), dt, kind="ExternalOutput" if debug else "Internal").ap()

    xp = din("xp", [T, D])
    xs = din("xs", [16, D])
    memp = din("memp", [256, D])
    w_in = din("w_in", [D, MIXIN])
    w_mk = din("w_mk", [D, 512])
    w_mv = din("w_mv", [D, 512])
    g_mix = din("g_mix", [128, 8])
    g_mem = din("g_mem", [128, 8])
    ident = din("ident", [128, 128])
    lbl0 = din("lbl0", [128, 512])
    lbl1 = din("lbl1", [128, 512])
    triU_d = din("triU", [128, 128])
    tri4_d = din("tri4", [128, 512])
    rowmask_d = din("rowmask", [128, 1])
    st_h = din("st_h", [4, 4, 128, 128])
    maskC_d = din("maskC", [128, 1024])
    maskP_d = din("maskP", [128, 1024])
    ck = din("ck", [4, 2048, 512])
    cv = din("cv", [4, 2048, 512])
    bmask_d = din("bmask", [8, 528])
    w_out = din("w_out", [D, D])
    g_outc = din("g_outc", [128, 8])
    g_cross = din("g_cross", [128, 8])
    w_cq = din("w_cq", [D, 512])
    w_co = din("w_co", [512, D])
    cmk = din("cmk", [4, 256, 512])
    w_pq = din("w_pq", [D, 2048])
    g_ffn = din("g_ffn", [128, 8])
    g_ffnx = din("g_ffnx", [128, 1024])
    keysT = din("keysT", [128, 2048])
    ut_h = din("ut_h", [128, 128, 1024])
    v_h = din("v_h", [128, 128, 1024])
    g_fin = din("g_fin", [128, D])
    iota16_d = din("iota16", [128, 16])
    iota128_d = din("iota128", [128, 128])
    cmv = din("cmv", [4, 256, 512])
    y_p = dout("y_p", [T, D])
    y_s = dout("y_s", [16, D])
    o_pk = dout("o_pk", [2048, 512])
    o_pv = dout("o_pv", [2048, 512])
    o_ph = dout("o_ph", [4, 128, 128])
    o_mk = dout("o_mk", [256, 512])
    o_mv = dout("o_mv", [256, 512])
    o_sk = dout("o_sk", [16, 512])
    o_sv = dout("o_sv", [16, 512])
    o_sh = dout("o_sh", [4, 4, 128, 128])
    Z = dscr("Z", [NTT * 128, MIXIN])
    OB = dscr("OB", [NTT * 128, 512])
    OA = dscr("OA", [3, NTT * 128, 528])
    X2 = dscr("X2", [NTT * 128, D])
    X3 = dscr("X3", [NTT * 128, D]) if debug else None
    UTb = dscr("UTb", [32, 128, 4096], BF16)
    Vb = dscr("Vb", [32, 128, 4096], BF16)

    with contextlib.ExitStack() as es:
        S = Sync(nc, es)

        cur = [es]

        def sb(name, shape, dt=F32):
            return cur[0].enter_context(nc.sbuf_tensor(name, list(shape), dt))

        def ps(name, shape, dt=F32):
            return cur[0].enter_context(nc.psum_tensor(name, list(shape), dt))

        def tl(name, shape, dt=F32):
            return Tl(sb(name, shape, dt), name)

        def ptl(name, shape, dt=F32):
            return Tl(ps(name, shape, dt), name)

        ident_f = sb("ident_f", [128, 128])
        ident_b = sb("ident_b", [128, 128], BF16)
        gmix = sb("gmix", [128, 8])
        gmem = sb("gmem", [128, 8])
        S.dma('sp', ident_f[:], ident[:, :], writes=['ident_f'])
        S.dma('sp', gmix[:], g_mix[:, :], writes=['gmix'])
        S.dma('sp', gmem[:], g_mem[:, :], writes=['gmem'])
        S.op('dve', lambda e: e.tensor_copy(out=ident_b[:], in_=ident_f[:]), reads=['ident_f'], writes=['ident_b'])

        es_a = contextlib.ExitStack()
        es_a.__enter__()
        cur[0] = es_a
        win_b = sb("win_b", [128, 8 * MIXIN], BF16)
        wmk_b = sb("wmk_b", [128, 8 * 512], BF16)
        wmv_b = sb("wmv_b", [128, 8 * 512], BF16)
        wst = [sb("wst%d" % i, [128, MIXIN]) for i in range(2)]
        for kc in range(8):
            st = wst[kc % 2]
            S.dma('sp', st[:], w_in[kc * 128:(kc + 1) * 128, :], writes=['wst%d' % (kc % 2)])
            S.op('dve' if kc % 2 == 0 else 'pool',
                 lambda e, st=st, kc=kc: e.tensor_scalar(out=win_b[:, kc * MIXIN:(kc + 1) * MIXIN], in0=st[:],
                                                         scalar1=gmix[:, kc:kc + 1], scalar2=None, op0=ALU.mult),
                 reads=['wst%d' % (kc % 2), 'gmix'], writes=['win_b'])
        for kc in range(8):
            st = wst[kc % 2]
            S.dma('sp', st[:, 0:512], w_mk[kc * 128:(kc + 1) * 128, :], writes=['wst%d' % (kc % 2)])
            S.dma('sp', st[:, 512:1024], w_mv[kc * 128:(kc + 1) * 128, :], writes=['wst%d' % (kc % 2)])
            S.op('dve', lambda e, st=st, kc=kc: e.tensor_scalar(out=wmk_b[:, kc * 512:(kc + 1) * 512], in0=st[:, 0:512],
                                                                scalar1=gmem[:, kc:kc + 1], scalar2=None, op0=ALU.mult),
                 reads=['wst%d' % (kc % 2), 'gmem'], writes=['wmk_b'])
            S.op('pool', lambda e, st=st, kc=kc: e.tensor_scalar(out=wmv_b[:, kc * 512:(kc + 1) * 512], in0=st[:, 512:1024],
                                                                 scalar1=gmem[:, kc:kc + 1], scalar2=None, op0=ALU.mult),
                 reads=['wst%d' % (kc % 2), 'gmem'], writes=['wmv_b'])

        xt = [sb("xt%d" % i, [128, D]) for i in range(2)]
        junk = sb("junk", [128, D])
        ss = sb("ss", [128, 1])
        rstd = sb("rstd", [128, 1])
        hb = sb("hb", [128, D], BF16)
        hT = [sb("hT%d" % i, [128, D], BF16) for i in range(2)]
        pT = ps("pT", [128, D], BF16)
        pz = [ps("pz%d" % i, [128, 512]) for i in range(2)]
        zt = [sb("zt%d" % i, [128, MIXIN]) for i in range(2)]

        def front(xtile, xkey, hT_t, hkey):
            S.op('act', lambda e: e.activation(out=junk[:], in_=xtile[:], func=AF.Square, accum_out=ss[:]),
                 reads=[xkey], writes=['junk', 'ss'])
            S.op('dve', lambda e: e.tensor_scalar(out=rstd[:], in0=ss[:], scalar1=1.0 / D, scalar2=EPS,
                                                  op0=ALU.mult, op1=ALU.add), reads=['ss'], writes=['rstd'])
            S.op('act', lambda e: e.sqrt(out=rstd[:], in_=rstd[:]), reads=['rstd'], writes=['rstd'])
            S.op('dve', lambda e: e.reciprocal(out=rstd[:], in_=rstd[:]), reads=['rstd'], writes=['rstd'])
            S.op('dve', lambda e: e.tensor_scalar(out=hb[:], in0=xtile[:], scalar1=rstd[:, 0:1], scalar2=None,
                                                  op0=ALU.mult), reads=[xkey, 'rstd'], writes=['hb'])
            for kc in range(8):
                S.op('pe', lambda e, kc=kc: e.transpose(out=pT[:, kc * 128:(kc + 1) * 128],
                                                        in_=hb[:, kc * 128:(kc + 1) * 128], identity=ident_b[:]),
                     reads=['hb', 'ident_b'], writes=['pT'])
            S.op('act', lambda e: e.copy(out=hT_t[:], in_=pT[:]), reads=['pT'], writes=[hkey])

        for i in range(NTT):
            b = i % 2
            xkey = 'xt%d' % b
            if i < NT:
                S.dma('sp', xt[b][:], xp[i * 128:(i + 1) * 128, :], writes=[xkey])
            else:
                S.op('dve', lambda e, b=b: e.memset(xt[b][:], 0.0), writes=[xkey])
                S.dma('sp', xt[b][0:16, :], xs[:, :], writes=[xkey])
            front(xt[b], xkey, hT[b], 'hT%d' % b)
            for g in range(7):
                pzg = pz[g % 2]
                for kc in range(8):
                    S.op('pe', lambda e, kc=kc, g=g, pzg=pzg, b=b: e.matmul(
                        pzg[:], lhsT=hT[b][:, kc * 128:(kc + 1) * 128],
                        rhs=win_b[:, kc * MIXIN + g * 512: kc * MIXIN + (g + 1) * 512],
                        start=(kc == 0), stop=(kc == 7)),
                        reads=['hT%d' % b, 'win_b'], writes=['pz%d' % (g % 2)])
                if g % 2 == 0:
                    S.op('act', lambda e, g=g, pzg=pzg, b=b: e.copy(out=zt[b][:, g * 512:(g + 1) * 512], in_=pzg[:]),
                         reads=['pz%d' % (g % 2)], writes=['zt%d' % b])
                else:
                    S.op('dve', lambda e, g=g, pzg=pzg, b=b: e.tensor_copy(out=zt[b][:, g * 512:(g + 1) * 512], in_=pzg[:]),
                         reads=['pz%d' % (g % 2)], writes=['zt%d' % b])
            S.dma('pool', Z[i * 128:(i + 1) * 128, :], zt[b][:], reads=['zt%d' % b], writes=[('Z', i)])
            if 16 <= i < NT:
                r0 = (i - 16) * 128
                S.dma('pool', o_pk[r0:r0 + 128, :], zt[b][:, 512:1024], reads=['zt%d' % b])
                S.dma('pool', o_pv[r0:r0 + 128, :], zt[b][:, 1024:1536], reads=['zt%d' % b])
            if i == NT:
                S.dma('pool', o_sk[:, :], zt[b][0:16, 512:1024], reads=['zt%d' % b])
                S.dma('pool', o_sv[:, :], zt[b][0:16, 1024:1536], reads=['zt%d' % b])

        for i in range(2):
            b = i % 2
            xkey = 'xt%d' % b
            S.dma('sp', xt[b][:], memp[i * 128:(i + 1) * 128, :], writes=[xkey])
            front(xt[b], xkey, hT[b], 'hT%d' % b)
            for j, wb in enumerate((wmk_b, wmv_b)):
                pzg = pz[j]
                for kc in range(8):
                    S.op('pe', lambda e, kc=kc, wb=wb, pzg=pzg, b=b: e.matmul(
                        pzg[:], lhsT=hT[b][:, kc * 128:(kc + 1) * 128], rhs=wb[:, kc * 512:(kc + 1) * 512],
                        start=(kc == 0), stop=(kc == 7)),
                        reads=['hT%d' % b, 'wmk_b', 'wmv_b'], writes=['pz%d' % j])
                S.op('act' if j == 0 else 'dve',
                     (lambda e, j=j, pzg=pzg, b=b: e.copy(out=zt[b][:, j * 512:(j + 1) * 512], in_=pzg[:])) if j == 0 else
                     (lambda e, j=j, pzg=pzg, b=b: e.tensor_copy(out=zt[b][:, j * 512:(j + 1) * 512], in_=pzg[:])),
                     reads=['pz%d' % j], writes=['zt%d' % b])
            S.dma('pool', o_mk[i * 128:(i + 1) * 128, :], zt[b][:, 0:512], reads=['zt%d' % b])
            S.dma('pool', o_mv[i * 128:(i + 1) * 128, :], zt[b][:, 512:1024], reads=['zt%d' % b])

        es_a.close()
        cur[0] = es
        S.barrier()
        es_d = contextlib.ExitStack()
        es_d.__enter__()
        cur[0] = es_d
        triU = tl("triU_s", [128, 128])
        tri4 = tl("tri4_s", [128, 512])
        rowmask = tl("rowmask_s", [128, 1])
        lb_t = tl("lb_t", [128, 512])
        oml_t = tl("oml_t", [128, 512])
        l1_t = tl("l1_t", [128, 512])
        S.dma('sp', triU[:], triU_d[:, :], writes=[triU])
        S.dma('sp', tri4[:], tri4_d[:, :], writes=[tri4])
        S.dma('sp', rowmask[:], rowmask_d[:, :], writes=[rowmask])
        S.dma('sp', lb_t[:], lbl0[:, :], writes=[lb_t])
        S.dma('sp', l1_t[:], lbl1[:, :], writes=[l1_t])
        S.op('dve', lambda e: e.tensor_tensor(out=lb_t[:], in0=lb_t[:], in1=l1_t[:], op=ALU.subtract),
             reads=[lb_t, l1_t], writes=[lb_t])
        S.op('act', lambda e: e.activation(out=lb_t[:], in_=lb_t[:], func=AF.Sigmoid), reads=[lb_t], writes=[lb_t])
        S.op('dve', lambda e: e.tensor_scalar(out=oml_t[:], in0=lb_t[:], scalar1=-1.0, scalar2=1.0,
                                              op0=ALU.mult, op1=ALU.add), reads=[lb_t], writes=[oml_t])
        hz = [tl("hz%d" % i, [128, 2048]) for i in range(2)]
        sig = tl("sig", [128, 512])
        logf = tl("logf", [128, 512])
        kk = tl("kk", [128, 512])
        qs = tl("qs", [128, 512])
        gs = tl("gs", [128, 512])
        ib_b = tl("ib_b", [128, 512], BF16)
        ec = tl("ec", [128, 512])
        emc = tl("emc", [128, 512])
        ecl = tl("ecl", [128, 4])
        qd_b = tl("qd_b", [128, 512], BF16)
        kd_b = tl("kd_b", [128, 512], BF16)
        qkT = tl("qkT", [128, 1024], BF16)
        AT_b = tl("AT_b", [128, 512], BF16)
        Sst = tl("Sst", [128, 512])
        S_b = tl("S_b", [128, 512], BF16)
        ssq = tl("ssq", [128, 4])
        rb = tl("rb", [128, 4])
        hjunk = tl("hjunk", [128, 128])
        obn = [tl("obn%d" % i, [128, 512]) for i in range(2)]
        pc = ptl("pc", [128, 512])
        pcT = ptl("pcT", [128, 512])
        pTq = ptl("pTq", [128, 1024], BF16)
        pA = ptl("pA", [128, 512])
        po = ptl("po", [128, 512])
        pdS = ptl("pdS", [128, 512])

        P = 64

        def hs(h):
            return slice(h * 128, (h + 1) * 128)

        def hp(h):
            return slice(h * P, (h + 1) * P)

        def hgrn_chunk(ci, row0, nrows, masked):
            z = hz[ci % 2]
            ob = obn[ci % 2]
            if nrows < P:
                S.op('dve', lambda e: e.memset(z[:], 0.0), writes=[z])
            S.dma('sp', z[0:nrows, :], Z[row0:row0 + nrows, 1536:3584], reads=[('Z', row0 // 128)], writes=[z])
            S.op('act', lambda e: e.activation(out=sig[0:P, :], in_=z[0:P, 512:1024], func=AF.Sigmoid), reads=[z], writes=[sig])
            S.op('dve', lambda e: e.tensor_tensor(out=sig[0:P, :], in0=sig[0:P, :], in1=oml_t[0:P, :], op=ALU.mult),
                 reads=[sig, oml_t], writes=[sig])
            S.op('dve', lambda e: e.tensor_tensor(out=sig[0:P, :], in0=sig[0:P, :], in1=lb_t[0:P, :], op=ALU.add),
                 reads=[sig, lb_t], writes=[sig])
            S.op('act', lambda e: e.activation(out=logf[0:P, :], in_=sig[0:P, :], func=AF.Ln), reads=[sig], writes=[logf])
            S.op('dve', lambda e: e.tensor_scalar(out=kk[0:P, :], in0=sig[0:P, :], scalar1=-1.0, scalar2=1.0,
                                                  op0=ALU.mult, op1=ALU.add), reads=[sig], writes=[kk])
            if masked:
                S.op('dve', lambda e: e.tensor_scalar(out=logf[0:P, :], in0=logf[0:P, :], scalar1=rowmask[0:P, 0:1],
                                                      scalar2=None, op0=ALU.mult), reads=[logf, rowmask], writes=[logf])
                S.op('dve', lambda e: e.tensor_scalar(out=kk[0:P, :], in0=kk[0:P, :], scalar1=rowmask[0:P, 0:1],
                                                      scalar2=None, op0=ALU.mult), reads=[kk, rowmask], writes=[kk])
            S.op('act', lambda e: e.activation(out=qs[0:P, :], in_=z[0:P, 0:512], func=AF.Silu), reads=[z], writes=[qs])
            S.op('act', lambda e: e.activation(out=gs[0:P, :], in_=z[0:P, 1536:2048], func=AF.Silu), reads=[z], writes=[gs])
            S.op('dve', lambda e: e.tensor_copy(out=ib_b[0:P, :], in_=z[0:P, 1024:1536]), reads=[z], writes=[ib_b])
            S.op('pe', lambda e: e.matmul(pc[0:P, :], lhsT=triU[0:P, 0:P], rhs=logf[0:P, :], start=True, stop=True),
                 reads=[triU, logf], writes=[pc])
            for h in range(4):
                S.op('pe', lambda e, h=h: e.matmul(pcT[:, hp(h)], lhsT=logf[0:P, hs(h)], rhs=triU[0:P, 0:P],
                                                   start=True, stop=True), reads=[triU, logf], writes=[pcT])
            S.op('act', lambda e: e.activation(out=ec[0:P, :], in_=pc[0:P, :], func=AF.Exp), reads=[pc], writes=[ec])
            S.op('act', lambda e: e.activation(out=emc[0:P, :], in_=pc[0:P, :], func=AF.Exp, scale=-1.0),
                 reads=[pc], writes=[emc])
            S.op('act', lambda e: e.activation(out=ecl[:], in_=pcT[:, P - 1:4 * P:P], func=AF.Exp), reads=[pcT], writes=[ecl])
            S.op('dve', lambda e: e.tensor_tensor(out=qd_b[0:P, :], in0=qs[0:P, :], in1=ec[0:P, :], op=ALU.mult),
                 reads=[qs, ec], writes=[qd_b])
            S.op('dve', lambda e: e.tensor_tensor(out=kd_b[0:P, :], in0=kk[0:P, :], in1=emc[0:P, :], op=ALU.mult),
                 reads=[kk, emc], writes=[kd_b])
            for h in range(4):
                S.op('pe', lambda e, h=h: e.transpose(out=pTq[:, hp(h)], in_=qd_b[0:P, hs(h)], identity=ident_b[0:P, 0:P]),
                     reads=[qd_b, 'ident_b'], writes=[pTq])
                S.op('pe', lambda e, h=h: e.transpose(out=pTq[:, hp(4 + h)], in_=kd_b[0:P, hs(h)],
                                                      identity=ident_b[0:P, 0:P]), reads=[kd_b, 'ident_b'], writes=[pTq])
            S.op('act', lambda e: e.copy(out=qkT[:, 0:8 * P], in_=pTq[:, 0:8 * P]), reads=[pTq], writes=[qkT])
            for h in range(4):
                S.op('pe', lambda e, h=h: e.matmul(pA[0:P, hp(h)], lhsT=qkT[:, hp(4 + h)],
                                                   rhs=qkT[:, hp(h)], start=True, stop=True), reads=[qkT], writes=[pA])
            S.op('dve', lambda e: e.tensor_tensor(out=AT_b[0:P, 0:4 * P], in0=pA[0:P, 0:4 * P], in1=tri4[0:P, 0:4 * P],
                                                  op=ALU.mult), reads=[pA, tri4], writes=[AT_b])
            for h in range(4):
                S.op('pe', lambda e, h=h: e.matmul(po[0:P, hs(h)], lhsT=AT_b[0:P, hp(h)], rhs=ib_b[0:P, hs(h)],
                                                   start=True, stop=False), reads=[AT_b, ib_b], writes=[po])
                S.op('pe', lambda e, h=h: e.matmul(po[0:P, hs(h)], lhsT=qkT[:, hp(h)], rhs=S_b[:, hs(h)],
                                                   start=False, stop=True), reads=[qkT, S_b], writes=[po])
                S.op('pe', lambda e, h=h: e.matmul(pdS[:, hs(h)], lhsT=kd_b[0:P, hs(h)], rhs=ib_b[0:P, hs(h)],
                                                   start=True, stop=True), reads=[kd_b, ib_b], writes=[pdS])
            S.op('dve', lambda e: e.tensor_tensor(out=Sst[:], in0=Sst[:], in1=pdS[:], op=ALU.add),
                 reads=[Sst, pdS], writes=[Sst])
            for h in range(4):
                S.op('dve', lambda e, h=h: e.tensor_scalar(out=Sst[:, hs(h)], in0=Sst[:, hs(h)], scalar1=ecl[:, h:h + 1],
                                                           scalar2=None, op0=ALU.mult), reads=[Sst, ecl], writes=[Sst])
            S.op('dve', lambda e: e.tensor_copy(out=S_b[:], in_=Sst[:]), reads=[Sst], writes=[S_b])
            for h in range(4):
                S.op('act', lambda e, h=h: e.activation(out=hjunk[0:P, :], in_=po[0:P, hs(h)], func=AF.Square,
                                                        accum_out=ssq[0:P, h:h + 1]), reads=[po], writes=[hjunk, ssq])
            S.op('dve', lambda e: e.tensor_scalar(out=rb[0:P, :], in0=ssq[0:P, :], scalar1=1.0 / 128, scalar2=EPS,
                                                  op0=ALU.mult, op1=ALU.add), reads=[ssq], writes=[rb])
            S.op('act', lambda e: e.sqrt(out=rb[0:P, :], in_=rb[0:P, :]), reads=[rb], writes=[rb])
            S.op('dve', lambda e: e.reciprocal(out=rb[0:P, :], in_=rb[0:P, :]), reads=[rb], writes=[rb])
            for h in range(4):
                S.op('dve', lambda e, h=h: e.tensor_scalar(out=ob[0:P, hs(h)], in0=po[0:P, hs(h)], scalar1=rb[0:P, h:h + 1],
                                                           scalar2=None, op0=ALU.mult), reads=[po, rb], writes=[ob])
            S.op('dve', lambda e: e.tensor_tensor(out=ob[0:P, :], in0=ob[0:P, :], in1=gs[0:P, :], op=ALU.mult),
                 reads=[ob, gs], writes=[ob])
            S.dma('pool', OB[row0:row0 + nrows, :], ob[0:nrows, :], reads=[ob], writes=[('OB', row0 // 128)])

        S.op('dve', lambda e: e.memset(Sst[:], 0.0), writes=[Sst])
        S.op('dve', lambda e: e.memset(S_b[:], 0.0), writes=[S_b])
        for i in range(T // P):
            hgrn_chunk(i, i * P, P, False)
        for h in range(4):
            S.dma('pool', o_ph[h, :, :], Sst[:, hs(h)], reads=[Sst])
        for bb in range(4):
            for h in range(4):
                S.dma('sp', Sst[:, hs(h)], st_h[bb, h, :, :], writes=[Sst])
            S.op('dve', lambda e: e.tensor_copy(out=S_b[:], in_=Sst[:]), reads=[Sst], writes=[S_b])
            hgrn_chunk(bb, T + 4 * bb, 4, True)
            for h in range(4):
                S.dma('pool', o_sh[bb, h, :, :], Sst[:, hs(h)], reads=[Sst])
        es_d.close()
        cur[0] = es
        S.barrier()
        es_c = contextlib.ExitStack()
        es_c.__enter__()
        cur[0] = es_c
        mstage = tl("mstage", [128, 1024])
        maskC = tl("maskC_s", [128, 1024], BF16)
        maskP = tl("maskP_s", [128, 1024], BF16)
        S.dma('sp', mstage[:], maskC_d[:, :], writes=[mstage])
        S.op('dve', lambda e: e.tensor_copy(out=maskC[:], in_=mstage[:]), reads=[mstage], writes=[maskC])
        S.dma('sp', mstage[:], maskP_d[:, :], writes=[mstage])
        S.op('dve', lambda e: e.tensor_copy(out=maskP[:], in_=mstage[:]), reads=[mstage], writes=[maskP])
        az = [tl("az%d" % i, [128, 1536]) for i in range(2)]
        qk_b = tl("qk_b", [128, 1024], BF16)
        vaug = [tl("vaug%d" % i, [128, 8 * 66], BF16) for i in range(2)]
        qT = tl("qT", [64, 1024], BF16)
        kT = [tl("kT%d" % i, [64, 1024], BF16) for i in range(2)]
        PTc = tl("PTc", [128, 1024], BF16)
        PTp = tl("PTp", [128, 1024], BF16)
        osb = [tl("osb%d" % i, [128, 528]) for i in range(2)]
        pTt = ptl("pTt", [128, 1024], BF16)
        pTk = ptl("pTk", [128, 1024], BF16)
        psC = [ptl("psC%d" % i, [128, 512]) for i in range(2)]
        psP = [ptl("psP%d" % i, [128, 512]) for i in range(2)]
        pO = [ptl("pO%d" % i, [128, 512]) for i in range(2)]
        for i in range(2):
            S.op('dve', lambda e, i=i: e.memset(vaug[i][:], 1.0), writes=[vaug[i]])
        blk = 0
        for br, dil in enumerate(_BRANCHES):
            nb = T // dil // 128
            if dil == 1:
                Zv = Z[0:T, :].rearrange("(o m) c -> o m c", o=1)
                OAv = OA[br, 0:T, :].rearrange("(o m) c -> o m c", o=1)
            else:
                Zv = Z[0:T, :].rearrange("(m d) c -> d m c", d=dil)
                OAv = OA[br, 0:T, :].rearrange("(m d) c -> d m c", d=dil)
            for r in range(dil):
                for b in range(nb):
                    cb = blk % 2
                    pb = 1 - cb
                    a = az[cb]
                    S.dma('sp', a[:], Zv[r, b * 128:(b + 1) * 128, 0:1536], reads=[('Z', i) for i in range(NT)] if blk == 0 else [],
                          writes=[a])
                    S.op('dve', lambda e, a=a: e.tensor_copy(out=qk_b[:], in_=a[:, 0:1024]), reads=[a], writes=[qk_b])
                    S.op('act', lambda e, a=a, cb=cb: e.copy(
                        out=vaug[cb][:].rearrange("p (h e) -> p h e", e=66)[:, :, 0:64],
                        in_=a[:, 1024:1536].rearrange("p (h e) -> p h e", e=64)), reads=[a], writes=[vaug[cb]])
                    if blk >= _ATT_MAXB or _ATT_LVL < 2:
                        blk += 1
                        continue
                    for h in range(8):
                        S.op('pe', lambda e, h=h: e.transpose(out=pTt[0:64, h * 128:(h + 1) * 128],
                                                              in_=qk_b[:, h * 64:(h + 1) * 64], identity=ident_b[:]),
                             reads=[qk_b, 'ident_b'], writes=[pTt])
                        S.op('pe', lambda e, h=h: e.transpose(out=pTk[0:64, h * 128:(h + 1) * 128],
                                                              in_=qk_b[:, 512 + h * 64:512 + (h + 1) * 64],
                                                              identity=ident_b[:]),
                             reads=[qk_b, 'ident_b'], writes=[pTk])
                    S.op('act', lambda e: e.copy(out=qT[0:64, :], in_=pTt[0:64, :]), reads=[pTt], writes=[qT])
                    S.op('act', lambda e, cb=cb: e.copy(out=kT[cb][0:64, :], in_=pTk[0:64, :]), reads=[pTk], writes=[kT[cb]])
                    if _ATT_LVL < 3:
                        blk += 1
                        continue
                    for h in range(8):
                        p, j = h // 2, h % 2
                        S.op('pe', lambda e, h=h, p=p, j=j, cb=cb: e.matmul(
                            psC[h // 4][:, (h % 4) * 128:(h % 4 + 1) * 128], lhsT=kT[cb][0:64, h * 128:(h + 1) * 128],
                            rhs=qT[0:64, h * 128:(h + 1) * 128], start=True, stop=True),
                            reads=[kT[cb], qT], writes=[psC[h // 4]])
                    if b > 0:
                        for h in range(8):
                            p, j = h // 2, h % 2
                            S.op('pe', lambda e, h=h, p=p, j=j, pb=pb: e.matmul(
                                psP[h // 4][:, (h % 4) * 128:(h % 4 + 1) * 128], lhsT=kT[pb][0:64, h * 128:(h + 1) * 128],
                                rhs=qT[0:64, h * 128:(h + 1) * 128], start=True, stop=True),
                                reads=[kT[pb], qT], writes=[psP[h // 4]])
                    if _ATT_LVL < 4:
                        blk += 1
                        continue
                    for hh in range(2):
                        S.op('act', lambda e, hh=hh: e.activation(out=PTc[:, hh * 512:(hh + 1) * 512], in_=psC[hh][:],
                                                                  func=AF.Exp, scale=0.125), reads=[psC[hh]], writes=[PTc])
                    S.op('dve', lambda e: e.tensor_tensor(out=PTc[:], in0=PTc[:], in1=maskC[:], op=ALU.mult),
                         reads=[PTc, maskC], writes=[PTc])
                    if b > 0:
                        for hh in range(2):
                            S.op('act', lambda e, hh=hh: e.activation(out=PTp[:, hh * 512:(hh + 1) * 512], in_=psP[hh][:],
                                                                      func=AF.Exp, scale=0.125), reads=[psP[hh]], writes=[PTp])
                        S.op('pool', lambda e: e.tensor_tensor(out=PTp[:], in0=PTp[:], in1=maskP[:], op=ALU.mult),
                             reads=[PTp, maskP], writes=[PTp])
                    if _ATT_LVL < 5:
                        blk += 1
                        continue
                    for h in range(8):
                        po_t = pO[h // 4]
                        osl = slice((h % 4) * 66, (h % 4 + 1) * 66)
                        S.op('pe', lambda e, h=h, po_t=po_t, osl=osl, cb=cb: e.matmul(
                            po_t[:, osl], lhsT=PTc[:, h * 128:(h + 1) * 128], rhs=vaug[cb][:, h * 66:(h + 1) * 66],
                            start=True, stop=(b == 0)), reads=[PTc, vaug[cb]], writes=[po_t])
                        if b > 0:
                            S.op('pe', lambda e, h=h, po_t=po_t, osl=osl, pb=pb: e.matmul(
                                po_t[:, osl], lhsT=PTp[:, h * 128:(h + 1) * 128], rhs=vaug[pb][:, h * 66:(h + 1) * 66],
                                start=False, stop=True), reads=[PTp, vaug[pb]], writes=[po_t])
                    o = osb[cb]
                    S.op('act', lambda e, o=o: e.copy(out=o[:, 0:264], in_=pO[0][:, 0:264]), reads=[pO[0]], writes=[o])
                    S.op('dve', lambda e, o=o: e.tensor_copy(out=o[:, 264:528], in_=pO[1][:, 0:264]), reads=[pO[1]], writes=[o])
                    S.dma('pool', OAv[r, b * 128:(b + 1) * 128, :], o[:], reads=[o], writes=[('OA', br)])
                    blk += 1
        es_c.close()
        cur[0] = es
        S.barrier()
        es_e = contextlib.ExitStack()
        es_e.__enter__()
        cur[0] = es_e
        bmask = tl("bmask_s", [8, 528])
        S.dma('sp', bmask[:], bmask_d[:, :], writes=[bmask])
        skv = [tl("skv%d" % i, [128, 1024]) for i in range(2)]
        sqb = tl("sqb", [128, 512])
        sprod = tl("sprod", [128, 512])
        ssc = tl("ssc", [128, 8])
        sp_b = tl("sp_b", [128, 8], BF16)
        svaug = [tl("svaug%d" % i, [128, 528], BF16) for i in range(2)]
        sq8 = tl("sq8", [8, 64])
        sk8 = tl("sk8", [8, 64])
        sv8 = tl("sv8", [8, 64])
        sj8 = tl("sj8", [8, 64])
        ss8 = tl("ss8", [8, 1])
        sdiag = tl("sdiag", [8, 528])
        sres = [tl("sres%d" % i, [8, 66]) for i in range(2)]
        pSa = ptl("pSa", [8, 512])
        pSb = ptl("pSb", [8, 512])
        for i in range(2):
            S.op('dve', lambda e, i=i: e.memset(svaug[i][:], 1.0), writes=[svaug[i]])
        it = 0
        for bb in range(4):
            for t in range(4):
                row = T + 4 * bb + t
                S.dma('sp', sqb[:], bass.AP(tensor=Z.tensor, offset=row * MIXIN, ap=[[0, 128], [1, 512]]),
                      reads=[('Z', NT)], writes=[sqb])
                S.dma('sp', sq8[:], Z[row, 0:512].rearrange("(h e) -> h e", e=64), writes=[sq8])
                S.dma('sp', sk8[:], Z[row, 512:1024].rearrange("(h e) -> h e", e=64), writes=[sk8])
                S.dma('sp', sv8[:], Z[row, 1024:1536].rearrange("(h e) -> h e", e=64), writes=[sv8])
                for br, dil in enumerate((1, 4, 16)):
                    kv = skv[it % 2]
                    va = svaug[it % 2]
                    it += 1
                    start = 2048 + t - dil * 128
                    if dil == 1:
                        ncache = 128 - t
                        S.dma('sp', kv[0:ncache, 0:512], ck[bb, start:2048, :], writes=[kv])
                        S.dma('sp', kv[0:ncache, 512:1024], cv[bb, start:2048, :], writes=[kv])
                        if t > 0:
                            S.dma('sp', kv[ncache:128, :], Z[T + 4 * bb:T + 4 * bb + t, 512:1536], reads=[('Z', NT)], writes=[kv])
                    else:
                        S.dma('sp', kv[:, 0:512], bass.AP(tensor=ck.tensor, offset=(bb * 2048 + start) * 512,
                                                          ap=[[dil * 512, 128], [1, 512]]), writes=[kv])
                        S.dma('sp', kv[:, 512:1024], bass.AP(tensor=cv.tensor, offset=(bb * 2048 + start) * 512,
                                                             ap=[[dil * 512, 128], [1, 512]]), writes=[kv])
                    S.op('act', lambda e, kv=kv, va=va: e.copy(
                        out=va[:].rearrange("p (h e) -> p h e", e=66)[:, :, 0:64],
                        in_=kv[:, 512:1024].rearrange("p (h e) -> p h e", e=64)), reads=[kv], writes=[va])
                    S.op('dve', lambda e, kv=kv: e.tensor_tensor(out=sprod[:], in0=kv[:, 0:512], in1=sqb[:], op=ALU.mult),
                         reads=[kv, sqb], writes=[sprod])
                    S.op('dve', lambda e: e.tensor_reduce(out=ssc[:], in_=sprod[:].rearrange("p (h e) -> p h e", e=64),
                                                          axis=AX.X, op=ALU.add), reads=[sprod], writes=[ssc])
                    S.op('act', lambda e: e.activation(out=sp_b[:], in_=ssc[:], func=AF.Exp, scale=0.125),
                         reads=[ssc], writes=[sp_b])
                    S.op('pe', lambda e, va=va, br=br: e.matmul(pSa[:, 0:264], lhsT=sp_b[:], rhs=va[:, 0:264],
                                                                start=(br == 0), stop=(br == 2)),
                         reads=[sp_b, va], writes=[pSa])
                    S.op('pe', lambda e, va=va, br=br: e.matmul(pSb[:, 0:264], lhsT=sp_b[:], rhs=va[:, 264:528],
                                                                start=(br == 0), stop=(br == 2)),
                         reads=[sp_b, va], writes=[pSb])
                res = sres[(4 * bb + t) % 2]
                S.op('dve', lambda e: e.tensor_tensor(out=sdiag[:, 0:264], in0=pSa[:, 0:264], in1=bmask[:, 0:264], op=ALU.mult),
                     reads=[pSa, bmask], writes=[sdiag])
                S.op('dve', lambda e: e.tensor_tensor(out=sdiag[:, 264:528], in0=pSb[:, 0:264], in1=bmask[:, 264:528], op=ALU.mult),
                     reads=[pSb, bmask], writes=[sdiag])
                S.op('dve', lambda e, res=res: e.tensor_reduce(out=res[:], in_=sdiag[:].rearrange("p (h e) -> p e h", e=66),
                                                               axis=AX.X, op=ALU.add), reads=[sdiag], writes=[res])
                S.op('dve', lambda e: e.tensor_tensor(out=sj8[:], in0=sq8[:], in1=sk8[:], op=ALU.mult),
                     reads=[sq8, sk8], writes=[sj8])
                S.op('dve', lambda e: e.tensor_reduce(out=ss8[:], in_=sj8[:], axis=AX.X, op=ALU.add), reads=[sj8], writes=[ss8])
                S.op('act', lambda e: e.activation(out=ss8[:], in_=ss8[:], func=AF.Exp, scale=0.125), reads=[ss8], writes=[ss8])
                S.op('dve', lambda e: e.tensor_scalar(out=ss8[:], in0=ss8[:], scalar1=3.0, scalar2=None, op0=ALU.mult),
                     reads=[ss8], writes=[ss8])
                S.op('dve', lambda e, res=res: e.scalar_tensor_tensor(out=res[:, 0:64], in0=sv8[:], scalar=ss8[:, 0:1],
                                                                      in1=res[:, 0:64], op0=ALU.mult, op1=ALU.add),
                     reads=[sv8, ss8, res], writes=[res])
                S.op('dve', lambda e, res=res: e.tensor_tensor(out=res[:, 64:65], in0=res[:, 64:65], in1=ss8[:], op=ALU.add),
                     reads=[res, ss8], writes=[res])
                S.dma('pool', OA[0, row, :].rearrange("(h e) -> h e", e=66), res[:], reads=[res], writes=[('OA', 0)])
        es_e.close()
        cur[0] = es
        S.barrier()
        es_f = contextlib.ExitStack()
        es_f.__enter__()
        cur[0] = es_f
        goutc = tl("goutc", [128, 8])
        gcross = tl("gcross", [128, 8])
        S.dma('sp', goutc[:], g_outc[:, :], writes=[goutc])
        S.dma('sp', gcross[:], g_cross[:, :], writes=[gcross])
        wout_b = tl("wout_b", [128, 8 * 1024], BF16)
        wcq_b = tl("wcq_b", [128, 8 * 512], BF16)
        wco_b = tl("wco_b", [128, 4 * 1024], BF16)
        fst = [tl("fst%d" % i, [128, 1024]) for i in range(2)]
        for kc in range(8):
            st = fst[kc % 2]
            S.dma('sp', st[:], w_out[kc * 128:(kc + 1) * 128, :], writes=[st])
            S.op('dve', lambda e, st=st, kc=kc: e.tensor_scalar(out=wout_b[:, kc * 1024:(kc + 1) * 1024], in0=st[:],
                                                                scalar1=goutc[:, kc:kc + 1], scalar2=None, op0=ALU.mult),
                 reads=[st, goutc], writes=[wout_b])
        for kc in range(8):
            st = fst[kc % 2]
            S.dma('sp', st[:, 0:512], w_cq[kc * 128:(kc + 1) * 128, :], writes=[st])
            S.op('dve', lambda e, st=st, kc=kc: e.tensor_scalar(out=wcq_b[:, kc * 512:(kc + 1) * 512], in0=st[:, 0:512],
                                                                scalar1=gcross[:, kc:kc + 1], scalar2=None, op0=ALU.mult),
                 reads=[st, gcross], writes=[wcq_b])
        for kc in range(4):
            st = fst[kc % 2]
            S.dma('sp', st[:], w_co[kc * 128:(kc + 1) * 128, :], writes=[st])
            S.op('dve', lambda e, st=st, kc=kc: e.tensor_copy(out=wco_b[:, kc * 1024:(kc + 1) * 1024], in_=st[:]),
                 reads=[st], writes=[wco_b])
        ones_b = tl("ones_b", [128, 128], BF16)
        S.op('dve', lambda e: e.memset(ones_b[:], 1.0), writes=[ones_b])
        mkT = [tl("mkT%d" % i, [128, 4 * 256], BF16) for i in range(5)]
        mvb = [tl("mvb%d" % i, [128, 2 * 512], BF16) for i in range(5)]
        mkb = tl("mkb", [128, 512], BF16)
        pFT = ptl("pFT", [128, 1024], BF16)
        pTm = pFT
        for g in range(5):
            src_k = o_mk if g == 0 else cmk[g - 1]
            src_v = o_mv if g == 0 else cmv[g - 1]
            for mb in range(2):
                st = fst[mb]
                S.dma('sp', st[:, 0:512], src_k[mb * 128:(mb + 1) * 128, :], writes=[st])
                S.dma('sp', st[:, 512:1024], src_v[mb * 128:(mb + 1) * 128, :], writes=[st])
                S.op('dve', lambda e, st=st: e.tensor_copy(out=mkb[:], in_=st[:, 0:512]), reads=[st], writes=[mkb])
                S.op('dve', lambda e, st=st, g=g, mb=mb: e.tensor_copy(out=mvb[g][:, mb * 512:(mb + 1) * 512], in_=st[:, 512:1024]),
                     reads=[st], writes=[mvb[g]])
                for hh in range(4):
                    S.op('pe', lambda e, hh=hh: e.transpose(out=pTm[:, hh * 128:(hh + 1) * 128], in_=mkb[:, hh * 128:(hh + 1) * 128],
                                                            identity=ident_b[:]), reads=[mkb, 'ident_b'], writes=[pTm])
                S.op('act', lambda e, g=g, mb=mb: e.copy(
                    out=mkT[g][:].rearrange("p (h m) -> p h m", m=256)[:, :, mb * 128:(mb + 1) * 128],
                    in_=pTm[:, 0:512].rearrange("p (h m) -> p h m", m=128)), reads=[pTm], writes=[mkT[g]])
        xa = [tl("xa%d" % i, [128, D]) for i in range(2)]
        oat = [tl("oat%d" % i, [128, 528]) for i in range(3)]
        obt = tl("obt", [128, 512])
        rden = tl("rden", [128, 8])
        oan = tl("oan", [128, 512])
        cat = tl("cat", [128, D], BF16)
        fjunk = tl("fjunk", [128, D])
        fss = tl("fss", [128, 1])
        frs = tl("frs", [128, 1])
        fhb = tl("fhb", [128, D], BF16)
        catT = tl("catT", [128, D], BF16)
        h2T = tl("h2T", [128, D], BF16)
        qcT = tl("qcT", [128, 512], BF16)
        PTx = tl("PTx", [128, 1024], BF16)
        rdx = tl("rdx", [128, 512])
        oTx = tl("oTx", [128, 512], BF16)
        py = [ptl("py%d" % i, [128, 512]) for i in range(2)]
        pq = ptl("pq", [128, 512])
        psx = [ptl("psx%d" % i, [128, 512]) for i in range(2)]
        pox = ptl("pox", [128, 512])
        pdx = ptl("pdx", [128, 512])

        def normT(xtile, outT):
            S.op('act', lambda e: e.activation(out=fjunk[:], in_=xtile[:], func=AF.Square, accum_out=fss[:]),
                 reads=[xtile], writes=[fjunk, fss])
            S.op('dve', lambda e: e.tensor_scalar(out=frs[:], in0=fss[:], scalar1=1.0 / D, scalar2=EPS,
                                                  op0=ALU.mult, op1=ALU.add), reads=[fss], writes=[frs])
            S.op('act', lambda e: e.sqrt(out=frs[:], in_=frs[:]), reads=[frs], writes=[frs])
            S.op('dve', lambda e: e.reciprocal(out=frs[:], in_=frs[:]), reads=[frs], writes=[frs])
            S.op('dve', lambda e: e.tensor_scalar(out=fhb[:], in0=xtile[:], scalar1=frs[:, 0:1], scalar2=None,
                                                  op0=ALU.mult), reads=[xtile, frs], writes=[fhb])
            for kc in range(8):
                S.op('pe', lambda e, kc=kc: e.transpose(out=pFT[:, kc * 128:(kc + 1) * 128],
                                                        in_=fhb[:, kc * 128:(kc + 1) * 128], identity=ident_b[:]),
                     reads=[fhb, 'ident_b'], writes=[pFT])
            S.op('act', lambda e: e.copy(out=outT[:], in_=pFT[:]), reads=[pFT], writes=[outT])

        for i in range(NTT):
            x = xa[i % 2]
            nbr = 3 if i < NT else 1
            if i < NT:
                S.dma('sp', x[:], xp[i * 128:(i + 1) * 128, :], writes=[x])
            else:
                S.op('dve', lambda e, x=x: e.memset(x[:], 0.0), writes=[x])
                S.dma('sp', x[0:16, :], xs[:, :], writes=[x])
                for t3 in (oat[0], obt):
                    S.op('dve', lambda e, t3=t3: e.memset(t3[:], 1.0), writes=[t3])
            nr = 128 if i < NT else 16
            for br in range(nbr):
                S.dma('sp', oat[br][0:nr, :], OA[br, i * 128:i * 128 + nr, :], reads=[('OA', br)], writes=[oat[br]])
            S.dma('sp', obt[0:nr, :], OB[i * 128:i * 128 + nr, :], reads=[('OB', j) for j in range(NTT)], writes=[obt])
            for br in range(1, nbr):
                S.op('dve', lambda e, br=br: e.tensor_tensor(out=oat[0][:], in0=oat[0][:], in1=oat[br][:], op=ALU.add),
                     reads=[oat[0], oat[br]], writes=[oat[0]])
            S.op('dve', lambda e: e.reciprocal(out=rden[:], in_=oat[0][:].rearrange("p (h e) -> p h e", e=66)[:, :, 64]),
                 reads=[oat[0]], writes=[rden])
            for h in range(8):
                S.op('dve', lambda e, h=h: e.tensor_scalar(out=oan[:, h * 64:(h + 1) * 64], in0=oat[0][:, h * 66:h * 66 + 64],
                                                           scalar1=rden[:, h:h + 1], scalar2=None, op0=ALU.mult),
                     reads=[oat[0], rden], writes=[oan])
            S.op('act', lambda e: e.activation(out=fjunk[:, 0:512], in_=oan[:], func=AF.Square, accum_out=fss[:]),
                 reads=[oan], writes=[fjunk, fss])
            S.op('dve', lambda e: e.tensor_scalar(out=frs[:], in0=fss[:], scalar1=1.0 / 512, scalar2=EPS,
                                                  op0=ALU.mult, op1=ALU.add), reads=[fss], writes=[frs])
            S.op('act', lambda e: e.sqrt(out=frs[:], in_=frs[:]), reads=[frs], writes=[frs])
            S.op('dve', lambda e: e.reciprocal(out=frs[:], in_=frs[:]), reads=[frs], writes=[frs])
            S.op('dve', lambda e: e.tensor_scalar(out=cat[:, 0:512], in0=oan[:], scalar1=frs[:, 0:1], scalar2=None,
                                                  op0=ALU.mult), reads=[oan, frs], writes=[cat])
            S.op('dve', lambda e: e.tensor_copy(out=cat[:, 512:1024], in_=obt[:]), reads=[obt], writes=[cat])
            for kc in range(8):
                S.op('pe', lambda e, kc=kc: e.transpose(out=pFT[:, kc * 128:(kc + 1) * 128],
                                                        in_=cat[:, kc * 128:(kc + 1) * 128], identity=ident_b[:]),
                     reads=[cat, 'ident_b'], writes=[pFT])
            S.op('act', lambda e: e.copy(out=catT[:], in_=pFT[:]), reads=[pFT], writes=[catT])
            for half in range(2):
                for kc in range(8):
                    S.op('pe', lambda e, kc=kc, half=half: e.matmul(
                        py[half][:], lhsT=catT[:, kc * 128:(kc + 1) * 128],
                        rhs=wout_b[:, kc * 1024 + half * 512:kc * 1024 + (half + 1) * 512],
                        start=(kc == 0), stop=(kc == 7)), reads=[catT, wout_b], writes=[py[half]])
                S.op('dve', lambda e, half=half, x=x: e.tensor_tensor(out=x[:, half * 512:(half + 1) * 512],
                                                                     in0=x[:, half * 512:(half + 1) * 512], in1=py[half][:],
                                                                     op=ALU.add), reads=[x, py[half]], writes=[x])
            normT(x, h2T)
            for hh in range(4):
                for kc in range(8):
                    S.op('pe', lambda e, kc=kc, hh=hh: e.matmul(
                        pq[:, hh * 128:(hh + 1) * 128], lhsT=wcq_b[:, kc * 512 + hh * 128:kc * 512 + (hh + 1) * 128],
                        rhs=h2T[:, kc * 128:(kc + 1) * 128], start=(kc == 0), stop=(kc == 7)),
                        reads=[wcq_b, h2T], writes=[pq])
            S.op('act', lambda e: e.copy(out=qcT[:], in_=pq[:]), reads=[pq], writes=[qcT])
            groups = [(0, 128, 0)] if i < NT else [(4 * bb, 4, 1 + bb) for bb in range(4)]
            for (c0, ncol, g) in groups:
                for hh in range(4):
                    for mb in range(2):
                        S.op('pe', lambda e, hh=hh, mb=mb, c0=c0, ncol=ncol, g=g: e.matmul(
                            psx[mb][:, hh * 128 + c0:hh * 128 + c0 + ncol],
                            lhsT=mkT[g][:, hh * 256 + mb * 128:hh * 256 + (mb + 1) * 128],
                            rhs=qcT[:, hh * 128 + c0:hh * 128 + c0 + ncol], start=True, stop=True),
                            reads=[mkT[g], qcT], writes=[psx[mb]])
            for mb in range(2):
                S.op('act', lambda e, mb=mb: e.activation(out=PTx[:, mb * 512:(mb + 1) * 512], in_=psx[mb][:], func=AF.Exp,
                                                          scale=float(128 ** -0.5)), reads=[psx[mb]], writes=[PTx])
            for (c0, ncol, g) in groups:
                for hh in range(4):
                    for mb in range(2):
                        S.op('pe', lambda e, hh=hh, mb=mb, c0=c0, ncol=ncol, g=g: e.matmul(
                            pox[:, hh * 128 + c0:hh * 128 + c0 + ncol],
                            lhsT=mvb[g][:, mb * 512 + hh * 128:mb * 512 + (hh + 1) * 128],
                            rhs=PTx[:, mb * 512 + hh * 128 + c0:mb * 512 + hh * 128 + c0 + ncol],
                            start=(mb == 0), stop=(mb == 1)), reads=[mvb[g], PTx], writes=[pox])
                        S.op('pe', lambda e, hh=hh, mb=mb, c0=c0, ncol=ncol: e.matmul(
                            pdx[:, hh * 128 + c0:hh * 128 + c0 + ncol], lhsT=ones_b[:],
                            rhs=PTx[:, mb * 512 + hh * 128 + c0:mb * 512 + hh * 128 + c0 + ncol],
                            start=(mb == 0), stop=(mb == 1)), reads=[ones_b, PTx], writes=[pdx])
            S.op('dve', lambda e: e.reciprocal(out=rdx[:], in_=pdx[:]), reads=[pdx], writes=[rdx])
            S.op('dve', lambda e: e.tensor_tensor(out=oTx[:], in0=pox[:], in1=rdx[:], op=ALU.mult),
                 reads=[pox, rdx], writes=[oTx])
            for half in range(2):
                for hh in range(4):
                    S.op('pe', lambda e, hh=hh, half=half: e.matmul(
                        py[half][:], lhsT=oTx[:, hh * 128:(hh + 1) * 128],
                        rhs=wco_b[:, hh * 1024 + half * 512:hh * 1024 + (half + 1) * 512],
                        start=(hh == 0), stop=(hh == 3)), reads=[oTx, wco_b], writes=[py[half]])
                S.op('dve', lambda e, half=half, x=x: e.tensor_tensor(out=x[:, half * 512:(half + 1) * 512],
                                                                     in0=x[:, half * 512:(half + 1) * 512], in1=py[half][:],
                                                                     op=ALU.add), reads=[x, py[half]], writes=[x])
            S.dma('pool', X2[i * 128:(i + 1) * 128, :], x[:], reads=[x], writes=[('X2', i)])
        es_f.close()
        cur[0] = es
        S.barrier()
        es_g = contextlib.ExitStack()
        es_g.__enter__()
        cur[0] = es_g

        def cap(tile, off, dims):
            return bass.AP(tensor=tile.t, offset=off, ap=dims)

        gffn = tl("gffn", [128, 8])
        gfin = tl("gfin", [128, D])
        iota16 = tl("iota16_s", [128, 16])
        iota128 = tl("iota128_s", [128, 128])
        S.dma('sp', gffn[:], g_ffn[:, :], writes=[gffn])
        S.dma('sp', gfin[:], g_fin[:, :], writes=[gfin])
        S.dma('sp', iota16[:], iota16_d[:, :], writes=[iota16])
        S.dma('sp', iota128[:], iota128_d[:, :], writes=[iota128])
        sc = tl("sc", [128, 2048])
        scw = tl("scw", [128, 2048])
        cand = tl("cand", [128, 2048])
        gffnx = cand
        wpq_b = tl("wpq_b", [128, 8 * 2048], BF16)
        keys_b = tl("keys_b", [128, 2048], BF16)
        gst = [sc, scw]
        S.dma('sp', gffnx[:, 0:1024], g_ffnx[:, :], writes=[gffnx])
        for kc in range(8):
            for hf in range(2):
                st = gst[hf]
                S.dma('sp', st[:, 0:1024], w_pq[kc * 128:(kc + 1) * 128, hf * 1024:(hf + 1) * 1024], writes=[st])
                S.op('dve', lambda e, st=st, kc=kc, hf=hf: e.tensor_scalar(
                    out=wpq_b[:, kc * 2048 + hf * 1024:kc * 2048 + (hf + 1) * 1024], in0=st[:, 0:1024],
                    scalar1=gffn[:, kc:kc + 1], scalar2=None, op0=ALU.mult), reads=[st, gffn], writes=[wpq_b])
        for hf in range(2):
            S.dma('sp', gst[hf][:, 0:1024], keysT[:, hf * 1024:(hf + 1) * 1024], writes=[gst[hf]])
            S.op('dve', lambda e, hf=hf: e.tensor_copy(out=keys_b[:, hf * 1024:(hf + 1) * 1024], in_=gst[hf][:, 0:1024]),
                 reads=[gst[hf]], writes=[keys_b])
        ut4 = [tl("ut4_%d" % i, [128, 4096], BF16) for i in range(2)]
        vt4 = [tl("vt4_%d" % i, [128, 4096], BF16) for i in range(2)]
        for j in range(128):
            su, sv = gst[0], gst[1]
            S.dma('sp', su[:, 0:1024], ut_h[j, :, :], writes=[su])
            S.dma('sp', sv[:, 0:1024], v_h[j, :, :], writes=[sv])
            cu, cv2 = ut4[j % 2], vt4[j % 2]
            S.op('dve', lambda e, cu=cu: e.tensor_tensor(out=cu[:, 0:1024], in0=su[:, 0:1024], in1=gffnx[:, 0:1024], op=ALU.mult),
                 reads=[su, gffnx], writes=[cu])
            S.op('act', lambda e, cv2=cv2: e.copy(out=cv2[:, 0:1024], in_=sv[:, 0:1024]), reads=[sv], writes=[cv2])
            S.dma('pool', UTb[j // 4, :, (j % 4) * 1024:(j % 4 + 1) * 1024], cu[:, 0:1024], reads=[cu], writes=[('UTb', j // 4)])
            S.dma('pool', Vb[j // 4, :, (j % 4) * 1024:(j % 4 + 1) * 1024], cv2[:, 0:1024], reads=[cv2], writes=[('Vb', j // 4)])
        xg = [tl("xg%d" % i, [128, D]) for i in range(2)]
        gjunk = tl("gjunk", [128, D])
        gss = tl("gss", [128, 1])
        grs = tl("grs", [128, 1])
        ghb = tl("ghb", [128, D], BF16)
        h3Tb = [tl("h3T%d" % i, [128, D], BF16) for i in range(2)]
        qTb = tl("qTb", [128, 2048], BF16)
        v16 = tl("v16", [128, 256])
        i16 = tl("i16", [128, 256], U32)
        i16f = tl("i16f", [128, 256])
        candw = scw
        c16 = tl("c16", [128, 128])
        ci = tl("ci", [128, 128], U32)
        ca_u = tl("ca_u", [128, 128], U32)
        cb_u = tl("cb_u", [128, 128], U32)
        ca_f = tl("ca_f", [128, 128])
        cb_f = tl("cb_f", [128, 128])
        eq = cand
        IG = tl("IG", [128, 384])
        gsum = tl("gsum", [128, 8])
        IGT = tl("IGT", [128, 384])
        NQ = 8
        Lh = tl("Lh", [128, NQ * 128], BF16)
        Rh = tl("Rh", [128, NQ * 128], BF16)
        Gsbb = [tl("Gsb%d" % i, [128, 16384], BF16) for i in range(2)]
        ga4 = [tl("ga4_%d" % i, [128, 512], BF16) for i in range(2)]
        gT4 = [tl("gT4_%d" % i, [128, 512], BF16) for i in range(2)]
        Wb4 = [tl("Wb4_%d" % i, [128, 512], BF16) for i in range(2)]
        yo = gjunk
        pGT = ptl("pGT", [128, 1024], BF16)
        pqs = ptl("pqs", [128, 512])
        pG = [ptl("pG%d" % i, [128, 512]) for i in range(2)]
        pA = [ptl("pA0", [128, 512])] * 2
        pT4 = ptl("pT4", [128, 1024], BF16)
        py3 = [ptl("py3_%d" % i, [128, 512]) for i in range(2)]

        def prep(i):
            x = xg[i % 2]
            h3T = h3Tb[i % 2]
            Gsb = Gsbb[i % 2]
            S.dma('sp', x[:], X2[i * 128:(i + 1) * 128, :], reads=[('X2', i)], writes=[x])
            S.op('act', lambda e, x=x: e.activation(out=gjunk[:], in_=x[:], func=AF.Square, accum_out=gss[:]),
                 reads=[x], writes=[gjunk, gss])
            S.op('dve', lambda e: e.tensor_scalar(out=grs[:], in0=gss[:], scalar1=1.0 / D, scalar2=EPS,
                                                  op0=ALU.mult, op1=ALU.add), reads=[gss], writes=[grs])
            S.op('act', lambda e: e.sqrt(out=grs[:], in_=grs[:]), reads=[grs], writes=[grs])
            S.op('dve', lambda e: e.reciprocal(out=grs[:], in_=grs[:]), reads=[grs], writes=[grs])
            S.op('dve', lambda e, x=x: e.tensor_scalar(out=ghb[:], in0=x[:], scalar1=grs[:, 0:1], scalar2=None,
                                                       op0=ALU.mult), reads=[x, grs], writes=[ghb])
            for kc in range(8):
                S.op('pe', lambda e, kc=kc: e.transpose(out=pGT[:, kc * 128:(kc + 1) * 128],
                                                        in_=ghb[:, kc * 128:(kc + 1) * 128], identity=ident_b[:]),
                     reads=[ghb, 'ident_b'], writes=[pGT])
            S.op('act', lambda e: e.copy(out=h3T[:], in_=pGT[:]), reads=[pGT], writes=[h3T])
            for cg in range(4):
                for cc in range(4):
                    c = cg * 4 + cc
                    for kc in range(8):
                        S.op('pe', lambda e, kc=kc, c=c, cc=cc: e.matmul(
                            pqs[:, cc * 128:(cc + 1) * 128], lhsT=wpq_b[:, kc * 2048 + c * 128:kc * 2048 + (c + 1) * 128],
                            rhs=h3T[:, kc * 128:(kc + 1) * 128], start=(kc == 0), stop=(kc == 7)),
                            reads=[wpq_b, h3T], writes=[pqs])
                S.op('act', lambda e, cg=cg: e.copy(out=qTb[:, cg * 512:(cg + 1) * 512], in_=pqs[:]), reads=[pqs], writes=[qTb])
            for cg in range(4):
                for cc in range(4):
                    c = cg * 4 + cc
                    S.op('pe', lambda e, c=c, cc=cc: e.matmul(
                        pqs[:, cc * 128:(cc + 1) * 128], lhsT=qTb[:, c * 128:(c + 1) * 128],
                        rhs=keys_b[:, c * 128:(c + 1) * 128], start=True, stop=True), reads=[qTb, keys_b], writes=[pqs])
                S.op('act', lambda e, cg=cg: e.copy(out=sc[:, cg * 512:(cg + 1) * 512], in_=pqs[:]), reads=[pqs], writes=[sc])
            for c in range(16):
                cs = slice(c * 128, (c + 1) * 128)
                S.op('dve', lambda e, c=c, cs=cs: e.max(out=v16[:, c * 16:c * 16 + 8], in_=sc[:, cs]), reads=[sc], writes=[v16])
                S.op('dve', lambda e, c=c, cs=cs: e.max_index(out=i16[:, c * 16:c * 16 + 8], in_max=v16[:, c * 16:c * 16 + 8],
                                                              in_values=sc[:, cs]), reads=[sc, v16], writes=[i16])
                S.op('dve', lambda e, c=c, cs=cs: e.match_replace(out=scw[:, cs], in_to_replace=v16[:, c * 16:c * 16 + 8],
                                                                  in_values=sc[:, cs], imm_value=-1e30),
                     reads=[sc, v16], writes=[scw])
                S.op('dve', lambda e, c=c, cs=cs: e.max(out=v16[:, c * 16 + 8:c * 16 + 16], in_=scw[:, cs]),
                     reads=[scw], writes=[v16])
                S.op('dve', lambda e, c=c, cs=cs: e.max_index(out=i16[:, c * 16 + 8:c * 16 + 16],
                                                              in_max=v16[:, c * 16 + 8:c * 16 + 16], in_values=scw[:, cs]),
                     reads=[scw, v16], writes=[i16])
            S.op('dve', lambda e: e.tensor_copy(out=i16f[:], in_=i16[:]), reads=[i16], writes=[i16f])
            S.op('dve', lambda e: e.tensor_tensor(
                out=cand[:].rearrange("p (h a b) -> p h a b", h=8, a=16),
                in0=cap(v16, 0, [[256, 128], [32, 8], [1, 16], [0, 16]]),
                in1=cap(v16, 16, [[256, 128], [32, 8], [0, 16], [1, 16]]), op=ALU.add), reads=[v16], writes=[cand])
            for h in range(8):
                cs = slice(h * 256, (h + 1) * 256)
                S.op('dve', lambda e, h=h, cs=cs: e.max(out=c16[:, h * 16:h * 16 + 8], in_=cand[:, cs]), reads=[cand], writes=[c16])
                S.op('dve', lambda e, h=h, cs=cs: e.max_index(out=ci[:, h * 16:h * 16 + 8], in_max=c16[:, h * 16:h * 16 + 8],
                                                              in_values=cand[:, cs]), reads=[cand, c16], writes=[ci])
                S.op('dve', lambda e, h=h, cs=cs: e.match_replace(out=candw[:, cs], in_to_replace=c16[:, h * 16:h * 16 + 8],
                                                                  in_values=cand[:, cs], imm_value=-1e30),
                     reads=[cand, c16], writes=[candw])
                S.op('dve', lambda e, h=h, cs=cs: e.max(out=c16[:, h * 16 + 8:h * 16 + 16], in_=candw[:, cs]),
                     reads=[candw], writes=[c16])
                S.op('dve', lambda e, h=h, cs=cs: e.max_index(out=ci[:, h * 16 + 8:h * 16 + 16],
                                                              in_max=c16[:, h * 16 + 8:h * 16 + 16], in_values=candw[:, cs]),
                     reads=[candw, c16], writes=[ci])
            S.op('dve', lambda e: e.tensor_single_scalar(out=ca_u[:], in_=ci[:], scalar=4, op=ALU.logical_shift_right),
                 reads=[ci], writes=[ca_u])
            S.op('dve', lambda e: e.tensor_single_scalar(out=cb_u[:], in_=ci[:], scalar=15, op=ALU.bitwise_and),
                 reads=[ci], writes=[cb_u])
            S.op('dve', lambda e: e.tensor_copy(out=ca_f[:], in_=ca_u[:]), reads=[ca_u], writes=[ca_f])
            S.op('dve', lambda e: e.tensor_copy(out=cb_f[:], in_=cb_u[:]), reads=[cb_u], writes=[cb_f])
            for which, (src_f, off) in enumerate(((ca_f, 0), (cb_f, 16))):
                S.op('dve', lambda e, src_f=src_f: e.tensor_tensor(
                    out=eq[:].rearrange("p (s a) -> p s a", a=16),
                    in0=cap(src_f, 0, [[128, 128], [1, 128], [0, 16]]),
                    in1=cap(iota16, 0, [[16, 128], [0, 128], [1, 16]]), op=ALU.is_equal),
                    reads=[src_f, iota16], writes=[eq])
                S.op('dve', lambda e, off=off: e.tensor_tensor(
                    out=eq[:].rearrange("p (h k a) -> p h k a", h=8, k=16),
                    in0=eq[:].rearrange("p (h k a) -> p h k a", h=8, k=16),
                    in1=cap(i16f, off, [[256, 128], [32, 8], [0, 16], [1, 16]]), op=ALU.mult),
                    reads=[eq, i16f], writes=[eq])
                S.op('dve', lambda e, which=which: e.tensor_reduce(
                    out=IG[:, which * 128:(which + 1) * 128], in_=eq[:].rearrange("p (s a) -> p s a", a=16),
                    axis=AX.X, op=ALU.add), reads=[eq], writes=[IG])
            S.op('dve', lambda e: e.tensor_tensor(
                out=IG[:, 256:384].rearrange("p (h k) -> p h k", k=16), in0=c16[:].rearrange("p (h k) -> p h k", k=16),
                in1=cap(c16, 0, [[128, 128], [16, 8], [0, 16]]), op=ALU.subtract), reads=[c16], writes=[IG])
            S.op('act', lambda e: e.activation(out=IG[:, 256:384], in_=IG[:, 256:384], func=AF.Exp), reads=[IG], writes=[IG])
            S.op('dve', lambda e: e.tensor_reduce(out=gsum[:], in_=IG[:, 256:384].rearrange("p (h k) -> p h k", k=16),
                                                  axis=AX.X, op=ALU.add), reads=[IG], writes=[gsum])
            S.op('dve', lambda e: e.reciprocal(out=gsum[:], in_=gsum[:]), reads=[gsum], writes=[gsum])
            S.op('dve', lambda e: e.tensor_tensor(
                out=IG[:, 256:384].rearrange("p (h k) -> p h k", k=16), in0=IG[:, 256:384].rearrange("p (h k) -> p h k", k=16),
                in1=cap(gsum, 0, [[8, 128], [1, 8], [0, 16]]), op=ALU.mult), reads=[IG, gsum], writes=[IG])
            for w3 in range(3):
                S.op('pe', lambda e, w3=w3: e.transpose(out=pqs[:, w3 * 128:(w3 + 1) * 128], in_=IG[:, w3 * 128:(w3 + 1) * 128],
                                                        identity=ident_f[:]), reads=[IG, 'ident_f'], writes=[pqs])
            S.op('act', lambda e: e.copy(out=IGT[:], in_=pqs[:, 0:384]), reads=[pqs], writes=[IGT])
            for hf in range(128 // NQ):
                S.op('dve', lambda e, hf=hf: e.tensor_tensor(
                    out=Lh[:].rearrange("p (t i) -> p t i", i=128),
                    in0=cap(iota128, 0, [[128, 128], [0, NQ], [1, 128]]),
                    in1=cap(IGT, hf * NQ, [[384, 128], [1, NQ], [0, 128]]), op=ALU.is_equal),
                    reads=[iota128, IGT], writes=[Lh])
                S.op('dve', lambda e, hf=hf: e.tensor_tensor(
                    out=Rh[:].rearrange("p (t i) -> p t i", i=128),
                    in0=cap(iota128, 0, [[128, 128], [0, NQ], [1, 128]]),
                    in1=cap(IGT, 128 + hf * NQ, [[384, 128], [1, NQ], [0, 128]]), op=ALU.is_equal),
                    reads=[iota128, IGT], writes=[Rh])
                S.op('pool', lambda e, hf=hf: e.tensor_tensor(
                    out=Rh[:].rearrange("p (t i) -> p t i", i=128),
                    in0=Rh[:].rearrange("p (t i) -> p t i", i=128),
                    in1=cap(IGT, 256 + hf * NQ, [[384, 128], [1, NQ], [0, 128]]), op=ALU.mult),
                    reads=[Rh, IGT], writes=[Rh])
                for t4 in range(NQ // 4):
                    pg = pG[t4 % 2]
                    for tt in range(4):
                        tl_ = t4 * 4 + tt
                        S.op('pe', lambda e, pg=pg, tt=tt, tl_=tl_: e.matmul(
                            pg[:, tt * 128:(tt + 1) * 128], lhsT=Lh[:, tl_ * 128:(tl_ + 1) * 128],
                            rhs=Rh[:, tl_ * 128:(tl_ + 1) * 128], start=True, stop=True), reads=[Lh, Rh], writes=[pg])
                    g0 = (hf * NQ + t4 * 4) * 128
                    if t4 % 2 == 0:
                        S.op('act', lambda e, pg=pg, g0=g0: e.copy(out=Gsb[:, g0:g0 + 512], in_=pg[:]), reads=[pg], writes=[Gsb])
                    else:
                        S.op('dve', lambda e, pg=pg, g0=g0: e.tensor_copy(out=Gsb[:, g0:g0 + 512], in_=pg[:]),
                             reads=[pg], writes=[Gsb])

        def record(fn, *args):
            rec = []
            o_op, o_dma = S.op, S.dma
            S.op = lambda *a, **k: rec.append((o_op, a, k))
            S.dma = lambda *a, **k: rec.append((o_dma, a, k))
            try:
                fn(*args)
            finally:
                S.op, S.dma = o_op, o_dma
            return rec

        def dense(i, nxt):
            x = xg[i % 2]
            h3T = h3Tb[i % 2]
            Gsb = Gsbb[i % 2]
            pos = [0]

            def pump(upto):
                while pos[0] < min(upto, len(nxt)):
                    f, a, k = nxt[pos[0]]
                    f(*a, **k)
                    pos[0] += 1
            def stage_u1(g):
                gb = g % 2
                S.dma('sp', ut4[gb][:], UTb[g, :, :], reads=[('UTb', g)], writes=[ut4[gb]])
                S.dma('sp', vt4[gb][:], Vb[g, :, :], reads=[('Vb', g)], writes=[vt4[gb]])
                for kc in range(8):
                    S.op('pe', lambda e, kc=kc, gb=gb: e.matmul(
                        pA[gb][:], lhsT=h3T[:, kc * 128:(kc + 1) * 128],
                        rhs=cap(ut4[gb], kc * 128, [[4096, 128], [1024, 4], [1, 128]]),
                        start=(kc == 0), stop=(kc == 7)), reads=[ut4[gb], h3T], writes=[pA[gb]])
                S.op('act', lambda e, gb=gb: e.activation(out=ga4[gb][:], in_=pA[gb][:], func=AF.Gelu),
                     reads=[pA[gb]], writes=[ga4[gb]])

            def stage_u2(g):
                gb = g % 2
                for c in range(4):
                    S.op('pe', lambda e, c=c, gb=gb: e.transpose(out=pT4[:, c * 128:(c + 1) * 128],
                                                                 in_=ga4[gb][:, c * 128:(c + 1) * 128], identity=ident_b[:]),
                         reads=[ga4[gb], 'ident_b'], writes=[pT4])
                S.op('act', lambda e, gb=gb: e.copy(out=gT4[gb][:], in_=pT4[:, 0:512]), reads=[pT4], writes=[gT4[gb]])
                S.op('dve', lambda e, gb=gb, g=g: e.tensor_tensor(
                    out=Wb4[gb][:].rearrange("p (c t) -> p c t", c=4), in0=gT4[gb][:].rearrange("p (c t) -> p c t", c=4),
                    in1=cap(Gsb, 4 * g, [[16384, 128], [1, 4], [128, 128]]), op=ALU.mult),
                    reads=[gT4[gb], Gsb], writes=[Wb4[gb]])

            def stage_v(g):
                gb = g % 2
                for c in range(4):
                    for half in range(2):
                        S.op('pe', lambda e, gb=gb, half=half, c=c, g=g: e.matmul(
                            py3[half][:], lhsT=Wb4[gb][:, c * 128:(c + 1) * 128],
                            rhs=vt4[gb][:, c * 1024 + half * 512:c * 1024 + (half + 1) * 512],
                            start=(g == 0 and c == 0), stop=(g == 31 and c == 3)),
                            reads=[Wb4[gb], vt4[gb]], writes=[py3[half]])

            stage_u1(0)
            stage_u2(0)
            for g in range(32):
                if g + 1 < 32:
                    stage_u1(g + 1)
                stage_v(g)
                if g + 1 < 32:
                    stage_u2(g + 1)
                pump(((g + 1) * len(nxt) + 29) // 30)
            pump(len(nxt))
            for half in range(2):
                S.op('dve', lambda e, half=half, x=x: e.tensor_tensor(out=x[:, half * 512:(half + 1) * 512],
                                                                     in0=x[:, half * 512:(half + 1) * 512], in1=py3[half][:],
                                                                     op=ALU.add), reads=[x, py3[half]], writes=[x])
            if debug:
                S.dma('pool', X3[i * 128:(i + 1) * 128, :], x[:], reads=[x])
            S.op('act', lambda e, x=x: e.activation(out=gjunk[:], in_=x[:], func=AF.Square, accum_out=gss[:]),
                 reads=[x], writes=[gjunk, gss])
            S.op('dve', lambda e: e.tensor_scalar(out=grs[:], in0=gss[:], scalar1=1.0 / D, scalar2=EPS,
                                                  op0=ALU.mult, op1=ALU.add), reads=[gss], writes=[grs])
            S.op('act', lambda e: e.sqrt(out=grs[:], in_=grs[:]), reads=[grs], writes=[grs])
            S.op('dve', lambda e: e.reciprocal(out=grs[:], in_=grs[:]), reads=[grs], writes=[grs])
            S.op('dve', lambda e, x=x: e.scalar_tensor_tensor(out=yo[:], in0=x[:], scalar=grs[:, 0:1], in1=gfin[:],
                                                              op0=ALU.mult, op1=ALU.mult), reads=[x, grs, gfin], writes=[yo])
            if i < NT:
                S.dma('pool', y_p[i * 128:(i + 1) * 128, :], yo[:], reads=[yo])
            else:
                S.dma('pool', y_s[:, :], yo[0:16, :], reads=[yo])

        prep(0)
        for i in range(NTT):
            nxt = record(prep, i + 1) if i + 1 < NTT else []
            dense(i, nxt)
        es_g.close()
        cur[0] = es
        S.finish()
    return nc


_PROGRAM = None
_DEBUG_HOOK = None


def kernel(x_prompt, x_sample, cache_swa_k, cache_swa_v, state_hgrn, cache_mem_k, cache_mem_v,
           mem_prompt, norm_mix, w_in, lb_logits, beta_a, gnorm_b, w_out, norm_cross, norm_mem,
           w_cq, w_mk, w_mv, w_co, norm_ffn, w_pq, peer_k1, peer_k2, peer_u, peer_v, norm_final):
    global _PROGRAM
    f = lambda a: np.ascontiguousarray(np.asarray(a, dtype=np.float32))
    if _PROGRAM is None:
        _PROGRAM = build_program()
    nc = _PROGRAM

    def col(g):
        return f(np.asarray(g).reshape(-1, 128).T)

    common = {
        "w_in": f(w_in[0]), "w_mk": f(w_mk[0]), "w_mv": f(w_mv[0]),
        "g_mix": col(norm_mix[0]), "g_mem": col(norm_mem[0]),
        "w_out": f(w_out[0]), "w_cq": f(w_cq[0]), "w_co": f(w_co[0]),
        "g_outc": col(np.concatenate([np.asarray(beta_a[0]).reshape(-1), np.asarray(gnorm_b[0]).reshape(-1)])),
        "g_cross": col(norm_cross[0]),
        "w_pq": f(w_pq[0]), "g_ffn": col(norm_ffn[0]),
        "g_ffnx": f(np.repeat(np.asarray(norm_ffn[0]).reshape(8, 128).T[:, :, None], 128, axis=2).reshape(128, 1024)),
        "keysT": f(np.stack([np.asarray(peer_k1[0]), np.asarray(peer_k2[0])], axis=1).reshape(16, 128, 128)
                   .transpose(2, 0, 1).reshape(128, 2048)),
        "ut_h": f(np.asarray(peer_u[0]).reshape(128, 128, 8, 128).transpose(1, 3, 2, 0).reshape(128, 128, 1024)),
        "v_h": f(np.asarray(peer_v[0]).reshape(128, 128, 1024).transpose(1, 0, 2)),
        "g_fin": f(np.broadcast_to(np.asarray(norm_final)[None, :], (128, D))),
        "iota16": f(np.broadcast_to(np.arange(16)[None, :], (128, 16))),
        "iota128": f(np.broadcast_to(np.arange(128)[None, :], (128, 128))),
        "ident": np.eye(128, dtype=np.float32),
        "lbl0": f(np.broadcast_to(np.asarray(lb_logits)[0][None, :], (128, 512))),
        "lbl1": f(np.broadcast_to(np.asarray(lb_logits)[1][None, :], (128, 512))),
        "triU": np.triu(np.ones((128, 128), np.float32)),
        "tri4": np.tile(np.triu(np.ones((128, 64), np.float32)), (1, 8)),
        "rowmask": (np.arange(128) < 4).astype(np.float32).reshape(128, 1),
        "bmask": np.kron(np.eye(8, dtype=np.float32), np.ones((1, 66), np.float32)),
        "maskC": np.tile(np.triu(np.ones((128, 128), np.float32)), (1, 8)),
        "maskP": np.tile(np.tril(np.ones((128, 128), np.float32)), (1, 8)),
    }
    in_maps = []
    for c in range(NCORES):
        m = dict(common)
        m["xp"] = f(x_prompt[c])
        m["xs"] = f(np.asarray(x_sample[4 * c:4 * c + 4]).reshape(16, D))
        m["memp"] = f(mem_prompt[c])
        m["st_h"] = f(state_hgrn[0, 4 * c:4 * c + 4])
        m["cmk"] = f(np.asarray(cache_mem_k[0, 4 * c:4 * c + 4]).reshape(4, 256, 512))
        m["cmv"] = f(np.asarray(cache_mem_v[0, 4 * c:4 * c + 4]).reshape(4, 256, 512))
        m["ck"] = f(np.asarray(cache_swa_k[0, 4 * c:4 * c + 4]).reshape(4, 2048, 512))
        m["cv"] = f(np.asarray(cache_swa_v[0, 4 * c:4 * c + 4]).reshape(4, 2048, 512))
        in_maps.append(m)
    if _DEBUG_HOOK is not None:
        return _DEBUG_HOOK(in_maps)
    res = run_bass_kernel_spmd(nc, in_maps, core_ids=list(range(NCORES)))
    R = res.results
    y_prompt = np.stack([R[c]["y_p"] for c in range(NCORES)]).reshape(8, T, D)
    y_sample = np.concatenate([R[c]["y_s"].reshape(4, 4, D) for c in range(NCORES)], axis=0)
    p_k = np.stack([R[c]["o_pk"].reshape(2048, 8, 64) for c in range(NCORES)])[None]
    p_v = np.stack([R[c]["o_pv"].reshape(2048, 8, 64) for c in range(NCORES)])[None]
    p_h = np.stack([R[c]["o_ph"] for c in range(NCORES)])[None]
    p_mk = np.stack([R[c]["o_mk"].reshape(256, 4, 128) for c in range(NCORES)])[None]
    p_mv = np.stack([R[c]["o_mv"].reshape(256, 4, 128) for c in range(NCORES)])[None]
    s_k = np.concatenate([R[c]["o_sk"].reshape(4, 4, 8, 64) for c in range(NCORES)], axis=0)[None]
    s_v = np.concatenate([R[c]["o_sv"].reshape(4, 4, 8, 64) for c in range(NCORES)], axis=0)[None]
    s_h = np.concatenate([R[c]["o_sh"] for c in range(NCORES)], axis=0)[None]
    outs = (y_prompt, y_sample, p_k, p_v, p_h, p_mk, p_mv, s_k, s_v, s_h)
    return tuple(np.ascontiguousarray(o.astype(np.float32)) for o in outs)
```

```python
import contextlib
import numpy as np
import concourse.bass as bass
import concourse.mybir as mybir
from concourse.bass_utils import run_bass_kernel_spmd

F32 = mybir.dt.float32
BF16 = mybir.dt.bfloat16
U32 = mybir.dt.uint32
AF = mybir.ActivationFunctionType
ALU = mybir.AluOpType
AX = mybir.AxisListType

NCORES = 8
_ATT_LVL = 9
_ATT_MAXB = 10 ** 9
_BRANCHES = (1, 4, 16)
T = 4096
NT = 32
NTT = 33
D = 1024
MIXIN = 3584
EPS = 1e-6


class Sync:
    def __init__(self, nc, es):
        self.nc = nc
        self.eng = {'pe': nc.tensor, 'dve': nc.vector, 'act': nc.scalar, 'pool': nc.gpsimd, 'sp': nc.sync}
        self.sem = {}
        self.cnt = {}
        for e in self.eng:
            self.sem[e] = es.enter_context(nc.semaphore('c_' + e))
            self.cnt[e] = 0
        self.R = 8
        for q in ('sp', 'pool'):
            for r in range(self.R):
                k = ('d', q, r)
                self.sem[k] = es.enter_context(nc.semaphore('d_%s%d' % (q, r)))
                self.cnt[k] = 0
        self.dnext = {'sp': 0, 'pool': 0}
        self.waited = {}
        self.last_w = {}
        self.readers = {}

    def _wait(self, eng, dep):
        k, v = dep
        if k == 'pe' and eng == 'pe':
            return
        if self.waited.get((eng, k), 0) >= v:
            return
        self.eng[eng].wait_ge(self.sem[k], v)
        self.waited[(eng, k)] = v

    def _deps(self, eng, reads, writes):
        deps = {}
        def add(d):
            if d is None:
                return
            if deps.get(d[0], 0) < d[1]:
                deps[d[0]] = d[1]
        for r in reads:
            add(self.last_w.get(r))
        for w in writes:
            add(self.last_w.get(w))
            for d in self.readers.get(w, ()):
                add(d)
        for k, v in deps.items():
            self._wait(eng, (k, v))

    def _record(self, me, reads, writes):
        for r in reads:
            self.readers.setdefault(r, []).append(me)
        for w in writes:
            self.last_w[w] = me
            self.readers[w] = []

    def op(self, eng, inst_fn, reads=(), writes=()):
        reads = [getattr(r, 'k', r) for r in reads]
        writes = [getattr(w, 'k', w) for w in writes]
        self._deps(eng, reads, writes)
        inst = inst_fn(self.eng[eng])
        self.cnt[eng] += 1
        inst.then_inc(self.sem[eng], 1)
        self._record((eng, self.cnt[eng]), reads, writes)

    def dma(self, q, out, in_, reads=(), writes=(), **kw):
        reads = [getattr(r, 'k', r) for r in reads]
        writes = [getattr(w, 'k', w) for w in writes]
        self._deps(q, reads, writes)
        r = self.dnext[q]
        self.dnext[q] = (r + 1) % self.R
        k = ('d', q, r)
        inst = self.eng[q].dma_start(out=out, in_=in_, **kw)
        self.cnt[k] += 16
        inst.then_inc(self.sem[k], 16)
        self._record((k, self.cnt[k]), reads, writes)

    def barrier(self):
        for e in self.eng:
            for k, v in self.cnt.items():
                if v > 0 and k != e:
                    self._wait(e, (k, v))

    def finish(self):
        for k, v in self.cnt.items():
            if v > 0 and k != 'sp':
                self._wait('sp', (k, v))


class Tl:
    def __init__(self, t, k):
        self.t, self.k = t, k

    def __getitem__(self, idx):
        return self.t[idx]


def build_program(debug=False):
    nc = bass.Bass("TRN2", target_bir_lowering=False)

    def din(name, shape, dt=F32):
        return nc.dram_tensor(name, list(shape), dt, kind="ExternalInput").ap()

    def dout(name, shape, dt=F32):
        return nc.dram_tensor(name, list(shape), dt, kind="ExternalOutput").ap()

    def dscr(name, shape, dt=F32):
        return nc.dram_tensor(name, list(shape), dt, kind="ExternalOutput" if debug else "Internal").ap()

    xp = din("xp", [T, D])
    xs = din("xs", [16, D])
    memp = din("memp", [256, D])
    w_in = din("w_in", [D, MIXIN])
    w_mk = din("w_mk", [D, 512])
    w_mv = din("w_mv", [D, 512])
    g_mix = din("g_mix", [128, 8])
    g_mem = din("g_mem", [128, 8])
    ident = din("ident", [128, 128])
    lbl0 = din("lbl0", [128, 512])
    lbl1 = din("lbl1", [128, 512])
    triU_d = din("triU", [128, 128])
    tri4_d = din("tri4", [128, 512])
    rowmask_d = din("rowmask", [128, 1])
    st_h = din("st_h", [4, 4, 128, 128])
    maskC_d = din("maskC", [128, 1024])
    maskP_d = din("maskP", [128, 1024])
    ck = din("ck", [4, 2048, 512])
    cv = din("cv", [4, 2048, 512])
    bmask_d = din("bmask", [8, 528])
    w_out = din("w_out", [D, D])
    g_outc = din("g_outc", [128, 8])
    g_cross = din("g_cross", [128, 8])
    w_cq = din("w_cq", [D, 512])
    w_co = din("w_co", [512, D])
    cmk = din("cmk", [4, 256, 512])
    w_pq = din("w_pq", [D, 2048])
    g_ffn = din("g_ffn", [128, 8])
    g_ffnx = din("g_ffnx", [128, 1024])
    keysT = din("keysT", [128, 2048])
    ut_h = din("ut_h", [128, 128, 1024])
    v_h = din("v_h", [128, 128, 1024])
    g_fin = din("g_fin", [128, D])
    iota16_d = din("iota16", [128, 16])
    iota128_d = din("iota128", [128, 128])
    cmv = din("cmv", [4, 256, 512])
    y_p = dout("y_p", [T, D])
    y_s = dout("y_s", [16, D])
    o_pk = dout("o_pk", [2048, 512])
    o_pv = dout("o_pv", [2048, 512])
    o_ph = dout("o_ph", [4, 128, 128])
    o_mk = dout("o_mk", [256, 512])
    o_mv = dout("o_mv", [256, 512])
    o_sk = dout("o_sk", [16, 512])
    o_sv = dout("o_sv", [16, 512])
    o_sh = dout("o_sh", [4, 4, 128, 128])
    Z = dscr("Z", [NTT * 128, MIXIN])
    OB = dscr("OB", [NTT * 128, 512])
    OA = dscr("OA", [3, NTT * 128, 528])
    X2 = dscr("X2", [NTT * 128, D])
    X3 = dscr("X3", [NTT * 128, D]) if debug else None
    UTb = dscr("UTb", [128, 128, 1024], BF16)
    Vb = dscr("Vb", [128, 128, 1024], BF16)

    with contextlib.ExitStack() as es:
        S = Sync(nc, es)

        cur = [es]

        def sb(name, shape, dt=F32):
            return cur[0].enter_context(nc.sbuf_tensor(name, list(shape), dt))

        def ps(name, shape, dt=F32):
            return cur[0].enter_context(nc.psum_tensor(name, list(shape), dt))

        def tl(name, shape, dt=F32):
            return Tl(sb(name, shape, dt), name)

        def ptl(name, shape, dt=F32):
            return Tl(ps(name, shape, dt), name)

        ident_f = sb("ident_f", [128, 128])
        ident_b = sb("ident_b", [128, 128], BF16)
        gmix = sb("gmix", [128, 8])
        gmem = sb("gmem", [128, 8])
        S.dma('sp', ident_f[:], ident[:, :], writes=['ident_f'])
        S.dma('sp', gmix[:], g_mix[:, :], writes=['gmix'])
        S.dma('sp', gmem[:], g_mem[:, :], writes=['gmem'])
        S.op('dve', lambda e: e.tensor_copy(out=ident_b[:], in_=ident_f[:]), reads=['ident_f'], writes=['ident_b'])

        es_a = contextlib.ExitStack()
        es_a.__enter__()
        cur[0] = es_a
        win_b = sb("win_b", [128, 8 * MIXIN], BF16)
        wmk_b = sb("wmk_b", [128, 8 * 512], BF16)
        wmv_b = sb("wmv_b", [128, 8 * 512], BF16)
        wst = [sb("wst%d" % i, [128, MIXIN]) for i in range(2)]
        for kc in range(8):
            st = wst[kc % 2]
            S.dma('sp', st[:], w_in[kc * 128:(kc + 1) * 128, :], writes=['wst%d' % (kc % 2)])
            S.op('dve' if kc % 2 == 0 else 'pool',
                 lambda e, st=st, kc=kc: e.tensor_scalar(out=win_b[:, kc * MIXIN:(kc + 1) * MIXIN], in0=st[:],
                                                         scalar1=gmix[:, kc:kc + 1], scalar2=None, op0=ALU.mult),
                 reads=['wst%d' % (kc % 2), 'gmix'], writes=['win_b'])
        for kc in range(8):
            st = wst[kc % 2]
            S.dma('sp', st[:, 0:512], w_mk[kc * 128:(kc + 1) * 128, :], writes=['wst%d' % (kc % 2)])
            S.dma('sp', st[:, 512:1024], w_mv[kc * 128:(kc + 1) * 128, :], writes=['wst%d' % (kc % 2)])
            S.op('dve', lambda e, st=st, kc=kc: e.tensor_scalar(out=wmk_b[:, kc * 512:(kc + 1) * 512], in0=st[:, 0:512],
                                                                scalar1=gmem[:, kc:kc + 1], scalar2=None, op0=ALU.mult),
                 reads=['wst%d' % (kc % 2), 'gmem'], writes=['wmk_b'])
            S.op('pool', lambda e, st=st, kc=kc: e.tensor_scalar(out=wmv_b[:, kc * 512:(kc + 1) * 512], in0=st[:, 512:1024],
                                                                 scalar1=gmem[:, kc:kc + 1], scalar2=None, op0=ALU.mult),
                 reads=['wst%d' % (kc % 2), 'gmem'], writes=['wmv_b'])

        xt = [sb("xt%d" % i, [128, D]) for i in range(2)]
        junk = sb("junk", [128, D])
        ss = sb("ss", [128, 1])
        rstd = sb("rstd", [128, 1])
        hb = sb("hb", [128, D], BF16)
        hT = [sb("hT%d" % i, [128, D], BF16) for i in range(2)]
        pT = ps("pT", [128, D], BF16)
        pz = [ps("pz%d" % i, [128, 512]) for i in range(2)]
        zt = [sb("zt%d" % i, [128, MIXIN]) for i in range(2)]

        def front(xtile, xkey, hT_t, hkey):
            S.op('act', lambda e: e.activation(out=junk[:], in_=xtile[:], func=AF.Square, accum_out=ss[:]),
                 reads=[xkey], writes=['junk', 'ss'])
            S.op('dve', lambda e: e.tensor_scalar(out=rstd[:], in0=ss[:], scalar1=1.0 / D, scalar2=EPS,
                                                  op0=ALU.mult, op1=ALU.add), reads=['ss'], writes=['rstd'])
            S.op('act', lambda e: e.sqrt(out=rstd[:], in_=rstd[:]), reads=['rstd'], writes=['rstd'])
            S.op('dve', lambda e: e.reciprocal(out=rstd[:], in_=rstd[:]), reads=['rstd'], writes=['rstd'])
            S.op('dve', lambda e: e.tensor_scalar(out=hb[:], in0=xtile[:], scalar1=rstd[:, 0:1], scalar2=None,
                                                  op0=ALU.mult), reads=[xkey, 'rstd'], writes=['hb'])
            for kc in range(8):
                S.op('pe', lambda e, kc=kc: e.transpose(out=pT[:, kc * 128:(kc + 1) * 128],
                                                        in_=hb[:, kc * 128:(kc + 1) * 128], identity=ident_b[:]),
                     reads=['hb', 'ident_b'], writes=['pT'])
            S.op('act', lambda e: e.copy(out=hT_t[:], in_=pT[:]), reads=['pT'], writes=[hkey])

        for i in range(NTT):
            b = i % 2
            xkey = 'xt%d' % b
            if i < NT:
                S.dma('sp', xt[b][:], xp[i * 128:(i + 1) * 128, :], writes=[xkey])
            else:
                S.op('dve', lambda e, b=b: e.memset(xt[b][:], 0.0), writes=[xkey])
                S.dma('sp', xt[b][0:16, :], xs[:, :], writes=[xkey])
            front(xt[b], xkey, hT[b], 'hT%d' % b)
            for g in range(7):
                pzg = pz[g % 2]
                for kc in range(8):
                    S.op('pe', lambda e, kc=kc, g=g, pzg=pzg, b=b: e.matmul(
                        pzg[:], lhsT=hT[b][:, kc * 128:(kc + 1) * 128],
                        rhs=win_b[:, kc * MIXIN + g * 512: kc * MIXIN + (g + 1) * 512],
                        start=(kc == 0), stop=(kc == 7)),
                        reads=['hT%d' % b, 'win_b'], writes=['pz%d' % (g % 2)])
                if g % 2 == 0:
                    S.op('act', lambda e, g=g, pzg=pzg, b=b: e.copy(out=zt[b][:, g * 512:(g + 1) * 512], in_=pzg[:]),
                         reads=['pz%d' % (g % 2)], writes=['zt%d' % b])
                else:
                    S.op('dve', lambda e, g=g, pzg=pzg, b=b: e.tensor_copy(out=zt[b][:, g * 512:(g + 1) * 512], in_=pzg[:]),
                         reads=['pz%d' % (g % 2)], writes=['zt%d' % b])
            S.dma('pool', Z[i * 128:(i + 1) * 128, :], zt[b][:], reads=['zt%d' % b], writes=[('Z', i)])
            if 16 <= i < NT:
                r0 = (i - 16) * 128
                S.dma('pool', o_pk[r0:r0 + 128, :], zt[b][:, 512:1024], reads=['zt%d' % b])
                S.dma('pool', o_pv[r0:r0 + 128, :], zt[b][:, 1024:1536], reads=['zt%d' % b])
            if i == NT:
                S.dma('pool', o_sk[:, :], zt[b][0:16, 512:1024], reads=['zt%d' % b])
                S.dma('pool', o_sv[:, :], zt[b][0:16, 1024:1536], reads=['zt%d' % b])

        for i in range(2):
            b = i % 2
            xkey = 'xt%d' % b
            S.dma('sp', xt[b][:], memp[i * 128:(i + 1) * 128, :], writes=[xkey])
            front(xt[b], xkey, hT[b], 'hT%d' % b)
            for j, wb in enumerate((wmk_b, wmv_b)):
                pzg = pz[j]
                for kc in range(8):
                    S.op('pe', lambda e, kc=kc, wb=wb, pzg=pzg, b=b: e.matmul(
                        pzg[:], lhsT=hT[b][:, kc * 128:(kc + 1) * 128], rhs=wb[:, kc * 512:(kc + 1) * 512],
                        start=(kc == 0), stop=(kc == 7)),
                        reads=['hT%d' % b, 'wmk_b', 'wmv_b'], writes=['pz%d' % j])
                S.op('act' if j == 0 else 'dve',
                     (lambda e, j=j, pzg=pzg, b=b: e.copy(out=zt[b][:, j * 512:(j + 1) * 512], in_=pzg[:])) if j == 0 else
                     (lambda e, j=j, pzg=pzg, b=b: e.tensor_copy(out=zt[b][:, j * 512:(j + 1) * 512], in_=pzg[:])),
                     reads=['pz%d' % j], writes=['zt%d' % b])
            S.dma('pool', o_mk[i * 128:(i + 1) * 128, :], zt[b][:, 0:512], reads=['zt%d' % b])
            S.dma('pool', o_mv[i * 128:(i + 1) * 128, :], zt[b][:, 512:1024], reads=['zt%d' % b])

        es_a.close()
        cur[0] = es
        S.barrier()
        es_d = contextlib.ExitStack()
        es_d.__enter__()
        cur[0] = es_d
        triU = tl("triU_s", [128, 128])
        tri4 = tl("tri4_s", [128, 512])
        rowmask = tl("rowmask_s", [128, 1])
        lb_t = tl("lb_t", [128, 512])
        oml_t = tl("oml_t", [128, 512])
        l1_t = tl("l1_t", [128, 512])
        S.dma('sp', triU[:], triU_d[:, :], writes=[triU])
        S.dma('sp', tri4[:], tri4_d[:, :], writes=[tri4])
        S.dma('sp', rowmask[:], rowmask_d[:, :], writes=[rowmask])
        S.dma('sp', lb_t[:], lbl0[:, :], writes=[lb_t])
        S.dma('sp', l1_t[:], lbl1[:, :], writes=[l1_t])
        S.op('dve', lambda e: e.tensor_tensor(out=lb_t[:], in0=lb_t[:], in1=l1_t[:], op=ALU.subtract),
             reads=[lb_t, l1_t], writes=[lb_t])
        S.op('act', lambda e: e.activation(out=lb_t[:], in_=lb_t[:], func=AF.Sigmoid), reads=[lb_t], writes=[lb_t])
        S.op('dve', lambda e: e.tensor_scalar(out=oml_t[:], in0=lb_t[:], scalar1=-1.0, scalar2=1.0,
                                              op0=ALU.mult, op1=ALU.add), reads=[lb_t], writes=[oml_t])
        hz = [tl("hz%d" % i, [128, 2048]) for i in range(2)]
        sig = tl("sig", [128, 512])
        logf = tl("logf", [128, 512])
        kk = tl("kk", [128, 512])
        qs = tl("qs", [128, 512])
        gs = tl("gs", [128, 512])
        ib_b = tl("ib_b", [128, 512], BF16)
        ec = tl("ec", [128, 512])
        emc = tl("emc", [128, 512])
        ecl = tl("ecl", [128, 4])
        qd_b = tl("qd_b", [128, 512], BF16)
        kd_b = tl("kd_b", [128, 512], BF16)
        qkT = tl("qkT", [128, 1024], BF16)
        AT_b = tl("AT_b", [128, 512], BF16)
        Sst = tl("Sst", [128, 512])
        S_b = tl("S_b", [128, 512], BF16)
        ssq = tl("ssq", [128, 4])
        rb = tl("rb", [128, 4])
        hjunk = tl("hjunk", [128, 128])
        obn = [tl("obn%d" % i, [128, 512]) for i in range(2)]
        pc = ptl("pc", [128, 512])
        pcT = ptl("pcT", [128, 512])
        pTq = ptl("pTq", [128, 1024], BF16)
        pA = ptl("pA", [128, 512])
        po = ptl("po", [128, 512])
        pdS = ptl("pdS", [128, 512])

        P = 64

        def hs(h):
            return slice(h * 128, (h + 1) * 128)

        def hp(h):
            return slice(h * P, (h + 1) * P)

        def hgrn_chunk(ci, row0, nrows, masked):
            z = hz[ci % 2]
            ob = obn[ci % 2]
            if nrows < P:
                S.op('dve', lambda e: e.memset(z[:], 0.0), writes=[z])
            S.dma('sp', z[0:nrows, :], Z[row0:row0 + nrows, 1536:3584], reads=[('Z', row0 // 128)], writes=[z])
            S.op('act', lambda e: e.activation(out=sig[0:P, :], in_=z[0:P, 512:1024], func=AF.Sigmoid), reads=[z], writes=[sig])
            S.op('dve', lambda e: e.tensor_tensor(out=sig[0:P, :], in0=sig[0:P, :], in1=oml_t[0:P, :], op=ALU.mult),
                 reads=[sig, oml_t], writes=[sig])
            S.op('dve', lambda e: e.tensor_tensor(out=sig[0:P, :], in0=sig[0:P, :], in1=lb_t[0:P, :], op=ALU.add),
                 reads=[sig, lb_t], writes=[sig])
            S.op('act', lambda e: e.activation(out=logf[0:P, :], in_=sig[0:P, :], func=AF.Ln), reads=[sig], writes=[logf])
            S.op('dve', lambda e: e.tensor_scalar(out=kk[0:P, :], in0=sig[0:P, :], scalar1=-1.0, scalar2=1.0,
                                                  op0=ALU.mult, op1=ALU.add), reads=[sig], writes=[kk])
            if masked:
                S.op('dve', lambda e: e.tensor_scalar(out=logf[0:P, :], in0=logf[0:P, :], scalar1=rowmask[0:P, 0:1],
                                                      scalar2=None, op0=ALU.mult), reads=[logf, rowmask], writes=[logf])
                S.op('dve', lambda e: e.tensor_scalar(out=kk[0:P, :], in0=kk[0:P, :], scalar1=rowmask[0:P, 0:1],
                                                      scalar2=None, op0=ALU.mult), reads=[kk, rowmask], writes=[kk])
            S.op('act', lambda e: e.activation(out=qs[0:P, :], in_=z[0:P, 0:512], func=AF.Silu), reads=[z], writes=[qs])
            S.op('act', lambda e: e.activation(out=gs[0:P, :], in_=z[0:P, 1536:2048], func=AF.Silu), reads=[z], writes=[gs])
            S.op('dve', lambda e: e.tensor_copy(out=ib_b[0:P, :], in_=z[0:P, 1024:1536]), reads=[z], writes=[ib_b])
            S.op('pe', lambda e: e.matmul(pc[0:P, :], lhsT=triU[0:P, 0:P], rhs=logf[0:P, :], start=True, stop=True),
                 reads=[triU, logf], writes=[pc])
            for h in range(4):
                S.op('pe', lambda e, h=h: e.matmul(pcT[:, hp(h)], lhsT=logf[0:P, hs(h)], rhs=triU[0:P, 0:P],
                                                   start=True, stop=True), reads=[triU, logf], writes=[pcT])
            S.op('act', lambda e: e.activation(out=ec[0:P, :], in_=pc[0:P, :], func=AF.Exp), reads=[pc], writes=[ec])
            S.op('act', lambda e: e.activation(out=emc[0:P, :], in_=pc[0:P, :], func=AF.Exp, scale=-1.0),
                 reads=[pc], writes=[emc])
            S.op('act', lambda e: e.activation(out=ecl[:], in_=pcT[:, P - 1:4 * P:P], func=AF.Exp), reads=[pcT], writes=[ecl])
            S.op('dve', lambda e: e.tensor_tensor(out=qd_b[0:P, :], in0=qs[0:P, :], in1=ec[0:P, :], op=ALU.mult),
                 reads=[qs, ec], writes=[qd_b])
            S.op('dve', lambda e: e.tensor_tensor(out=kd_b[0:P, :], in0=kk[0:P, :], in1=emc[0:P, :], op=ALU.mult),
                 reads=[kk, emc], writes=[kd_b])
            for h in range(4):
                S.op('pe', lambda e, h=h: e.transpose(out=pTq[:, hp(h)], in_=qd_b[0:P, hs(h)], identity=ident_b[0:P, 0:P]),
                     reads=[qd_b, 'ident_b'], writes=[pTq])
                S.op('pe', lambda e, h=h: e.transpose(out=pTq[:, hp(4 + h)], in_=kd_b[0:P, hs(h)],
                                                      identity=ident_b[0:P, 0:P]), reads=[kd_b, 'ident_b'], writes=[pTq])
            S.op('act', lambda e: e.copy(out=qkT[:, 0:8 * P], in_=pTq[:, 0:8 * P]), reads=[pTq], writes=[qkT])
            for h in range(4):
                S.op('pe', lambda e, h=h: e.matmul(pA[0:P, hp(h)], lhsT=qkT[:, hp(4 + h)],
                                                   rhs=qkT[:, hp(h)], start=True, stop=True), reads=[qkT], writes=[pA])
            S.op('dve', lambda e: e.tensor_tensor(out=AT_b[0:P, 0:4 * P], in0=pA[0:P, 0:4 * P], in1=tri4[0:P, 0:4 * P],
                                                  op=ALU.mult), reads=[pA, tri4], writes=[AT_b])
            for h in range(4):
                S.op('pe', lambda e, h=h: e.matmul(po[0:P, hs(h)], lhsT=AT_b[0:P, hp(h)], rhs=ib_b[0:P, hs(h)],
                                                   start=True, stop=False), reads=[AT_b, ib_b], writes=[po])
                S.op('pe', lambda e, h=h: e.matmul(po[0:P, hs(h)], lhsT=qkT[:, hp(h)], rhs=S_b[:, hs(h)],
                                                   start=False, stop=True), reads=[qkT, S_b], writes=[po])
                S.op('pe', lambda e, h=h: e.matmul(pdS[:, hs(h)], lhsT=kd_b[0:P, hs(h)], rhs=ib_b[0:P, hs(h)],
                                                   start=True, stop=True), reads=[kd_b, ib_b], writes=[pdS])
            S.op('dve', lambda e: e.tensor_tensor(out=Sst[:], in0=Sst[:], in1=pdS[:], op=ALU.add),
                 reads=[Sst, pdS], writes=[Sst])
            for h in range(4):
                S.op('dve', lambda e, h=h: e.tensor_scalar(out=Sst[:, hs(h)], in0=Sst[:, hs(h)], scalar1=ecl[:, h:h + 1],
                                                           scalar2=None, op0=ALU.mult), reads=[Sst, ecl], writes=[Sst])
            S.op('dve', lambda e: e.tensor_copy(out=S_b[:], in_=Sst[:]), reads=[Sst], writes=[S_b])
            for h in range(4):
                S.op('act', lambda e, h=h: e.activation(out=hjunk[0:P, :], in_=po[0:P, hs(h)], func=AF.Square,
                                                        accum_out=ssq[0:P, h:h + 1]), reads=[po], writes=[hjunk, ssq])
            S.op('dve', lambda e: e.tensor_scalar(out=rb[0:P, :], in0=ssq[0:P, :], scalar1=1.0 / 128, scalar2=EPS,
                                                  op0=ALU.mult, op1=ALU.add), reads=[ssq], writes=[rb])
            S.op('act', lambda e: e.sqrt(out=rb[0:P, :], in_=rb[0:P, :]), reads=[rb], writes=[rb])
            S.op('dve', lambda e: e.reciprocal(out=rb[0:P, :], in_=rb[0:P, :]), reads=[rb], writes=[rb])
            for h in range(4):
                S.op('dve', lambda e, h=h: e.tensor_scalar(out=ob[0:P, hs(h)], in0=po[0:P, hs(h)], scalar1=rb[0:P, h:h + 1],
                                                           scalar2=None, op0=ALU.mult), reads=[po, rb], writes=[ob])
            S.op('dve', lambda e: e.tensor_tensor(out=ob[0:P, :], in0=ob[0:P, :], in1=gs[0:P, :], op=ALU.mult),
                 reads=[ob, gs], writes=[ob])
            S.dma('pool', OB[row0:row0 + nrows, :], ob[0:nrows, :], reads=[ob], writes=[('OB', row0 // 128)])

        S.op('dve', lambda e: e.memset(Sst[:], 0.0), writes=[Sst])
        S.op('dve', lambda e: e.memset(S_b[:], 0.0), writes=[S_b])
        for i in range(T // P):
            hgrn_chunk(i, i * P, P, False)
        for h in range(4):
            S.dma('pool', o_ph[h, :, :], Sst[:, hs(h)], reads=[Sst])
        for bb in range(4):
            for h in range(4):
                S.dma('sp', Sst[:, hs(h)], st_h[bb, h, :, :], writes=[Sst])
            S.op('dve', lambda e: e.tensor_copy(out=S_b[:], in_=Sst[:]), reads=[Sst], writes=[S_b])
            hgrn_chunk(bb, T + 4 * bb, 4, True)
            for h in range(4):
                S.dma('pool', o_sh[bb, h, :, :], Sst[:, hs(h)], reads=[Sst])
        es_d.close()
        cur[0] = es
        S.barrier()
        es_c = contextlib.ExitStack()
        es_c.__enter__()
        cur[0] = es_c
        mstage = tl("mstage", [128, 1024])
        maskC = tl("maskC_s", [128, 1024], BF16)
        maskP = tl("maskP_s", [128, 1024], BF16)
        S.dma('sp', mstage[:], maskC_d[:, :], writes=[mstage])
        S.op('dve', lambda e: e.tensor_copy(out=maskC[:], in_=mstage[:]), reads=[mstage], writes=[maskC])
        S.dma('sp', mstage[:], maskP_d[:, :], writes=[mstage])
        S.op('dve', lambda e: e.tensor_copy(out=maskP[:], in_=mstage[:]), reads=[mstage], writes=[maskP])
        az = [tl("az%d" % i, [128, 1536]) for i in range(2)]
        qk_b = tl("qk_b", [128, 1024], BF16)
        vaug = [tl("vaug%d" % i, [128, 8 * 66], BF16) for i in range(2)]
        qT = tl("qT", [64, 1024], BF16)
        kT = [tl("kT%d" % i, [64, 1024], BF16) for i in range(2)]
        PTc = tl("PTc", [128, 1024], BF16)
        PTp = tl("PTp", [128, 1024], BF16)
        osb = [tl("osb%d" % i, [128, 528]) for i in range(2)]
        pTt = ptl("pTt", [128, 1024], BF16)
        pTk = ptl("pTk", [128, 1024], BF16)
        psC = [ptl("psC%d" % i, [128, 512]) for i in range(2)]
        psP = [ptl("psP%d" % i, [128, 512]) for i in range(2)]
        pO = [ptl("pO%d" % i, [128, 512]) for i in range(2)]
        for i in range(2):
            S.op('dve', lambda e, i=i: e.memset(vaug[i][:], 1.0), writes=[vaug[i]])
        blk = 0
        for br, dil in enumerate(_BRANCHES):
            nb = T // dil // 128
            if dil == 1:
                Zv = Z[0:T, :].rearrange("(o m) c -> o m c", o=1)
                OAv = OA[br, 0:T, :].rearrange("(o m) c -> o m c", o=1)
            else:
                Zv = Z[0:T, :].rearrange("(m d) c -> d m c", d=dil)
                OAv = OA[br, 0:T, :].rearrange("(m d) c -> d m c", d=dil)
            for r in range(dil):
                for b in range(nb):
                    cb = blk % 2
                    pb = 1 - cb
                    a = az[cb]
                    S.dma('sp', a[:], Zv[r, b * 128:(b + 1) * 128, 0:1536], reads=[('Z', i) for i in range(NT)] if blk == 0 else [],
                          writes=[a])
                    S.op('dve', lambda e, a=a: e.tensor_copy(out=qk_b[:], in_=a[:, 0:1024]), reads=[a], writes=[qk_b])
                    S.op('act', lambda e, a=a, cb=cb: e.copy(
                        out=vaug[cb][:].rearrange("p (h e) -> p h e", e=66)[:, :, 0:64],
                        in_=a[:, 1024:1536].rearrange("p (h e) -> p h e", e=64)), reads=[a], writes=[vaug[cb]])
                    if blk >= _ATT_MAXB or _ATT_LVL < 2:
                        blk += 1
                        continue
                    for h in range(8):
                        S.op('pe', lambda e, h=h: e.transpose(out=pTt[0:64, h * 128:(h + 1) * 128],
                                                              in_=qk_b[:, h * 64:(h + 1) * 64], identity=ident_b[:]),
                             reads=[qk_b, 'ident_b'], writes=[pTt])
                        S.op('pe', lambda e, h=h: e.transpose(out=pTk[0:64, h * 128:(h + 1) * 128],
                                                              in_=qk_b[:, 512 + h * 64:512 + (h + 1) * 64],
                                                              identity=ident_b[:]),
                             reads=[qk_b, 'ident_b'], writes=[pTk])
                    S.op('act', lambda e: e.copy(out=qT[0:64, :], in_=pTt[0:64, :]), reads=[pTt], writes=[qT])
                    S.op('act', lambda e, cb=cb: e.copy(out=kT[cb][0:64, :], in_=pTk[0:64, :]), reads=[pTk], writes=[kT[cb]])
                    if _ATT_LVL < 3:
                        blk += 1
                        continue
                    for h in range(8):
                        p, j = h // 2, h % 2
                        S.op('pe', lambda e, h=h, p=p, j=j, cb=cb: e.matmul(
                            psC[h // 4][:, (h % 4) * 128:(h % 4 + 1) * 128], lhsT=kT[cb][0:64, h * 128:(h + 1) * 128],
                            rhs=qT[0:64, h * 128:(h + 1) * 128], start=True, stop=True),
                            reads=[kT[cb], qT], writes=[psC[h // 4]])
                    if b > 0:
                        for h in range(8):
                            p, j = h // 2, h % 2
                            S.op('pe', lambda e, h=h, p=p, j=j, pb=pb: e.matmul(
                                psP[h // 4][:, (h % 4) * 128:(h % 4 + 1) * 128], lhsT=kT[pb][0:64, h * 128:(h + 1) * 128],
                                rhs=qT[0:64, h * 128:(h + 1) * 128], start=True, stop=True),
                                reads=[kT[pb], qT], writes=[psP[h // 4]])
                    if _ATT_LVL < 4:
                        blk += 1
                        continue
                    for hh in range(2):
                        S.op('act', lambda e, hh=hh: e.activation(out=PTc[:, hh * 512:(hh + 1) * 512], in_=psC[hh][:],
                                                                  func=AF.Exp, scale=0.125), reads=[psC[hh]], writes=[PTc])
                    S.op('dve', lambda e: e.tensor_tensor(out=PTc[:], in0=PTc[:], in1=maskC[:], op=ALU.mult),
                         reads=[PTc, maskC], writes=[PTc])
                    if b > 0:
                        for hh in range(2):
                            S.op('act', lambda e, hh=hh: e.activation(out=PTp[:, hh * 512:(hh + 1) * 512], in_=psP[hh][:],
                                                                      func=AF.Exp, scale=0.125), reads=[psP[hh]], writes=[PTp])
                        S.op('pool', lambda e: e.tensor_tensor(out=PTp[:], in0=PTp[:], in1=maskP[:], op=ALU.mult),
                             reads=[PTp, maskP], writes=[PTp])
                    if _ATT_LVL < 5:
                        blk += 1
                        continue
                    for h in range(8):
                        po_t = pO[h // 4]
                        osl = slice((h % 4) * 66, (h % 4 + 1) * 66)
                        S.op('pe', lambda e, h=h, po_t=po_t, osl=osl, cb=cb: e.matmul(
                            po_t[:, osl], lhsT=PTc[:, h * 128:(h + 1) * 128], rhs=vaug[cb][:, h * 66:(h + 1) * 66],
                            start=True, stop=(b == 0)), reads=[PTc, vaug[cb]], writes=[po_t])
                        if b > 0:
                            S.op('pe', lambda e, h=h, po_t=po_t, osl=osl, pb=pb: e.matmul(
                                po_t[:, osl], lhsT=PTp[:, h * 128:(h + 1) * 128], rhs=vaug[pb][:, h * 66:(h + 1) * 66],
                                start=False, stop=True), reads=[PTp, vaug[pb]], writes=[po_t])
                    o = osb[cb]
                    S.op('act', lambda e, o=o: e.copy(out=o[:, 0:264], in_=pO[0][:, 0:264]), reads=[pO[0]], writes=[o])
                    S.op('dve', lambda e, o=o: e.tensor_copy(out=o[:, 264:528], in_=pO[1][:, 0:264]), reads=[pO[1]], writes=[o])
                    S.dma('pool', OAv[r, b * 128:(b + 1) * 128, :], o[:], reads=[o], writes=[('OA', br)])
                    blk += 1
        es_c.close()
        cur[0] = es
        S.barrier()
        es_e = contextlib.ExitStack()
        es_e.__enter__()
        cur[0] = es_e
        bmask = tl("bmask_s", [8, 528])
        S.dma('sp', bmask[:], bmask_d[:, :], writes=[bmask])
        skv = [tl("skv%d" % i, [128, 1024]) for i in range(2)]
        sqb = tl("sqb", [128, 512])
        sprod = tl("sprod", [128, 512])
        ssc = tl("ssc", [128, 8])
        sp_b = tl("sp_b", [128, 8], BF16)
        svaug = [tl("svaug%d" % i, [128, 528], BF16) for i in range(2)]
        sq8 = tl("sq8", [8, 64])
        sk8 = tl("sk8", [8, 64])
        sv8 = tl("sv8", [8, 64])
        sj8 = tl("sj8", [8, 64])
        ss8 = tl("ss8", [8, 1])
        sdiag = tl("sdiag", [8, 528])
        sres = [tl("sres%d" % i, [8, 66]) for i in range(2)]
        pSa = ptl("pSa", [8, 512])
        pSb = ptl("pSb", [8, 512])
        for i in range(2):
            S.op('dve', lambda e, i=i: e.memset(svaug[i][:], 1.0), writes=[svaug[i]])
        it = 0
        for bb in range(4):
            for t in range(4):
                row = T + 4 * bb + t
                S.dma('sp', sqb[:], bass.AP(tensor=Z.tensor, offset=row * MIXIN, ap=[[0, 128], [1, 512]]),
                      reads=[('Z', NT)], writes=[sqb])
                S.dma('sp', sq8[:], Z[row, 0:512].rearrange("(h e) -> h e", e=64), writes=[sq8])
                S.dma('sp', sk8[:], Z[row, 512:1024].rearrange("(h e) -> h e", e=64), writes=[sk8])
                S.dma('sp', sv8[:], Z[row, 1024:1536].rearrange("(h e) -> h e", e=64), writes=[sv8])
                for br, dil in enumerate((1, 4, 16)):
                    kv = skv[it % 2]
                    va = svaug[it % 2]
                    it += 1
                    start = 2048 + t - dil * 128
                    if dil == 1:
                        ncache = 128 - t
                        S.dma('sp', kv[0:ncache, 0:512], ck[bb, start:2048, :], writes=[kv])
                        S.dma('sp', kv[0:ncache, 512:1024], cv[bb, start:2048, :], writes=[kv])
                        if t > 0:
                            S.dma('sp', kv[ncache:128, :], Z[T + 4 * bb:T + 4 * bb + t, 512:1536], reads=[('Z', NT)], writes=[kv])
                    else:
                        S.dma('sp', kv[:, 0:512], bass.AP(tensor=ck.tensor, offset=(bb * 2048 + start) * 512,
                                                          ap=[[dil * 512, 128], [1, 512]]), writes=[kv])
                        S.dma('sp', kv[:, 512:1024], bass.AP(tensor=cv.tensor, offset=(bb * 2048 + start) * 512,
                                                             ap=[[dil * 512, 128], [1, 512]]), writes=[kv])
                    S.op('act', lambda e, kv=kv, va=va: e.copy(
                        out=va[:].rearrange("p (h e) -> p h e", e=66)[:, :, 0:64],
                        in_=kv[:, 512:1024].rearrange("p (h e) -> p h e", e=64)), reads=[kv], writes=[va])
                    S.op('dve', lambda e, kv=kv: e.tensor_tensor(out=sprod[:], in0=kv[:, 0:512], in1=sqb[:], op=ALU.mult),
                         reads=[kv, sqb], writes=[sprod])
                    S.op('dve', lambda e: e.tensor_reduce(out=ssc[:], in_=sprod[:].rearrange("p (h e) -> p h e", e=64),
                                                          axis=AX.X, op=ALU.add), reads=[sprod], writes=[ssc])
                    S.op('act', lambda e: e.activation(out=sp_b[:], in_=ssc[:], func=AF.Exp, scale=0.125),
                         reads=[ssc], writes=[sp_b])
                    S.op('pe', lambda e, va=va, br=br: e.matmul(pSa[:, 0:264], lhsT=sp_b[:], rhs=va[:, 0:264],
                                                                start=(br == 0), stop=(br == 2)),
                         reads=[sp_b, va], writes=[pSa])
                    S.op('pe', lambda e, va=va, br=br: e.matmul(pSb[:, 0:264], lhsT=sp_b[:], rhs=va[:, 264:528],
                                                                start=(br == 0), stop=(br == 2)),
                         reads=[sp_b, va], writes=[pSb])
                res = sres[(4 * bb + t) % 2]
                S.op('dve', lambda e: e.tensor_tensor(out=sdiag[:, 0:264], in0=pSa[:, 0:264], in1=bmask[:, 0:264], op=ALU.mult),
                     reads=[pSa, bmask], writes=[sdiag])
                S.op('dve', lambda e: e.tensor_tensor(out=sdiag[:, 264:528], in0=pSb[:, 0:264], in1=bmask[:, 264:528], op=ALU.mult),
                     reads=[pSb, bmask], writes=[sdiag])
                S.op('dve', lambda e, res=res: e.tensor_reduce(out=res[:], in_=sdiag[:].rearrange("p (h e) -> p e h", e=66),
                                                               axis=AX.X, op=ALU.add), reads=[sdiag], writes=[res])
                S.op('dve', lambda e: e.tensor_tensor(out=sj8[:], in0=sq8[:], in1=sk8[:], op=ALU.mult),
                     reads=[sq8, sk8], writes=[sj8])
                S.op('dve', lambda e: e.tensor_reduce(out=ss8[:], in_=sj8[:], axis=AX.X, op=ALU.add), reads=[sj8], writes=[ss8])
                S.op('act', lambda e: e.activation(out=ss8[:], in_=ss8[:], func=AF.Exp, scale=0.125), reads=[ss8], writes=[ss8])
                S.op('dve', lambda e: e.tensor_scalar(out=ss8[:], in0=ss8[:], scalar1=3.0, scalar2=None, op0=ALU.mult),
                     reads=[ss8], writes=[ss8])
                S.op('dve', lambda e, res=res: e.scalar_tensor_tensor(out=res[:, 0:64], in0=sv8[:], scalar=ss8[:, 0:1],
                                                                      in1=res[:, 0:64], op0=ALU.mult, op1=ALU.add),
                     reads=[sv8, ss8, res], writes=[res])
                S.op('dve', lambda e, res=res: e.tensor_tensor(out=res[:, 64:65], in0=res[:, 64:65], in1=ss8[:], op=ALU.add),
                     reads=[res, ss8], writes=[res])
                S.dma('pool', OA[0, row, :].rearrange("(h e) -> h e", e=66), res[:], reads=[res], writes=[('OA', 0)])
        es_e.close()
        cur[0] = es
        S.barrier()
        es_f = contextlib.ExitStack()
        es_f.__enter__()
        cur[0] = es_f
        goutc = tl("goutc", [128, 8])
        gcross = tl("gcross", [128, 8])
        S.dma('sp', goutc[:], g_outc[:, :], writes=[goutc])
        S.dma('sp', gcross[:], g_cross[:, :], writes=[gcross])
        wout_b = tl("wout_b", [128, 8 * 1024], BF16)
        wcq_b = tl("wcq_b", [128, 8 * 512], BF16)
        wco_b = tl("wco_b", [128, 4 * 1024], BF16)
        fst = [tl("fst%d" % i, [128, 1024]) for i in range(2)]
        for kc in range(8):
            st = fst[kc % 2]
            S.dma('sp', st[:], w_out[kc * 128:(kc + 1) * 128, :], writes=[st])
            S.op('dve', lambda e, st=st, kc=kc: e.tensor_scalar(out=wout_b[:, kc * 1024:(kc + 1) * 1024], in0=st[:],
                                                                scalar1=goutc[:, kc:kc + 1], scalar2=None, op0=ALU.mult),
                 reads=[st, goutc], writes=[wout_b])
        for kc in range(8):
            st = fst[kc % 2]
            S.dma('sp', st[:, 0:512], w_cq[kc * 128:(kc + 1) * 128, :], writes=[st])
            S.op('dve', lambda e, st=st, kc=kc: e.tensor_scalar(out=wcq_b[:, kc * 512:(kc + 1) * 512], in0=st[:, 0:512],
                                                                scalar1=gcross[:, kc:kc + 1], scalar2=None, op0=ALU.mult),
                 reads=[st, gcross], writes=[wcq_b])
        for kc in range(4):
            st = fst[kc % 2]
            S.dma('sp', st[:], w_co[kc * 128:(kc + 1) * 128, :], writes=[st])
            S.op('dve', lambda e, st=st, kc=kc: e.tensor_copy(out=wco_b[:, kc * 1024:(kc + 1) * 1024], in_=st[:]),
                 reads=[st], writes=[wco_b])
        ones_b = tl("ones_b", [128, 128], BF16)
        S.op('dve', lambda e: e.memset(ones_b[:], 1.0), writes=[ones_b])
        mkT = [tl("mkT%d" % i, [128, 4 * 256], BF16) for i in range(5)]
        mvb = [tl("mvb%d" % i, [128, 2 * 512], BF16) for i in range(5)]
        mkb = tl("mkb", [128, 512], BF16)
        pFT = ptl("pFT", [128, 1024], BF16)
        pTm = pFT
        for g in range(5):
            src_k = o_mk if g == 0 else cmk[g - 1]
            src_v = o_mv if g == 0 else cmv[g - 1]
            for mb in range(2):
                st = fst[mb]
                S.dma('sp', st[:, 0:512], src_k[mb * 128:(mb + 1) * 128, :], writes=[st])
                S.dma('sp', st[:, 512:1024], src_v[mb * 128:(mb + 1) * 128, :], writes=[st])
                S.op('dve', lambda e, st=st: e.tensor_copy(out=mkb[:], in_=st[:, 0:512]), reads=[st], writes=[mkb])
                S.op('dve', lambda e, st=st, g=g, mb=mb: e.tensor_copy(out=mvb[g][:, mb * 512:(mb + 1) * 512], in_=st[:, 512:1024]),
                     reads=[st], writes=[mvb[g]])
                for hh in range(4):
                    S.op('pe', lambda e, hh=hh: e.transpose(out=pTm[:, hh * 128:(hh + 1) * 128], in_=mkb[:, hh * 128:(hh + 1) * 128],
                                                            identity=ident_b[:]), reads=[mkb, 'ident_b'], writes=[pTm])
                S.op('act', lambda e, g=g, mb=mb: e.copy(
                    out=mkT[g][:].rearrange("p (h m) -> p h m", m=256)[:, :, mb * 128:(mb + 1) * 128],
                    in_=pTm[:, 0:512].rearrange("p (h m) -> p h m", m=128)), reads=[pTm], writes=[mkT[g]])
        xa = [tl("xa%d" % i, [128, D]) for i in range(2)]
        oat = [tl("oat%d" % i, [128, 528]) for i in range(3)]
        obt = tl("obt", [128, 512])
        rden = tl("rden", [128, 8])
        oan = tl("oan", [128, 512])
        cat = tl("cat", [128, D], BF16)
        fjunk = tl("fjunk", [128, D])
        fss = tl("fss", [128, 1])
        frs = tl("frs", [128, 1])
        fhb = tl("fhb", [128, D], BF16)
        catT = tl("catT", [128, D], BF16)
        h2T = tl("h2T", [128, D], BF16)
        qcT = tl("qcT", [128, 512], BF16)
        PTx = tl("PTx", [128, 1024], BF16)
        rdx = tl("rdx", [128, 512])
        oTx = tl("oTx", [128, 512], BF16)
        py = [ptl("py%d" % i, [128, 512]) for i in range(2)]
        pq = ptl("pq", [128, 512])
        psx = [ptl("psx%d" % i, [128, 512]) for i in range(2)]
        pox = ptl("pox", [128, 512])
        pdx = ptl("pdx", [128, 512])

        def normT(xtile, outT):
            S.op('act', lambda e: e.activation(out=fjunk[:], in_=xtile[:], func=AF.Square, accum_out=fss[:]),
                 reads=[xtile], writes=[fjunk, fss])
            S.op('dve', lambda e: e.tensor_scalar(out=frs[:], in0=fss[:], scalar1=1.0 / D, scalar2=EPS,
                                                  op0=ALU.mult, op1=ALU.add), reads=[fss], writes=[frs])
            S.op('act', lambda e: e.sqrt(out=frs[:], in_=frs[:]), reads=[frs], writes=[frs])
            S.op('dve', lambda e: e.reciprocal(out=frs[:], in_=frs[:]), reads=[frs], writes=[frs])
            S.op('dve', lambda e: e.tensor_scalar(out=fhb[:], in0=xtile[:], scalar1=frs[:, 0:1], scalar2=None,
                                                  op0=ALU.mult), reads=[xtile, frs], writes=[fhb])
            for kc in range(8):
                S.op('pe', lambda e, kc=kc: e.transpose(out=pFT[:, kc * 128:(kc + 1) * 128],
                                                        in_=fhb[:, kc * 128:(kc + 1) * 128], identity=ident_b[:]),
                     reads=[fhb, 'ident_b'], writes=[pFT])
            S.op('act', lambda e: e.copy(out=outT[:], in_=pFT[:]), reads=[pFT], writes=[outT])

        for i in range(NTT):
            x = xa[i % 2]
            nbr = 3 if i < NT else 1
            if i < NT:
                S.dma('sp', x[:], xp[i * 128:(i + 1) * 128, :], writes=[x])
            else:
                S.op('dve', lambda e, x=x: e.memset(x[:], 0.0), writes=[x])
                S.dma('sp', x[0:16, :], xs[:, :], writes=[x])
                for t3 in (oat[0], obt):
                    S.op('dve', lambda e, t3=t3: e.memset(t3[:], 1.0), writes=[t3])
            nr = 128 if i < NT else 16
            for br in range(nbr):
                S.dma('sp', oat[br][0:nr, :], OA[br, i * 128:i * 128 + nr, :], reads=[('OA', br)], writes=[oat[br]])
            S.dma('sp', obt[0:nr, :], OB[i * 128:i * 128 + nr, :], reads=[('OB', j) for j in range(NTT)], writes=[obt])
            for br in range(1, nbr):
                S.op('dve', lambda e, br=br: e.tensor_tensor(out=oat[0][:], in0=oat[0][:], in1=oat[br][:], op=ALU.add),
                     reads=[oat[0], oat[br]], writes=[oat[0]])
            S.op('dve', lambda e: e.reciprocal(out=rden[:], in_=oat[0][:].rearrange("p (h e) -> p h e", e=66)[:, :, 64]),
                 reads=[oat[0]], writes=[rden])
            for h in range(8):
                S.op('dve', lambda e, h=h: e.tensor_scalar(out=oan[:, h * 64:(h + 1) * 64], in0=oat[0][:, h * 66:h * 66 + 64],
                                                           scalar1=rden[:, h:h + 1], scalar2=None, op0=ALU.mult),
                     reads=[oat[0], rden], writes=[oan])
            S.op('act', lambda e: e.activation(out=fjunk[:, 0:512], in_=oan[:], func=AF.Square, accum_out=fss[:]),
                 reads=[oan], writes=[fjunk, fss])
            S.op('dve', lambda e: e.tensor_scalar(out=frs[:], in0=fss[:], scalar1=1.0 / 512, scalar2=EPS,
                                                  op0=ALU.mult, op1=ALU.add), reads=[fss], writes=[frs])
            S.op('act', lambda e: e.sqrt(out=frs[:], in_=frs[:]), reads=[frs], writes=[frs])
            S.op('dve', lambda e: e.reciprocal(out=frs[:], in_=frs[:]), reads=[frs], writes=[frs])
            S.op('dve', lambda e: e.tensor_scalar(out=cat[:, 0:512], in0=oan[:], scalar1=frs[:, 0:1], scalar2=None,
                                                  op0=ALU.mult), reads=[oan, frs], writes=[cat])
            S.op('dve', lambda e: e.tensor_copy(out=cat[:, 512:1024], in_=obt[:]), reads=[obt], writes=[cat])
            for kc in range(8):
                S.op('pe', lambda e, kc=kc: e.transpose(out=pFT[:, kc * 128:(kc + 1) * 128],
                                                        in_=cat[:, kc * 128:(kc + 1) * 128], identity=ident_b[:]),
                     reads=[cat, 'ident_b'], writes=[pFT])
            S.op('act', lambda e: e.copy(out=catT[:], in_=pFT[:]), reads=[pFT], writes=[catT])
            for half in range(2):
                for kc in range(8):
                    S.op('pe', lambda e, kc=kc, half=half: e.matmul(
                        py[half][:], lhsT=catT[:, kc * 128:(kc + 1) * 128],
                        rhs=wout_b[:, kc * 1024 + half * 512:kc * 1024 + (half + 1) * 512],
                        start=(kc == 0), stop=(kc == 7)), reads=[catT, wout_b], writes=[py[half]])
                S.op('dve', lambda e, half=half, x=x: e.tensor_tensor(out=x[:, half * 512:(half + 1) * 512],
                                                                     in0=x[:, half * 512:(half + 1) * 512], in1=py[half][:],
                                                                     op=ALU.add), reads=[x, py[half]], writes=[x])
            normT(x, h2T)
            for hh in range(4):
                for kc in range(8):
                    S.op('pe', lambda e, kc=kc, hh=hh: e.matmul(
                        pq[:, hh * 128:(hh + 1) * 128], lhsT=wcq_b[:, kc * 512 + hh * 128:kc * 512 + (hh + 1) * 128],
                        rhs=h2T[:, kc * 128:(kc + 1) * 128], start=(kc == 0), stop=(kc == 7)),
                        reads=[wcq_b, h2T], writes=[pq])
            S.op('act', lambda e: e.copy(out=qcT[:], in_=pq[:]), reads=[pq], writes=[qcT])
            groups = [(0, 128, 0)] if i < NT else [(4 * bb, 4, 1 + bb) for bb in range(4)]
            for (c0, ncol, g) in groups:
                for hh in range(4):
                    for mb in range(2):
                        S.op('pe', lambda e, hh=hh, mb=mb, c0=c0, ncol=ncol, g=g: e.matmul(
                            psx[mb][:, hh * 128 + c0:hh * 128 + c0 + ncol],
                            lhsT=mkT[g][:, hh * 256 + mb * 128:hh * 256 + (mb + 1) * 128],
                            rhs=qcT[:, hh * 128 + c0:hh * 128 + c0 + ncol], start=True, stop=True),
                            reads=[mkT[g], qcT], writes=[psx[mb]])
            for mb in range(2):
                S.op('act', lambda e, mb=mb: e.activation(out=PTx[:, mb * 512:(mb + 1) * 512], in_=psx[mb][:], func=AF.Exp,
                                                          scale=float(128 ** -0.5)), reads=[psx[mb]], writes=[PTx])
            for (c0, ncol, g) in groups:
                for hh in range(4):
                    for mb in range(2):
                        S.op('pe', lambda e, hh=hh, mb=mb, c0=c0, ncol=ncol, g=g: e.matmul(
                            pox[:, hh * 128 + c0:hh * 128 + c0 + ncol],
                            lhsT=mvb[g][:, mb * 512 + hh * 128:mb * 512 + (hh + 1) * 128],
                            rhs=PTx[:, mb * 512 + hh * 128 + c0:mb * 512 + hh * 128 + c0 + ncol],
                            start=(mb == 0), stop=(mb == 1)), reads=[mvb[g], PTx], writes=[pox])
                        S.op('pe', lambda e, hh=hh, mb=mb, c0=c0, ncol=ncol: e.matmul(
                            pdx[:, hh * 128 + c0:hh * 128 + c0 + ncol], lhsT=ones_b[:],
                            rhs=PTx[:, mb * 512 + hh * 128 + c0:mb * 512 + hh * 128 + c0 + ncol],
                            start=(mb == 0), stop=(mb == 1)), reads=[ones_b, PTx], writes=[pdx])
            S.op('dve', lambda e: e.reciprocal(out=rdx[:], in_=pdx[:]), reads=[pdx], writes=[rdx])
            S.op('dve', lambda e: e.tensor_tensor(out=oTx[:], in0=pox[:], in1=rdx[:], op=ALU.mult),
                 reads=[pox, rdx], writes=[oTx])
            for half in range(2):
                for hh in range(4):
                    S.op('pe', lambda e, hh=hh, half=half: e.matmul(
                        py[half][:], lhsT=oTx[:, hh * 128:(hh + 1) * 128],
                        rhs=wco_b[:, hh * 1024 + half * 512:hh * 1024 + (half + 1) * 512],
                        start=(hh == 0), stop=(hh == 3)), reads=[oTx, wco_b], writes=[py[half]])
                S.op('dve', lambda e, half=half, x=x: e.tensor_tensor(out=x[:, half * 512:(half + 1) * 512],
                                                                     in0=x[:, half * 512:(half + 1) * 512], in1=py[half][:],
                                                                     op=ALU.add), reads=[x, py[half]], writes=[x])
            S.dma('pool', X2[i * 128:(i + 1) * 128, :], x[:], reads=[x], writes=[('X2', i)])
        es_f.close()
        cur[0] = es
        S.barrier()
        es_g = contextlib.ExitStack()
        es_g.__enter__()
        cur[0] = es_g

        def cap(tile, off, dims):
            return bass.AP(tensor=tile.t, offset=off, ap=dims)

        gffn = tl("gffn", [128, 8])
        gfin = tl("gfin", [128, D])
        iota16 = tl("iota16_s", [128, 16])
        iota128 = tl("iota128_s", [128, 128])
        S.dma('sp', gffn[:], g_ffn[:, :], writes=[gffn])
        S.dma('sp', gfin[:], g_fin[:, :], writes=[gfin])
        S.dma('sp', iota16[:], iota16_d[:, :], writes=[iota16])
        S.dma('sp', iota128[:], iota128_d[:, :], writes=[iota128])
        sc = tl("sc", [128, 2048])
        scw = tl("scw", [128, 2048])
        cand = tl("cand", [128, 2048])
        gffnx = cand
        wpq_b = tl("wpq_b", [128, 8 * 2048], BF16)
        keys_b = tl("keys_b", [128, 2048], BF16)
        gst = [sc, scw]
        S.dma('sp', gffnx[:, 0:1024], g_ffnx[:, :], writes=[gffnx])
        for kc in range(8):
            for hf in range(2):
                st = gst[hf]
                S.dma('sp', st[:, 0:1024], w_pq[kc * 128:(kc + 1) * 128, hf * 1024:(hf + 1) * 1024], writes=[st])
                S.op('dve', lambda e, st=st, kc=kc, hf=hf: e.tensor_scalar(
                    out=wpq_b[:, kc * 2048 + hf * 1024:kc * 2048 + (hf + 1) * 1024], in0=st[:, 0:1024],
                    scalar1=gffn[:, kc:kc + 1], scalar2=None, op0=ALU.mult), reads=[st, gffn], writes=[wpq_b])
        for hf in range(2):
            S.dma('sp', gst[hf][:, 0:1024], keysT[:, hf * 1024:(hf + 1) * 1024], writes=[gst[hf]])
            S.op('dve', lambda e, hf=hf: e.tensor_copy(out=keys_b[:, hf * 1024:(hf + 1) * 1024], in_=gst[hf][:, 0:1024]),
                 reads=[gst[hf]], writes=[keys_b])
        utb = [tl("utb%d" % i, [128, 1024], BF16) for i in range(8)]
        vtb = [tl("vtb%d" % i, [128, 1024], BF16) for i in range(8)]
        for j in range(128):
            su, sv = gst[0], gst[1]
            S.dma('sp', su[:, 0:1024], ut_h[j, :, :], writes=[su])
            S.dma('sp', sv[:, 0:1024], v_h[j, :, :], writes=[sv])
            cu, cv2 = utb[j % 2], vtb[j % 2]
            S.op('dve', lambda e, cu=cu: e.tensor_tensor(out=cu[:], in0=su[:, 0:1024], in1=gffnx[:, 0:1024], op=ALU.mult),
                 reads=[su, gffnx], writes=[cu])
            S.op('act', lambda e, cv2=cv2: e.copy(out=cv2[:], in_=sv[:, 0:1024]), reads=[sv], writes=[cv2])
            S.dma('pool', UTb[j, :, :], cu[:], reads=[cu], writes=[('UTb', j)])
            S.dma('pool', Vb[j, :, :], cv2[:], reads=[cv2], writes=[('Vb', j)])
        xg = [tl("xg%d" % i, [128, D]) for i in range(2)]
        gjunk = tl("gjunk", [128, D])
        gss = tl("gss", [128, 1])
        grs = tl("grs", [128, 1])
        ghb = tl("ghb", [128, D], BF16)
        h3Tb = [tl("h3T%d" % i, [128, D], BF16) for i in range(2)]
        qTb = tl("qTb", [128, 2048], BF16)
        v16 = tl("v16", [128, 256])
        i16 = tl("i16", [128, 256], U32)
        i16f = tl("i16f", [128, 256])
        candw = scw
        c16 = tl("c16", [128, 128])
        ci = tl("ci", [128, 128], U32)
        ca_u = tl("ca_u", [128, 128], U32)
        cb_u = tl("cb_u", [128, 128], U32)
        ca_f = tl("ca_f", [128, 128])
        cb_f = tl("cb_f", [128, 128])
        eq = cand
        IG = tl("IG", [128, 384])
        gsum = tl("gsum", [128, 8])
        IGT = tl("IGT", [128, 384])
        NQ = 8
        Lh = tl("Lh", [128, NQ * 128], BF16)
        Rh = tl("Rh", [128, NQ * 128], BF16)
        Gsbb = [tl("Gsb%d" % i, [128, 16384], BF16) for i in range(2)]
        ga = [tl("ga%d" % i, [128, 128]) for i in range(4)]
        Wb = [tl("Wb%d" % i, [128, 128], BF16) for i in range(4)]
        yo = gjunk
        pGT = ptl("pGT", [128, 1024], BF16)
        pqs = ptl("pqs", [128, 512])
        pG = [ptl("pG%d" % i, [128, 512]) for i in range(2)]
        pAbank = [ps("pAb%d" % i, [128, 512]) for i in range(2)]

        class PSlot:
            def __init__(self, bank, off, k):
                self.bank, self.off, self.k = bank, off, k

            def ap(self):
                return self.bank[:, self.off:self.off + 128]

        pA = [PSlot(pAbank[s_ % 2], 0, 'pAslot%d' % (s_ % 2)) for s_ in range(4)]
        py3 = [ptl("py3_%d" % i, [128, 512]) for i in range(2)]

        def prep(i):
            x = xg[i % 2]
            h3T = h3Tb[i % 2]
            Gsb = Gsbb[i % 2]
            S.dma('sp', x[:], X2[i * 128:(i + 1) * 128, :], reads=[('X2', i)], writes=[x])
            S.op('act', lambda e, x=x: e.activation(out=gjunk[:], in_=x[:], func=AF.Square, accum_out=gss[:]),
                 reads=[x], writes=[gjunk, gss])
            S.op('dve', lambda e: e.tensor_scalar(out=grs[:], in0=gss[:], scalar1=1.0 / D, scalar2=EPS,
                                                  op0=ALU.mult, op1=ALU.add), reads=[gss], writes=[grs])
            S.op('act', lambda e: e.sqrt(out=grs[:], in_=grs[:]), reads=[grs], writes=[grs])
            S.op('dve', lambda e: e.reciprocal(out=grs[:], in_=grs[:]), reads=[grs], writes=[grs])
            S.op('dve', lambda e, x=x: e.tensor_scalar(out=ghb[:], in0=x[:], scalar1=grs[:, 0:1], scalar2=None,
                                                       op0=ALU.mult), reads=[x, grs], writes=[ghb])
            for kc in range(8):
                S.op('pe', lambda e, kc=kc: e.transpose(out=pGT[:, kc * 128:(kc + 1) * 128],
                                                        in_=ghb[:, kc * 128:(kc + 1) * 128], identity=ident_b[:]),
                     reads=[ghb, 'ident_b'], writes=[pGT])
            S.op('act', lambda e: e.copy(out=h3T[:], in_=pGT[:]), reads=[pGT], writes=[h3T])
            for cg in range(4):
                for cc in range(4):
                    c = cg * 4 + cc
                    for kc in range(8):
                        S.op('pe', lambda e, kc=kc, c=c, cc=cc: e.matmul(
                            pqs[:, cc * 128:(cc + 1) * 128], lhsT=wpq_b[:, kc * 2048 + c * 128:kc * 2048 + (c + 1) * 128],
                            rhs=h3T[:, kc * 128:(kc + 1) * 128], start=(kc == 0), stop=(kc == 7)),
                            reads=[wpq_b, h3T], writes=[pqs])
                S.op('act', lambda e, cg=cg: e.copy(out=qTb[:, cg * 512:(cg + 1) * 512], in_=pqs[:]), reads=[pqs], writes=[qTb])
            for cg in range(4):
                for cc in range(4):
                    c = cg * 4 + cc
                    S.op('pe', lambda e, c=c, cc=cc: e.matmul(
                        pqs[:, cc * 128:(cc + 1) * 128], lhsT=qTb[:, c * 128:(c + 1) * 128],
                        rhs=keys_b[:, c * 128:(c + 1) * 128], start=True, stop=True), reads=[qTb, keys_b], writes=[pqs])
                S.op('act', lambda e, cg=cg: e.copy(out=sc[:, cg * 512:(cg + 1) * 512], in_=pqs[:]), reads=[pqs], writes=[sc])
            for c in range(16):
                cs = slice(c * 128, (c + 1) * 128)
                S.op('dve', lambda e, c=c, cs=cs: e.max(out=v16[:, c * 16:c * 16 + 8], in_=sc[:, cs]), reads=[sc], writes=[v16])
                S.op('dve', lambda e, c=c, cs=cs: e.max_index(out=i16[:, c * 16:c * 16 + 8], in_max=v16[:, c * 16:c * 16 + 8],
                                                              in_values=sc[:, cs]), reads=[sc, v16], writes=[i16])
                S.op('dve', lambda e, c=c, cs=cs: e.match_replace(out=scw[:, cs], in_to_replace=v16[:, c * 16:c * 16 + 8],
                                                                  in_values=sc[:, cs], imm_value=-1e30),
                     reads=[sc, v16], writes=[scw])
                S.op('dve', lambda e, c=c, cs=cs: e.max(out=v16[:, c * 16 + 8:c * 16 + 16], in_=scw[:, cs]),
                     reads=[scw], writes=[v16])
                S.op('dve', lambda e, c=c, cs=cs: e.max_index(out=i16[:, c * 16 + 8:c * 16 + 16],
                                                              in_max=v16[:, c * 16 + 8:c * 16 + 16], in_values=scw[:, cs]),
                     reads=[scw, v16], writes=[i16])
            S.op('dve', lambda e: e.tensor_copy(out=i16f[:], in_=i16[:]), reads=[i16], writes=[i16f])
            S.op('dve', lambda e: e.tensor_tensor(
                out=cand[:].rearrange("p (h a b) -> p h a b", h=8, a=16),
                in0=cap(v16, 0, [[256, 128], [32, 8], [1, 16], [0, 16]]),
                in1=cap(v16, 16, [[256, 128], [32, 8], [0, 16], [1, 16]]), op=ALU.add), reads=[v16], writes=[cand])
            for h in range(8):
                cs = slice(h * 256, (h + 1) * 256)
                S.op('dve', lambda e, h=h, cs=cs: e.max(out=c16[:, h * 16:h * 16 + 8], in_=cand[:, cs]), reads=[cand], writes=[c16])
                S.op('dve', lambda e, h=h, cs=cs: e.max_index(out=ci[:, h * 16:h * 16 + 8], in_max=c16[:, h * 16:h * 16 + 8],
                                                              in_values=cand[:, cs]), reads=[cand, c16], writes=[ci])
                S.op('dve', lambda e, h=h, cs=cs: e.match_replace(out=candw[:, cs], in_to_replace=c16[:, h * 16:h * 16 + 8],
                                                                  in_values=cand[:, cs], imm_value=-1e30),
                     reads=[cand, c16], writes=[candw])
                S.op('dve', lambda e, h=h, cs=cs: e.max(out=c16[:, h * 16 + 8:h * 16 + 16], in_=candw[:, cs]),
                     reads=[candw], writes=[c16])
                S.op('dve', lambda e, h=h, cs=cs: e.max_index(out=ci[:, h * 16 + 8:h * 16 + 16],
                                                              in_max=c16[:, h * 16 + 8:h * 16 + 16], in_values=candw[:, cs]),
                     reads=[candw, c16], writes=[ci])
            S.op('dve', lambda e: e.tensor_single_scalar(out=ca_u[:], in_=ci[:], scalar=4, op=ALU.logical_shift_right),
                 reads=[ci], writes=[ca_u])
            S.op('dve', lambda e: e.tensor_single_scalar(out=cb_u[:], in_=ci[:], scalar=15, op=ALU.bitwise_and),
                 reads=[ci], writes=[cb_u])
            S.op('dve', lambda e: e.tensor_copy(out=ca_f[:], in_=ca_u[:]), reads=[ca_u], writes=[ca_f])
            S.op('dve', lambda e: e.tensor_copy(out=cb_f[:], in_=cb_u[:]), reads=[cb_u], writes=[cb_f])
            for which, (src_f, off) in enumerate(((ca_f, 0), (cb_f, 16))):
                S.op('dve', lambda e, src_f=src_f: e.tensor_tensor(
                    out=eq[:].rearrange("p (s a) -> p s a", a=16),
                    in0=cap(src_f, 0, [[128, 128], [1, 128], [0, 16]]),
                    in1=cap(iota16, 0, [[16, 128], [0, 128], [1, 16]]), op=ALU.is_equal),
                    reads=[src_f, iota16], writes=[eq])
                S.op('dve', lambda e, off=off: e.tensor_tensor(
                    out=eq[:].rearrange("p (h k a) -> p h k a", h=8, k=16),
                    in0=eq[:].rearrange("p (h k a) -> p h k a", h=8, k=16),
                    in1=cap(i16f, off, [[256, 128], [32, 8], [0, 16], [1, 16]]), op=ALU.mult),
                    reads=[eq, i16f], writes=[eq])
                S.op('dve', lambda e, which=which: e.tensor_reduce(
                    out=IG[:, which * 128:(which + 1) * 128], in_=eq[:].rearrange("p (s a) -> p s a", a=16),
                    axis=AX.X, op=ALU.add), reads=[eq], writes=[IG])
            S.op('dve', lambda e: e.tensor_tensor(
                out=IG[:, 256:384].rearrange("p (h k) -> p h k", k=16), in0=c16[:].rearrange("p (h k) -> p h k", k=16),
                in1=cap(c16, 0, [[128, 128], [16, 8], [0, 16]]), op=ALU.subtract), reads=[c16], writes=[IG])
            S.op('act', lambda e: e.activation(out=IG[:, 256:384], in_=IG[:, 256:384], func=AF.Exp), reads=[IG], writes=[IG])
            S.op('dve', lambda e: e.tensor_reduce(out=gsum[:], in_=IG[:, 256:384].rearrange("p (h k) -> p h k", k=16),
                                                  axis=AX.X, op=ALU.add), reads=[IG], writes=[gsum])
            S.op('dve', lambda e: e.reciprocal(out=gsum[:], in_=gsum[:]), reads=[gsum], writes=[gsum])
            S.op('dve', lambda e: e.tensor_tensor(
                out=IG[:, 256:384].rearrange("p (h k) -> p h k", k=16), in0=IG[:, 256:384].rearrange("p (h k) -> p h k", k=16),
                in1=cap(gsum, 0, [[8, 128], [1, 8], [0, 16]]), op=ALU.mult), reads=[IG, gsum], writes=[IG])
            for w3 in range(3):
                S.op('pe', lambda e, w3=w3: e.transpose(out=pqs[:, w3 * 128:(w3 + 1) * 128], in_=IG[:, w3 * 128:(w3 + 1) * 128],
                                                        identity=ident_f[:]), reads=[IG, 'ident_f'], writes=[pqs])
            S.op('act', lambda e: e.copy(out=IGT[:], in_=pqs[:, 0:384]), reads=[pqs], writes=[IGT])
            for hf in range(128 // NQ):
                S.op('dve', lambda e, hf=hf: e.tensor_tensor(
                    out=Lh[:].rearrange("p (t i) -> p t i", i=128),
                    in0=cap(iota128, 0, [[128, 128], [0, NQ], [1, 128]]),
                    in1=cap(IGT, hf * NQ, [[384, 128], [1, NQ], [0, 128]]), op=ALU.is_equal),
                    reads=[iota128, IGT], writes=[Lh])
                S.op('dve', lambda e, hf=hf: e.tensor_tensor(
                    out=Rh[:].rearrange("p (t i) -> p t i", i=128),
                    in0=cap(iota128, 0, [[128, 128], [0, NQ], [1, 128]]),
                    in1=cap(IGT, 128 + hf * NQ, [[384, 128], [1, NQ], [0, 128]]), op=ALU.is_equal),
                    reads=[iota128, IGT], writes=[Rh])
                S.op('pool', lambda e, hf=hf: e.tensor_tensor(
                    out=Rh[:].rearrange("p (t i) -> p t i", i=128),
                    in0=Rh[:].rearrange("p (t i) -> p t i", i=128),
                    in1=cap(IGT, 256 + hf * NQ, [[384, 128], [1, NQ], [0, 128]]), op=ALU.mult),
                    reads=[Rh, IGT], writes=[Rh])
                for t4 in range(NQ // 4):
                    pg = pG[t4 % 2]
                    for tt in range(4):
                        tl_ = t4 * 4 + tt
                        S.op('pe', lambda e, pg=pg, tt=tt, tl_=tl_: e.matmul(
                            pg[:, tt * 128:(tt + 1) * 128], lhsT=Lh[:, tl_ * 128:(tl_ + 1) * 128],
                            rhs=Rh[:, tl_ * 128:(tl_ + 1) * 128], start=True, stop=True), reads=[Lh, Rh], writes=[pg])
                    g0 = (hf * NQ + t4 * 4) * 128
                    if t4 % 2 == 0:
                        S.op('act', lambda e, pg=pg, g0=g0: e.copy(out=Gsb[:, g0:g0 + 512], in_=pg[:]), reads=[pg], writes=[Gsb])
                    else:
                        S.op('dve', lambda e, pg=pg, g0=g0: e.tensor_copy(out=Gsb[:, g0:g0 + 512], in_=pg[:]),
                             reads=[pg], writes=[Gsb])

        def record(fn, *args):
            rec = []
            o_op, o_dma = S.op, S.dma
            S.op = lambda *a, **k: rec.append((o_op, a, k))
            S.dma = lambda *a, **k: rec.append((o_dma, a, k))
            try:
                fn(*args)
            finally:
                S.op, S.dma = o_op, o_dma
            return rec

        def dense(i, nxt):
            x = xg[i % 2]
            h3T = h3Tb[i % 2]
            Gsb = Gsbb[i % 2]
            pos = [0]

            def pump(upto):
                while pos[0] < min(upto, len(nxt)):
                    f, a, k = nxt[pos[0]]
                    f(*a, **k)
                    pos[0] += 1
            def stage_u(j):
                j4 = j % 4
                j8 = j % 8
                S.dma('sp', utb[j8][:], UTb[j, :, :], reads=[('UTb', j)], writes=[utb[j8]])
                S.dma('sp', vtb[j8][:], Vb[j, :, :], reads=[('Vb', j)], writes=[vtb[j8]])
                for kc in range(8):
                    S.op('pe', lambda e, kc=kc, j4=j4, j8=j8: e.matmul(
                        pA[j4].ap(), lhsT=utb[j8][:, kc * 128:(kc + 1) * 128], rhs=h3T[:, kc * 128:(kc + 1) * 128],
                        start=(kc == 0), stop=(kc == 7)), reads=[utb[j8], h3T], writes=[pA[j4]])
                S.op('act', lambda e, j4=j4: e.activation(out=ga[j4][:], in_=pA[j4].ap(), func=AF.Gelu),
                     reads=[pA[j4]], writes=[ga[j4]])
                S.op('dve', lambda e, j4=j4, j=j: e.tensor_tensor(
                    out=Wb[j4][:], in0=ga[j4][:], in1=cap(Gsb, j, [[16384, 128], [128, 128]]), op=ALU.mult),
                    reads=[ga[j4], Gsb], writes=[Wb[j4]])

            def stage_v(j):
                j4 = j % 4
                j8 = j % 8
                for half in range(2):
                    S.op('pe', lambda e, half=half, j=j, j4=j4, j8=j8: e.matmul(
                        py3[half][:], lhsT=Wb[j4][:], rhs=vtb[j8][:, half * 512:(half + 1) * 512],
                        start=(j == 0), stop=(j == 127)), reads=[Wb[j4], vtb[j8]], writes=[py3[half]])

            stage_u(0)
            stage_u(1)
            for j in range(128):
                if j + 2 < 128:
                    stage_u(j + 2)
                stage_v(j)
                pump(((j + 1) * len(nxt) + 119) // 120)
            pump(len(nxt))
            for half in range(2):
                S.op('dve', lambda e, half=half, x=x: e.tensor_tensor(out=x[:, half * 512:(half + 1) * 512],
                                                                     in0=x[:, half * 512:(half + 1) * 512], in1=py3[half][:],
                                                                     op=ALU.add), reads=[x, py3[half]], writes=[x])
            if debug:
                S.dma('pool', X3[i * 128:(i + 1) * 128, :], x[:], reads=[x])
            S.op('act', lambda e, x=x: e.activation(out=gjunk[:], in_=x[:], func=AF.Square, accum_out=gss[:]),
                 reads=[x], writes=[gjunk, gss])
            S.op('dve', lambda e: e.tensor_scalar(out=grs[:], in0=gss[:], scalar1=1.0 / D, scalar2=EPS,
                                                  op0=ALU.mult, op1=ALU.add), reads=[gss], writes=[grs])
            S.op('act', lambda e: e.sqrt(out=grs[:], in_=grs[:]), reads=[grs], writes=[grs])
            S.op('dve', lambda e: e.reciprocal(out=grs[:], in_=grs[:]), reads=[grs], writes=[grs])
            S.op('dve', lambda e, x=x: e.scalar_tensor_tensor(out=yo[:], in0=x[:], scalar=grs[:, 0:1], in1=gfin[:],
                                                              op0=ALU.mult, op1=ALU.mult), reads=[x, grs, gfin], writes=[yo])
            if i < NT:
                S.dma('pool', y_p[i * 128:(i + 1) * 128, :], yo[:], reads=[yo])
            else:
                S.dma('pool', y_s[:, :], yo[0:16, :], reads=[yo])

        prep(0)
        for i in range(NTT):
            nxt = record(prep, i + 1) if i + 1 < NTT else []
            dense(i, nxt)
        es_g.close()
        cur[0] = es
        S.finish()
    return nc


_PROGRAM = None
_DEBUG_HOOK = None


def kernel(x_prompt, x_sample, cache_swa_k, cache_swa_v, state_hgrn, cache_mem_k, cache_mem_v,
           mem_prompt, norm_mix, w_in, lb_logits, beta_a, gnorm_b, w_out, norm_cross, norm_mem,
           w_cq, w_mk, w_mv, w_co, norm_ffn, w_pq, peer_k1, peer_k2, peer_u, peer_v, norm_final):
    global _PROGRAM
    f = lambda a: np.ascontiguousarray(np.asarray(a, dtype=np.float32))
    if _PROGRAM is None:
        _PROGRAM = build_program()
    nc = _PROGRAM

    def col(g):
        return f(np.asarray(g).reshape(-1, 128).T)

    common = {
        "w_in": f(w_in[0]), "w_mk": f(w_mk[0]), "w_mv": f(w_mv[0]),
        "g_mix": col(norm_mix[0]), "g_mem": col(norm_mem[0]),
        "w_out": f(w_out[0]), "w_cq": f(w_cq[0]), "w_co": f(w_co[0]),
        "g_outc": col(np.concatenate([np.asarray(beta_a[0]).reshape(-1), np.asarray(gnorm_b[0]).reshape(-1)])),
        "g_cross": col(norm_cross[0]),
        "w_pq": f(w_pq[0]), "g_ffn": col(norm_ffn[0]),
        "g_ffnx": f(np.repeat(np.asarray(norm_ffn[0]).reshape(8, 128).T[:, :, None], 128, axis=2).reshape(128, 1024)),
        "keysT": f(np.stack([np.asarray(peer_k1[0]), np.asarray(peer_k2[0])], axis=1).reshape(16, 128, 128)
                   .transpose(2, 0, 1).reshape(128, 2048)),
        "ut_h": f(np.asarray(peer_u[0]).reshape(128, 128, 8, 128).transpose(1, 3, 2, 0).reshape(128, 128, 1024)),
        "v_h": f(np.asarray(peer_v[0]).reshape(128, 128, 1024).transpose(1, 0, 2)),
        "g_fin": f(np.broadcast_to(np.asarray(norm_final)[None, :], (128, D))),
        "iota16": f(np.broadcast_to(np.arange(16)[None, :], (128, 16))),
        "iota128": f(np.broadcast_to(np.arange(128)[None, :], (128, 128))),
        "ident": np.eye(128, dtype=np.float32),
        "lbl0": f(np.broadcast_to(np.asarray(lb_logits)[0][None, :], (128, 512))),
        "lbl1": f(np.broadcast_to(np.asarray(lb_logits)[1][None, :], (128, 512))),
        "triU": np.triu(np.ones((128, 128), np.float32)),
        "tri4": np.tile(np.triu(np.ones((128, 64), np.float32)), (1, 8)),
        "rowmask": (np.arange(128) < 4).astype(np.float32).reshape(128, 1),
        "bmask": np.kron(np.eye(8, dtype=np.float32), np.ones((1, 66), np.float32)),
        "maskC": np.tile(np.triu(np.ones((128, 128), np.float32)), (1, 8)),
        "maskP": np.tile(np.tril(np.ones((128, 128), np.float32)), (1, 8)),
    }
    in_maps = []
    for c in range(NCORES):
        m = dict(common)
        m["xp"] = f(x_prompt[c])
        m["xs"] = f(np.asarray(x_sample[4 * c:4 * c + 4]).reshape(16, D))
        m["memp"] = f(mem_prompt[c])
        m["st_h"] = f(state_hgrn[0, 4 * c:4 * c + 4])
        m["cmk"] = f(np.asarray(cache_mem_k[0, 4 * c:4 * c + 4]).reshape(4, 256, 512))
        m["cmv"] = f(np.asarray(cache_mem_v[0, 4 * c:4 * c + 4]).reshape(4, 256, 512))
        m["ck"] = f(np.asarray(cache_swa_k[0, 4 * c:4 * c + 4]).reshape(4, 2048, 512))
        m["cv"] = f(np.asarray(cache_swa_v[0, 4 * c:4 * c + 4]).reshape(4, 2048, 512))
        in_maps.append(m)
    if _DEBUG_HOOK is not None:
        return _DEBUG_HOOK(in_maps)
    res = run_bass_kernel_spmd(nc, in_maps, core_ids=list(range(NCORES)))
    R = res.results
    y_prompt = np.stack([R[c]["y_p"] for c in range(NCORES)]).reshape(8, T, D)
    y_sample = np.concatenate([R[c]["y_s"].reshape(4, 4, D) for c in range(NCORES)], axis=0)
    p_k = np.stack([R[c]["o_pk"].reshape(2048, 8, 64) for c in range(NCORES)])[None]
    p_v = np.stack([R[c]["o_pv"].reshape(2048, 8, 64) for c in range(NCORES)])[None]
    p_h = np.stack([R[c]["o_ph"] for c in range(NCORES)])[None]
    p_mk = np.stack([R[c]["o_mk"].reshape(256, 4, 128) for c in range(NCORES)])[None]
    p_mv = np.stack([R[c]["o_mv"].reshape(256, 4, 128) for c in range(NCORES)])[None]
    s_k = np.concatenate([R[c]["o_sk"].reshape(4, 4, 8, 64) for c in range(NCORES)], axis=0)[None]
    s_v = np.concatenate([R[c]["o_sv"].reshape(4, 4, 8, 64) for c in range(NCORES)], axis=0)[None]
    s_h = np.concatenate([R[c]["o_sh"] for c in range(NCORES)], axis=0)[None]
    outs = (y_prompt, y_sample, p_k, p_v, p_h, p_mk, p_mv, s_k, s_v, s_h)
    return tuple(np.ascontiguousarray(o.astype(np.float32)) for o in outs)
```

```python
import contextlib
import numpy as np
import concourse.bass as bass
import concourse.mybir as mybir
from concourse.bass_utils import run_bass_kernel_spmd

F32 = mybir.dt.float32
BF16 = mybir.dt.bfloat16
U32 = mybir.dt.uint32
AF = mybir.ActivationFunctionType
ALU = mybir.AluOpType
AX = mybir.AxisListType

NCORES = 8
_ATT_LVL = 9
_ATT_MAXB = 10 ** 9
_BRANCHES = (1, 4, 16)
T = 4096
NT = 32
NTT = 33
D = 1024
MIXIN = 3584
EPS = 1e-6


class Sync:
    def __init__(self, nc, es):
        self.nc = nc
        self.eng = {'pe': nc.tensor, 'dve': nc.vector, 'act': nc.scalar, 'pool': nc.gpsimd, 'sp': nc.sync}
        self.sem = {}
        self.cnt = {}
        for e in self.eng:
            self.sem[e] = es.enter_context(nc.semaphore('c_' + e))
            self.cnt[e] = 0
        self.R = 8
        for q in ('sp', 'pool'):
            for r in range(self.R):
                k = ('d', q, r)
                self.sem[k] = es.enter_context(nc.semaphore('d_%s%d' % (q, r)))
                self.cnt[k] = 0
        self.dnext = {'sp': 0, 'pool': 0}
        self.waited = {}
        self.last_w = {}
        self.readers = {}

    def _wait(self, eng, dep):
        k, v = dep
        if k == 'pe' and eng == 'pe':
            return
        if self.waited.get((eng, k), 0) >= v:
            return
        self.eng[eng].wait_ge(self.sem[k], v)
        self.waited[(eng, k)] = v

    def _deps(self, eng, reads, writes):
        deps = {}
        def add(d):
            if d is None:
                return
            if deps.get(d[0], 0) < d[1]:
                deps[d[0]] = d[1]
        for r in reads:
            add(self.last_w.get(r))
        for w in writes:
            add(self.last_w.get(w))
            for d in self.readers.get(w, ()):
                add(d)
        for k, v in deps.items():
            self._wait(eng, (k, v))

    def _record(self, me, reads, writes):
        for r in reads:
            self.readers.setdefault(r, []).append(me)
        for w in writes:
            self.last_w[w] = me
            self.readers[w] = []

    def op(self, eng, inst_fn, reads=(), writes=()):
        reads = [getattr(r, 'k', r) for r in reads]
        writes = [getattr(w, 'k', w) for w in writes]
        self._deps(eng, reads, writes)
        inst = inst_fn(self.eng[eng])
        self.cnt[eng] += 1
        inst.then_inc(self.sem[eng], 1)
        self._record((eng, self.cnt[eng]), reads, writes)

    def dma(self, q, out, in_, reads=(), writes=(), **kw):
        reads = [getattr(r, 'k', r) for r in reads]
        writes = [getattr(w, 'k', w) for w in writes]
        self._deps(q, reads, writes)
        r = self.dnext[q]
        self.dnext[q] = (r + 1) % self.R
        k = ('d', q, r)
        inst = self.eng[q].dma_start(out=out, in_=in_, **kw)
        self.cnt[k] += 16
        inst.then_inc(self.sem[k], 16)
        self._record((k, self.cnt[k]), reads, writes)

    def barrier(self):
        for e in self.eng:
            for k, v in self.cnt.items():
                if v > 0 and k != e:
                    self._wait(e, (k, v))

    def finish(self):
        for k, v in self.cnt.items():
            if v > 0 and k != 'sp':
                self._wait('sp', (k, v))


class Tl:
    def __init__(self, t, k):
        self.t, self.k = t, k

    def __getitem__(self, idx):
        return self.t[idx]


def build_program(debug=False):
    nc = bass.Bass("TRN2", target_bir_lowering=False)

    def din(name, shape, dt=F32):
        return nc.dram_tensor(name, list(shape), dt, kind="ExternalInput").ap()

    def dout(name, shape, dt=F32):
        return nc.dram_tensor(name, list(shape), dt, kind="ExternalOutput").ap()

    def dscr(name, shape, dt=F32):
        return nc.dram_tensor(name, list(shape), dt, kind="ExternalOutput" if debug else "Internal").ap()

    xp = din("xp", [T, D])
    xs = din("xs", [16, D])
    memp = din("memp", [256, D])
    w_in = din("w_in", [D, MIXIN])
    w_mk = din("w_mk", [D, 512])
    w_mv = din("w_mv", [D, 512])
    g_mix = din("g_mix", [128, 8])
    g_mem = din("g_mem", [128, 8])
    ident = din("ident", [128, 128])
    lbl0 = din("lbl0", [128, 512])
    lbl1 = din("lbl1", [128, 512])
    triU_d = din("triU", [128, 128])
    tri4_d = din("tri4", [128, 512])
    rowmask_d = din("rowmask", [128, 1])
    st_h = din("st_h", [4, 4, 128, 128])
    maskC_d = din("maskC", [128, 1024])
    maskP_d = din("maskP", [128, 1024])
    ck = din("ck", [4, 2048, 512])
    cv = din("cv", [4, 2048, 512])
    bmask_d = din("bmask", [8, 528])
    w_out = din("w_out", [D, D])
    g_outc = din("g_outc", [128, 8])
    g_cross = din("g_cross", [128, 8])
    w_cq = din("w_cq", [D, 512])
    w_co = din("w_co", [512, D])
    cmk = din("cmk", [4, 256, 512])
    w_pq = din("w_pq", [D, 2048])
    g_ffn = din("g_ffn", [128, 8])
    g_ffnx = din("g_ffnx", [128, 1024])
    keysT = din("keysT", [128, 2048])
    ut_h = din("ut_h", [128, 128, 1024])
    v_h = din("v_h", [128, 128, 1024])
    g_fin = din("g_fin", [128, D])
    iota16_d = din("iota16", [128, 16])
    iota128_d = din("iota128", [128, 128])
    cmv = din("cmv", [4, 256, 512])
    y_p = dout("y_p", [T, D])
    y_s = dout("y_s", [16, D])
    o_pk = dout("o_pk", [2048, 512])
    o_pv = dout("o_pv", [2048, 512])
    o_ph = dout("o_ph", [4, 128, 128])
    o_mk = dout("o_mk", [256, 512])
    o_mv = dout("o_mv", [256, 512])
    o_sk = dout("o_sk", [16, 512])
    o_sv = dout("o_sv", [16, 512])
    o_sh = dout("o_sh", [4, 4, 128, 128])
    Z = dscr("Z", [NTT * 128, MIXIN])
    OB = dscr("OB", [NTT * 128, 512])
    OA = dscr("OA", [3, NTT * 128, 528])
    X2 = dscr("X2", [NTT * 128, D])
    X3 = dscr("X3", [NTT * 128, D]) if debug else None
    UTb = dscr("UTb", [128, 128, 1024], BF16)
    Vb = dscr("Vb", [128, 128, 1024], BF16)

    with contextlib.ExitStack() as es:
        S = Sync(nc, es)

        cur = [es]

        def sb(name, shape, dt=F32):
            return cur[0].enter_context(nc.sbuf_tensor(name, list(shape), dt))

        def ps(name, shape, dt=F32):
            return cur[0].enter_context(nc.psum_tensor(name, list(shape), dt))

        def tl(name, shape, dt=F32):
            return Tl(sb(name, shape, dt), name)

        def ptl(name, shape, dt=F32):
            return Tl(ps(name, shape, dt), name)

        ident_f = sb("ident_f", [128, 128])
        ident_b = sb("ident_b", [128, 128], BF16)
        gmix = sb("gmix", [128, 8])
        gmem = sb("gmem", [128, 8])
        S.dma('sp', ident_f[:], ident[:, :], writes=['ident_f'])
        S.dma('sp', gmix[:], g_mix[:, :], writes=['gmix'])
        S.dma('sp', gmem[:], g_mem[:, :], writes=['gmem'])
        S.op('dve', lambda e: e.tensor_copy(out=ident_b[:], in_=ident_f[:]), reads=['ident_f'], writes=['ident_b'])

        es_a = contextlib.ExitStack()
        es_a.__enter__()
        cur[0] = es_a
        win_b = sb("win_b", [128, 8 * MIXIN], BF16)
        wmk_b = sb("wmk_b", [128, 8 * 512], BF16)
        wmv_b = sb("wmv_b", [128, 8 * 512], BF16)
        wst = [sb("wst%d" % i, [128, MIXIN]) for i in range(2)]
        for kc in range(8):
            st = wst[kc % 2]
            S.dma('sp', st[:], w_in[kc * 128:(kc + 1) * 128, :], writes=['wst%d' % (kc % 2)])
            S.op('dve' if kc % 2 == 0 else 'pool',
                 lambda e, st=st, kc=kc: e.tensor_scalar(out=win_b[:, kc * MIXIN:(kc + 1) * MIXIN], in0=st[:],
                                                         scalar1=gmix[:, kc:kc + 1], scalar2=None, op0=ALU.mult),
                 reads=['wst%d' % (kc % 2), 'gmix'], writes=['win_b'])
        for kc in range(8):
            st = wst[kc % 2]
            S.dma('sp', st[:, 0:512], w_mk[kc * 128:(kc + 1) * 128, :], writes=['wst%d' % (kc % 2)])
            S.dma('sp', st[:, 512:1024], w_mv[kc * 128:(kc + 1) * 128, :], writes=['wst%d' % (kc % 2)])
            S.op('dve', lambda e, st=st, kc=kc: e.tensor_scalar(out=wmk_b[:, kc * 512:(kc + 1) * 512], in0=st[:, 0:512],
                                                                scalar1=gmem[:, kc:kc + 1], scalar2=None, op0=ALU.mult),
                 reads=['wst%d' % (kc % 2), 'gmem'], writes=['wmk_b'])
            S.op('pool', lambda e, st=st, kc=kc: e.tensor_scalar(out=wmv_b[:, kc * 512:(kc + 1) * 512], in0=st[:, 512:1024],
                                                                 scalar1=gmem[:, kc:kc + 1], scalar2=None, op0=ALU.mult),
                 reads=['wst%d' % (kc % 2), 'gmem'], writes=['wmv_b'])

        xt = [sb("xt%d" % i, [128, D]) for i in range(2)]
        junk = sb("junk", [128, D])
        ss = sb("ss", [128, 1])
        rstd = sb("rstd", [128, 1])
        hb = sb("hb", [128, D], BF16)
        hT = [sb("hT%d" % i, [128, D], BF16) for i in range(2)]
        pT = ps("pT", [128, D], BF16)
        pz = [ps("pz%d" % i, [128, 512]) for i in range(2)]
        zt = [sb("zt%d" % i, [128, MIXIN]) for i in range(2)]

        def front(xtile, xkey, hT_t, hkey):
            S.op('act', lambda e: e.activation(out=junk[:], in_=xtile[:], func=AF.Square, accum_out=ss[:]),
                 reads=[xkey], writes=['junk', 'ss'])
            S.op('dve', lambda e: e.tensor_scalar(out=rstd[:], in0=ss[:], scalar1=1.0 / D, scalar2=EPS,
                                                  op0=ALU.mult, op1=ALU.add), reads=['ss'], writes=['rstd'])
            S.op('act', lambda e: e.sqrt(out=rstd[:], in_=rstd[:]), reads=['rstd'], writes=['rstd'])
            S.op('dve', lambda e: e.reciprocal(out=rstd[:], in_=rstd[:]), reads=['rstd'], writes=['rstd'])
            S.op('dve', lambda e: e.tensor_scalar(out=hb[:], in0=xtile[:], scalar1=rstd[:, 0:1], scalar2=None,
                                                  op0=ALU.mult), reads=[xkey, 'rstd'], writes=['hb'])
            for kc in range(8):
                S.op('pe', lambda e, kc=kc: e.transpose(out=pT[:, kc * 128:(kc + 1) * 128],
                                                        in_=hb[:, kc * 128:(kc + 1) * 128], identity=ident_b[:]),
                     reads=['hb', 'ident_b'], writes=['pT'])
            S.op('act', lambda e: e.copy(out=hT_t[:], in_=pT[:]), reads=['pT'], writes=[hkey])

        for i in range(NTT):
            b = i % 2
            xkey = 'xt%d' % b
            if i < NT:
                S.dma('sp', xt[b][:], xp[i * 128:(i + 1) * 128, :], writes=[xkey])
            else:
                S.op('dve', lambda e, b=b: e.memset(xt[b][:], 0.0), writes=[xkey])
                S.dma('sp', xt[b][0:16, :], xs[:, :], writes=[xkey])
            front(xt[b], xkey, hT[b], 'hT%d' % b)
            for g in range(7):
                pzg = pz[g % 2]
                for kc in range(8):
                    S.op('pe', lambda e, kc=kc, g=g, pzg=pzg, b=b: e.matmul(
                        pzg[:], lhsT=hT[b][:, kc * 128:(kc + 1) * 128],
                        rhs=win_b[:, kc * MIXIN + g * 512: kc * MIXIN + (g + 1) * 512],
                        start=(kc == 0), stop=(kc == 7)),
                        reads=['hT%d' % b, 'win_b'], writes=['pz%d' % (g % 2)])
                if g % 2 == 0:
                    S.op('act', lambda e, g=g, pzg=pzg, b=b: e.copy(out=zt[b][:, g * 512:(g + 1) * 512], in_=pzg[:]),
                         reads=['pz%d' % (g % 2)], writes=['zt%d' % b])
                else:
                    S.op('dve', lambda e, g=g, pzg=pzg, b=b: e.tensor_copy(out=zt[b][:, g * 512:(g + 1) * 512], in_=pzg[:]),
                         reads=['pz%d' % (g % 2)], writes=['zt%d' % b])
            S.dma('pool', Z[i * 128:(i + 1) * 128, :], zt[b][:], reads=['zt%d' % b], writes=[('Z', i)])
            if 16 <= i < NT:
                r0 = (i - 16) * 128
                S.dma('pool', o_pk[r0:r0 + 128, :], zt[b][:, 512:1024], reads=['zt%d' % b])
                S.dma('pool', o_pv[r0:r0 + 128, :], zt[b][:, 1024:1536], reads=['zt%d' % b])
            if i == NT:
                S.dma('pool', o_sk[:, :], zt[b][0:16, 512:1024], reads=['zt%d' % b])
                S.dma('pool', o_sv[:, :], zt[b][0:16, 1024:1536], reads=['zt%d' % b])

        for i in range(2):
            b = i % 2
            xkey = 'xt%d' % b
            S.dma('sp', xt[b][:], memp[i * 128:(i + 1) * 128, :], writes=[xkey])
            front(xt[b], xkey, hT[b], 'hT%d' % b)
            for j, wb in enumerate((wmk_b, wmv_b)):
                pzg = pz[j]
                for kc in range(8):
                    S.op('pe', lambda e, kc=kc, wb=wb, pzg=pzg, b=b: e.matmul(
                        pzg[:], lhsT=hT[b][:, kc * 128:(kc + 1) * 128], rhs=wb[:, kc * 512:(kc + 1) * 512],
                        start=(kc == 0), stop=(kc == 7)),
                        reads=['hT%d' % b, 'wmk_b', 'wmv_b'], writes=['pz%d' % j])
                S.op('act' if j == 0 else 'dve',
                     (lambda e, j=j, pzg=pzg, b=b: e.copy(out=zt[b][:, j * 512:(j + 1) * 512], in_=pzg[:])) if j == 0 else
                     (lambda e, j=j, pzg=pzg, b=b: e.tensor_copy(out=zt[b][:, j * 512:(j + 1) * 512], in_=pzg[:])),
                     reads=['pz%d' % j], writes=['zt%d' % b])
            S.dma('pool', o_mk[i * 128:(i + 1) * 128, :], zt[b][:, 0:512], reads=['zt%d' % b])
            S.dma('pool', o_mv[i * 128:(i + 1) * 128, :], zt[b][:, 512:1024], reads=['zt%d' % b])

        es_a.close()
        cur[0] = es
        S.barrier()
        es_d = contextlib.ExitStack()
        es_d.__enter__()
        cur[0] = es_d
        triU = tl("triU_s", [128, 128])
        tri4 = tl("tri4_s", [128, 512])
        rowmask = tl("rowmask_s", [128, 1])
        lb_t = tl("lb_t", [128, 512])
        oml_t = tl("oml_t", [128, 512])
        l1_t = tl("l1_t", [128, 512])
        S.dma('sp', triU[:], triU_d[:, :], writes=[triU])
        S.dma('sp', tri4[:], tri4_d[:, :], writes=[tri4])
        S.dma('sp', rowmask[:], rowmask_d[:, :], writes=[rowmask])
        S.dma('sp', lb_t[:], lbl0[:, :], writes=[lb_t])
        S.dma('sp', l1_t[:], lbl1[:, :], writes=[l1_t])
        S.op('dve', lambda e: e.tensor_tensor(out=lb_t[:], in0=lb_t[:], in1=l1_t[:], op=ALU.subtract),
             reads=[lb_t, l1_t], writes=[lb_t])
        S.op('act', lambda e: e.activation(out=lb_t[:], in_=lb_t[:], func=AF.Sigmoid), reads=[lb_t], writes=[lb_t])
        S.op('dve', lambda e: e.tensor_scalar(out=oml_t[:], in0=lb_t[:], scalar1=-1.0, scalar2=1.0,
                                              op0=ALU.mult, op1=ALU.add), reads=[lb_t], writes=[oml_t])
        hz = [tl("hz%d" % i, [128, 2048]) for i in range(2)]
        sig = tl("sig", [128, 512])
        logf = tl("logf", [128, 512])
        kk = tl("kk", [128, 512])
        qs = tl("qs", [128, 512])
        gs = tl("gs", [128, 512])
        ib_b = tl("ib_b", [128, 512], BF16)
        ec = tl("ec", [128, 512])
        emc = tl("emc", [128, 512])
        ecl = tl("ecl", [128, 4])
        qd_b = tl("qd_b", [128, 512], BF16)
        kd_b = tl("kd_b", [128, 512], BF16)
        qkT = tl("qkT", [128, 1024], BF16)
        AT_b = tl("AT_b", [128, 512], BF16)
        Sst = tl("Sst", [128, 512])
        S_b = tl("S_b", [128, 512], BF16)
        ssq = tl("ssq", [128, 4])
        rb = tl("rb", [128, 4])
        hjunk = tl("hjunk", [128, 128])
        obn = [tl("obn%d" % i, [128, 512]) for i in range(2)]
        pc = ptl("pc", [128, 512])
        pcT = ptl("pcT", [128, 512])
        pTq = ptl("pTq", [128, 1024], BF16)
        pA = ptl("pA", [128, 512])
        po = ptl("po", [128, 512])
        pdS = ptl("pdS", [128, 512])

        P = 64

        def hs(h):
            return slice(h * 128, (h + 1) * 128)

        def hp(h):
            return slice(h * P, (h + 1) * P)

        def hgrn_chunk(ci, row0, nrows, masked):
            z = hz[ci % 2]
            ob = obn[ci % 2]
            if nrows < P:
                S.op('dve', lambda e: e.memset(z[:], 0.0), writes=[z])
            S.dma('sp', z[0:nrows, :], Z[row0:row0 + nrows, 1536:3584], reads=[('Z', row0 // 128)], writes=[z])
            S.op('act', lambda e: e.activation(out=sig[0:P, :], in_=z[0:P, 512:1024], func=AF.Sigmoid), reads=[z], writes=[sig])
            S.op('dve', lambda e: e.tensor_tensor(out=sig[0:P, :], in0=sig[0:P, :], in1=oml_t[0:P, :], op=ALU.mult),
                 reads=[sig, oml_t], writes=[sig])
            S.op('dve', lambda e: e.tensor_tensor(out=sig[0:P, :], in0=sig[0:P, :], in1=lb_t[0:P, :], op=ALU.add),
                 reads=[sig, lb_t], writes=[sig])
            S.op('act', lambda e: e.activation(out=logf[0:P, :], in_=sig[0:P, :], func=AF.Ln), reads=[sig], writes=[logf])
            S.op('dve', lambda e: e.tensor_scalar(out=kk[0:P, :], in0=sig[0:P, :], scalar1=-1.0, scalar2=1.0,
                                                  op0=ALU.mult, op1=ALU.add), reads=[sig], writes=[kk])
            if masked:
                S.op('dve', lambda e: e.tensor_scalar(out=logf[0:P, :], in0=logf[0:P, :], scalar1=rowmask[0:P, 0:1],
                                                      scalar2=None, op0=ALU.mult), reads=[logf, rowmask], writes=[logf])
                S.op('dve', lambda e: e.tensor_scalar(out=kk[0:P, :], in0=kk[0:P, :], scalar1=rowmask[0:P, 0:1],
                                                      scalar2=None, op0=ALU.mult), reads=[kk, rowmask], writes=[kk])
            S.op('act', lambda e: e.activation(out=qs[0:P, :], in_=z[0:P, 0:512], func=AF.Silu), reads=[z], writes=[qs])
            S.op('act', lambda e: e.activation(out=gs[0:P, :], in_=z[0:P, 1536:2048], func=AF.Silu), reads=[z], writes=[gs])
            S.op('dve', lambda e: e.tensor_copy(out=ib_b[0:P, :], in_=z[0:P, 1024:1536]), reads=[z], writes=[ib_b])
            S.op('pe', lambda e: e.matmul(pc[0:P, :], lhsT=triU[0:P, 0:P], rhs=logf[0:P, :], start=True, stop=True),
                 reads=[triU, logf], writes=[pc])
            for h in range(4):
                S.op('pe', lambda e, h=h: e.matmul(pcT[:, hp(h)], lhsT=logf[0:P, hs(h)], rhs=triU[0:P, 0:P],
                                                   start=True, stop=True), reads=[triU, logf], writes=[pcT])
            S.op('act', lambda e: e.activation(out=ec[0:P, :], in_=pc[0:P, :], func=AF.Exp), reads=[pc], writes=[ec])
            S.op('act', lambda e: e.activation(out=emc[0:P, :], in_=pc[0:P, :], func=AF.Exp, scale=-1.0),
                 reads=[pc], writes=[emc])
            S.op('act', lambda e: e.activation(out=ecl[:], in_=pcT[:, P - 1:4 * P:P], func=AF.Exp), reads=[pcT], writes=[ecl])
            S.op('dve', lambda e: e.tensor_tensor(out=qd_b[0:P, :], in0=qs[0:P, :], in1=ec[0:P, :], op=ALU.mult),
                 reads=[qs, ec], writes=[qd_b])
            S.op('dve', lambda e: e.tensor_tensor(out=kd_b[0:P, :], in0=kk[0:P, :], in1=emc[0:P, :], op=ALU.mult),
                 reads=[kk, emc], writes=[kd_b])
            for h in range(4):
                S.op('pe', lambda e, h=h: e.transpose(out=pTq[:, hp(h)], in_=qd_b[0:P, hs(h)], identity=ident_b[0:P, 0:P]),
                     reads=[qd_b, 'ident_b'], writes=[pTq])
                S.op('pe', lambda e, h=h: e.transpose(out=pTq[:, hp(4 + h)], in_=kd_b[0:P, hs(h)],
                                                      identity=ident_b[0:P, 0:P]), reads=[kd_b, 'ident_b'], writes=[pTq])
            S.op('act', lambda e: e.copy(out=qkT[:, 0:8 * P], in_=pTq[:, 0:8 * P]), reads=[pTq], writes=[qkT])
            for h in range(4):
                S.op('pe', lambda e, h=h: e.matmul(pA[0:P, hp(h)], lhsT=qkT[:, hp(4 + h)],
                                                   rhs=qkT[:, hp(h)], start=True, stop=True), reads=[qkT], writes=[pA])
            S.op('dve', lambda e: e.tensor_tensor(out=AT_b[0:P, 0:4 * P], in0=pA[0:P, 0:4 * P], in1=tri4[0:P, 0:4 * P],
                                                  op=ALU.mult), reads=[pA, tri4], writes=[AT_b])
            for h in range(4):
                S.op('pe', lambda e, h=h: e.matmul(po[0:P, hs(h)], lhsT=AT_b[0:P, hp(h)], rhs=ib_b[0:P, hs(h)],
                                                   start=True, stop=False), reads=[AT_b, ib_b], writes=[po])
                S.op('pe', lambda e, h=h: e.matmul(po[0:P, hs(h)], lhsT=qkT[:, hp(h)], rhs=S_b[:, hs(h)],
                                                   start=False, stop=True), reads=[qkT, S_b], writes=[po])
                S.op('pe', lambda e, h=h: e.matmul(pdS[:, hs(h)], lhsT=kd_b[0:P, hs(h)], rhs=ib_b[0:P, hs(h)],
                                                   start=True, stop=True), reads=[kd_b, ib_b], writes=[pdS])
            S.op('dve', lambda e: e.tensor_tensor(out=Sst[:], in0=Sst[:], in1=pdS[:], op=ALU.add),
                 reads=[Sst, pdS], writes=[Sst])
            for h in range(4):
                S.op('dve', lambda e, h=h: e.tensor_scalar(out=Sst[:, hs(h)], in0=Sst[:, hs(h)], scalar1=ecl[:, h:h + 1],
                                                           scalar2=None, op0=ALU.mult), reads=[Sst, ecl], writes=[Sst])
            S.op('dve', lambda e: e.tensor_copy(out=S_b[:], in_=Sst[:]), reads=[Sst], writes=[S_b])
            for h in range(4):
                S.op('act', lambda e, h=h: e.activation(out=hjunk[0:P, :], in_=po[0:P, hs(h)], func=AF.Square,
                                                        accum_out=ssq[0:P, h:h + 1]), reads=[po], writes=[hjunk, ssq])
            S.op('dve', lambda e: e.tensor_scalar(out=rb[0:P, :], in0=ssq[0:P, :], scalar1=1.0 / 128, scalar2=EPS,
                                                  op0=ALU.mult, op1=ALU.add), reads=[ssq], writes=[rb])
            S.op('act', lambda e: e.sqrt(out=rb[0:P, :], in_=rb[0:P, :]), reads=[rb], writes=[rb])
            S.op('dve', lambda e: e.reciprocal(out=rb[0:P, :], in_=rb[0:P, :]), reads=[rb], writes=[rb])
            for h in range(4):
                S.op('dve', lambda e, h=h: e.tensor_scalar(out=ob[0:P, hs(h)], in0=po[0:P, hs(h)], scalar1=rb[0:P, h:h + 1],
                                                           scalar2=None, op0=ALU.mult), reads=[po, rb], writes=[ob])
            S.op('dve', lambda e: e.tensor_tensor(out=ob[0:P, :], in0=ob[0:P, :], in1=gs[0:P, :], op=ALU.mult),
                 reads=[ob, gs], writes=[ob])
            S.dma('pool', OB[row0:row0 + nrows, :], ob[0:nrows, :], reads=[ob], writes=[('OB', row0 // 128)])

        S.op('dve', lambda e: e.memset(Sst[:], 0.0), writes=[Sst])
        S.op('dve', lambda e: e.memset(S_b[:], 0.0), writes=[S_b])
        for i in range(T // P):
            hgrn_chunk(i, i * P, P, False)
        for h in range(4):
            S.dma('pool', o_ph[h, :, :], Sst[:, hs(h)], reads=[Sst])
        for bb in range(4):
            for h in range(4):
                S.dma('sp', Sst[:, hs(h)], st_h[bb, h, :, :], writes=[Sst])
            S.op('dve', lambda e: e.tensor_copy(out=S_b[:], in_=Sst[:]), reads=[Sst], writes=[S_b])
            hgrn_chunk(bb, T + 4 * bb, 4, True)
            for h in range(4):
                S.dma('pool', o_sh[bb, h, :, :], Sst[:, hs(h)], reads=[Sst])
        es_d.close()
        cur[0] = es
        S.barrier()
        es_c = contextlib.ExitStack()
        es_c.__enter__()
        cur[0] = es_c
        mstage = tl("mstage", [128, 1024])
        maskC = tl("maskC_s", [128, 1024], BF16)
        maskP = tl("maskP_s", [128, 1024], BF16)
        S.dma('sp', mstage[:], maskC_d[:, :], writes=[mstage])
        S.op('dve', lambda e: e.tensor_copy(out=maskC[:], in_=mstage[:]), reads=[mstage], writes=[maskC])
        S.dma('sp', mstage[:], maskP_d[:, :], writes=[mstage])
        S.op('dve', lambda e: e.tensor_copy(out=maskP[:], in_=mstage[:]), reads=[mstage], writes=[maskP])
        az = [tl("az%d" % i, [128, 1536]) for i in range(2)]
        qk_b = tl("qk_b", [128, 1024], BF16)
        vaug = [tl("vaug%d" % i, [128, 8 * 66], BF16) for i in range(2)]
        qT = tl("qT", [64, 1024], BF16)
        kT = [tl("kT%d" % i, [64, 1024], BF16) for i in range(2)]
        PTc = tl("PTc", [128, 1024], BF16)
        PTp = tl("PTp", [128, 1024], BF16)
        osb = [tl("osb%d" % i, [128, 528]) for i in range(2)]
        pTt = ptl("pTt", [128, 1024], BF16)
        pTk = ptl("pTk", [128, 1024], BF16)
        psC = [ptl("psC%d" % i, [128, 512]) for i in range(2)]
        psP = [ptl("psP%d" % i, [128, 512]) for i in range(2)]
        pO = [ptl("pO%d" % i, [128, 512]) for i in range(2)]
        for i in range(2):
            S.op('dve', lambda e, i=i: e.memset(vaug[i][:], 1.0), writes=[vaug[i]])
        blk = 0
        for br, dil in enumerate(_BRANCHES):
            nb = T // dil // 128
            if dil == 1:
                Zv = Z[0:T, :].rearrange("(o m) c -> o m c", o=1)
                OAv = OA[br, 0:T, :].rearrange("(o m) c -> o m c", o=1)
            else:
                Zv = Z[0:T, :].rearrange("(m d) c -> d m c", d=dil)
                OAv = OA[br, 0:T, :].rearrange("(m d) c -> d m c", d=dil)
            for r in range(dil):
                for b in range(nb):
                    cb = blk % 2
                    pb = 1 - cb
                    a = az[cb]
                    S.dma('sp', a[:], Zv[r, b * 128:(b + 1) * 128, 0:1536], reads=[('Z', i) for i in range(NT)] if blk == 0 else [],
                          writes=[a])
                    S.op('dve', lambda e, a=a: e.tensor_copy(out=qk_b[:], in_=a[:, 0:1024]), reads=[a], writes=[qk_b])
                    S.op('act', lambda e, a=a, cb=cb: e.copy(
                        out=vaug[cb][:].rearrange("p (h e) -> p h e", e=66)[:, :, 0:64],
                        in_=a[:, 1024:1536].rearrange("p (h e) -> p h e", e=64)), reads=[a], writes=[vaug[cb]])
                    if blk >= _ATT_MAXB or _ATT_LVL < 2:
                        blk += 1
                        continue
                    for h in range(8):
                        S.op('pe', lambda e, h=h: e.transpose(out=pTt[0:64, h * 128:(h + 1) * 128],
                                                              in_=qk_b[:, h * 64:(h + 1) * 64], identity=ident_b[:]),
                             reads=[qk_b, 'ident_b'], writes=[pTt])
                        S.op('pe', lambda e, h=h: e.transpose(out=pTk[0:64, h * 128:(h + 1) * 128],
                                                              in_=qk_b[:, 512 + h * 64:512 + (h + 1) * 64],
                                                              identity=ident_b[:]),
                             reads=[qk_b, 'ident_b'], writes=[pTk])
                    S.op('act', lambda e: e.copy(out=qT[0:64, :], in_=pTt[0:64, :]), reads=[pTt], writes=[qT])
                    S.op('act', lambda e, cb=cb: e.copy(out=kT[cb][0:64, :], in_=pTk[0:64, :]), reads=[pTk], writes=[kT[cb]])
                    if _ATT_LVL < 3:
                        blk += 1
                        continue
                    for h in range(8):
                        p, j = h // 2, h % 2
                        S.op('pe', lambda e, h=h, p=p, j=j, cb=cb: e.matmul(
                            psC[h // 4][:, (h % 4) * 128:(h % 4 + 1) * 128], lhsT=kT[cb][0:64, h * 128:(h + 1) * 128],
                            rhs=qT[0:64, h * 128:(h + 1) * 128], start=True, stop=True),
                            reads=[kT[cb], qT], writes=[psC[h // 4]])
                    if b > 0:
                        for h in range(8):
                            p, j = h // 2, h % 2
                            S.op('pe', lambda e, h=h, p=p, j=j, pb=pb: e.matmul(
                                psP[h // 4][:, (h % 4) * 128:(h % 4 + 1) * 128], lhsT=kT[pb][0:64, h * 128:(h + 1) * 128],
                                rhs=qT[0:64, h * 128:(h + 1) * 128], start=True, stop=True),
                                reads=[kT[pb], qT], writes=[psP[h // 4]])
                    if _ATT_LVL < 4:
                        blk += 1
                        continue
                    for hh in range(2):
                        S.op('act', lambda e, hh=hh: e.activation(out=PTc[:, hh * 512:(hh + 1) * 512], in_=psC[hh][:],
                                                                  func=AF.Exp, scale=0.125), reads=[psC[hh]], writes=[PTc])
                    S.op('dve', lambda e: e.tensor_tensor(out=PTc[:], in0=PTc[:], in1=maskC[:], op=ALU.mult),
                         reads=[PTc, maskC], writes=[PTc])
                    if b > 0:
                        for hh in range(2):
                            S.op('act', lambda e, hh=hh: e.activation(out=PTp[:, hh * 512:(hh + 1) * 512], in_=psP[hh][:],
                                                                      func=AF.Exp, scale=0.125), reads=[psP[hh]], writes=[PTp])
                        S.op('pool', lambda e: e.tensor_tensor(out=PTp[:], in0=PTp[:], in1=maskP[:], op=ALU.mult),
                             reads=[PTp, maskP], writes=[PTp])
                    if _ATT_LVL < 5:
                        blk += 1
                        continue
                    for h in range(8):
                        po_t = pO[h // 4]
                        osl = slice((h % 4) * 66, (h % 4 + 1) * 66)
                        S.op('pe', lambda e, h=h, po_t=po_t, osl=osl, cb=cb: e.matmul(
                            po_t[:, osl], lhsT=PTc[:, h * 128:(h + 1) * 128], rhs=vaug[cb][:, h * 66:(h + 1) * 66],
                            start=True, stop=(b == 0)), reads=[PTc, vaug[cb]], writes=[po_t])
                        if b > 0:
                            S.op('pe', lambda e, h=h, po_t=po_t, osl=osl, pb=pb: e.matmul(
                                po_t[:, osl], lhsT=PTp[:, h * 128:(h + 1) * 128], rhs=vaug[pb][:, h * 66:(h + 1) * 66],
                                start=False, stop=True), reads=[PTp, vaug[pb]], writes=[po_t])
                    o = osb[cb]
                    S.op('act', lambda e, o=o: e.copy(out=o[:, 0:264], in_=pO[0][:, 0:264]), reads=[pO[0]], writes=[o])
                    S.op('dve', lambda e, o=o: e.tensor_copy(out=o[:, 264:528], in_=pO[1][:, 0:264]), reads=[pO[1]], writes=[o])
                    S.dma('pool', OAv[r, b * 128:(b + 1) * 128, :], o[:], reads=[o], writes=[('OA', br)])
                    blk += 1
        es_c.close()
        cur[0] = es
        S.barrier()
        es_e = contextlib.ExitStack()
        es_e.__enter__()
        cur[0] = es_e
        bmask = tl("bmask_s", [8, 528])
        S.dma('sp', bmask[:], bmask_d[:, :], writes=[bmask])
        skv = [tl("skv%d" % i, [128, 1024]) for i in range(2)]
        sqb = tl("sqb", [128, 512])
        sprod = tl("sprod", [128, 512])
        ssc = tl("ssc", [128, 8])
        sp_b = tl("sp_b", [128, 8], BF16)
        svaug = [tl("svaug%d" % i, [128, 528], BF16) for i in range(2)]
        sq8 = tl("sq8", [8, 64])
        sk8 = tl("sk8", [8, 64])
        sv8 = tl("sv8", [8, 64])
        sj8 = tl("sj8", [8, 64])
        ss8 = tl("ss8", [8, 1])
        sdiag = tl("sdiag", [8, 528])
        sres = [tl("sres%d" % i, [8, 66]) for i in range(2)]
        pSa = ptl("pSa", [8, 512])
        pSb = ptl("pSb", [8, 512])
        for i in range(2):
            S.op('dve', lambda e, i=i: e.memset(svaug[i][:], 1.0), writes=[svaug[i]])
        it = 0
        for bb in range(4):
            for t in range(4):
                row = T + 4 * bb + t
                S.dma('sp', sqb[:], bass.AP(tensor=Z.tensor, offset=row * MIXIN, ap=[[0, 128], [1, 512]]),
                      reads=[('Z', NT)], writes=[sqb])
                S.dma('sp', sq8[:], Z[row, 0:512].rearrange("(h e) -> h e", e=64), writes=[sq8])
                S.dma('sp', sk8[:], Z[row, 512:1024].rearrange("(h e) -> h e", e=64), writes=[sk8])
                S.dma('sp', sv8[:], Z[row, 1024:1536].rearrange("(h e) -> h e", e=64), writes=[sv8])
                for br, dil in enumerate((1, 4, 16)):
                    kv = skv[it % 2]
                    va = svaug[it % 2]
                    it += 1
                    start = 2048 + t - dil * 128
                    if dil == 1:
                        ncache = 128 - t
                        S.dma('sp', kv[0:ncache, 0:512], ck[bb, start:2048, :], writes=[kv])
                        S.dma('sp', kv[0:ncache, 512:1024], cv[bb, start:2048, :], writes=[kv])
                        if t > 0:
                            S.dma('sp', kv[ncache:128, :], Z[T + 4 * bb:T + 4 * bb + t, 512:1536], reads=[('Z', NT)], writes=[kv])
                    else:
                        S.dma('sp', kv[:, 0:512], bass.AP(tensor=ck.tensor, offset=(bb * 2048 + start) * 512,
                                                          ap=[[dil * 512, 128], [1, 512]]), writes=[kv])
                        S.dma('sp', kv[:, 512:1024], bass.AP(tensor=cv.tensor, offset=(bb * 2048 + start) * 512,
                                                             ap=[[dil * 512, 128], [1, 512]]), writes=[kv])
                    S.op('act', lambda e, kv=kv, va=va: e.copy(
                        out=va[:].rearrange("p (h e) -> p h e", e=66)[:, :, 0:64],
                        in_=kv[:, 512:1024].rearrange("p (h e) -> p h e", e=64)), reads=[kv], writes=[va])
                    S.op('dve', lambda e, kv=kv: e.tensor_tensor(out=sprod[:], in0=kv[:, 0:512], in1=sqb[:], op=ALU.mult),
                         reads=[kv, sqb], writes=[sprod])
                    S.op('dve', lambda e: e.tensor_reduce(out=ssc[:], in_=sprod[:].rearrange("p (h e) -> p h e", e=64),
                                                          axis=AX.X, op=ALU.add), reads=[sprod], writes=[ssc])
                    S.op('act', lambda e: e.activation(out=sp_b[:], in_=ssc[:], func=AF.Exp, scale=0.125),
                         reads=[ssc], writes=[sp_b])
                    S.op('pe', lambda e, va=va, br=br: e.matmul(pSa[:, 0:264], lhsT=sp_b[:], rhs=va[:, 0:264],
                                                                start=(br == 0), stop=(br == 2)),
                         reads=[sp_b, va], writes=[pSa])
                    S.op('pe', lambda e, va=va, br=br: e.matmul(pSb[:, 0:264], lhsT=sp_b[:], rhs=va[:, 264:528],
                                                                start=(br == 0), stop=(br == 2)),
                         reads=[sp_b, va], writes=[pSb])
                res = sres[(4 * bb + t) % 2]
                S.op('dve', lambda e: e.tensor_tensor(out=sdiag[:, 0:264], in0=pSa[:, 0:264], in1=bmask[:, 0:264], op=ALU.mult),
                     reads=[pSa, bmask], writes=[sdiag])
                S.op('dve', lambda e: e.tensor_tensor(out=sdiag[:, 264:528], in0=pSb[:, 0:264], in1=bmask[:, 264:528], op=ALU.mult),
                     reads=[pSb, bmask], writes=[sdiag])
                S.op('dve', lambda e, res=res: e.tensor_reduce(out=res[:], in_=sdiag[:].rearrange("p (h e) -> p e h", e=66),
                                                               axis=AX.X, op=ALU.add), reads=[sdiag], writes=[res])
                S.op('dve', lambda e: e.tensor_tensor(out=sj8[:], in0=sq8[:], in1=sk8[:], op=ALU.mult),
                     reads=[sq8, sk8], writes=[sj8])
                S.op('dve', lambda e: e.tensor_reduce(out=ss8[:], in_=sj8[:], axis=AX.X, op=ALU.add), reads=[sj8], writes=[ss8])
                S.op('act', lambda e: e.activation(out=ss8[:], in_=ss8[:], func=AF.Exp, scale=0.125), reads=[ss8], writes=[ss8])
                S.op('dve', lambda e: e.tensor_scalar(out=ss8[:], in0=ss8[:], scalar1=3.0, scalar2=None, op0=ALU.mult),
                     reads=[ss8], writes=[ss8])
                S.op('dve', lambda e, res=res: e.scalar_tensor_tensor(out=res[:, 0:64], in0=sv8[:], scalar=ss8[:, 0:1],
                                                                      in1=res[:, 0:64], op0=ALU.mult, op1=ALU.add),
                     reads=[sv8, ss8, res], writes=[res])
                S.op('dve', lambda e, res=res: e.tensor_tensor(out=res[:, 64:65], in0=res[:, 64:65], in1=ss8[:], op=ALU.add),
                     reads=[res, ss8], writes=[res])
                S.dma('pool', OA[0, row, :].rearrange("(h e) -> h e", e=66), res[:], reads=[res], writes=[('OA', 0)])
        es_e.close()
        cur[0] = es
        S.barrier()
        es_f = contextlib.ExitStack()
        es_f.__enter__()
        cur[0] = es_f
        goutc = tl("goutc", [128, 8])
        gcross = tl("gcross", [128, 8])
        S.dma('sp', goutc[:], g_outc[:, :], writes=[goutc])
        S.dma('sp', gcross[:], g_cross[:, :], writes=[gcross])
        wout_b = tl("wout_b", [128, 8 * 1024], BF16)
        wcq_b = tl("wcq_b", [128, 8 * 512], BF16)
        wco_b = tl("wco_b", [128, 4 * 1024], BF16)
        fst = [tl("fst%d" % i, [128, 1024]) for i in range(2)]
        for kc in range(8):
            st = fst[kc % 2]
            S.dma('sp', st[:], w_out[kc * 128:(kc + 1) * 128, :], writes=[st])
            S.op('dve', lambda e, st=st, kc=kc: e.tensor_scalar(out=wout_b[:, kc * 1024:(kc + 1) * 1024], in0=st[:],
                                                                scalar1=goutc[:, kc:kc + 1], scalar2=None, op0=ALU.mult),
                 reads=[st, goutc], writes=[wout_b])
        for kc in range(8):
            st = fst[kc % 2]
            S.dma('sp', st[:, 0:512], w_cq[kc * 128:(kc + 1) * 128, :], writes=[st])
            S.op('dve', lambda e, st=st, kc=kc: e.tensor_scalar(out=wcq_b[:, kc * 512:(kc + 1) * 512], in0=st[:, 0:512],
                                                                scalar1=gcross[:, kc:kc + 1], scalar2=None, op0=ALU.mult),
                 reads=[st, gcross], writes=[wcq_b])
        for kc in range(4):
            st = fst[kc % 2]
            S.dma('sp', st[:], w_co[kc * 128:(kc + 1) * 128, :], writes=[st])
            S.op('dve', lambda e, st=st, kc=kc: e.tensor_copy(out=wco_b[:, kc * 1024:(kc + 1) * 1024], in_=st[:]),
                 reads=[st], writes=[wco_b])
        ones_b = tl("ones_b", [128, 128], BF16)
        S.op('dve', lambda e: e.memset(ones_b[:], 1.0), writes=[ones_b])
        mkT = [tl("mkT%d" % i, [128, 4 * 256], BF16) for i in range(5)]
        mvb = [tl("mvb%d" % i, [128, 2 * 512], BF16) for i in range(5)]
        mkb = tl("mkb", [128, 512], BF16)
        pFT = ptl("pFT", [128, 1024], BF16)
        pTm = pFT
        for g in range(5):
            src_k = o_mk if g == 0 else cmk[g - 1]
            src_v = o_mv if g == 0 else cmv[g - 1]
            for mb in range(2):
                st = fst[mb]
                S.dma('sp', st[:, 0:512], src_k[mb * 128:(mb + 1) * 128, :], writes=[st])
                S.dma('sp', st[:, 512:1024], src_v[mb * 128:(mb + 1) * 128, :], writes=[st])
                S.op('dve', lambda e, st=st: e.tensor_copy(out=mkb[:], in_=st[:, 0:512]), reads=[st], writes=[mkb])
                S.op('dve', lambda e, st=st, g=g, mb=mb: e.tensor_copy(out=mvb[g][:, mb * 512:(mb + 1) * 512], in_=st[:, 512:1024]),
                     reads=[st], writes=[mvb[g]])
                for hh in range(4):
                    S.op('pe', lambda e, hh=hh: e.transpose(out=pTm[:, hh * 128:(hh + 1) * 128], in_=mkb[:, hh * 128:(hh + 1) * 128],
                                                            identity=ident_b[:]), reads=[mkb, 'ident_b'], writes=[pTm])
                S.op('act', lambda e, g=g, mb=mb: e.copy(
                    out=mkT[g][:].rearrange("p (h m) -> p h m", m=256)[:, :, mb * 128:(mb + 1) * 128],
                    in_=pTm[:, 0:512].rearrange("p (h m) -> p h m", m=128)), reads=[pTm], writes=[mkT[g]])
        xa = [tl("xa%d" % i, [128, D]) for i in range(2)]
        oat = [tl("oat%d" % i, [128, 528]) for i in range(3)]
        obt = tl("obt", [128, 512])
        rden = tl("rden", [128, 8])
        oan = tl("oan", [128, 512])
        cat = tl("cat", [128, D], BF16)
        fjunk = tl("fjunk", [128, D])
        fss = tl("fss", [128, 1])
        frs = tl("frs", [128, 1])
        fhb = tl("fhb", [128, D], BF16)
        catT = tl("catT", [128, D], BF16)
        h2T = tl("h2T", [128, D], BF16)
        qcT = tl("qcT", [128, 512], BF16)
        PTx = tl("PTx", [128, 1024], BF16)
        rdx = tl("rdx", [128, 512])
        oTx = tl("oTx", [128, 512], BF16)
        py = [ptl("py%d" % i, [128, 512]) for i in range(2)]
        pq = ptl("pq", [128, 512])
        psx = [ptl("psx%d" % i, [128, 512]) for i in range(2)]
        pox = ptl("pox", [128, 512])
        pdx = ptl("pdx", [128, 512])

        def normT(xtile, outT):
            S.op('act', lambda e: e.activation(out=fjunk[:], in_=xtile[:], func=AF.Square, accum_out=fss[:]),
                 reads=[xtile], writes=[fjunk, fss])
            S.op('dve', lambda e: e.tensor_scalar(out=frs[:], in0=fss[:], scalar1=1.0 / D, scalar2=EPS,
                                                  op0=ALU.mult, op1=ALU.add), reads=[fss], writes=[frs])
            S.op('act', lambda e: e.sqrt(out=frs[:], in_=frs[:]), reads=[frs], writes=[frs])
            S.op('dve', lambda e: e.reciprocal(out=frs[:], in_=frs[:]), reads=[frs], writes=[frs])
            S.op('dve', lambda e: e.tensor_scalar(out=fhb[:], in0=xtile[:], scalar1=frs[:, 0:1], scalar2=None,
                                                  op0=ALU.mult), reads=[xtile, frs], writes=[fhb])
            for kc in range(8):
                S.op('pe', lambda e, kc=kc: e.transpose(out=pFT[:, kc * 128:(kc + 1) * 128],
                                                        in_=fhb[:, kc * 128:(kc + 1) * 128], identity=ident_b[:]),
                     reads=[fhb, 'ident_b'], writes=[pFT])
            S.op('act', lambda e: e.copy(out=outT[:], in_=pFT[:]), reads=[pFT], writes=[outT])

        for i in range(NTT):
            x = xa[i % 2]
            nbr = 3 if i < NT else 1
            if i < NT:
                S.dma('sp', x[:], xp[i * 128:(i + 1) * 128, :], writes=[x])
            else:
                S.op('dve', lambda e, x=x: e.memset(x[:], 0.0), writes=[x])
                S.dma('sp', x[0:16, :], xs[:, :], writes=[x])
                for t3 in (oat[0], obt):
                    S.op('dve', lambda e, t3=t3: e.memset(t3[:], 1.0), writes=[t3])
            nr = 128 if i < NT else 16
            for br in range(nbr):
                S.dma('sp', oat[br][0:nr, :], OA[br, i * 128:i * 128 + nr, :], reads=[('OA', br)], writes=[oat[br]])
            S.dma('sp', obt[0:nr, :], OB[i * 128:i * 128 + nr, :], reads=[('OB', j) for j in range(NTT)], writes=[obt])
            for br in range(1, nbr):
                S.op('dve', lambda e, br=br: e.tensor_tensor(out=oat[0][:], in0=oat[0][:], in1=oat[br][:], op=ALU.add),
                     reads=[oat[0], oat[br]], writes=[oat[0]])
            S.op('dve', lambda e: e.reciprocal(out=rden[:], in_=oat[0][:].rearrange("p (h e) -> p h e", e=66)[:, :, 64]),
                 reads=[oat[0]], writes=[rden])
            for h in range(8):
                S.op('dve', lambda e, h=h: e.tensor_scalar(out=oan[:, h * 64:(h + 1) * 64], in0=oat[0][:, h * 66:h * 66 + 64],
                                                           scalar1=rden[:, h:h + 1], scalar2=None, op0=ALU.mult),
                     reads=[oat[0], rden], writes=[oan])
            S.op('act', lambda e: e.activation(out=fjunk[:, 0:512], in_=oan[:], func=AF.Square, accum_out=fss[:]),
                 reads=[oan], writes=[fjunk, fss])
            S.op('dve', lambda e: e.tensor_scalar(out=frs[:], in0=fss[:], scalar1=1.0 / 512, scalar2=EPS,
                                                  op0=ALU.mult, op1=ALU.add), reads=[fss], writes=[frs])
            S.op('act', lambda e: e.sqrt(out=frs[:], in_=frs[:]), reads=[frs], writes=[frs])
            S.op('dve', lambda e: e.reciprocal(out=frs[:], in_=frs[:]), reads=[frs], writes=[frs])
            S.op('dve', lambda e: e.tensor_scalar(out=cat[:, 0:512], in0=oan[:], scalar1=frs[:, 0:1], scalar2=None,
                                                  op0=ALU.mult), reads=[oan, frs], writes=[cat])
            S.op('dve', lambda e: e.tensor_copy(out=cat[:, 512:1024], in_=obt[:]), reads=[obt], writes=[cat])
            for kc in range(8):
                S.op('pe', lambda e, kc=kc: e.transpose(out=pFT[:, kc * 128:(kc + 1) * 128],
                                                        in_=cat[:, kc * 128:(kc + 1) * 128], identity=ident_b[:]),
                     reads=[cat, 'ident_b'], writes=[pFT])
            S.op('act', lambda e: e.copy(out=catT[:], in_=pFT[:]), reads=[pFT], writes=[catT])
            for half in range(2):
                for kc in range(8):
                    S.op('pe', lambda e, kc=kc, half=half: e.matmul(
                        py[half][:], lhsT=catT[:, kc * 128:(kc + 1) * 128],
                        rhs=wout_b[:, kc * 1024 + half * 512:kc * 1024 + (half + 1) * 512],
                        start=(kc == 0), stop=(kc == 7)), reads=[catT, wout_b], writes=[py[half]])
                S.op('dve', lambda e, half=half, x=x: e.tensor_tensor(out=x[:, half * 512:(half + 1) * 512],
                                                                     in0=x[:, half * 512:(half + 1) * 512], in1=py[half][:],
                                                                     op=ALU.add), reads=[x, py[half]], writes=[x])
            normT(x, h2T)
            for hh in range(4):
                for kc in range(8):
                    S.op('pe', lambda e, kc=kc, hh=hh: e.matmul(
                        pq[:, hh * 128:(hh + 1) * 128], lhsT=wcq_b[:, kc * 512 + hh * 128:kc * 512 + (hh + 1) * 128],
                        rhs=h2T[:, kc * 128:(kc + 1) * 128], start=(kc == 0), stop=(kc == 7)),
                        reads=[wcq_b, h2T], writes=[pq])
            S.op('act', lambda e: e.copy(out=qcT[:], in_=pq[:]), reads=[pq], writes=[qcT])
            groups = [(0, 128, 0)] if i < NT else [(4 * bb, 4, 1 + bb) for bb in range(4)]
            for (c0, ncol, g) in groups:
                for hh in range(4):
                    for mb in range(2):
                        S.op('pe', lambda e, hh=hh, mb=mb, c0=c0, ncol=ncol, g=g: e.matmul(
                            psx[mb][:, hh * 128 + c0:hh * 128 + c0 + ncol],
                            lhsT=mkT[g][:, hh * 256 + mb * 128:hh * 256 + (mb + 1) * 128],
                            rhs=qcT[:, hh * 128 + c0:hh * 128 + c0 + ncol], start=True, stop=True),
                            reads=[mkT[g], qcT], writes=[psx[mb]])
            for mb in range(2):
                S.op('act', lambda e, mb=mb: e.activation(out=PTx[:, mb * 512:(mb + 1) * 512], in_=psx[mb][:], func=AF.Exp,
                                                          scale=float(128 ** -0.5)), reads=[psx[mb]], writes=[PTx])
            for (c0, ncol, g) in groups:
                for hh in range(4):
                    for mb in range(2):
                        S.op('pe', lambda e, hh=hh, mb=mb, c0=c0, ncol=ncol, g=g: e.matmul(
                            pox[:, hh * 128 + c0:hh * 128 + c0 + ncol],
                            lhsT=mvb[g][:, mb * 512 + hh * 128:mb * 512 + (hh + 1) * 128],
                            rhs=PTx[:, mb * 512 + hh * 128 + c0:mb * 512 + hh * 128 + c0 + ncol],
                            start=(mb == 0), stop=(mb == 1)), reads=[mvb[g], PTx], writes=[pox])
                        S.op('pe', lambda e, hh=hh, mb=mb, c0=c0, ncol=ncol: e.matmul(
                            pdx[:, hh * 128 + c0:hh * 128 + c0 + ncol], lhsT=ones_b[:],
                            rhs=PTx[:, mb * 512 + hh * 128 + c0:mb * 512 + hh * 128 + c0 + ncol],
                            start=(mb == 0), stop=(mb == 1)), reads=[ones_b, PTx], writes=[pdx])
            S.op('dve', lambda e: e.reciprocal(out=rdx[:], in_=pdx[:]), reads=[pdx], writes=[rdx])
            S.op('dve', lambda e: e.tensor_tensor(out=oTx[:], in0=pox[:], in1=rdx[:], op=ALU.mult),
                 reads=[pox, rdx], writes=[oTx])
            for half in range(2):
                for hh in range(4):
                    S.op('pe', lambda e, hh=hh, half=half: e.matmul(
                        py[half][:], lhsT=oTx[:, hh * 128:(hh + 1) * 128],
                        rhs=wco_b[:, hh * 1024 + half * 512:hh * 1024 + (half + 1) * 512],
                        start=(hh == 0), stop=(hh == 3)), reads=[oTx, wco_b], writes=[py[half]])
                S.op('dve', lambda e, half=half, x=x: e.tensor_tensor(out=x[:, half * 512:(half + 1) * 512],
                                                                     in0=x[:, half * 512:(half + 1) * 512], in1=py[half][:],
                                                                     op=ALU.add), reads=[x, py[half]], writes=[x])
            S.dma('pool', X2[i * 128:(i + 1) * 128, :], x[:], reads=[x], writes=[('X2', i)])
        es_f.close()
        cur[0] = es
        S.barrier()
        es_g = contextlib.ExitStack()
        es_g.__enter__()
        cur[0] = es_g

        def cap(tile, off, dims):
            return bass.AP(tensor=tile.t, offset=off, ap=dims)

        gffn = tl("gffn", [128, 8])
        gfin = tl("gfin", [128, D])
        iota16 = tl("iota16_s", [128, 16])
        iota128 = tl("iota128_s", [128, 128])
        S.dma('sp', gffn[:], g_ffn[:, :], writes=[gffn])
        S.dma('sp', gfin[:], g_fin[:, :], writes=[gfin])
        S.dma('sp', iota16[:], iota16_d[:, :], writes=[iota16])
        S.dma('sp', iota128[:], iota128_d[:, :], writes=[iota128])
        sc = tl("sc", [128, 2048])
        scw = tl("scw", [128, 2048])
        cand = tl("cand", [128, 2048])
        gffnx = cand
        wpq_b = tl("wpq_b", [128, 8 * 2048], BF16)
        keys_b = tl("keys_b", [128, 2048], BF16)
        gst = [sc, scw]
        S.dma('sp', gffnx[:, 0:1024], g_ffnx[:, :], writes=[gffnx])
        for kc in range(8):
            for hf in range(2):
                st = gst[hf]
                S.dma('sp', st[:, 0:1024], w_pq[kc * 128:(kc + 1) * 128, hf * 1024:(hf + 1) * 1024], writes=[st])
                S.op('dve', lambda e, st=st, kc=kc, hf=hf: e.tensor_scalar(
                    out=wpq_b[:, kc * 2048 + hf * 1024:kc * 2048 + (hf + 1) * 1024], in0=st[:, 0:1024],
                    scalar1=gffn[:, kc:kc + 1], scalar2=None, op0=ALU.mult), reads=[st, gffn], writes=[wpq_b])
        for hf in range(2):
            S.dma('sp', gst[hf][:, 0:1024], keysT[:, hf * 1024:(hf + 1) * 1024], writes=[gst[hf]])
            S.op('dve', lambda e, hf=hf: e.tensor_copy(out=keys_b[:, hf * 1024:(hf + 1) * 1024], in_=gst[hf][:, 0:1024]),
                 reads=[gst[hf]], writes=[keys_b])
        utb = [tl("utb%d" % i, [128, 1024], BF16) for i in range(8)]
        vtb = [tl("vtb%d" % i, [128, 1024], BF16) for i in range(8)]
        for j in range(128):
            su, sv = gst[0], gst[1]
            S.dma('sp', su[:, 0:1024], ut_h[j, :, :], writes=[su])
            S.dma('sp', sv[:, 0:1024], v_h[j, :, :], writes=[sv])
            cu, cv2 = utb[j % 2], vtb[j % 2]
            S.op('dve', lambda e, cu=cu: e.tensor_tensor(out=cu[:], in0=su[:, 0:1024], in1=gffnx[:, 0:1024], op=ALU.mult),
                 reads=[su, gffnx], writes=[cu])
            S.op('act', lambda e, cv2=cv2: e.copy(out=cv2[:], in_=sv[:, 0:1024]), reads=[sv], writes=[cv2])
            S.dma('pool', UTb[j, :, :], cu[:], reads=[cu], writes=[('UTb', j)])
            S.dma('pool', Vb[j, :, :], cv2[:], reads=[cv2], writes=[('Vb', j)])
        xg = [tl("xg%d" % i, [128, D]) for i in range(2)]
        gjunk = tl("gjunk", [128, D])
        gss = tl("gss", [128, 1])
        grs = tl("grs", [128, 1])
        ghb = tl("ghb", [128, D], BF16)
        h3Tb = [tl("h3T%d" % i, [128, D], BF16) for i in range(2)]
        qTb = tl("qTb", [128, 2048], BF16)
        v16 = tl("v16", [128, 256])
        i16 = tl("i16", [128, 256], U32)
        i16f = tl("i16f", [128, 256])
        candw = scw
        c16 = tl("c16", [128, 128])
        ci = tl("ci", [128, 128], U32)
        ca_u = tl("ca_u", [128, 128], U32)
        cb_u = tl("cb_u", [128, 128], U32)
        ca_f = tl("ca_f", [128, 128])
        cb_f = tl("cb_f", [128, 128])
        eq = cand
        IG = tl("IG", [128, 384])
        gsum = tl("gsum", [128, 8])
        IGT = tl("IGT", [128, 384])
        NQ = 8
        Lh = tl("Lh", [128, NQ * 128], BF16)
        Rh = tl("Rh", [128, NQ * 128], BF16)
        Gsbb = [tl("Gsb%d" % i, [128, 16384], BF16) for i in range(2)]
        ga = [tl("ga%d" % i, [128, 128]) for i in range(4)]
        Wb = [tl("Wb%d" % i, [128, 128], BF16) for i in range(4)]
        yo = gjunk
        pGT = ptl("pGT", [128, 1024], BF16)
        pqs = ptl("pqs", [128, 512])
        pG = [ptl("pG%d" % i, [128, 512]) for i in range(2)]
        pAbank = [ps("pAb%d" % i, [128, 512]) for i in range(2)]

        class PSlot:
            def __init__(self, bank, off, k):
                self.bank, self.off, self.k = bank, off, k

            def ap(self):
                return self.bank[:, self.off:self.off + 128]

        pA = [PSlot(pAbank[s_ % 2], 0, 'pAslot%d' % (s_ % 2)) for s_ in range(4)]
        py3 = [ptl("py3_%d" % i, [128, 512]) for i in range(2)]

        def prep(i):
            x = xg[i % 2]
            h3T = h3Tb[i % 2]
            Gsb = Gsbb[i % 2]
            S.dma('sp', x[:], X2[i * 128:(i + 1) * 128, :], reads=[('X2', i)], writes=[x])
            S.op('act', lambda e, x=x: e.activation(out=gjunk[:], in_=x[:], func=AF.Square, accum_out=gss[:]),
                 reads=[x], writes=[gjunk, gss])
            S.op('dve', lambda e: e.tensor_scalar(out=grs[:], in0=gss[:], scalar1=1.0 / D, scalar2=EPS,
                                                  op0=ALU.mult, op1=ALU.add), reads=[gss], writes=[grs])
            S.op('act', lambda e: e.sqrt(out=grs[:], in_=grs[:]), reads=[grs], writes=[grs])
            S.op('dve', lambda e: e.reciprocal(out=grs[:], in_=grs[:]), reads=[grs], writes=[grs])
            S.op('dve', lambda e, x=x: e.tensor_scalar(out=ghb[:], in0=x[:], scalar1=grs[:, 0:1], scalar2=None,
                                                       op0=ALU.mult), reads=[x, grs], writes=[ghb])
            for kc in range(8):
                S.op('pe', lambda e, kc=kc: e.transpose(out=pGT[:, kc * 128:(kc + 1) * 128],
                                                        in_=ghb[:, kc * 128:(kc + 1) * 128], identity=ident_b[:]),
                     reads=[ghb, 'ident_b'], writes=[pGT])
            S.op('act', lambda e: e.copy(out=h3T[:], in_=pGT[:]), reads=[pGT], writes=[h3T])
            for cg in range(4):
                for cc in range(4):
                    c = cg * 4 + cc
                    for kc in range(8):
                        S.op('pe', lambda e, kc=kc, c=c, cc=cc: e.matmul(
                            pqs[:, cc * 128:(cc + 1) * 128], lhsT=wpq_b[:, kc * 2048 + c * 128:kc * 2048 + (c + 1) * 128],
                            rhs=h3T[:, kc * 128:(kc + 1) * 128], start=(kc == 0), stop=(kc == 7)),
                            reads=[wpq_b, h3T], writes=[pqs])
                S.op('act', lambda e, cg=cg: e.copy(out=qTb[:, cg * 512:(cg + 1) * 512], in_=pqs[:]), reads=[pqs], writes=[qTb])
            for cg in range(4):
                for cc in range(4):
                    c = cg * 4 + cc
                    S.op('pe', lambda e, c=c, cc=cc: e.matmul(
                        pqs[:, cc * 128:(cc + 1) * 128], lhsT=qTb[:, c * 128:(c + 1) * 128],
                        rhs=keys_b[:, c * 128:(c + 1) * 128], start=True, stop=True), reads=[qTb, keys_b], writes=[pqs])
                S.op('act', lambda e, cg=cg: e.copy(out=sc[:, cg * 512:(cg + 1) * 512], in_=pqs[:]), reads=[pqs], writes=[sc])
            for c in range(16):
                cs = slice(c * 128, (c + 1) * 128)
                S.op('dve', lambda e, c=c, cs=cs: e.max(out=v16[:, c * 16:c * 16 + 8], in_=sc[:, cs]), reads=[sc], writes=[v16])
                S.op('dve', lambda e, c=c, cs=cs: e.max_index(out=i16[:, c * 16:c * 16 + 8], in_max=v16[:, c * 16:c * 16 + 8],
                                                              in_values=sc[:, cs]), reads=[sc, v16], writes=[i16])
                S.op('dve', lambda e, c=c, cs=cs: e.match_replace(out=scw[:, cs], in_to_replace=v16[:, c * 16:c * 16 + 8],
                                                                  in_values=sc[:, cs], imm_value=-1e30),
                     reads=[sc, v16], writes=[scw])
                S.op('dve', lambda e, c=c, cs=cs: e.max(out=v16[:, c * 16 + 8:c * 16 + 16], in_=scw[:, cs]),
                     reads=[scw], writes=[v16])
                S.op('dve', lambda e, c=c, cs=cs: e.max_index(out=i16[:, c * 16 + 8:c * 16 + 16],
                                                              in_max=v16[:, c * 16 + 8:c * 16 + 16], in_values=scw[:, cs]),
                     reads=[scw, v16], writes=[i16])
            S.op('dve', lambda e: e.tensor_copy(out=i16f[:], in_=i16[:]), reads=[i16], writes=[i16f])
            S.op('dve', lambda e: e.tensor_tensor(
                out=cand[:].rearrange("p (h a b) -> p h a b", h=8, a=16),
                in0=cap(v16, 0, [[256, 128], [32, 8], [1, 16], [0, 16]]),
                in1=cap(v16, 16, [[256, 128], [32, 8], [0, 16], [1, 16]]), op=ALU.add), reads=[v16], writes=[cand])
            for h in range(8):
                cs = slice(h * 256, (h + 1) * 256)
                S.op('dve', lambda e, h=h, cs=cs: e.max(out=c16[:, h * 16:h * 16 + 8], in_=cand[:, cs]), reads=[cand], writes=[c16])
                S.op('dve', lambda e, h=h, cs=cs: e.max_index(out=ci[:, h * 16:h * 16 + 8], in_max=c16[:, h * 16:h * 16 + 8],
                                                              in_values=cand[:, cs]), reads=[cand, c16], writes=[ci])
                S.op('dve', lambda e, h=h, cs=cs: e.match_replace(out=candw[:, cs], in_to_replace=c16[:, h * 16:h * 16 + 8],
                                                                  in_values=cand[:, cs], imm_value=-1e30),
                     reads=[cand, c16], writes=[candw])
                S.op('dve', lambda e, h=h, cs=cs: e.max(out=c16[:, h * 16 + 8:h * 16 + 16], in_=candw[:, cs]),
                     reads=[candw], writes=[c16])
                S.op('dve', lambda e, h=h, cs=cs: e.max_index(out=ci[:, h * 16 + 8:h * 16 + 16],
                                                              in_max=c16[:, h * 16 + 8:h * 16 + 16], in_values=candw[:, cs]),
                     reads=[candw, c16], writes=[ci])
            S.op('dve', lambda e: e.tensor_single_scalar(out=ca_u[:], in_=ci[:], scalar=4, op=ALU.logical_shift_right),
                 reads=[ci], writes=[ca_u])
            S.op('dve', lambda e: e.tensor_single_scalar(out=cb_u[:], in_=ci[:], scalar=15, op=ALU.bitwise_and),
                 reads=[ci], writes=[cb_u])
            S.op('dve', lambda e: e.tensor_copy(out=ca_f[:], in_=ca_u[:]), reads=[ca_u], writes=[ca_f])
            S.op('dve', lambda e: e.tensor_copy(out=cb_f[:], in_=cb_u[:]), reads=[cb_u], writes=[cb_f])
            for which, (src_f, off) in enumerate(((ca_f, 0), (cb_f, 16))):
                S.op('dve', lambda e, src_f=src_f: e.tensor_tensor(
                    out=eq[:].rearrange("p (s a) -> p s a", a=16),
                    in0=cap(src_f, 0, [[128, 128], [1, 128], [0, 16]]),
                    in1=cap(iota16, 0, [[16, 128], [0, 128], [1, 16]]), op=ALU.is_equal),
                    reads=[src_f, iota16], writes=[eq])
                S.op('dve', lambda e, off=off: e.tensor_tensor(
                    out=eq[:].rearrange("p (h k a) -> p h k a", h=8, k=16),
                    in0=eq[:].rearrange("p (h k a) -> p h k a", h=8, k=16),
                    in1=cap(i16f, off, [[256, 128], [32, 8], [0, 16], [1, 16]]), op=ALU.mult),
                    reads=[eq, i16f], writes=[eq])
                S.op('dve', lambda e, which=which: e.tensor_reduce(
                    out=IG[:, which * 128:(which + 1) * 128], in_=eq[:].rearrange("p (s a) -> p s a", a=16),
                    axis=AX.X, op=ALU.add), reads=[eq], writes=[IG])
            S.op('dve', lambda e: e.tensor_tensor(
                out=IG[:, 256:384].rearrange("p (h k) -> p h k", k=16), in0=c16[:].rearrange("p (h k) -> p h k", k=16),
                in1=cap(c16, 0, [[128, 128], [16, 8], [0, 16]]), op=ALU.subtract), reads=[c16], writes=[IG])
            mark()
            S.op('act', lambda e: e.activation(out=IG[:, 256:384], in_=IG[:, 256:384], func=AF.Exp), reads=[IG], writes=[IG])
            S.op('dve', lambda e: e.tensor_reduce(out=gsum[:], in_=IG[:, 256:384].rearrange("p (h k) -> p h k", k=16),
                                                  axis=AX.X, op=ALU.add), reads=[IG], writes=[gsum])
            S.op('dve', lambda e: e.reciprocal(out=gsum[:], in_=gsum[:]), reads=[gsum], writes=[gsum])
            S.op('dve', lambda e: e.tensor_tensor(
                out=IG[:, 256:384].rearrange("p (h k) -> p h k", k=16), in0=IG[:, 256:384].rearrange("p (h k) -> p h k", k=16),
                in1=cap(gsum, 0, [[8, 128], [1, 8], [0, 16]]), op=ALU.mult), reads=[IG, gsum], writes=[IG])
            for w3 in range(3):
                S.op('pe', lambda e, w3=w3: e.transpose(out=pqs[:, w3 * 128:(w3 + 1) * 128], in_=IG[:, w3 * 128:(w3 + 1) * 128],
                                                        identity=ident_f[:]), reads=[IG, 'ident_f'], writes=[pqs])
            S.op('act', lambda e: e.copy(out=IGT[:], in_=pqs[:, 0:384]), reads=[pqs], writes=[IGT])
            for hf in range(128 // NQ):
                S.op('dve', lambda e, hf=hf: e.tensor_tensor(
                    out=Lh[:].rearrange("p (t i) -> p t i", i=128),
                    in0=cap(iota128, 0, [[128, 128], [0, NQ], [1, 128]]),
                    in1=cap(IGT, hf * NQ, [[384, 128], [1, NQ], [0, 128]]), op=ALU.is_equal),
                    reads=[iota128, IGT], writes=[Lh])
                S.op('dve', lambda e, hf=hf: e.tensor_tensor(
                    out=Rh[:].rearrange("p (t i) -> p t i", i=128),
                    in0=cap(iota128, 0, [[128, 128], [0, NQ], [1, 128]]),
                    in1=cap(IGT, 128 + hf * NQ, [[384, 128], [1, NQ], [0, 128]]), op=ALU.is_equal),
                    reads=[iota128, IGT], writes=[Rh])
                S.op('dve', lambda e, hf=hf: e.tensor_tensor(
                    out=Rh[:].rearrange("p (t i) -> p t i", i=128),
                    in0=Rh[:].rearrange("p (t i) -> p t i", i=128),
                    in1=cap(IGT, 256 + hf * NQ, [[384, 128], [1, NQ], [0, 128]]), op=ALU.mult),
                    reads=[Rh, IGT], writes=[Rh])
                for t4 in range(NQ // 4):
                    pg = pG[t4 % 2]
                    for tt in range(4):
                        tl_ = t4 * 4 + tt
                        S.op('pe', lambda e, pg=pg, tt=tt, tl_=tl_: e.matmul(
                            pg[:, tt * 128:(tt + 1) * 128], lhsT=Lh[:, tl_ * 128:(tl_ + 1) * 128],
                            rhs=Rh[:, tl_ * 128:(tl_ + 1) * 128], start=True, stop=True), reads=[Lh, Rh], writes=[pg])
                    g0 = (hf * NQ + t4 * 4) * 128
                    if t4 % 2 == 0:
                        S.op('act', lambda e, pg=pg, g0=g0: e.copy(out=Gsb[:, g0:g0 + 512], in_=pg[:]), reads=[pg], writes=[Gsb])
                    else:
                        S.op('dve', lambda e, pg=pg, g0=g0: e.tensor_copy(out=Gsb[:, g0:g0 + 512], in_=pg[:]),
                             reads=[pg], writes=[Gsb])

        cur_rec = [None]

        def mark():
            if cur_rec[0] is not None:
                cur_rec[0].append(None)

        def record(fn, *args):
            rec = []
            cur_rec[0] = rec
            o_op, o_dma = S.op, S.dma
            S.op = lambda *a, **k: rec.append((o_op, a, k))
            S.dma = lambda *a, **k: rec.append((o_dma, a, k))
            try:
                fn(*args)
            finally:
                S.op, S.dma = o_op, o_dma
                cur_rec[0] = None
            return rec

        def dense(i, nxt):
            x = xg[i % 2]
            h3T = h3Tb[i % 2]
            Gsb = Gsbb[i % 2]
            pos = [0]

            nsplit = nxt.index(None) if None in nxt else len(nxt)

            def pump(upto):
                while pos[0] < min(upto, len(nxt)):
                    if nxt[pos[0]] is not None:
                        f, a, k = nxt[pos[0]]
                        f(*a, **k)
                    pos[0] += 1

            def sched(j):
                if j < 64:
                    return ((j + 1) * nsplit + 63) // 64
                if j < 80:
                    return nsplit
                return nsplit + ((j - 79) * (len(nxt) - nsplit) + 39) // 40
            def stage_u(j):
                j4 = j % 4
                j8 = j % 8
                S.dma('sp', utb[j8][:], UTb[j, :, :], reads=[('UTb', j)], writes=[utb[j8]])
                S.dma('sp', vtb[j8][:], Vb[j, :, :], reads=[('Vb', j)], writes=[vtb[j8]])
                for kc in range(8):
                    S.op('pe', lambda e, kc=kc, j4=j4, j8=j8: e.matmul(
                        pA[j4].ap(), lhsT=utb[j8][:, kc * 128:(kc + 1) * 128], rhs=h3T[:, kc * 128:(kc + 1) * 128],
                        start=(kc == 0), stop=(kc == 7)), reads=[utb[j8], h3T], writes=[pA[j4]])
                S.op('act', lambda e, j4=j4: e.activation(out=ga[j4][:], in_=pA[j4].ap(), func=AF.Gelu),
                     reads=[pA[j4]], writes=[ga[j4]])
                S.op('dve', lambda e, j4=j4, j=j: e.tensor_tensor(
                    out=Wb[j4][:], in0=ga[j4][:], in1=cap(Gsb, j, [[16384, 128], [128, 128]]), op=ALU.mult),
                    reads=[ga[j4], Gsb], writes=[Wb[j4]])

            def stage_v(j):
                j4 = j % 4
                j8 = j % 8
                for half in range(2):
                    S.op('pe', lambda e, half=half, j=j, j4=j4, j8=j8: e.matmul(
                        py3[half][:], lhsT=Wb[j4][:], rhs=vtb[j8][:, half * 512:(half + 1) * 512],
                        start=(j == 0), stop=(j == 127)), reads=[Wb[j4], vtb[j8]], writes=[py3[half]])

            stage_u(0)
            stage_u(1)
            for j in range(128):
                if j + 2 < 128:
                    stage_u(j + 2)
                stage_v(j)
                pump(sched(j))
            pump(len(nxt))
            for half in range(2):
                S.op('dve', lambda e, half=half, x=x: e.tensor_tensor(out=x[:, half * 512:(half + 1) * 512],
                                                                     in0=x[:, half * 512:(half + 1) * 512], in1=py3[half][:],
                                                                     op=ALU.add), reads=[x, py3[half]], writes=[x])
            if debug:
                S.dma('pool', X3[i * 128:(i + 1) * 128, :], x[:], reads=[x])
            S.op('act', lambda e, x=x: e.activation(out=gjunk[:], in_=x[:], func=AF.Square, accum_out=gss[:]),
                 reads=[x], writes=[gjunk, gss])
            S.op('dve', lambda e: e.tensor_scalar(out=grs[:], in0=gss[:], scalar1=1.0 / D, scalar2=EPS,
                                                  op0=ALU.mult, op1=ALU.add), reads=[gss], writes=[grs])
            S.op('act', lambda e: e.sqrt(out=grs[:], in_=grs[:]), reads=[grs], writes=[grs])
            S.op('dve', lambda e: e.reciprocal(out=grs[:], in_=grs[:]), reads=[grs], writes=[grs])
            S.op('dve', lambda e, x=x: e.scalar_tensor_tensor(out=yo[:], in0=x[:], scalar=grs[:, 0:1], in1=gfin[:],
                                                              op0=ALU.mult, op1=ALU.mult), reads=[x, grs, gfin], writes=[yo])
            if i < NT:
                S.dma('pool', y_p[i * 128:(i + 1) * 128, :], yo[:], reads=[yo])
            else:
                S.dma('pool', y_s[:, :], yo[0:16, :], reads=[yo])

        prep(0)
        for i in range(NTT):
            nxt = record(prep, i + 1) if i + 1 < NTT else []
            dense(i, nxt)
        es_g.close()
        cur[0] = es
        S.finish()
    return nc


_PROGRAM = None
_DEBUG_HOOK = None


def kernel(x_prompt, x_sample, cache_swa_k, cache_swa_v, state_hgrn, cache_mem_k, cache_mem_v,
           mem_prompt, norm_mix, w_in, lb_logits, beta_a, gnorm_b, w_out, norm_cross, norm_mem,
           w_cq, w_mk, w_mv, w_co, norm_ffn, w_pq, peer_k1, peer_k2, peer_u, peer_v, norm_final):
    global _PROGRAM
    f = lambda a: np.ascontiguousarray(np.asarray(a, dtype=np.float32))
    if _PROGRAM is None:
        _PROGRAM = build_program()
    nc = _PROGRAM

    def col(g):
        return f(np.asarray(g).reshape(-1, 128).T)

    common = {
        "w_in": f(w_in[0]), "w_mk": f(w_mk[0]), "w_mv": f(w_mv[0]),
        "g_mix": col(norm_mix[0]), "g_mem": col(norm_mem[0]),
        "w_out": f(w_out[0]), "w_cq": f(w_cq[0]), "w_co": f(w_co[0]),
        "g_outc": col(np.concatenate([np.asarray(beta_a[0]).reshape(-1), np.asarray(gnorm_b[0]).reshape(-1)])),
        "g_cross": col(norm_cross[0]),
        "w_pq": f(w_pq[0]), "g_ffn": col(norm_ffn[0]),
        "g_ffnx": f(np.repeat(np.asarray(norm_ffn[0]).reshape(8, 128).T[:, :, None], 128, axis=2).reshape(128, 1024)),
        "keysT": f(np.stack([np.asarray(peer_k1[0]), np.asarray(peer_k2[0])], axis=1).reshape(16, 128, 128)
                   .transpose(2, 0, 1).reshape(128, 2048)),
        "ut_h": f(np.asarray(peer_u[0]).reshape(128, 128, 8, 128).transpose(1, 3, 2, 0).reshape(128, 128, 1024)),
        "v_h": f(np.asarray(peer_v[0]).reshape(128, 128, 1024).transpose(1, 0, 2)),
        "g_fin": f(np.broadcast_to(np.asarray(norm_final)[None, :], (128, D))),
        "iota16": f(np.broadcast_to(np.arange(16)[None, :], (128, 16))),
        "iota128": f(np.broadcast_to(np.arange(128)[None, :], (128, 128))),
        "ident": np.eye(128, dtype=np.float32),
        "lbl0": f(np.broadcast_to(np.asarray(lb_logits)[0][None, :], (128, 512))),
        "lbl1": f(np.broadcast_to(np.asarray(lb_logits)[1][None, :], (128, 512))),
        "triU": np.triu(np.ones((128, 128), np.float32)),
        "tri4": np.tile(np.triu(np.ones((128, 64), np.float32)), (1, 8)),
        "rowmask": (np.arange(128) < 4).astype(np.float32).reshape(128, 1),
        "bmask": np.kron(np.eye(8, dtype=np.float32), np.ones((1, 66), np.float32)),
        "maskC": np.tile(np.triu(np.ones((128, 128), np.float32)), (1, 8)),
        "maskP": np.tile(np.tril(np.ones((128, 128), np.float32)), (1, 8)),
    }
    in_maps = []
    for c in range(NCORES):
        m = dict(common)
        m["xp"] = f(x_prompt[c])
        m["xs"] = f(np.asarray(x_sample[4 * c:4 * c + 4]).reshape(16, D))
        m["memp"] = f(mem_prompt[c])
        m["st_h"] = f(state_hgrn[0, 4 * c:4 * c + 4])
        m["cmk"] = f(np.asarray(cache_mem_k[0, 4 * c:4 * c + 4]).reshape(4, 256, 512))
        m["cmv"] = f(np.asarray(cache_mem_v[0, 4 * c:4 * c + 4]).reshape(4, 256, 512))
        m["ck"] = f(np.asarray(cache_swa_k[0, 4 * c:4 * c + 4]).reshape(4, 2048, 512))
        m["cv"] = f(np.asarray(cache_swa_v[0, 4 * c:4 * c + 4]).reshape(4, 2048, 512))
        in_maps.append(m)
    if _DEBUG_HOOK is not None:
        return _DEBUG_HOOK(in_maps)
    res = run_bass_kernel_spmd(nc, in_maps, core_ids=list(range(NCORES)))
    R = res.results
    y_prompt = np.stack([R[c]["y_p"] for c in range(NCORES)]).reshape(8, T, D)
    y_sample = np.concatenate([R[c]["y_s"].reshape(4, 4, D) for c in range(NCORES)], axis=0)
    p_k = np.stack([R[c]["o_pk"].reshape(2048, 8, 64) for c in range(NCORES)])[None]
    p_v = np.stack([R[c]["o_pv"].reshape(2048, 8, 64) for c in range(NCORES)])[None]
    p_h = np.stack([R[c]["o_ph"] for c in range(NCORES)])[None]
    p_mk = np.stack([R[c]["o_mk"].reshape(256, 4, 128) for c in range(NCORES)])[None]
    p_mv = np.stack([R[c]["o_mv"].reshape(256, 4, 128) for c in range(NCORES)])[None]
    s_k = np.concatenate([R[c]["o_sk"].reshape(4, 4, 8, 64) for c in range(NCORES)], axis=0)[None]
    s_v = np.concatenate([R[c]["o_sv"].reshape(4, 4, 8, 64) for c in range(NCORES)], axis=0)[None]
    s_h = np.concatenate([R[c]["o_sh"] for c in range(NCORES)], axis=0)[None]
    outs = (y_prompt, y_sample, p_k, p_v, p_h, p_mk, p_mv, s_k, s_v, s_h)
    return tuple(np.ascontiguousarray(o.astype(np.float32)) for o in outs)
```

```python
import contextlib
import numpy as np
import concourse.bass as bass
import concourse.mybir as mybir
from concourse.bass_utils import run_bass_kernel_spmd

F32 = mybir.dt.float32
BF16 = mybir.dt.bfloat16
U32 = mybir.dt.uint32
AF = mybir.ActivationFunctionType
ALU = mybir.AluOpType
AX = mybir.AxisListType

NCORES = 8
_ATT_LVL = 9
_ATT_MAXB = 10 ** 9
_BRANCHES = (1, 4, 16)
T = 4096
NT = 32
NTT = 33
D = 1024
MIXIN = 3584
EPS = 1e-6


class Sync:
    def __init__(self, nc, es):
        self.nc = nc
        self.eng = {'pe': nc.tensor, 'dve': nc.vector, 'act': nc.scalar, 'pool': nc.gpsimd, 'sp': nc.sync}
        self.sem = {}
        self.cnt = {}
        for e in self.eng:
            self.sem[e] = es.enter_context(nc.semaphore('c_' + e))
            self.cnt[e] = 0
        self.R = 8
        for q in ('sp', 'pool'):
            for r in range(self.R):
                k = ('d', q, r)
                self.sem[k] = es.enter_context(nc.semaphore('d_%s%d' % (q, r)))
                self.cnt[k] = 0
        self.dnext = {'sp': 0, 'pool': 0}
        self.waited = {}
        self.last_w = {}
        self.readers = {}

    def _wait(self, eng, dep):
        k, v = dep
        if k == 'pe' and eng == 'pe':
            return
        if self.waited.get((eng, k), 0) >= v:
            return
        self.eng[eng].wait_ge(self.sem[k], v)
        self.waited[(eng, k)] = v

    def _deps(self, eng, reads, writes):
        deps = {}
        def add(d):
            if d is None:
                return
            if deps.get(d[0], 0) < d[1]:
                deps[d[0]] = d[1]
        for r in reads:
            add(self.last_w.get(r))
        for w in writes:
            add(self.last_w.get(w))
            for d in self.readers.get(w, ()):
                add(d)
        for k, v in deps.items():
            self._wait(eng, (k, v))

    def _record(self, me, reads, writes):
        for r in reads:
            self.readers.setdefault(r, []).append(me)
        for w in writes:
            self.last_w[w] = me
            self.readers[w] = []

    def op(self, eng, inst_fn, reads=(), writes=()):
        reads = [getattr(r, 'k', r) for r in reads]
        writes = [getattr(w, 'k', w) for w in writes]
        self._deps(eng, reads, writes)
        inst = inst_fn(self.eng[eng])
        self.cnt[eng] += 1
        inst.then_inc(self.sem[eng], 1)
        self._record((eng, self.cnt[eng]), reads, writes)

    def dma(self, q, out, in_, reads=(), writes=(), **kw):
        reads = [getattr(r, 'k', r) for r in reads]
        writes = [getattr(w, 'k', w) for w in writes]
        self._deps(q, reads, writes)
        r = self.dnext[q]
        self.dnext[q] = (r + 1) % self.R
        k = ('d', q, r)
        inst = self.eng[q].dma_start(out=out, in_=in_, **kw)
        self.cnt[k] += 16
        inst.then_inc(self.sem[k], 16)
        self._record((k, self.cnt[k]), reads, writes)

    def barrier(self):
        for e in self.eng:
            for k, v in self.cnt.items():
                if v > 0 and k != e:
                    self._wait(e, (k, v))

    def finish(self):
        for k, v in self.cnt.items():
            if v > 0 and k != 'sp':
                self._wait('sp', (k, v))


class Tl:
    def __init__(self, t, k):
        self.t, self.k = t, k

    def __getitem__(self, idx):
        return self.t[idx]


def build_program(debug=False):
    nc = bass.Bass("TRN2", target_bir_lowering=False)

    def din(name, shape, dt=F32):
        return nc.dram_tensor(name, list(shape), dt, kind="ExternalInput").ap()

    def dout(name, shape, dt=F32):
        return nc.dram_tensor(name, list(shape), dt, kind="ExternalOutput").ap()

    def dscr(name, shape, dt=F32):
        return nc.dram_tensor(name, list(shape), dt, kind="ExternalOutput" if debug else "Internal").ap()

    xp = din("xp", [T, D])
    xs = din("xs", [16, D])
    memp = din("memp", [256, D])
    w_in = din("w_in", [D, MIXIN])
    w_mk = din("w_mk", [D, 512])
    w_mv = din("w_mv", [D, 512])
    g_mix = din("g_mix", [128, 8])
    g_mem = din("g_mem", [128, 8])
    ident = din("ident", [128, 128])
    lbl0 = din("lbl0", [128, 512])
    lbl1 = din("lbl1", [128, 512])
    triU_d = din("triU", [128, 128])
    tri4_d = din("tri4", [128, 512])
    rowmask_d = din("rowmask", [128, 1])
    st_h = din("st_h", [4, 4, 128, 128])
    maskC_d = din("maskC", [128, 1024])
    maskP_d = din("maskP", [128, 1024])
    ck = din("ck", [4, 2048, 512])
    cv = din("cv", [4, 2048, 512])
    bmask_d = din("bmask", [8, 528])
    w_out = din("w_out", [D, D])
    g_outc = din("g_outc", [128, 8])
    g_cross = din("g_cross", [128, 8])
    w_cq = din("w_cq", [D, 512])
    w_co = din("w_co", [512, D])
    cmk = din("cmk", [4, 256, 512])
    w_pq = din("w_pq", [D, 2048])
    g_ffn = din("g_ffn", [128, 8])
    g_ffnx = din("g_ffnx", [128, 1024])
    keysT = din("keysT", [128, 2048])
    ut_h = din("ut_h", [128, 128, 1024])
    v_h = din("v_h", [128, 128, 1024])
    g_fin = din("g_fin", [128, D])
    iota16_d = din("iota16", [128, 16])
    iota128_d = din("iota128", [128, 128])
    cmv = din("cmv", [4, 256, 512])
    y_p = dout("y_p", [T, D])
    y_s = dout("y_s", [16, D])
    o_pk = dout("o_pk", [2048, 512])
    o_pv = dout("o_pv", [2048, 512])
    o_ph = dout("o_ph", [4, 128, 128])
    o_mk = dout("o_mk", [256, 512])
    o_mv = dout("o_mv", [256, 512])
    o_sk = dout("o_sk", [16, 512])
    o_sv = dout("o_sv", [16, 512])
    o_sh = dout("o_sh", [4, 4, 128, 128])
    Z = dscr("Z", [NTT * 128, MIXIN])
    OB = dscr("OB", [NTT * 128, 512])
    OA = dscr("OA", [3, NTT * 128, 528])
    X2 = dscr("X2", [NTT * 128, D])
    X3 = dscr("X3", [NTT * 128, D]) if debug else None
    UTb = dscr("UTb", [128, 128, 1024], BF16)
    Vb = dscr("Vb", [128, 128, 1024], BF16)

    with contextlib.ExitStack() as es:
        S = Sync(nc, es)

        cur = [es]

        def sb(name, shape, dt=F32):
            return cur[0].enter_context(nc.sbuf_tensor(name, list(shape), dt))

        def ps(name, shape, dt=F32):
            return cur[0].enter_context(nc.psum_tensor(name, list(shape), dt))

        def tl(name, shape, dt=F32):
            return Tl(sb(name, shape, dt), name)

        def ptl(name, shape, dt=F32):
            return Tl(ps(name, shape, dt), name)

        ident_f = sb("ident_f", [128, 128])
        ident_b = sb("ident_b", [128, 128], BF16)
        gmix = sb("gmix", [128, 8])
        gmem = sb("gmem", [128, 8])
        S.dma('sp', ident_f[:], ident[:, :], writes=['ident_f'])
        S.dma('sp', gmix[:], g_mix[:, :], writes=['gmix'])
        S.dma('sp', gmem[:], g_mem[:, :], writes=['gmem'])
        S.op('dve', lambda e: e.tensor_copy(out=ident_b[:], in_=ident_f[:]), reads=['ident_f'], writes=['ident_b'])

        es_a = contextlib.ExitStack()
        es_a.__enter__()
        cur[0] = es_a
        win_b = sb("win_b", [128, 8 * MIXIN], BF16)
        wmk_b = sb("wmk_b", [128, 8 * 512], BF16)
        wmv_b = sb("wmv_b", [128, 8 * 512], BF16)
        wst = [sb("wst%d" % i, [128, MIXIN]) for i in range(2)]
        for kc in range(8):
            st = wst[kc % 2]
            S.dma('sp', st[:], w_in[kc * 128:(kc + 1) * 128, :], writes=['wst%d' % (kc % 2)])
            S.op('dve' if kc % 2 == 0 else 'pool',
                 lambda e, st=st, kc=kc: e.tensor_scalar(out=win_b[:, kc * MIXIN:(kc + 1) * MIXIN], in0=st[:],
                                                         scalar1=gmix[:, kc:kc + 1], scalar2=None, op0=ALU.mult),
                 reads=['wst%d' % (kc % 2), 'gmix'], writes=['win_b'])
        for kc in range(8):
            st = wst[kc % 2]
            S.dma('sp', st[:, 0:512], w_mk[kc * 128:(kc + 1) * 128, :], writes=['wst%d' % (kc % 2)])
            S.dma('sp', st[:, 512:1024], w_mv[kc * 128:(kc + 1) * 128, :], writes=['wst%d' % (kc % 2)])
            S.op('dve', lambda e, st=st, kc=kc: e.tensor_scalar(out=wmk_b[:, kc * 512:(kc + 1) * 512], in0=st[:, 0:512],
                                                                scalar1=gmem[:, kc:kc + 1], scalar2=None, op0=ALU.mult),
                 reads=['wst%d' % (kc % 2), 'gmem'], writes=['wmk_b'])
            S.op('pool', lambda e, st=st, kc=kc: e.tensor_scalar(out=wmv_b[:, kc * 512:(kc + 1) * 512], in0=st[:, 512:1024],
                                                                 scalar1=gmem[:, kc:kc + 1], scalar2=None, op0=ALU.mult),
                 reads=['wst%d' % (kc % 2), 'gmem'], writes=['wmv_b'])

        xt = [sb("xt%d" % i, [128, D]) for i in range(2)]
        junk = sb("junk", [128, D])
        ss = sb("ss", [128, 1])
        rstd = sb("rstd", [128, 1])
        hb = sb("hb", [128, D], BF16)
        hT = [sb("hT%d" % i, [128, D], BF16) for i in range(2)]
        pT = ps("pT", [128, D], BF16)
        pz = [ps("pz%d" % i, [128, 512]) for i in range(2)]
        zt = [sb("zt%d" % i, [128, MIXIN]) for i in range(2)]

        def front(xtile, xkey, hT_t, hkey):
            S.op('act', lambda e: e.activation(out=junk[:], in_=xtile[:], func=AF.Square, accum_out=ss[:]),
                 reads=[xkey], writes=['junk', 'ss'])
            S.op('dve', lambda e: e.tensor_scalar(out=rstd[:], in0=ss[:], scalar1=1.0 / D, scalar2=EPS,
                                                  op0=ALU.mult, op1=ALU.add), reads=['ss'], writes=['rstd'])
            S.op('act', lambda e: e.sqrt(out=rstd[:], in_=rstd[:]), reads=['rstd'], writes=['rstd'])
            S.op('dve', lambda e: e.reciprocal(out=rstd[:], in_=rstd[:]), reads=['rstd'], writes=['rstd'])
            S.op('dve', lambda e: e.tensor_scalar(out=hb[:], in0=xtile[:], scalar1=rstd[:, 0:1], scalar2=None,
                                                  op0=ALU.mult), reads=[xkey, 'rstd'], writes=['hb'])
            for kc in range(8):
                S.op('pe', lambda e, kc=kc: e.transpose(out=pT[:, kc * 128:(kc + 1) * 128],
                                                        in_=hb[:, kc * 128:(kc + 1) * 128], identity=ident_b[:]),
                     reads=['hb', 'ident_b'], writes=['pT'])
            S.op('act', lambda e: e.copy(out=hT_t[:], in_=pT[:]), reads=['pT'], writes=[hkey])

        for i in range(NTT):
            b = i % 2
            xkey = 'xt%d' % b
            if i < NT:
                S.dma('sp', xt[b][:], xp[i * 128:(i + 1) * 128, :], writes=[xkey])
            else:
                S.op('dve', lambda e, b=b: e.memset(xt[b][:], 0.0), writes=[xkey])
                S.dma('sp', xt[b][0:16, :], xs[:, :], writes=[xkey])
            front(xt[b], xkey, hT[b], 'hT%d' % b)
            for g in range(7):
                pzg = pz[g % 2]
                for kc in range(8):
                    S.op('pe', lambda e, kc=kc, g=g, pzg=pzg, b=b: e.matmul(
                        pzg[:], lhsT=hT[b][:, kc * 128:(kc + 1) * 128],
                        rhs=win_b[:, kc * MIXIN + g * 512: kc * MIXIN + (g + 1) * 512],
                        start=(kc == 0), stop=(kc == 7)),
                        reads=['hT%d' % b, 'win_b'], writes=['pz%d' % (g % 2)])
                if g % 2 == 0:
                    S.op('act', lambda e, g=g, pzg=pzg, b=b: e.copy(out=zt[b][:, g * 512:(g + 1) * 512], in_=pzg[:]),
                         reads=['pz%d' % (g % 2)], writes=['zt%d' % b])
                else:
                    S.op('dve', lambda e, g=g, pzg=pzg, b=b: e.tensor_copy(out=zt[b][:, g * 512:(g + 1) * 512], in_=pzg[:]),
                         reads=['pz%d' % (g % 2)], writes=['zt%d' % b])
            S.dma('pool', Z[i * 128:(i + 1) * 128, :], zt[b][:], reads=['zt%d' % b], writes=[('Z', i)])
            if 16 <= i < NT:
                r0 = (i - 16) * 128
                S.dma('pool', o_pk[r0:r0 + 128, :], zt[b][:, 512:1024], reads=['zt%d' % b])
                S.dma('pool', o_pv[r0:r0 + 128, :], zt[b][:, 1024:1536], reads=['zt%d' % b])
            if i == NT:
                S.dma('pool', o_sk[:, :], zt[b][0:16, 512:1024], reads=['zt%d' % b])
                S.dma('pool', o_sv[:, :], zt[b][0:16, 1024:1536], reads=['zt%d' % b])

        for i in range(2):
            b = i % 2
            xkey = 'xt%d' % b
            S.dma('sp', xt[b][:], memp[i * 128:(i + 1) * 128, :], writes=[xkey])
            front(xt[b], xkey, hT[b], 'hT%d' % b)
            for j, wb in enumerate((wmk_b, wmv_b)):
                pzg = pz[j]
                for kc in range(8):
                    S.op('pe', lambda e, kc=kc, wb=wb, pzg=pzg, b=b: e.matmul(
                        pzg[:], lhsT=hT[b][:, kc * 128:(kc + 1) * 128], rhs=wb[:, kc * 512:(kc + 1) * 512],
                        start=(kc == 0), stop=(kc == 7)),
                        reads=['hT%d' % b, 'wmk_b', 'wmv_b'], writes=['pz%d' % j])
                S.op('act' if j == 0 else 'dve',
                     (lambda e, j=j, pzg=pzg, b=b: e.copy(out=zt[b][:, j * 512:(j + 1) * 512], in_=pzg[:])) if j == 0 else
                     (lambda e, j=j, pzg=pzg, b=b: e.tensor_copy(out=zt[b][:, j * 512:(j + 1) * 512], in_=pzg[:])),
                     reads=['pz%d' % j], writes=['zt%d' % b])
            S.dma('pool', o_mk[i * 128:(i + 1) * 128, :], zt[b][:, 0:512], reads=['zt%d' % b])
            S.dma('pool', o_mv[i * 128:(i + 1) * 128, :], zt[b][:, 512:1024], reads=['zt%d' % b])

        es_a.close()
        cur[0] = es
        S.barrier()
        es_d = contextlib.ExitStack()
        es_d.__enter__()
        cur[0] = es_d
        triU = tl("triU_s", [128, 128])
        tri4 = tl("tri4_s", [128, 512])
        rowmask = tl("rowmask_s", [128, 1])
        lb_t = tl("lb_t", [128, 512])
        oml_t = tl("oml_t", [128, 512])
        l1_t = tl("l1_t", [128, 512])
        S.dma('sp', triU[:], triU_d[:, :], writes=[triU])
        S.dma('sp', tri4[:], tri4_d[:, :], writes=[tri4])
        S.dma('sp', rowmask[:], rowmask_d[:, :], writes=[rowmask])
        S.dma('sp', lb_t[:], lbl0[:, :], writes=[lb_t])
        S.dma('sp', l1_t[:], lbl1[:, :], writes=[l1_t])
        S.op('dve', lambda e: e.tensor_tensor(out=lb_t[:], in0=lb_t[:], in1=l1_t[:], op=ALU.subtract),
             reads=[lb_t, l1_t], writes=[lb_t])
        S.op('act', lambda e: e.activation(out=lb_t[:], in_=lb_t[:], func=AF.Sigmoid), reads=[lb_t], writes=[lb_t])
        S.op('dve', lambda e: e.tensor_scalar(out=oml_t[:], in0=lb_t[:], scalar1=-1.0, scalar2=1.0,
                                              op0=ALU.mult, op1=ALU.add), reads=[lb_t], writes=[oml_t])
        hz = [tl("hz%d" % i, [128, 2048]) for i in range(2)]
        sig = tl("sig", [128, 512])
        logf = tl("logf", [128, 512])
        kk = tl("kk", [128, 512])
        qs = tl("qs", [128, 512])
        gs = tl("gs", [128, 512])
        ib_b = tl("ib_b", [128, 512], BF16)
        ec = tl("ec", [128, 512])
        emc = tl("emc", [128, 512])
        ecl = tl("ecl", [128, 4])
        qd_b = tl("qd_b", [128, 512], BF16)
        kd_b = tl("kd_b", [128, 512], BF16)
        qkT = tl("qkT", [128, 1024], BF16)
        AT_b = tl("AT_b", [128, 512], BF16)
        Sst = tl("Sst", [128, 512])
        S_b = tl("S_b", [128, 512], BF16)
        ssq = tl("ssq", [128, 4])
        rb = tl("rb", [128, 4])
        hjunk = tl("hjunk", [128, 128])
        obn = [tl("obn%d" % i, [128, 512]) for i in range(2)]
        pc = ptl("pc", [128, 512])
        pcT = ptl("pcT", [128, 512])
        pTq = ptl("pTq", [128, 1024], BF16)
        pA = ptl("pA", [128, 512])
        po = ptl("po", [128, 512])
        pdS = ptl("pdS", [128, 512])

        P = 64

        def hs(h):
            return slice(h * 128, (h + 1) * 128)

        def hp(h):
            return slice(h * P, (h + 1) * P)

        def hgrn_chunk(ci, row0, nrows, masked):
            z = hz[ci % 2]
            ob = obn[ci % 2]
            if nrows < P:
                S.op('dve', lambda e: e.memset(z[:], 0.0), writes=[z])
            S.dma('sp', z[0:nrows, :], Z[row0:row0 + nrows, 1536:3584], reads=[('Z', row0 // 128)], writes=[z])
            S.op('act', lambda e: e.activation(out=sig[0:P, :], in_=z[0:P, 512:1024], func=AF.Sigmoid), reads=[z], writes=[sig])
            S.op('dve', lambda e: e.tensor_tensor(out=sig[0:P, :], in0=sig[0:P, :], in1=oml_t[0:P, :], op=ALU.mult),
                 reads=[sig, oml_t], writes=[sig])
            S.op('dve', lambda e: e.tensor_tensor(out=sig[0:P, :], in0=sig[0:P, :], in1=lb_t[0:P, :], op=ALU.add),
                 reads=[sig, lb_t], writes=[sig])
            S.op('act', lambda e: e.activation(out=logf[0:P, :], in_=sig[0:P, :], func=AF.Ln), reads=[sig], writes=[logf])
            S.op('dve', lambda e: e.tensor_scalar(out=kk[0:P, :], in0=sig[0:P, :], scalar1=-1.0, scalar2=1.0,
                                                  op0=ALU.mult, op1=ALU.add), reads=[sig], writes=[kk])
            if masked:
                S.op('dve', lambda e: e.tensor_scalar(out=logf[0:P, :], in0=logf[0:P, :], scalar1=rowmask[0:P, 0:1],
                                                      scalar2=None, op0=ALU.mult), reads=[logf, rowmask], writes=[logf])
                S.op('dve', lambda e: e.tensor_scalar(out=kk[0:P, :], in0=kk[0:P, :], scalar1=rowmask[0:P, 0:1],
                                                      scalar2=None, op0=ALU.mult), reads=[kk, rowmask], writes=[kk])
            S.op('act', lambda e: e.activation(out=qs[0:P, :], in_=z[0:P, 0:512], func=AF.Silu), reads=[z], writes=[qs])
            S.op('act', lambda e: e.activation(out=gs[0:P, :], in_=z[0:P, 1536:2048], func=AF.Silu), reads=[z], writes=[gs])
            S.op('dve', lambda e: e.tensor_copy(out=ib_b[0:P, :], in_=z[0:P, 1024:1536]), reads=[z], writes=[ib_b])
            S.op('pe', lambda e: e.matmul(pc[0:P, :], lhsT=triU[0:P, 0:P], rhs=logf[0:P, :], start=True, stop=True),
                 reads=[triU, logf], writes=[pc])
            for h in range(4):
                S.op('pe', lambda e, h=h: e.matmul(pcT[:, hp(h)], lhsT=logf[0:P, hs(h)], rhs=triU[0:P, 0:P],
                                                   start=True, stop=True), reads=[triU, logf], writes=[pcT])
            S.op('act', lambda e: e.activation(out=ec[0:P, :], in_=pc[0:P, :], func=AF.Exp), reads=[pc], writes=[ec])
            S.op('act', lambda e: e.activation(out=emc[0:P, :], in_=pc[0:P, :], func=AF.Exp, scale=-1.0),
                 reads=[pc], writes=[emc])
            S.op('act', lambda e: e.activation(out=ecl[:], in_=pcT[:, P - 1:4 * P:P], func=AF.Exp), reads=[pcT], writes=[ecl])
            S.op('dve', lambda e: e.tensor_tensor(out=qd_b[0:P, :], in0=qs[0:P, :], in1=ec[0:P, :], op=ALU.mult),
                 reads=[qs, ec], writes=[qd_b])
            S.op('dve', lambda e: e.tensor_tensor(out=kd_b[0:P, :], in0=kk[0:P, :], in1=emc[0:P, :], op=ALU.mult),
                 reads=[kk, emc], writes=[kd_b])
            for h in range(4):
                S.op('pe', lambda e, h=h: e.transpose(out=pTq[:, hp(h)], in_=qd_b[0:P, hs(h)], identity=ident_b[0:P, 0:P]),
                     reads=[qd_b, 'ident_b'], writes=[pTq])
                S.op('pe', lambda e, h=h: e.transpose(out=pTq[:, hp(4 + h)], in_=kd_b[0:P, hs(h)],
                                                      identity=ident_b[0:P, 0:P]), reads=[kd_b, 'ident_b'], writes=[pTq])
            S.op('act', lambda e: e.copy(out=qkT[:, 0:8 * P], in_=pTq[:, 0:8 * P]), reads=[pTq], writes=[qkT])
            for h in range(4):
                S.op('pe', lambda e, h=h: e.matmul(pA[0:P, hp(h)], lhsT=qkT[:, hp(4 + h)],
                                                   rhs=qkT[:, hp(h)], start=True, stop=True), reads=[qkT], writes=[pA])
            S.op('dve', lambda e: e.tensor_tensor(out=AT_b[0:P, 0:4 * P], in0=pA[0:P, 0:4 * P], in1=tri4[0:P, 0:4 * P],
                                                  op=ALU.mult), reads=[pA, tri4], writes=[AT_b])
            for h in range(4):
                S.op('pe', lambda e, h=h: e.matmul(po[0:P, hs(h)], lhsT=AT_b[0:P, hp(h)], rhs=ib_b[0:P, hs(h)],
                                                   start=True, stop=False), reads=[AT_b, ib_b], writes=[po])
                S.op('pe', lambda e, h=h: e.matmul(po[0:P, hs(h)], lhsT=qkT[:, hp(h)], rhs=S_b[:, hs(h)],
                                                   start=False, stop=True), reads=[qkT, S_b], writes=[po])
                S.op('pe', lambda e, h=h: e.matmul(pdS[:, hs(h)], lhsT=kd_b[0:P, hs(h)], rhs=ib_b[0:P, hs(h)],
                                                   start=True, stop=True), reads=[kd_b, ib_b], writes=[pdS])
            S.op('dve', lambda e: e.tensor_tensor(out=Sst[:], in0=Sst[:], in1=pdS[:], op=ALU.add),
                 reads=[Sst, pdS], writes=[Sst])
            for h in range(4):
                S.op('dve', lambda e, h=h: e.tensor_scalar(out=Sst[:, hs(h)], in0=Sst[:, hs(h)], scalar1=ecl[:, h:h + 1],
                                                           scalar2=None, op0=ALU.mult), reads=[Sst, ecl], writes=[Sst])
            S.op('dve', lambda e: e.tensor_copy(out=S_b[:], in_=Sst[:]), reads=[Sst], writes=[S_b])
            for h in range(4):
                S.op('act', lambda e, h=h: e.activation(out=hjunk[0:P, :], in_=po[0:P, hs(h)], func=AF.Square,
                                                        accum_out=ssq[0:P, h:h + 1]), reads=[po], writes=[hjunk, ssq])
            S.op('dve', lambda e: e.tensor_scalar(out=rb[0:P, :], in0=ssq[0:P, :], scalar1=1.0 / 128, scalar2=EPS,
                                                  op0=ALU.mult, op1=ALU.add), reads=[ssq], writes=[rb])
            S.op('act', lambda e: e.sqrt(out=rb[0:P, :], in_=rb[0:P, :]), reads=[rb], writes=[rb])
            S.op('dve', lambda e: e.reciprocal(out=rb[0:P, :], in_=rb[0:P, :]), reads=[rb], writes=[rb])
            for h in range(4):
                S.op('dve', lambda e, h=h: e.tensor_scalar(out=ob[0:P, hs(h)], in0=po[0:P, hs(h)], scalar1=rb[0:P, h:h + 1],
                                                           scalar2=None, op0=ALU.mult), reads=[po, rb], writes=[ob])
            S.op('dve', lambda e: e.tensor_tensor(out=ob[0:P, :], in0=ob[0:P, :], in1=gs[0:P, :], op=ALU.mult),
                 reads=[ob, gs], writes=[ob])
            S.dma('pool', OB[row0:row0 + nrows, :], ob[0:nrows, :], reads=[ob], writes=[('OB', row0 // 128)])

        S.op('dve', lambda e: e.memset(Sst[:], 0.0), writes=[Sst])
        S.op('dve', lambda e: e.memset(S_b[:], 0.0), writes=[S_b])
        for i in range(T // P):
            hgrn_chunk(i, i * P, P, False)
        for h in range(4):
            S.dma('pool', o_ph[h, :, :], Sst[:, hs(h)], reads=[Sst])
        for bb in range(4):
            for h in range(4):
                S.dma('sp', Sst[:, hs(h)], st_h[bb, h, :, :], writes=[Sst])
            S.op('dve', lambda e: e.tensor_copy(out=S_b[:], in_=Sst[:]), reads=[Sst], writes=[S_b])
            hgrn_chunk(bb, T + 4 * bb, 4, True)
            for h in range(4):
                S.dma('pool', o_sh[bb, h, :, :], Sst[:, hs(h)], reads=[Sst])
        es_d.close()
        cur[0] = es
        S.barrier()
        es_c = contextlib.ExitStack()
        es_c.__enter__()
        cur[0] = es_c
        mstage = tl("mstage", [128, 1024])
        maskC = tl("maskC_s", [128, 1024], BF16)
        maskP = tl("maskP_s", [128, 1024], BF16)
        S.dma('sp', mstage[:], maskC_d[:, :], writes=[mstage])
        S.op('dve', lambda e: e.tensor_copy(out=maskC[:], in_=mstage[:]), reads=[mstage], writes=[maskC])
        S.dma('sp', mstage[:], maskP_d[:, :], writes=[mstage])
        S.op('dve', lambda e: e.tensor_copy(out=maskP[:], in_=mstage[:]), reads=[mstage], writes=[maskP])
        az = [tl("az%d" % i, [128, 1536]) for i in range(2)]
        qk_b = tl("qk_b", [128, 1024], BF16)
        vaug = [tl("vaug%d" % i, [128, 8 * 66], BF16) for i in range(2)]
        qT = tl("qT", [64, 1024], BF16)
        kT = [tl("kT%d" % i, [64, 1024], BF16) for i in range(2)]
        PTc = tl("PTc", [128, 1024], BF16)
        PTp = tl("PTp", [128, 1024], BF16)
        osb = [tl("osb%d" % i, [128, 528]) for i in range(2)]
        pTt = ptl("pTt", [128, 1024], BF16)
        pTk = ptl("pTk", [128, 1024], BF16)
        psC = [ptl("psC%d" % i, [128, 512]) for i in range(2)]
        psP = [ptl("psP%d" % i, [128, 512]) for i in range(2)]
        pO = [ptl("pO%d" % i, [128, 512]) for i in range(2)]
        for i in range(2):
            S.op('dve', lambda e, i=i: e.memset(vaug[i][:], 1.0), writes=[vaug[i]])
        blk = 0
        for br, dil in enumerate(_BRANCHES):
            nb = T // dil // 128
            if dil == 1:
                Zv = Z[0:T, :].rearrange("(o m) c -> o m c", o=1)
                OAv = OA[br, 0:T, :].rearrange("(o m) c -> o m c", o=1)
            else:
                Zv = Z[0:T, :].rearrange("(m d) c -> d m c", d=dil)
                OAv = OA[br, 0:T, :].rearrange("(m d) c -> d m c", d=dil)
            for r in range(dil):
                for b in range(nb):
                    cb = blk % 2
                    pb = 1 - cb
                    a = az[cb]
                    S.dma('sp', a[:], Zv[r, b * 128:(b + 1) * 128, 0:1536], reads=[('Z', i) for i in range(NT)] if blk == 0 else [],
                          writes=[a])
                    S.op('dve', lambda e, a=a: e.tensor_copy(out=qk_b[:], in_=a[:, 0:1024]), reads=[a], writes=[qk_b])
                    S.op('act', lambda e, a=a, cb=cb: e.copy(
                        out=vaug[cb][:].rearrange("p (h e) -> p h e", e=66)[:, :, 0:64],
                        in_=a[:, 1024:1536].rearrange("p (h e) -> p h e", e=64)), reads=[a], writes=[vaug[cb]])
                    if blk >= _ATT_MAXB or _ATT_LVL < 2:
                        blk += 1
                        continue
                    for h in range(8):
                        S.op('pe', lambda e, h=h: e.transpose(out=pTt[0:64, h * 128:(h + 1) * 128],
                                                              in_=qk_b[:, h * 64:(h + 1) * 64], identity=ident_b[:]),
                             reads=[qk_b, 'ident_b'], writes=[pTt])
                        S.op('pe', lambda e, h=h: e.transpose(out=pTk[0:64, h * 128:(h + 1) * 128],
                                                              in_=qk_b[:, 512 + h * 64:512 + (h + 1) * 64],
                                                              identity=ident_b[:]),
                             reads=[qk_b, 'ident_b'], writes=[pTk])
                    S.op('act', lambda e: e.copy(out=qT[0:64, :], in_=pTt[0:64, :]), reads=[pTt], writes=[qT])
                    S.op('act', lambda e, cb=cb: e.copy(out=kT[cb][0:64, :], in_=pTk[0:64, :]), reads=[pTk], writes=[kT[cb]])
                    if _ATT_LVL < 3:
                        blk += 1
                        continue
                    for h in range(8):
                        p, j = h // 2, h % 2
                        S.op('pe', lambda e, h=h, p=p, j=j, cb=cb: e.matmul(
                            psC[h // 4][:, (h % 4) * 128:(h % 4 + 1) * 128], lhsT=kT[cb][0:64, h * 128:(h + 1) * 128],
                            rhs=qT[0:64, h * 128:(h + 1) * 128], start=True, stop=True),
                            reads=[kT[cb], qT], writes=[psC[h // 4]])
                    if b > 0:
                        for h in range(8):
                            p, j = h // 2, h % 2
                            S.op('pe', lambda e, h=h, p=p, j=j, pb=pb: e.matmul(
                                psP[h // 4][:, (h % 4) * 128:(h % 4 + 1) * 128], lhsT=kT[pb][0:64, h * 128:(h + 1) * 128],
                                rhs=qT[0:64, h * 128:(h + 1) * 128], start=True, stop=True),
                                reads=[kT[pb], qT], writes=[psP[h // 4]])
                    if _ATT_LVL < 4:
                        blk += 1
                        continue
                    for hh in range(2):
                        S.op('act', lambda e, hh=hh: e.activation(out=PTc[:, hh * 512:(hh + 1) * 512], in_=psC[hh][:],
                                                                  func=AF.Exp, scale=0.125), reads=[psC[hh]], writes=[PTc])
                    S.op('dve', lambda e: e.tensor_tensor(out=PTc[:], in0=PTc[:], in1=maskC[:], op=ALU.mult),
                         reads=[PTc, maskC], writes=[PTc])
                    if b > 0:
                        for hh in range(2):
                            S.op('act', lambda e, hh=hh: e.activation(out=PTp[:, hh * 512:(hh + 1) * 512], in_=psP[hh][:],
                                                                      func=AF.Exp, scale=0.125), reads=[psP[hh]], writes=[PTp])
                        S.op('pool', lambda e: e.tensor_tensor(out=PTp[:], in0=PTp[:], in1=maskP[:], op=ALU.mult),
                             reads=[PTp, maskP], writes=[PTp])
                    if _ATT_LVL < 5:
                        blk += 1
                        continue
                    for h in range(8):
                        po_t = pO[h // 4]
                        osl = slice((h % 4) * 66, (h % 4 + 1) * 66)
                        S.op('pe', lambda e, h=h, po_t=po_t, osl=osl, cb=cb: e.matmul(
                            po_t[:, osl], lhsT=PTc[:, h * 128:(h + 1) * 128], rhs=vaug[cb][:, h * 66:(h + 1) * 66],
                            start=True, stop=(b == 0)), reads=[PTc, vaug[cb]], writes=[po_t])
                        if b > 0:
                            S.op('pe', lambda e, h=h, po_t=po_t, osl=osl, pb=pb: e.matmul(
                                po_t[:, osl], lhsT=PTp[:, h * 128:(h + 1) * 128], rhs=vaug[pb][:, h * 66:(h + 1) * 66],
                                start=False, stop=True), reads=[PTp, vaug[pb]], writes=[po_t])
                    o = osb[cb]
                    S.op('act', lambda e, o=o: e.copy(out=o[:, 0:264], in_=pO[0][:, 0:264]), reads=[pO[0]], writes=[o])
                    S.op('dve', lambda e, o=o: e.tensor_copy(out=o[:, 264:528], in_=pO[1][:, 0:264]), reads=[pO[1]], writes=[o])
                    S.dma('pool', OAv[r, b * 128:(b + 1) * 128, :], o[:], reads=[o], writes=[('OA', br)])
                    blk += 1
        es_c.close()
        cur[0] = es
        S.barrier()
        es_e = contextlib.ExitStack()
        es_e.__enter__()
        cur[0] = es_e
        bmask = tl("bmask_s", [8, 528])
        S.dma('sp', bmask[:], bmask_d[:, :], writes=[bmask])
        skv = [tl("skv%d" % i, [128, 1024]) for i in range(2)]
        sqb = tl("sqb", [128, 512])
        sprod = tl("sprod", [128, 512])
        ssc = tl("ssc", [128, 8])
        sp_b = tl("sp_b", [128, 8], BF16)
        svaug = [tl("svaug%d" % i, [128, 528], BF16) for i in range(2)]
        sq8 = tl("sq8", [8, 64])
        sk8 = tl("sk8", [8, 64])
        sv8 = tl("sv8", [8, 64])
        sj8 = tl("sj8", [8, 64])
        ss8 = tl("ss8", [8, 1])
        sdiag = tl("sdiag", [8, 528])
        sres = [tl("sres%d" % i, [8, 66]) for i in range(2)]
        pSa = ptl("pSa", [8, 512])
        pSb = ptl("pSb", [8, 512])
        for i in range(2):
            S.op('dve', lambda e, i=i: e.memset(svaug[i][:], 1.0), writes=[svaug[i]])
        it = 0
        for bb in range(4):
            for t in range(4):
                row = T + 4 * bb + t
                S.dma('sp', sqb[:], bass.AP(tensor=Z.tensor, offset=row * MIXIN, ap=[[0, 128], [1, 512]]),
                      reads=[('Z', NT)], writes=[sqb])
                S.dma('sp', sq8[:], Z[row, 0:512].rearrange("(h e) -> h e", e=64), writes=[sq8])
                S.dma('sp', sk8[:], Z[row, 512:1024].rearrange("(h e) -> h e", e=64), writes=[sk8])
                S.dma('sp', sv8[:], Z[row, 1024:1536].rearrange("(h e) -> h e", e=64), writes=[sv8])
                for br, dil in enumerate((1, 4, 16)):
                    kv = skv[it % 2]
                    va = svaug[it % 2]
                    it += 1
                    start = 2048 + t - dil * 128
                    if dil == 1:
                        ncache = 128 - t
                        S.dma('sp', kv[0:ncache, 0:512], ck[bb, start:2048, :], writes=[kv])
                        S.dma('sp', kv[0:ncache, 512:1024], cv[bb, start:2048, :], writes=[kv])
                        if t > 0:
                            S.dma('sp', kv[ncache:128, :], Z[T + 4 * bb:T + 4 * bb + t, 512:1536], reads=[('Z', NT)], writes=[kv])
                    else:
                        S.dma('sp', kv[:, 0:512], bass.AP(tensor=ck.tensor, offset=(bb * 2048 + start) * 512,
                                                          ap=[[dil * 512, 128], [1, 512]]), writes=[kv])
                        S.dma('sp', kv[:, 512:1024], bass.AP(tensor=cv.tensor, offset=(bb * 2048 + start) * 512,
                                                             ap=[[dil * 512, 128], [1, 512]]), writes=[kv])
                    S.op('act', lambda e, kv=kv, va=va: e.copy(
                        out=va[:].rearrange("p (h e) -> p h e", e=66)[:, :, 0:64],
                        in_=kv[:, 512:1024].rearrange("p (h e) -> p h e", e=64)), reads=[kv], writes=[va])
                    S.op('dve', lambda e, kv=kv: e.tensor_tensor(out=sprod[:], in0=kv[:, 0:512], in1=sqb[:], op=ALU.mult),
                         reads=[kv, sqb], writes=[sprod])
                    S.op('dve', lambda e: e.tensor_reduce(out=ssc[:], in_=sprod[:].rearrange("p (h e) -> p h e", e=64),
                                                          axis=AX.X, op=ALU.add), reads=[sprod], writes=[ssc])
                    S.op('act', lambda e: e.activation(out=sp_b[:], in_=ssc[:], func=AF.Exp, scale=0.125),
                         reads=[ssc], writes=[sp_b])
                    S.op('pe', lambda e, va=va, br=br: e.matmul(pSa[:, 0:264], lhsT=sp_b[:], rhs=va[:, 0:264],
                                                                start=(br == 0), stop=(br == 2)),
                         reads=[sp_b, va], writes=[pSa])
                    S.op('pe', lambda e, va=va, br=br: e.matmul(pSb[:, 0:264], lhsT=sp_b[:], rhs=va[:, 264:528],
                                                                start=(br == 0), stop=(br == 2)),
                         reads=[sp_b, va], writes=[pSb])
                res = sres[(4 * bb + t) % 2]
                S.op('dve', lambda e: e.tensor_tensor(out=sdiag[:, 0:264], in0=pSa[:, 0:264], in1=bmask[:, 0:264], op=ALU.mult),
                     reads=[pSa, bmask], writes=[sdiag])
                S.op('dve', lambda e: e.tensor_tensor(out=sdiag[:, 264:528], in0=pSb[:, 0:264], in1=bmask[:, 264:528], op=ALU.mult),
                     reads=[pSb, bmask], writes=[sdiag])
                S.op('dve', lambda e, res=res: e.tensor_reduce(out=res[:], in_=sdiag[:].rearrange("p (h e) -> p e h", e=66),
                                                               axis=AX.X, op=ALU.add), reads=[sdiag], writes=[res])
                S.op('dve', lambda e: e.tensor_tensor(out=sj8[:], in0=sq8[:], in1=sk8[:], op=ALU.mult),
                     reads=[sq8, sk8], writes=[sj8])
                S.op('dve', lambda e: e.tensor_reduce(out=ss8[:], in_=sj8[:], axis=AX.X, op=ALU.add), reads=[sj8], writes=[ss8])
                S.op('act', lambda e: e.activation(out=ss8[:], in_=ss8[:], func=AF.Exp, scale=0.125), reads=[ss8], writes=[ss8])
                S.op('dve', lambda e: e.tensor_scalar(out=ss8[:], in0=ss8[:], scalar1=3.0, scalar2=None, op0=ALU.mult),
                     reads=[ss8], writes=[ss8])
                S.op('dve', lambda e, res=res: e.scalar_tensor_tensor(out=res[:, 0:64], in0=sv8[:], scalar=ss8[:, 0:1],
                                                                      in1=res[:, 0:64], op0=ALU.mult, op1=ALU.add),
                     reads=[sv8, ss8, res], writes=[res])
                S.op('dve', lambda e, res=res: e.tensor_tensor(out=res[:, 64:65], in0=res[:, 64:65], in1=ss8[:], op=ALU.add),
                     reads=[res, ss8], writes=[res])
                S.dma('pool', OA[0, row, :].rearrange("(h e) -> h e", e=66), res[:], reads=[res], writes=[('OA', 0)])
        es_e.close()
        cur[0] = es
        S.barrier()
        es_f = contextlib.ExitStack()
        es_f.__enter__()
        cur[0] = es_f
        goutc = tl("goutc", [128, 8])
        gcross = tl("gcross", [128, 8])
        S.dma('sp', goutc[:], g_outc[:, :], writes=[goutc])
        S.dma('sp', gcross[:], g_cross[:, :], writes=[gcross])
        wout_b = tl("wout_b", [128, 8 * 1024], BF16)
        wcq_b = tl("wcq_b", [128, 8 * 512], BF16)
        wco_b = tl("wco_b", [128, 4 * 1024], BF16)
        fst = [tl("fst%d" % i, [128, 1024]) for i in range(2)]
        for kc in range(8):
            st = fst[kc % 2]
            S.dma('sp', st[:], w_out[kc * 128:(kc + 1) * 128, :], writes=[st])
            S.op('dve', lambda e, st=st, kc=kc: e.tensor_scalar(out=wout_b[:, kc * 1024:(kc + 1) * 1024], in0=st[:],
                                                                scalar1=goutc[:, kc:kc + 1], scalar2=None, op0=ALU.mult),
                 reads=[st, goutc], writes=[wout_b])
        for kc in range(8):
            st = fst[kc % 2]
            S.dma('sp', st[:, 0:512], w_cq[kc * 128:(kc + 1) * 128, :], writes=[st])
            S.op('dve', lambda e, st=st, kc=kc: e.tensor_scalar(out=wcq_b[:, kc * 512:(kc + 1) * 512], in0=st[:, 0:512],
                                                                scalar1=gcross[:, kc:kc + 1], scalar2=None, op0=ALU.mult),
                 reads=[st, gcross], writes=[wcq_b])
        for kc in range(4):
            st = fst[kc % 2]
            S.dma('sp', st[:], w_co[kc * 128:(kc + 1) * 128, :], writes=[st])
            S.op('dve', lambda e, st=st, kc=kc: e.tensor_copy(out=wco_b[:, kc * 1024:(kc + 1) * 1024], in_=st[:]),
                 reads=[st], writes=[wco_b])
        ones_b = tl("ones_b", [128, 128], BF16)
        S.op('dve', lambda e: e.memset(ones_b[:], 1.0), writes=[ones_b])
        mkT = [tl("mkT%d" % i, [128, 4 * 256], BF16) for i in range(5)]
        mvb = [tl("mvb%d" % i, [128, 2 * 512], BF16) for i in range(5)]
        mkb = tl("mkb", [128, 512], BF16)
        pFT = ptl("pFT", [128, 1024], BF16)
        pTm = pFT
        for g in range(5):
            src_k = o_mk if g == 0 else cmk[g - 1]
            src_v = o_mv if g == 0 else cmv[g - 1]
            for mb in range(2):
                st = fst[mb]
                S.dma('sp', st[:, 0:512], src_k[mb * 128:(mb + 1) * 128, :], writes=[st])
                S.dma('sp', st[:, 512:1024], src_v[mb * 128:(mb + 1) * 128, :], writes=[st])
                S.op('dve', lambda e, st=st: e.tensor_copy(out=mkb[:], in_=st[:, 0:512]), reads=[st], writes=[mkb])
                S.op('dve', lambda e, st=st, g=g, mb=mb: e.tensor_copy(out=mvb[g][:, mb * 512:(mb + 1) * 512], in_=st[:, 512:1024]),
                     reads=[st], writes=[mvb[g]])
                for hh in range(4):
                    S.op('pe', lambda e, hh=hh: e.transpose(out=pTm[:, hh * 128:(hh + 1) * 128], in_=mkb[:, hh * 128:(hh + 1) * 128],
                                                            identity=ident_b[:]), reads=[mkb, 'ident_b'], writes=[pTm])
                S.op('act', lambda e, g=g, mb=mb: e.copy(
                    out=mkT[g][:].rearrange("p (h m) -> p h m", m=256)[:, :, mb * 128:(mb + 1) * 128],
                    in_=pTm[:, 0:512].rearrange("p (h m) -> p h m", m=128)), reads=[pTm], writes=[mkT[g]])
        xa = [tl("xa%d" % i, [128, D]) for i in range(2)]
        oat = [tl("oat%d" % i, [128, 528]) for i in range(3)]
        obt = tl("obt", [128, 512])
        rden = tl("rden", [128, 8])
        oan = tl("oan", [128, 512])
        cat = tl("cat", [128, D], BF16)
        fjunk = tl("fjunk", [128, D])
        fss = tl("fss", [128, 1])
        frs = tl("frs", [128, 1])
        fhb = tl("fhb", [128, D], BF16)
        catT = tl("catT", [128, D], BF16)
        h2T = tl("h2T", [128, D], BF16)
        qcT = tl("qcT", [128, 512], BF16)
        PTx = tl("PTx", [128, 1024], BF16)
        rdx = tl("rdx", [128, 512])
        oTx = tl("oTx", [128, 512], BF16)
        py = [ptl("py%d" % i, [128, 512]) for i in range(2)]
        pq = ptl("pq", [128, 512])
        psx = [ptl("psx%d" % i, [128, 512]) for i in range(2)]
        pox = ptl("pox", [128, 512])
        pdx = ptl("pdx", [128, 512])

        def normT(xtile, outT):
            S.op('act', lambda e: e.activation(out=fjunk[:], in_=xtile[:], func=AF.Square, accum_out=fss[:]),
                 reads=[xtile], writes=[fjunk, fss])
            S.op('dve', lambda e: e.tensor_scalar(out=frs[:], in0=fss[:], scalar1=1.0 / D, scalar2=EPS,
                                                  op0=ALU.mult, op1=ALU.add), reads=[fss], writes=[frs])
            S.op('act', lambda e: e.sqrt(out=frs[:], in_=frs[:]), reads=[frs], writes=[frs])
            S.op('dve', lambda e: e.reciprocal(out=frs[:], in_=frs[:]), reads=[frs], writes=[frs])
            S.op('dve', lambda e: e.tensor_scalar(out=fhb[:], in0=xtile[:], scalar1=frs[:, 0:1], scalar2=None,
                                                  op0=ALU.mult), reads=[xtile, frs], writes=[fhb])
            for kc in range(8):
                S.op('pe', lambda e, kc=kc: e.transpose(out=pFT[:, kc * 128:(kc + 1) * 128],
                                                        in_=fhb[:, kc * 128:(kc + 1) * 128], identity=ident_b[:]),
                     reads=[fhb, 'ident_b'], writes=[pFT])
            S.op('act', lambda e: e.copy(out=outT[:], in_=pFT[:]), reads=[pFT], writes=[outT])

        for i in range(NTT):
            x = xa[i % 2]
            nbr = 3 if i < NT else 1
            if i < NT:
                S.dma('sp', x[:], xp[i * 128:(i + 1) * 128, :], writes=[x])
            else:
                S.op('dve', lambda e, x=x: e.memset(x[:], 0.0), writes=[x])
                S.dma('sp', x[0:16, :], xs[:, :], writes=[x])
                for t3 in (oat[0], obt):
                    S.op('dve', lambda e, t3=t3: e.memset(t3[:], 1.0), writes=[t3])
            nr = 128 if i < NT else 16
            for br in range(nbr):
                S.dma('sp', oat[br][0:nr, :], OA[br, i * 128:i * 128 + nr, :], reads=[('OA', br)], writes=[oat[br]])
            S.dma('sp', obt[0:nr, :], OB[i * 128:i * 128 + nr, :], reads=[('OB', j) for j in range(NTT)], writes=[obt])
            for br in range(1, nbr):
                S.op('dve', lambda e, br=br: e.tensor_tensor(out=oat[0][:], in0=oat[0][:], in1=oat[br][:], op=ALU.add),
                     reads=[oat[0], oat[br]], writes=[oat[0]])
            S.op('dve', lambda e: e.reciprocal(out=rden[:], in_=oat[0][:].rearrange("p (h e) -> p h e", e=66)[:, :, 64]),
                 reads=[oat[0]], writes=[rden])
            for h in range(8):
                S.op('dve', lambda e, h=h: e.tensor_scalar(out=oan[:, h * 64:(h + 1) * 64], in0=oat[0][:, h * 66:h * 66 + 64],
                                                           scalar1=rden[:, h:h + 1], scalar2=None, op0=ALU.mult),
                     reads=[oat[0], rden], writes=[oan])
            S.op('act', lambda e: e.activation(out=fjunk[:, 0:512], in_=oan[:], func=AF.Square, accum_out=fss[:]),
                 reads=[oan], writes=[fjunk, fss])
            S.op('dve', lambda e: e.tensor_scalar(out=frs[:], in0=fss[:], scalar1=1.0 / 512, scalar2=EPS,
                                                  op0=ALU.mult, op1=ALU.add), reads=[fss], writes=[frs])
            S.op('act', lambda e: e.sqrt(out=frs[:], in_=frs[:]), reads=[frs], writes=[frs])
            S.op('dve', lambda e: e.reciprocal(out=frs[:], in_=frs[:]), reads=[frs], writes=[frs])
            S.op('dve', lambda e: e.tensor_scalar(out=cat[:, 0:512], in0=oan[:], scalar1=frs[:, 0:1], scalar2=None,
                                                  op0=ALU.mult), reads=[oan, frs], writes=[cat])
            S.op('dve', lambda e: e.tensor_copy(out=cat[:, 512:1024], in_=obt[:]), reads=[obt], writes=[cat])
            for kc in range(8):
                S.op('pe', lambda e, kc=kc: e.transpose(out=pFT[:, kc * 128:(kc + 1) * 128],
                                                        in_=cat[:, kc * 128:(kc + 1) * 128], identity=ident_b[:]),
                     reads=[cat, 'ident_b'], writes=[pFT])
            S.op('act', lambda e: e.copy(out=catT[:], in_=pFT[:]), reads=[pFT], writes=[catT])
            for half in range(2):
                for kc in range(8):
                    S.op('pe', lambda e, kc=kc, half=half: e.matmul(
                        py[half][:], lhsT=catT[:, kc * 128:(kc + 1) * 128],
                        rhs=wout_b[:, kc * 1024 + half * 512:kc * 1024 + (half + 1) * 512],
                        start=(kc == 0), stop=(kc == 7)), reads=[catT, wout_b], writes=[py[half]])
                S.op('dve', lambda e, half=half, x=x: e.tensor_tensor(out=x[:, half * 512:(half + 1) * 512],
                                                                     in0=x[:, half * 512:(half + 1) * 512], in1=py[half][:],
                                                                     op=ALU.add), reads=[x, py[half]], writes=[x])
            normT(x, h2T)
            for hh in range(4):
                for kc in range(8):
                    S.op('pe', lambda e, kc=kc, hh=hh: e.matmul(
                        pq[:, hh * 128:(hh + 1) * 128], lhsT=wcq_b[:, kc * 512 + hh * 128:kc * 512 + (hh + 1) * 128],
                        rhs=h2T[:, kc * 128:(kc + 1) * 128], start=(kc == 0), stop=(kc == 7)),
                        reads=[wcq_b, h2T], writes=[pq])
            S.op('act', lambda e: e.copy(out=qcT[:], in_=pq[:]), reads=[pq], writes=[qcT])
            groups = [(0, 128, 0)] if i < NT else [(4 * bb, 4, 1 + bb) for bb in range(4)]
            for (c0, ncol, g) in groups:
                for hh in range(4):
                    for mb in range(2):
                        S.op('pe', lambda e, hh=hh, mb=mb, c0=c0, ncol=ncol, g=g: e.matmul(
                            psx[mb][:, hh * 128 + c0:hh * 128 + c0 + ncol],
                            lhsT=mkT[g][:, hh * 256 + mb * 128:hh * 256 + (mb + 1) * 128],
                            rhs=qcT[:, hh * 128 + c0:hh * 128 + c0 + ncol], start=True, stop=True),
                            reads=[mkT[g], qcT], writes=[psx[mb]])
            for mb in range(2):
                S.op('act', lambda e, mb=mb: e.activation(out=PTx[:, mb * 512:(mb + 1) * 512], in_=psx[mb][:], func=AF.Exp,
                                                          scale=float(128 ** -0.5)), reads=[psx[mb]], writes=[PTx])
            for (c0, ncol, g) in groups:
                for hh in range(4):
                    for mb in range(2):
                        S.op('pe', lambda e, hh=hh, mb=mb, c0=c0, ncol=ncol, g=g: e.matmul(
                            pox[:, hh * 128 + c0:hh * 128 + c0 + ncol],
                            lhsT=mvb[g][:, mb * 512 + hh * 128:mb * 512 + (hh + 1) * 128],
                            rhs=PTx[:, mb * 512 + hh * 128 + c0:mb * 512 + hh * 128 + c0 + ncol],
                            start=(mb == 0), stop=(mb == 1)), reads=[mvb[g], PTx], writes=[pox])
                        S.op('pe', lambda e, hh=hh, mb=mb, c0=c0, ncol=ncol: e.matmul(
                            pdx[:, hh * 128 + c0:hh * 128 + c0 + ncol], lhsT=ones_b[:],
                            rhs=PTx[:, mb * 512 + hh * 128 + c0:mb * 512 + hh * 128 + c0 + ncol],
                            start=(mb == 0), stop=(mb == 1)), reads=[ones_b, PTx], writes=[pdx])
            S.op('dve', lambda e: e.reciprocal(out=rdx[:], in_=pdx[:]), reads=[pdx], writes=[rdx])
            S.op('dve', lambda e: e.tensor_tensor(out=oTx[:], in0=pox[:], in1=rdx[:], op=ALU.mult),
                 reads=[pox, rdx], writes=[oTx])
            for half in range(2):
                for hh in range(4):
                    S.op('pe', lambda e, hh=hh, half=half: e.matmul(
                        py[half][:], lhsT=oTx[:, hh * 128:(hh + 1) * 128],
                        rhs=wco_b[:, hh * 1024 + half * 512:hh * 1024 + (half + 1) * 512],
                        start=(hh == 0), stop=(hh == 3)), reads=[oTx, wco_b], writes=[py[half]])
                S.op('dve', lambda e, half=half, x=x: e.tensor_tensor(out=x[:, half * 512:(half + 1) * 512],
                                                                     in0=x[:, half * 512:(half + 1) * 512], in1=py[half][:],
                                                                     op=ALU.add), reads=[x, py[half]], writes=[x])
            S.dma('pool', X2[i * 128:(i + 1) * 128, :], x[:], reads=[x], writes=[('X2', i)])
        es_f.close()
        cur[0] = es
        S.barrier()
        es_g = contextlib.ExitStack()
        es_g.__enter__()
        cur[0] = es_g

        def cap(tile, off, dims):
            return bass.AP(tensor=tile.t, offset=off, ap=dims)

        gffn = tl("gffn", [128, 8])
        gfin = tl("gfin", [128, D])
        iota16 = tl("iota16_s", [128, 16])
        iota128 = tl("iota128_s", [128, 128])
        S.dma('sp', gffn[:], g_ffn[:, :], writes=[gffn])
        S.dma('sp', gfin[:], g_fin[:, :], writes=[gfin])
        S.dma('sp', iota16[:], iota16_d[:, :], writes=[iota16])
        S.dma('sp', iota128[:], iota128_d[:, :], writes=[iota128])
        sc = tl("sc", [128, 2048])
        scw = tl("scw", [128, 2048])
        cand = tl("cand", [128, 2048])
        gffnx = cand
        wpq_b = tl("wpq_b", [128, 8 * 2048], BF16)
        keys_b = tl("keys_b", [128, 2048], BF16)
        gst = [sc, scw]
        S.dma('sp', gffnx[:, 0:1024], g_ffnx[:, :], writes=[gffnx])
        for kc in range(8):
            for hf in range(2):
                st = gst[hf]
                S.dma('sp', st[:, 0:1024], w_pq[kc * 128:(kc + 1) * 128, hf * 1024:(hf + 1) * 1024], writes=[st])
                S.op('dve', lambda e, st=st, kc=kc, hf=hf: e.tensor_scalar(
                    out=wpq_b[:, kc * 2048 + hf * 1024:kc * 2048 + (hf + 1) * 1024], in0=st[:, 0:1024],
                    scalar1=gffn[:, kc:kc + 1], scalar2=None, op0=ALU.mult), reads=[st, gffn], writes=[wpq_b])
        for hf in range(2):
            S.dma('sp', gst[hf][:, 0:1024], keysT[:, hf * 1024:(hf + 1) * 1024], writes=[gst[hf]])
            S.op('dve', lambda e, hf=hf: e.tensor_copy(out=keys_b[:, hf * 1024:(hf + 1) * 1024], in_=gst[hf][:, 0:1024]),
                 reads=[gst[hf]], writes=[keys_b])
        utb = [tl("utb%d" % i, [128, 1024], BF16) for i in range(8)]
        vtb = [tl("vtb%d" % i, [128, 1024], BF16) for i in range(8)]
        for j in range(128):
            su, sv = gst[0], gst[1]
            S.dma('sp', su[:, 0:1024], ut_h[j, :, :], writes=[su])
            S.dma('sp', sv[:, 0:1024], v_h[j, :, :], writes=[sv])
            cu, cv2 = utb[j % 2], vtb[j % 2]
            S.op('dve', lambda e, cu=cu: e.tensor_tensor(out=cu[:], in0=su[:, 0:1024], in1=gffnx[:, 0:1024], op=ALU.mult),
                 reads=[su, gffnx], writes=[cu])
            S.op('act', lambda e, cv2=cv2: e.copy(out=cv2[:], in_=sv[:, 0:1024]), reads=[sv], writes=[cv2])
            S.dma('pool', UTb[j, :, :], cu[:], reads=[cu], writes=[('UTb', j)])
            S.dma('pool', Vb[j, :, :], cv2[:], reads=[cv2], writes=[('Vb', j)])
        xg = [tl("xg%d" % i, [128, D]) for i in range(2)]
        gjunk = tl("gjunk", [128, D])
        gss = tl("gss", [128, 1])
        grs = tl("grs", [128, 1])
        ghb = tl("ghb", [128, D], BF16)
        h3Tb = [tl("h3T%d" % i, [128, D], BF16) for i in range(2)]
        qTb = tl("qTb", [128, 2048], BF16)
        v16 = tl("v16", [128, 256])
        i16 = tl("i16", [128, 256], U32)
        i16f = tl("i16f", [128, 256])
        candw = scw
        c16 = tl("c16", [128, 128])
        ci = tl("ci", [128, 128], U32)
        ca_u = tl("ca_u", [128, 128], U32)
        cb_u = tl("cb_u", [128, 128], U32)
        ca_f = tl("ca_f", [128, 128])
        cb_f = tl("cb_f", [128, 128])
        eq = cand
        IG = tl("IG", [128, 384])
        gsum = tl("gsum", [128, 8])
        IGT = tl("IGT", [128, 384])
        NQ = 8
        Lh = tl("Lh", [128, NQ * 128], BF16)
        Rh = tl("Rh", [128, NQ * 128], BF16)
        Gsbb = [tl("Gsb%d" % i, [128, 16384], BF16) for i in range(2)]
        ga = [tl("ga%d" % i, [128, 128]) for i in range(4)]
        Wb = [tl("Wb%d" % i, [128, 128], BF16) for i in range(4)]
        yo = gjunk
        pGT = ptl("pGT", [128, 1024], BF16)
        pqs = ptl("pqs", [128, 512])
        pG = [ptl("pG%d" % i, [128, 512]) for i in range(2)]
        pAbank = [ps("pAb%d" % i, [128, 512]) for i in range(2)]

        class PSlot:
            def __init__(self, bank, off, k):
                self.bank, self.off, self.k = bank, off, k

            def ap(self):
                return self.bank[:, self.off:self.off + 128]

        pA = [PSlot(pAbank[s_ % 2], 0, 'pAslot%d' % (s_ % 2)) for s_ in range(4)]
        py3 = [ptl("py3_%d" % i, [128, 512]) for i in range(2)]

        def prep(i):
            x = xg[i % 2]
            h3T = h3Tb[i % 2]
            Gsb = Gsbb[i % 2]
            S.dma('sp', x[:], X2[i * 128:(i + 1) * 128, :], reads=[('X2', i)], writes=[x])
            S.op('act', lambda e, x=x: e.activation(out=gjunk[:], in_=x[:], func=AF.Square, accum_out=gss[:]),
                 reads=[x], writes=[gjunk, gss])
            S.op('dve', lambda e: e.tensor_scalar(out=grs[:], in0=gss[:], scalar1=1.0 / D, scalar2=EPS,
                                                  op0=ALU.mult, op1=ALU.add), reads=[gss], writes=[grs])
            S.op('act', lambda e: e.sqrt(out=grs[:], in_=grs[:]), reads=[grs], writes=[grs])
            S.op('dve', lambda e: e.reciprocal(out=grs[:], in_=grs[:]), reads=[grs], writes=[grs])
            S.op('dve', lambda e, x=x: e.tensor_scalar(out=ghb[:], in0=x[:], scalar1=grs[:, 0:1], scalar2=None,
                                                       op0=ALU.mult), reads=[x, grs], writes=[ghb])
            for kc in range(8):
                S.op('pe', lambda e, kc=kc: e.transpose(out=pGT[:, kc * 128:(kc + 1) * 128],
                                                        in_=ghb[:, kc * 128:(kc + 1) * 128], identity=ident_b[:]),
                     reads=[ghb, 'ident_b'], writes=[pGT])
            S.op('act', lambda e: e.copy(out=h3T[:], in_=pGT[:]), reads=[pGT], writes=[h3T])
            for cg in range(4):
                for cc in range(4):
                    c = cg * 4 + cc
                    for kc in range(8):
                        S.op('pe', lambda e, kc=kc, c=c, cc=cc: e.matmul(
                            pqs[:, cc * 128:(cc + 1) * 128], lhsT=wpq_b[:, kc * 2048 + c * 128:kc * 2048 + (c + 1) * 128],
                            rhs=h3T[:, kc * 128:(kc + 1) * 128], start=(kc == 0), stop=(kc == 7)),
                            reads=[wpq_b, h3T], writes=[pqs])
                S.op('act', lambda e, cg=cg: e.copy(out=qTb[:, cg * 512:(cg + 1) * 512], in_=pqs[:]), reads=[pqs], writes=[qTb])
            for cg in range(4):
                for cc in range(4):
                    c = cg * 4 + cc
                    S.op('pe', lambda e, c=c, cc=cc: e.matmul(
                        pqs[:, cc * 128:(cc + 1) * 128], lhsT=qTb[:, c * 128:(c + 1) * 128],
                        rhs=keys_b[:, c * 128:(c + 1) * 128], start=True, stop=True), reads=[qTb, keys_b], writes=[pqs])
                S.op('act', lambda e, cg=cg: e.copy(out=sc[:, cg * 512:(cg + 1) * 512], in_=pqs[:]), reads=[pqs], writes=[sc])
            for c in range(16):
                cs = slice(c * 128, (c + 1) * 128)
                S.op('dve', lambda e, c=c, cs=cs: e.max(out=v16[:, c * 16:c * 16 + 8], in_=sc[:, cs]), reads=[sc], writes=[v16])
                S.op('dve', lambda e, c=c, cs=cs: e.max_index(out=i16[:, c * 16:c * 16 + 8], in_max=v16[:, c * 16:c * 16 + 8],
                                                              in_values=sc[:, cs]), reads=[sc, v16], writes=[i16])
                S.op('dve', lambda e, c=c, cs=cs: e.match_replace(out=scw[:, cs], in_to_replace=v16[:, c * 16:c * 16 + 8],
                                                                  in_values=sc[:, cs], imm_value=-1e30),
                     reads=[sc, v16], writes=[scw])
                S.op('dve', lambda e, c=c, cs=cs: e.max(out=v16[:, c * 16 + 8:c * 16 + 16], in_=scw[:, cs]),
                     reads=[scw], writes=[v16])
                S.op('dve', lambda e, c=c, cs=cs: e.max_index(out=i16[:, c * 16 + 8:c * 16 + 16],
                                                              in_max=v16[:, c * 16 + 8:c * 16 + 16], in_values=scw[:, cs]),
                     reads=[scw, v16], writes=[i16])
            S.op('dve', lambda e: e.tensor_copy(out=i16f[:], in_=i16[:]), reads=[i16], writes=[i16f])
            S.op('dve', lambda e: e.tensor_tensor(
                out=cand[:].rearrange("p (h a b) -> p h a b", h=8, a=16),
                in0=cap(v16, 0, [[256, 128], [32, 8], [1, 16], [0, 16]]),
                in1=cap(v16, 16, [[256, 128], [32, 8], [0, 16], [1, 16]]), op=ALU.add), reads=[v16], writes=[cand])
            for h in range(8):
                cs = slice(h * 256, (h + 1) * 256)
                S.op('dve', lambda e, h=h, cs=cs: e.max(out=c16[:, h * 16:h * 16 + 8], in_=cand[:, cs]), reads=[cand], writes=[c16])
                S.op('dve', lambda e, h=h, cs=cs: e.max_index(out=ci[:, h * 16:h * 16 + 8], in_max=c16[:, h * 16:h * 16 + 8],
                                                              in_values=cand[:, cs]), reads=[cand, c16], writes=[ci])
                S.op('dve', lambda e, h=h, cs=cs: e.match_replace(out=candw[:, cs], in_to_replace=c16[:, h * 16:h * 16 + 8],
                                                                  in_values=cand[:, cs], imm_value=-1e30),
                     reads=[cand, c16], writes=[candw])
                S.op('dve', lambda e, h=h, cs=cs: e.max(out=c16[:, h * 16 + 8:h * 16 + 16], in_=candw[:, cs]),
                     reads=[candw], writes=[c16])
                S.op('dve', lambda e, h=h, cs=cs: e.max_index(out=ci[:, h * 16 + 8:h * 16 + 16],
                                                              in_max=c16[:, h * 16 + 8:h * 16 + 16], in_values=candw[:, cs]),
                     reads=[candw, c16], writes=[ci])
            S.op('dve', lambda e: e.tensor_single_scalar(out=ca_u[:], in_=ci[:], scalar=4, op=ALU.logical_shift_right),
                 reads=[ci], writes=[ca_u])
            S.op('dve', lambda e: e.tensor_single_scalar(out=cb_u[:], in_=ci[:], scalar=15, op=ALU.bitwise_and),
                 reads=[ci], writes=[cb_u])
            S.op('dve', lambda e: e.tensor_copy(out=ca_f[:], in_=ca_u[:]), reads=[ca_u], writes=[ca_f])
            S.op('dve', lambda e: e.tensor_copy(out=cb_f[:], in_=cb_u[:]), reads=[cb_u], writes=[cb_f])
            for which, (src_f, off) in enumerate(((ca_f, 0), (cb_f, 16))):
                S.op('dve', lambda e, src_f=src_f: e.tensor_tensor(
                    out=eq[:].rearrange("p (s a) -> p s a", a=16),
                    in0=cap(src_f, 0, [[128, 128], [1, 128], [0, 16]]),
                    in1=cap(iota16, 0, [[16, 128], [0, 128], [1, 16]]), op=ALU.is_equal),
                    reads=[src_f, iota16], writes=[eq])
                S.op('dve', lambda e, off=off: e.tensor_tensor(
                    out=eq[:].rearrange("p (h k a) -> p h k a", h=8, k=16),
                    in0=eq[:].rearrange("p (h k a) -> p h k a", h=8, k=16),
                    in1=cap(i16f, off, [[256, 128], [32, 8], [0, 16], [1, 16]]), op=ALU.mult),
                    reads=[eq, i16f], writes=[eq])
                S.op('dve', lambda e, which=which: e.tensor_reduce(
                    out=IG[:, which * 128:(which + 1) * 128], in_=eq[:].rearrange("p (s a) -> p s a", a=16),
                    axis=AX.X, op=ALU.add), reads=[eq], writes=[IG])
            S.op('dve', lambda e: e.tensor_tensor(
                out=IG[:, 256:384].rearrange("p (h k) -> p h k", k=16), in0=c16[:].rearrange("p (h k) -> p h k", k=16),
                in1=cap(c16, 0, [[128, 128], [16, 8], [0, 16]]), op=ALU.subtract), reads=[c16], writes=[IG])
            mark()
            S.op('act', lambda e: e.activation(out=IG[:, 256:384], in_=IG[:, 256:384], func=AF.Exp), reads=[IG], writes=[IG])
            S.op('dve', lambda e: e.tensor_reduce(out=gsum[:], in_=IG[:, 256:384].rearrange("p (h k) -> p h k", k=16),
                                                  axis=AX.X, op=ALU.add), reads=[IG], writes=[gsum])
            S.op('dve', lambda e: e.reciprocal(out=gsum[:], in_=gsum[:]), reads=[gsum], writes=[gsum])
            S.op('dve', lambda e: e.tensor_tensor(
                out=IG[:, 256:384].rearrange("p (h k) -> p h k", k=16), in0=IG[:, 256:384].rearrange("p (h k) -> p h k", k=16),
                in1=cap(gsum, 0, [[8, 128], [1, 8], [0, 16]]), op=ALU.mult), reads=[IG, gsum], writes=[IG])
            for w3 in range(3):
                S.op('pe', lambda e, w3=w3: e.transpose(out=pqs[:, w3 * 128:(w3 + 1) * 128], in_=IG[:, w3 * 128:(w3 + 1) * 128],
                                                        identity=ident_f[:]), reads=[IG, 'ident_f'], writes=[pqs])
            S.op('act', lambda e: e.copy(out=IGT[:], in_=pqs[:, 0:384]), reads=[pqs], writes=[IGT])
            for hf in range(128 // NQ):
                S.op('dve', lambda e, hf=hf: e.tensor_tensor(
                    out=Lh[:].rearrange("p (t i) -> p t i", i=128),
                    in0=cap(iota128, 0, [[128, 128], [0, NQ], [1, 128]]),
                    in1=cap(IGT, hf * NQ, [[384, 128], [1, NQ], [0, 128]]), op=ALU.is_equal),
                    reads=[iota128, IGT], writes=[Lh])
                S.op('dve', lambda e, hf=hf: e.tensor_tensor(
                    out=Rh[:].rearrange("p (t i) -> p t i", i=128),
                    in0=cap(iota128, 0, [[128, 128], [0, NQ], [1, 128]]),
                    in1=cap(IGT, 128 + hf * NQ, [[384, 128], [1, NQ], [0, 128]]), op=ALU.is_equal),
                    reads=[iota128, IGT], writes=[Rh])
                S.op('dve', lambda e, hf=hf: e.tensor_tensor(
                    out=Rh[:].rearrange("p (t i) -> p t i", i=128),
                    in0=Rh[:].rearrange("p (t i) -> p t i", i=128),
                    in1=cap(IGT, 256 + hf * NQ, [[384, 128], [1, NQ], [0, 128]]), op=ALU.mult),
                    reads=[Rh, IGT], writes=[Rh])
                for t4 in range(NQ // 4):
                    pg = pG[t4 % 2]
                    for tt in range(4):
                        tl_ = t4 * 4 + tt
                        S.op('pe', lambda e, pg=pg, tt=tt, tl_=tl_: e.matmul(
                            pg[:, tt * 128:(tt + 1) * 128], lhsT=Lh[:, tl_ * 128:(tl_ + 1) * 128],
                            rhs=Rh[:, tl_ * 128:(tl_ + 1) * 128], start=True, stop=True), reads=[Lh, Rh], writes=[pg])
                    g0 = (hf * NQ + t4 * 4) * 128
                    if t4 % 2 == 0:
                        S.op('act', lambda e, pg=pg, g0=g0: e.copy(out=Gsb[:, g0:g0 + 512], in_=pg[:]), reads=[pg], writes=[Gsb])
                    else:
                        S.op('dve', lambda e, pg=pg, g0=g0: e.tensor_copy(out=Gsb[:, g0:g0 + 512], in_=pg[:]),
                             reads=[pg], writes=[Gsb])

        cur_rec = [None]

        def mark():
            if cur_rec[0] is not None:
                cur_rec[0].append(None)

        def record(fn, *args):
            rec = []
            cur_rec[0] = rec
            o_op, o_dma = S.op, S.dma
            S.op = lambda *a, **k: rec.append((o_op, a, k))
            S.dma = lambda *a, **k: rec.append((o_dma, a, k))
            try:
                fn(*args)
            finally:
                S.op, S.dma = o_op, o_dma
                cur_rec[0] = None
            return rec

        def dense(i, nxt):
            x = xg[i % 2]
            h3T = h3Tb[i % 2]
            Gsb = Gsbb[i % 2]
            pos = [0]

            nsplit = nxt.index(None) if None in nxt else len(nxt)

            def pump(upto):
                while pos[0] < min(upto, len(nxt)):
                    if nxt[pos[0]] is not None:
                        f, a, k = nxt[pos[0]]
                        f(*a, **k)
                    pos[0] += 1

            def sched(j):
                if j < 64:
                    return ((j + 1) * nsplit + 63) // 64
                if j < 80:
                    return nsplit
                return nsplit + ((j - 79) * (len(nxt) - nsplit) + 39) // 40
            def stage_u(j):
                j4 = j % 4
                j8 = j % 8
                S.dma('sp', utb[j8][:], UTb[j, :, :], reads=[('UTb', j)], writes=[utb[j8]])
                S.dma('sp', vtb[j8][:], Vb[j, :, :], reads=[('Vb', j)], writes=[vtb[j8]])
                for kc in range(8):
                    S.op('pe', lambda e, kc=kc, j4=j4, j8=j8: e.matmul(
                        pA[j4].ap(), lhsT=utb[j8][:, kc * 128:(kc + 1) * 128], rhs=h3T[:, kc * 128:(kc + 1) * 128],
                        start=(kc == 0), stop=(kc == 7)), reads=[utb[j8], h3T], writes=[pA[j4]])
                S.op('act', lambda e, j4=j4: e.activation(out=ga[j4][:], in_=pA[j4].ap(), func=AF.Gelu),
                     reads=[pA[j4]], writes=[ga[j4]])
                S.op('pool', lambda e, j4=j4, j=j: e.tensor_tensor(
                    out=Wb[j4][:], in0=ga[j4][:], in1=cap(Gsb, j, [[16384, 128], [128, 128]]), op=ALU.mult),
                    reads=[ga[j4], Gsb], writes=[Wb[j4]])

            def stage_v(j):
                j4 = j % 4
                j8 = j % 8
                for half in range(2):
                    S.op('pe', lambda e, half=half, j=j, j4=j4, j8=j8: e.matmul(
                        py3[half][:], lhsT=Wb[j4][:], rhs=vtb[j8][:, half * 512:(half + 1) * 512],
                        start=(j == 0), stop=(j == 127)), reads=[Wb[j4], vtb[j8]], writes=[py3[half]])

            stage_u(0)
            stage_u(1)
            for j in range(128):
                if j + 2 < 128:
                    stage_u(j + 2)
                stage_v(j)
                pump(sched(j))
            pump(len(nxt))
            for half in range(2):
                S.op('dve', lambda e, half=half, x=x: e.tensor_tensor(out=x[:, half * 512:(half + 1) * 512],
                                                                     in0=x[:, half * 512:(half + 1) * 512], in1=py3[half][:],
                                                                     op=ALU.add), reads=[x, py3[half]], writes=[x])
            if debug:
                S.dma('pool', X3[i * 128:(i + 1) * 128, :], x[:], reads=[x])
            S.op('act', lambda e, x=x: e.activation(out=gjunk[:], in_=x[:], func=AF.Square, accum_out=gss[:]),
                 reads=[x], writes=[gjunk, gss])
            S.op('dve', lambda e: e.tensor_scalar(out=grs[:], in0=gss[:], scalar1=1.0 / D, scalar2=EPS,
                                                  op0=ALU.mult, op1=ALU.add), reads=[gss], writes=[grs])
            S.op('act', lambda e: e.sqrt(out=grs[:], in_=grs[:]), reads=[grs], writes=[grs])
            S.op('dve', lambda e: e.reciprocal(out=grs[:], in_=grs[:]), reads=[grs], writes=[grs])
            S.op('dve', lambda e, x=x: e.scalar_tensor_tensor(out=yo[:], in0=x[:], scalar=grs[:, 0:1], in1=gfin[:],
                                                              op0=ALU.mult, op1=ALU.mult), reads=[x, grs, gfin], writes=[yo])
            if i < NT:
                S.dma('pool', y_p[i * 128:(i + 1) * 128, :], yo[:], reads=[yo])
            else:
                S.dma('pool', y_s[:, :], yo[0:16, :], reads=[yo])

        prep(0)
        for i in range(NTT):
            nxt = record(prep, i + 1) if i + 1 < NTT else []
            dense(i, nxt)
        es_g.close()
        cur[0] = es
        S.finish()
    return nc


_PROGRAM = None
_DEBUG_HOOK = None


def kernel(x_prompt, x_sample, cache_swa_k, cache_swa_v, state_hgrn, cache_mem_k, cache_mem_v,
           mem_prompt, norm_mix, w_in, lb_logits, beta_a, gnorm_b, w_out, norm_cross, norm_mem,
           w_cq, w_mk, w_mv, w_co, norm_ffn, w_pq, peer_k1, peer_k2, peer_u, peer_v, norm_final):
    global _PROGRAM
    f = lambda a: np.ascontiguousarray(np.asarray(a, dtype=np.float32))
    if _PROGRAM is None:
        _PROGRAM = build_program()
    nc = _PROGRAM

    def col(g):
        return f(np.asarray(g).reshape(-1, 128).T)

    common = {
        "w_in": f(w_in[0]), "w_mk": f(w_mk[0]), "w_mv": f(w_mv[0]),
        "g_mix": col(norm_mix[0]), "g_mem": col(norm_mem[0]),
        "w_out": f(w_out[0]), "w_cq": f(w_cq[0]), "w_co": f(w_co[0]),
        "g_outc": col(np.concatenate([np.asarray(beta_a[0]).reshape(-1), np.asarray(gnorm_b[0]).reshape(-1)])),
        "g_cross": col(norm_cross[0]),
        "w_pq": f(w_pq[0]), "g_ffn": col(norm_ffn[0]),
        "g_ffnx": f(np.repeat(np.asarray(norm_ffn[0]).reshape(8, 128).T[:, :, None], 128, axis=2).reshape(128, 1024)),
        "keysT": f(np.stack([np.asarray(peer_k1[0]), np.asarray(peer_k2[0])], axis=1).reshape(16, 128, 128)
                   .transpose(2, 0, 1).reshape(128, 2048)),
        "ut_h": f(np.asarray(peer_u[0]).reshape(128, 128, 8, 128).transpose(1, 3, 2, 0).reshape(128, 128, 1024)),
        "v_h": f(np.asarray(peer_v[0]).reshape(128, 128, 1024).transpose(1, 0, 2)),
        "g_fin": f(np.broadcast_to(np.asarray(norm_final)[None, :], (128, D))),
        "iota16": f(np.broadcast_to(np.arange(16)[None, :], (128, 16))),
        "iota128": f(np.broadcast_to(np.arange(128)[None, :], (128, 128))),
        "ident": np.eye(128, dtype=np.float32),
        "lbl0": f(np.broadcast_to(np.asarray(lb_logits)[0][None, :], (128, 512))),
        "lbl1": f(np.broadcast_to(np.asarray(lb_logits)[1][None, :], (128, 512))),
        "triU": np.triu(np.ones((128, 128), np.float32)),
        "tri4": np.tile(np.triu(np.ones((128, 64), np.float32)), (1, 8)),
        "rowmask": (np.arange(128) < 4).astype(np.float32).reshape(128, 1),
        "bmask": np.kron(np.eye(8, dtype=np.float32), np.ones((1, 66), np.float32)),
        "maskC": np.tile(np.triu(np.ones((128, 128), np.float32)), (1, 8)),
        "maskP": np.tile(np.tril(np.ones((128, 128), np.float32)), (1, 8)),
    }
    in_maps = []
    for c in range(NCORES):
        m = dict(common)
        m["xp"] = f(x_prompt[c])
        m["xs"] = f(np.asarray(x_sample[4 * c:4 * c + 4]).reshape(16, D))
        m["memp"] = f(mem_prompt[c])
        m["st_h"] = f(state_hgrn[0, 4 * c:4 * c + 4])
        m["cmk"] = f(np.asarray(cache_mem_k[0, 4 * c:4 * c + 4]).reshape(4, 256, 512))
        m["cmv"] = f(np.asarray(cache_mem_v[0, 4 * c:4 * c + 4]).reshape(4, 256, 512))
        m["ck"] = f(np.asarray(cache_swa_k[0, 4 * c:4 * c + 4]).reshape(4, 2048, 512))
        m["cv"] = f(np.asarray(cache_swa_v[0, 4 * c:4 * c + 4]).reshape(4, 2048, 512))
        in_maps.append(m)
    if _DEBUG_HOOK is not None:
        return _DEBUG_HOOK(in_maps)
    res = run_bass_kernel_spmd(nc, in_maps, core_ids=list(range(NCORES)))
    R = res.results
    y_prompt = np.stack([R[c]["y_p"] for c in range(NCORES)]).reshape(8, T, D)
    y_sample = np.concatenate([R[c]["y_s"].reshape(4, 4, D) for c in range(NCORES)], axis=0)
    p_k = np.stack([R[c]["o_pk"].reshape(2048, 8, 64) for c in range(NCORES)])[None]
    p_v = np.stack([R[c]["o_pv"].reshape(2048, 8, 64) for c in range(NCORES)])[None]
    p_h = np.stack([R[c]["o_ph"] for c in range(NCORES)])[None]
    p_mk = np.stack([R[c]["o_mk"].reshape(256, 4, 128) for c in range(NCORES)])[None]
    p_mv = np.stack([R[c]["o_mv"].reshape(256, 4, 128) for c in range(NCORES)])[None]
    s_k = np.concatenate([R[c]["o_sk"].reshape(4, 4, 8, 64) for c in range(NCORES)], axis=0)[None]
    s_v = np.concatenate([R[c]["o_sv"].reshape(4, 4, 8, 64) for c in range(NCORES)], axis=0)[None]
    s_h = np.concatenate([R[c]["o_sh"] for c in range(NCORES)], axis=0)[None]
    outs = (y_prompt, y_sample, p_k, p_v, p_h, p_mk, p_mv, s_k, s_v, s_h)
    return tuple(np.ascontiguousarray(o.astype(np.float32)) for o in outs)
```

```python
import contextlib
import numpy as np
import concourse.bass as bass
import concourse.mybir as mybir
from concourse.bass_utils import run_bass_kernel_spmd

F32 = mybir.dt.float32
BF16 = mybir.dt.bfloat16
U32 = mybir.dt.uint32
AF = mybir.ActivationFunctionType
ALU = mybir.AluOpType
AX = mybir.AxisListType

NCORES = 8
_ATT_LVL = 9
_ATT_MAXB = 10 ** 9
_BRANCHES = (1, 4, 16)
T = 4096
NT = 32
NTT = 33
D = 1024
MIXIN = 3584
EPS = 1e-6


class Sync:
    def __init__(self, nc, es):
        self.nc = nc
        self.eng = {'pe': nc.tensor, 'dve': nc.vector, 'act': nc.scalar, 'pool': nc.gpsimd, 'sp': nc.sync}
        self.sem = {}
        self.cnt = {}
        for e in self.eng:
            self.sem[e] = es.enter_context(nc.semaphore('c_' + e))
            self.cnt[e] = 0
        self.R = 8
        for q in ('sp', 'pool'):
            for r in range(self.R):
                k = ('d', q, r)
                self.sem[k] = es.enter_context(nc.semaphore('d_%s%d' % (q, r)))
                self.cnt[k] = 0
        self.dnext = {'sp': 0, 'pool': 0}
        self.waited = {}
        self.last_w = {}
        self.readers = {}

    def _wait(self, eng, dep):
        k, v = dep
        if k == 'pe' and eng == 'pe':
            return
        if self.waited.get((eng, k), 0) >= v:
            return
        self.eng[eng].wait_ge(self.sem[k], v)
        self.waited[(eng, k)] = v

    def _deps(self, eng, reads, writes):
        deps = {}
        def add(d):
            if d is None:
                return
            if deps.get(d[0], 0) < d[1]:
                deps[d[0]] = d[1]
        for r in reads:
            add(self.last_w.get(r))
        for w in writes:
            add(self.last_w.get(w))
            for d in self.readers.get(w, ()):
                add(d)
        for k, v in deps.items():
            self._wait(eng, (k, v))

    def _record(self, me, reads, writes):
        for r in reads:
            self.readers.setdefault(r, []).append(me)
        for w in writes:
            self.last_w[w] = me
            self.readers[w] = []

    def op(self, eng, inst_fn, reads=(), writes=()):
        reads = [getattr(r, 'k', r) for r in reads]
        writes = [getattr(w, 'k', w) for w in writes]
        self._deps(eng, reads, writes)
        inst = inst_fn(self.eng[eng])
        self.cnt[eng] += 1
        inst.then_inc(self.sem[eng], 1)
        self._record((eng, self.cnt[eng]), reads, writes)

    def dma(self, q, out, in_, reads=(), writes=(), **kw):
        reads = [getattr(r, 'k', r) for r in reads]
        writes = [getattr(w, 'k', w) for w in writes]
        self._deps(q, reads, writes)
        r = self.dnext[q]
        self.dnext[q] = (r + 1) % self.R
        k = ('d', q, r)
        inst = self.eng[q].dma_start(out=out, in_=in_, **kw)
        self.cnt[k] += 16
        inst.then_inc(self.sem[k], 16)
        self._record((k, self.cnt[k]), reads, writes)

    def barrier(self):
        for e in self.eng:
            for k, v in self.cnt.items():
                if v > 0 and k != e:
                    self._wait(e, (k, v))

    def finish(self):
        for k, v in self.cnt.items():
            if v > 0 and k != 'sp':
                self._wait('sp', (k, v))


class Tl:
    def __init__(self, t, k):
        self.t, self.k = t, k

    def __getitem__(self, idx):
        return self.t[idx]


def build_program(debug=False):
    nc = bass.Bass("TRN2", target_bir_lowering=False)

    def din(name, shape, dt=F32):
        return nc.dram_tensor(name, list(shape), dt, kind="ExternalInput").ap()

    def dout(name, shape, dt=F32):
        return nc.dram_tensor(name, list(shape), dt, kind="ExternalOutput").ap()

    def dscr(name, shape, dt=F32):
        return nc.dram_tensor(name, list(shape), dt, kind="ExternalOutput" if debug else "Internal").ap()

    xp = din("xp", [T, D])
    xs = din("xs", [16, D])
    memp = din("memp", [256, D])
    w_in = din("w_in", [D, MIXIN])
    w_mk = din("w_mk", [D, 512])
    w_mv = din("w_mv", [D, 512])
    g_mix = din("g_mix", [128, 8])
    g_mem = din("g_mem", [128, 8])
    ident = din("ident", [128, 128])
    lbl0 = din("lbl0", [128, 512])
    lbl1 = din("lbl1", [128, 512])
    triU_d = din("triU", [128, 128])
    tri4_d = din("tri4", [128, 512])
    rowmask_d = din("rowmask", [128, 1])
    st_h = din("st_h", [4, 4, 128, 128])
    maskC_d = din("maskC", [128, 1024])
    maskP_d = din("maskP", [128, 1024])
    ck = din("ck", [4, 2048, 512])
    cv = din("cv", [4, 2048, 512])
    bmask_d = din("bmask", [8, 528])
    w_out = din("w_out", [D, D])
    g_outc = din("g_outc", [128, 8])
    g_cross = din("g_cross", [128, 8])
    w_cq = din("w_cq", [D, 512])
    w_co = din("w_co", [512, D])
    cmk = din("cmk", [4, 256, 512])
    w_pq = din("w_pq", [D, 2048])
    g_ffn = din("g_ffn", [128, 8])
    g_ffnx = din("g_ffnx", [128, 1024])
    keysT = din("keysT", [128, 2048])
    ut_h = din("ut_h", [128, 128, 1024])
    v_h = din("v_h", [128, 128, 1024])
    g_fin = din("g_fin", [128, D])
    iota16_d = din("iota16", [128, 16])
    iota128_d = din("iota128", [128, 128])
    cmv = din("cmv", [4, 256, 512])
    y_p = dout("y_p", [T, D])
    y_s = dout("y_s", [16, D])
    o_pk = dout("o_pk", [2048, 512])
    o_pv = dout("o_pv", [2048, 512])
    o_ph = dout("o_ph", [4, 128, 128])
    o_mk = dout("o_mk", [256, 512])
    o_mv = dout("o_mv", [256, 512])
    o_sk = dout("o_sk", [16, 512])
    o_sv = dout("o_sv", [16, 512])
    o_sh = dout("o_sh", [4, 4, 128, 128])
    Z = dscr("Z", [NTT * 128, MIXIN])
    OB = dscr("OB", [NTT * 128, 512])
    OA = dscr("OA", [3, NTT * 128, 528])
    X2 = dscr("X2", [NTT * 128, D])
    X3 = dscr("X3", [NTT * 128, D]) if debug else None
    UTb = dscr("UTb", [128, 128, 1024], BF16)
    Vb = dscr("Vb", [128, 128, 1024], BF16)

    with contextlib.ExitStack() as es:
        S = Sync(nc, es)

        cur = [es]

        def sb(name, shape, dt=F32):
            return cur[0].enter_context(nc.sbuf_tensor(name, list(shape), dt))

        def ps(name, shape, dt=F32):
            return cur[0].enter_context(nc.psum_tensor(name, list(shape), dt))

        def tl(name, shape, dt=F32):
            return Tl(sb(name, shape, dt), name)

        def ptl(name, shape, dt=F32):
            return Tl(ps(name, shape, dt), name)

        ident_f = sb("ident_f", [128, 128])
        ident_b = sb("ident_b", [128, 128], BF16)
        gmix = sb("gmix", [128, 8])
        gmem = sb("gmem", [128, 8])
        S.dma('sp', ident_f[:], ident[:, :], writes=['ident_f'])
        S.dma('sp', gmix[:], g_mix[:, :], writes=['gmix'])
        S.dma('sp', gmem[:], g_mem[:, :], writes=['gmem'])
        S.op('dve', lambda e: e.tensor_copy(out=ident_b[:], in_=ident_f[:]), reads=['ident_f'], writes=['ident_b'])

        es_a = contextlib.ExitStack()
        es_a.__enter__()
        cur[0] = es_a
        win_b = sb("win_b", [128, 8 * MIXIN], BF16)
        wmk_b = sb("wmk_b", [128, 8 * 512], BF16)
        wmv_b = sb("wmv_b", [128, 8 * 512], BF16)
        wst = [sb("wst%d" % i, [128, MIXIN]) for i in range(2)]
        for kc in range(8):
            st = wst[kc % 2]
            S.dma('sp', st[:], w_in[kc * 128:(kc + 1) * 128, :], writes=['wst%d' % (kc % 2)])
            S.op('dve' if kc % 2 == 0 else 'pool',
                 lambda e, st=st, kc=kc: e.tensor_scalar(out=win_b[:, kc * MIXIN:(kc + 1) * MIXIN], in0=st[:],
                                                         scalar1=gmix[:, kc:kc + 1], scalar2=None, op0=ALU.mult),
                 reads=['wst%d' % (kc % 2), 'gmix'], writes=['win_b'])
        for kc in range(8):
            st = wst[kc % 2]
            S.dma('sp', st[:, 0:512], w_mk[kc * 128:(kc + 1) * 128, :], writes=['wst%d' % (kc % 2)])
            S.dma('sp', st[:, 512:1024], w_mv[kc * 128:(kc + 1) * 128, :], writes=['wst%d' % (kc % 2)])
            S.op('dve', lambda e, st=st, kc=kc: e.tensor_scalar(out=wmk_b[:, kc * 512:(kc + 1) * 512], in0=st[:, 0:512],
                                                                scalar1=gmem[:, kc:kc + 1], scalar2=None, op0=ALU.mult),
                 reads=['wst%d' % (kc % 2), 'gmem'], writes=['wmk_b'])
            S.op('pool', lambda e, st=st, kc=kc: e.tensor_scalar(out=wmv_b[:, kc * 512:(kc + 1) * 512], in0=st[:, 512:1024],
                                                                 scalar1=gmem[:, kc:kc + 1], scalar2=None, op0=ALU.mult),
                 reads=['wst%d' % (kc % 2), 'gmem'], writes=['wmv_b'])

        xt = [sb("xt%d" % i, [128, D]) for i in range(2)]
        junk = sb("junk", [128, D])
        ss = sb("ss", [128, 1])
        rstd = sb("rstd", [128, 1])
        hb = sb("hb", [128, D], BF16)
        hT = [sb("hT%d" % i, [128, D], BF16) for i in range(2)]
        pT = ps("pT", [128, D], BF16)
        pz = [ps("pz%d" % i, [128, 512]) for i in range(2)]
        zt = [sb("zt%d" % i, [128, MIXIN]) for i in range(2)]

        def front(xtile, xkey, hT_t, hkey):
            S.op('act', lambda e: e.activation(out=junk[:], in_=xtile[:], func=AF.Square, accum_out=ss[:]),
                 reads=[xkey], writes=['junk', 'ss'])
            S.op('dve', lambda e: e.tensor_scalar(out=rstd[:], in0=ss[:], scalar1=1.0 / D, scalar2=EPS,
                                                  op0=ALU.mult, op1=ALU.add), reads=['ss'], writes=['rstd'])
            S.op('act', lambda e: e.sqrt(out=rstd[:], in_=rstd[:]), reads=['rstd'], writes=['rstd'])
            S.op('dve', lambda e: e.reciprocal(out=rstd[:], in_=rstd[:]), reads=['rstd'], writes=['rstd'])
            S.op('dve', lambda e: e.tensor_scalar(out=hb[:], in0=xtile[:], scalar1=rstd[:, 0:1], scalar2=None,
                                                  op0=ALU.mult), reads=[xkey, 'rstd'], writes=['hb'])
            for kc in range(8):
                S.op('pe', lambda e, kc=kc: e.transpose(out=pT[:, kc * 128:(kc + 1) * 128],
                                                        in_=hb[:, kc * 128:(kc + 1) * 128], identity=ident_b[:]),
                     reads=['hb', 'ident_b'], writes=['pT'])
            S.op('act', lambda e: e.copy(out=hT_t[:], in_=pT[:]), reads=['pT'], writes=[hkey])

        for i in range(NTT):
            b = i % 2
            xkey = 'xt%d' % b
            if i < NT:
                S.dma('sp', xt[b][:], xp[i * 128:(i + 1) * 128, :], writes=[xkey])
            else:
                S.op('dve', lambda e, b=b: e.memset(xt[b][:], 0.0), writes=[xkey])
                S.dma('sp', xt[b][0:16, :], xs[:, :], writes=[xkey])
            front(xt[b], xkey, hT[b], 'hT%d' % b)
            for g in range(7):
                pzg = pz[g % 2]
                for kc in range(8):
                    S.op('pe', lambda e, kc=kc, g=g, pzg=pzg, b=b: e.matmul(
                        pzg[:], lhsT=hT[b][:, kc * 128:(kc + 1) * 128],
                        rhs=win_b[:, kc * MIXIN + g * 512: kc * MIXIN + (g + 1) * 512],
                        start=(kc == 0), stop=(kc == 7)),
                        reads=['hT%d' % b, 'win_b'], writes=['pz%d' % (g % 2)])
                if g % 2 == 0:
                    S.op('act', lambda e, g=g, pzg=pzg, b=b: e.copy(out=zt[b][:, g * 512:(g + 1) * 512], in_=pzg[:]),
                         reads=['pz%d' % (g % 2)], writes=['zt%d' % b])
                else:
                    S.op('dve', lambda e, g=g, pzg=pzg, b=b: e.tensor_copy(out=zt[b][:, g * 512:(g + 1) * 512], in_=pzg[:]),
                         reads=['pz%d' % (g % 2)], writes=['zt%d' % b])
            S.dma('pool', Z[i * 128:(i + 1) * 128, :], zt[b][:], reads=['zt%d' % b], writes=[('Z', i)])
            if 16 <= i < NT:
                r0 = (i - 16) * 128
                S.dma('pool', o_pk[r0:r0 + 128, :], zt[b][:, 512:1024], reads=['zt%d' % b])
                S.dma('pool', o_pv[r0:r0 + 128, :], zt[b][:, 1024:1536], reads=['zt%d' % b])
            if i == NT:
                S.dma('pool', o_sk[:, :], zt[b][0:16, 512:1024], reads=['zt%d' % b])
                S.dma('pool', o_sv[:, :], zt[b][0:16, 1024:1536], reads=['zt%d' % b])

        for i in range(2):
            b = i % 2
            xkey = 'xt%d' % b
            S.dma('sp', xt[b][:], memp[i * 128:(i + 1) * 128, :], writes=[xkey])
            front(xt[b], xkey, hT[b], 'hT%d' % b)
            for j, wb in enumerate((wmk_b, wmv_b)):
                pzg = pz[j]
                for kc in range(8):
                    S.op('pe', lambda e, kc=kc, wb=wb, pzg=pzg, b=b: e.matmul(
                        pzg[:], lhsT=hT[b][:, kc * 128:(kc + 1) * 128], rhs=wb[:, kc * 512:(kc + 1) * 512],
                        start=(kc == 0), stop=(kc == 7)),
                        reads=['hT%d' % b, 'wmk_b', 'wmv_b'], writes=['pz%d' % j])
                S.op('act' if j == 0 else 'dve',
                     (lambda e, j=j, pzg=pzg, b=b: e.copy(out=zt[b][:, j * 512:(j + 1) * 512], in_=pzg[:])) if j == 0 else
                     (lambda e, j=j, pzg=pzg, b=b: e.tensor_copy(out=zt[b][:, j * 512:(j + 1) * 512], in_=pzg[:])),
                     reads=['pz%d' % j], writes=['zt%d' % b])
            S.dma('pool', o_mk[i * 128:(i + 1) * 128, :], zt[b][:, 0:512], reads=['zt%d' % b])
            S.dma('pool', o_mv[i * 128:(i + 1) * 128, :], zt[b][:, 512:1024], reads=['zt%d' % b])

        es_a.close()
        cur[0] = es
        S.barrier()
        es_d = contextlib.ExitStack()
        es_d.__enter__()
        cur[0] = es_d
        triU = tl("triU_s", [128, 128])
        tri4 = tl("tri4_s", [128, 512])
        rowmask = tl("rowmask_s", [128, 1])
        lb_t = tl("lb_t", [128, 512])
        oml_t = tl("oml_t", [128, 512])
        l1_t = tl("l1_t", [128, 512])
        S.dma('sp', triU[:], triU_d[:, :], writes=[triU])
        S.dma('sp', tri4[:], tri4_d[:, :], writes=[tri4])
        S.dma('sp', rowmask[:], rowmask_d[:, :], writes=[rowmask])
        S.dma('sp', lb_t[:], lbl0[:, :], writes=[lb_t])
        S.dma('sp', l1_t[:], lbl1[:, :], writes=[l1_t])
        S.op('dve', lambda e: e.tensor_tensor(out=lb_t[:], in0=lb_t[:], in1=l1_t[:], op=ALU.subtract),
             reads=[lb_t, l1_t], writes=[lb_t])
        S.op('act', lambda e: e.activation(out=lb_t[:], in_=lb_t[:], func=AF.Sigmoid), reads=[lb_t], writes=[lb_t])
        S.op('dve', lambda e: e.tensor_scalar(out=oml_t[:], in0=lb_t[:], scalar1=-1.0, scalar2=1.0,
                                              op0=ALU.mult, op1=ALU.add), reads=[lb_t], writes=[oml_t])
        hz = [tl("hz%d" % i, [128, 2048]) for i in range(2)]
        sig = tl("sig", [128, 512])
        logf = tl("logf", [128, 512])
        kk = tl("kk", [128, 512])
        qs = tl("qs", [128, 512])
        gs = tl("gs", [128, 512])
        ib_b = tl("ib_b", [128, 512], BF16)
        ec = tl("ec", [128, 512])
        emc = tl("emc", [128, 512])
        ecl = tl("ecl", [128, 4])
        qd_b = tl("qd_b", [128, 512], BF16)
        kd_b = tl("kd_b", [128, 512], BF16)
        qkT = tl("qkT", [128, 1024], BF16)
        AT_b = tl("AT_b", [128, 512], BF16)
        Sst = tl("Sst", [128, 512])
        S_b = tl("S_b", [128, 512], BF16)
        ssq = tl("ssq", [128, 4])
        rb = tl("rb", [128, 4])
        hjunk = tl("hjunk", [128, 128])
        obn = [tl("obn%d" % i, [128, 512]) for i in range(2)]
        pc = ptl("pc", [128, 512])
        pcT = ptl("pcT", [128, 512])
        pTq = ptl("pTq", [128, 1024], BF16)
        pA = ptl("pA", [128, 512])
        po = ptl("po", [128, 512])
        pdS = ptl("pdS", [128, 512])

        P = 64

        def hs(h):
            return slice(h * 128, (h + 1) * 128)

        def hp(h):
            return slice(h * P, (h + 1) * P)

        def hgrn_chunk(ci, row0, nrows, masked):
            z = hz[ci % 2]
            ob = obn[ci % 2]
            if nrows < P:
                S.op('dve', lambda e: e.memset(z[:], 0.0), writes=[z])
            S.dma('sp', z[0:nrows, :], Z[row0:row0 + nrows, 1536:3584], reads=[('Z', row0 // 128)], writes=[z])
            S.op('act', lambda e: e.activation(out=sig[0:P, :], in_=z[0:P, 512:1024], func=AF.Sigmoid), reads=[z], writes=[sig])
            S.op('dve', lambda e: e.tensor_tensor(out=sig[0:P, :], in0=sig[0:P, :], in1=oml_t[0:P, :], op=ALU.mult),
                 reads=[sig, oml_t], writes=[sig])
            S.op('dve', lambda e: e.tensor_tensor(out=sig[0:P, :], in0=sig[0:P, :], in1=lb_t[0:P, :], op=ALU.add),
                 reads=[sig, lb_t], writes=[sig])
            S.op('act', lambda e: e.activation(out=logf[0:P, :], in_=sig[0:P, :], func=AF.Ln), reads=[sig], writes=[logf])
            S.op('dve', lambda e: e.tensor_scalar(out=kk[0:P, :], in0=sig[0:P, :], scalar1=-1.0, scalar2=1.0,
                                                  op0=ALU.mult, op1=ALU.add), reads=[sig], writes=[kk])
            if masked:
                S.op('dve', lambda e: e.tensor_scalar(out=logf[0:P, :], in0=logf[0:P, :], scalar1=rowmask[0:P, 0:1],
                                                      scalar2=None, op0=ALU.mult), reads=[logf, rowmask], writes=[logf])
                S.op('dve', lambda e: e.tensor_scalar(out=kk[0:P, :], in0=kk[0:P, :], scalar1=rowmask[0:P, 0:1],
                                                      scalar2=None, op0=ALU.mult), reads=[kk, rowmask], writes=[kk])
            S.op('act', lambda e: e.activation(out=qs[0:P, :], in_=z[0:P, 0:512], func=AF.Silu), reads=[z], writes=[qs])
            S.op('act', lambda e: e.activation(out=gs[0:P, :], in_=z[0:P, 1536:2048], func=AF.Silu), reads=[z], writes=[gs])
            S.op('dve', lambda e: e.tensor_copy(out=ib_b[0:P, :], in_=z[0:P, 1024:1536]), reads=[z], writes=[ib_b])
            S.op('pe', lambda e: e.matmul(pc[0:P, :], lhsT=triU[0:P, 0:P], rhs=logf[0:P, :], start=True, stop=True),
                 reads=[triU, logf], writes=[pc])
            for h in range(4):
                S.op('pe', lambda e, h=h: e.matmul(pcT[:, hp(h)], lhsT=logf[0:P, hs(h)], rhs=triU[0:P, 0:P],
                                                   start=True, stop=True), reads=[triU, logf], writes=[pcT])
            S.op('act', lambda e: e.activation(out=ec[0:P, :], in_=pc[0:P, :], func=AF.Exp), reads=[pc], writes=[ec])
            S.op('act', lambda e: e.activation(out=emc[0:P, :], in_=pc[0:P, :], func=AF.Exp, scale=-1.0),
                 reads=[pc], writes=[emc])
            S.op('act', lambda e: e.activation(out=ecl[:], in_=pcT[:, P - 1:4 * P:P], func=AF.Exp), reads=[pcT], writes=[ecl])
            S.op('dve', lambda e: e.tensor_tensor(out=qd_b[0:P, :], in0=qs[0:P, :], in1=ec[0:P, :], op=ALU.mult),
                 reads=[qs, ec], writes=[qd_b])
            S.op('dve', lambda e: e.tensor_tensor(out=kd_b[0:P, :], in0=kk[0:P, :], in1=emc[0:P, :], op=ALU.mult),
                 reads=[kk, emc], writes=[kd_b])
            for h in range(4):
                S.op('pe', lambda e, h=h: e.transpose(out=pTq[:, hp(h)], in_=qd_b[0:P, hs(h)], identity=ident_b[0:P, 0:P]),
                     reads=[qd_b, 'ident_b'], writes=[pTq])
                S.op('pe', lambda e, h=h: e.transpose(out=pTq[:, hp(4 + h)], in_=kd_b[0:P, hs(h)],
                                                      identity=ident_b[0:P, 0:P]), reads=[kd_b, 'ident_b'], writes=[pTq])
            S.op('act', lambda e: e.copy(out=qkT[:, 0:8 * P], in_=pTq[:, 0:8 * P]), reads=[pTq], writes=[qkT])
            for h in range(4):
                S.op('pe', lambda e, h=h: e.matmul(pA[0:P, hp(h)], lhsT=qkT[:, hp(4 + h)],
                                                   rhs=qkT[:, hp(h)], start=True, stop=True), reads=[qkT], writes=[pA])
            S.op('dve', lambda e: e.tensor_tensor(out=AT_b[0:P, 0:4 * P], in0=pA[0:P, 0:4 * P], in1=tri4[0:P, 0:4 * P],
                                                  op=ALU.mult), reads=[pA, tri4], writes=[AT_b])
            for h in range(4):
                S.op('pe', lambda e, h=h: e.matmul(po[0:P, hs(h)], lhsT=AT_b[0:P, hp(h)], rhs=ib_b[0:P, hs(h)],
                                                   start=True, stop=False), reads=[AT_b, ib_b], writes=[po])
                S.op('pe', lambda e, h=h: e.matmul(po[0:P, hs(h)], lhsT=qkT[:, hp(h)], rhs=S_b[:, hs(h)],
                                                   start=False, stop=True), reads=[qkT, S_b], writes=[po])
                S.op('pe', lambda e, h=h: e.matmul(pdS[:, hs(h)], lhsT=kd_b[0:P, hs(h)], rhs=ib_b[0:P, hs(h)],
                                                   start=True, stop=True), reads=[kd_b, ib_b], writes=[pdS])
            S.op('dve', lambda e: e.tensor_tensor(out=Sst[:], in0=Sst[:], in1=pdS[:], op=ALU.add),
                 reads=[Sst, pdS], writes=[Sst])
            for h in range(4):
                S.op('dve', lambda e, h=h: e.tensor_scalar(out=Sst[:, hs(h)], in0=Sst[:, hs(h)], scalar1=ecl[:, h:h + 1],
                                                           scalar2=None, op0=ALU.mult), reads=[Sst, ecl], writes=[Sst])
            S.op('dve', lambda e: e.tensor_copy(out=S_b[:], in_=Sst[:]), reads=[Sst], writes=[S_b])
            for h in range(4):
                S.op('act', lambda e, h=h: e.activation(out=hjunk[0:P, :], in_=po[0:P, hs(h)], func=AF.Square,
                                                        accum_out=ssq[0:P, h:h + 1]), reads=[po], writes=[hjunk, ssq])
            S.op('dve', lambda e: e.tensor_scalar(out=rb[0:P, :], in0=ssq[0:P, :], scalar1=1.0 / 128, scalar2=EPS,
                                                  op0=ALU.mult, op1=ALU.add), reads=[ssq], writes=[rb])
            S.op('act', lambda e: e.sqrt(out=rb[0:P, :], in_=rb[0:P, :]), reads=[rb], writes=[rb])
            S.op('dve', lambda e: e.reciprocal(out=rb[0:P, :], in_=rb[0:P, :]), reads=[rb], writes=[rb])
            for h in range(4):
                S.op('dve', lambda e, h=h: e.tensor_scalar(out=ob[0:P, hs(h)], in0=po[0:P, hs(h)], scalar1=rb[0:P, h:h + 1],
                                                           scalar2=None, op0=ALU.mult), reads=[po, rb], writes=[ob])
            S.op('dve', lambda e: e.tensor_tensor(out=ob[0:P, :], in0=ob[0:P, :], in1=gs[0:P, :], op=ALU.mult),
                 reads=[ob, gs], writes=[ob])
            S.dma('pool', OB[row0:row0 + nrows, :], ob[0:nrows, :], reads=[ob], writes=[('OB', row0 // 128)])

        S.op('dve', lambda e: e.memset(Sst[:], 0.0), writes=[Sst])
        S.op('dve', lambda e: e.memset(S_b[:], 0.0), writes=[S_b])
        for i in range(T // P):
            hgrn_chunk(i, i * P, P, False)
        for h in range(4):
            S.dma('pool', o_ph[h, :, :], Sst[:, hs(h)], reads=[Sst])
        for bb in range(4):
            for h in range(4):
                S.dma('sp', Sst[:, hs(h)], st_h[bb, h, :, :], writes=[Sst])
            S.op('dve', lambda e: e.tensor_copy(out=S_b[:], in_=Sst[:]), reads=[Sst], writes=[S_b])
            hgrn_chunk(bb, T + 4 * bb, 4, True)
            for h in range(4):
                S.dma('pool', o_sh[bb, h, :, :], Sst[:, hs(h)], reads=[Sst])
        es_d.close()
        cur[0] = es
        S.barrier()
        es_c = contextlib.ExitStack()
        es_c.__enter__()
        cur[0] = es_c
        mstage = tl("mstage", [128, 1024])
        maskC = tl("maskC_s", [128, 1024], BF16)
        maskP = tl("maskP_s", [128, 1024], BF16)
        S.dma('sp', mstage[:], maskC_d[:, :], writes=[mstage])
        S.op('dve', lambda e: e.tensor_copy(out=maskC[:], in_=mstage[:]), reads=[mstage], writes=[maskC])
        S.dma('sp', mstage[:], maskP_d[:, :], writes=[mstage])
        S.op('dve', lambda e: e.tensor_copy(out=maskP[:], in_=mstage[:]), reads=[mstage], writes=[maskP])
        az = [tl("az%d" % i, [128, 1536]) for i in range(2)]
        qk_b = tl("qk_b", [128, 1024], BF16)
        vaug = [tl("vaug%d" % i, [128, 8 * 66], BF16) for i in range(2)]
        qT = tl("qT", [64, 1024], BF16)
        kT = [tl("kT%d" % i, [64, 1024], BF16) for i in range(2)]
        PTc = tl("PTc", [128, 1024], BF16)
        PTp = tl("PTp", [128, 1024], BF16)
        osb = [tl("osb%d" % i, [128, 528]) for i in range(2)]
        pTt = ptl("pTt", [128, 1024], BF16)
        pTk = ptl("pTk", [128, 1024], BF16)
        psC = [ptl("psC%d" % i, [128, 512]) for i in range(2)]
        psP = [ptl("psP%d" % i, [128, 512]) for i in range(2)]
        pO = [ptl("pO%d" % i, [128, 512]) for i in range(2)]
        for i in range(2):
            S.op('dve', lambda e, i=i: e.memset(vaug[i][:], 1.0), writes=[vaug[i]])
        blk = 0
        for br, dil in enumerate(_BRANCHES):
            nb = T // dil // 128
            if dil == 1:
                Zv = Z[0:T, :].rearrange("(o m) c -> o m c", o=1)
                OAv = OA[br, 0:T, :].rearrange("(o m) c -> o m c", o=1)
            else:
                Zv = Z[0:T, :].rearrange("(m d) c -> d m c", d=dil)
                OAv = OA[br, 0:T, :].rearrange("(m d) c -> d m c", d=dil)
            for r in range(dil):
                for b in range(nb):
                    cb = blk % 2
                    pb = 1 - cb
                    a = az[cb]
                    S.dma('sp', a[:], Zv[r, b * 128:(b + 1) * 128, 0:1536], reads=[('Z', i) for i in range(NT)] if blk == 0 else [],
                          writes=[a])
                    S.op('dve', lambda e, a=a: e.tensor_copy(out=qk_b[:], in_=a[:, 0:1024]), reads=[a], writes=[qk_b])
                    S.op('act', lambda e, a=a, cb=cb: e.copy(
                        out=vaug[cb][:].rearrange("p (h e) -> p h e", e=66)[:, :, 0:64],
                        in_=a[:, 1024:1536].rearrange("p (h e) -> p h e", e=64)), reads=[a], writes=[vaug[cb]])
                    if blk >= _ATT_MAXB or _ATT_LVL < 2:
                        blk += 1
                        continue
                    for h in range(8):
                        S.op('pe', lambda e, h=h: e.transpose(out=pTt[0:64, h * 128:(h + 1) * 128],
                                                              in_=qk_b[:, h * 64:(h + 1) * 64], identity=ident_b[:]),
                             reads=[qk_b, 'ident_b'], writes=[pTt])
                        S.op('pe', lambda e, h=h: e.transpose(out=pTk[0:64, h * 128:(h + 1) * 128],
                                                              in_=qk_b[:, 512 + h * 64:512 + (h + 1) * 64],
                                                              identity=ident_b[:]),
                             reads=[qk_b, 'ident_b'], writes=[pTk])
                    S.op('act', lambda e: e.copy(out=qT[0:64, :], in_=pTt[0:64, :]), reads=[pTt], writes=[qT])
                    S.op('act', lambda e, cb=cb: e.copy(out=kT[cb][0:64, :], in_=pTk[0:64, :]), reads=[pTk], writes=[kT[cb]])
                    if _ATT_LVL < 3:
                        blk += 1
                        continue
                    for h in range(8):
                        p, j = h // 2, h % 2
                        S.op('pe', lambda e, h=h, p=p, j=j, cb=cb: e.matmul(
                            psC[h // 4][:, (h % 4) * 128:(h % 4 + 1) * 128], lhsT=kT[cb][0:64, h * 128:(h + 1) * 128],
                            rhs=qT[0:64, h * 128:(h + 1) * 128], start=True, stop=True),
                            reads=[kT[cb], qT], writes=[psC[h // 4]])
                    if b > 0:
                        for h in range(8):
                            p, j = h // 2, h % 2
                            S.op('pe', lambda e, h=h, p=p, j=j, pb=pb: e.matmul(
                                psP[h // 4][:, (h % 4) * 128:(h % 4 + 1) * 128], lhsT=kT[pb][0:64, h * 128:(h + 1) * 128],
                                rhs=qT[0:64, h * 128:(h + 1) * 128], start=True, stop=True),
                                reads=[kT[pb], qT], writes=[psP[h // 4]])
                    if _ATT_LVL < 4:
                        blk += 1
                        continue
                    for hh in range(2):
                        S.op('act', lambda e, hh=hh: e.activation(out=PTc[:, hh * 512:(hh + 1) * 512], in_=psC[hh][:],
                                                                  func=AF.Exp, scale=0.125), reads=[psC[hh]], writes=[PTc])
                    S.op('dve', lambda e: e.tensor_tensor(out=PTc[:], in0=PTc[:], in1=maskC[:], op=ALU.mult),
                         reads=[PTc, maskC], writes=[PTc])
                    if b > 0:
                        for hh in range(2):
                            S.op('act', lambda e, hh=hh: e.activation(out=PTp[:, hh * 512:(hh + 1) * 512], in_=psP[hh][:],
                                                                      func=AF.Exp, scale=0.125), reads=[psP[hh]], writes=[PTp])
                        S.op('pool', lambda e: e.tensor_tensor(out=PTp[:], in0=PTp[:], in1=maskP[:], op=ALU.mult),
                             reads=[PTp, maskP], writes=[PTp])
                    if _ATT_LVL < 5:
                        blk += 1
                        continue
                    for h in range(8):
                        po_t = pO[h // 4]
                        osl = slice((h % 4) * 66, (h % 4 + 1) * 66)
                        S.op('pe', lambda e, h=h, po_t=po_t, osl=osl, cb=cb: e.matmul(
                            po_t[:, osl], lhsT=PTc[:, h * 128:(h + 1) * 128], rhs=vaug[cb][:, h * 66:(h + 1) * 66],
                            start=True, stop=(b == 0)), reads=[PTc, vaug[cb]], writes=[po_t])
                        if b > 0:
                            S.op('pe', lambda e, h=h, po_t=po_t, osl=osl, pb=pb: e.matmul(
                                po_t[:, osl], lhsT=PTp[:, h * 128:(h + 1) * 128], rhs=vaug[pb][:, h * 66:(h + 1) * 66],
                                start=False, stop=True), reads=[PTp, vaug[pb]], writes=[po_t])
                    o = osb[cb]
                    S.op('act', lambda e, o=o: e.copy(out=o[:, 0:264], in_=pO[0][:, 0:264]), reads=[pO[0]], writes=[o])
                    S.op('dve', lambda e, o=o: e.tensor_copy(out=o[:, 264:528], in_=pO[1][:, 0:264]), reads=[pO[1]], writes=[o])
                    S.dma('pool', OAv[r, b * 128:(b + 1) * 128, :], o[:], reads=[o], writes=[('OA', br)])
                    blk += 1
        es_c.close()
        cur[0] = es
        S.barrier()
        es_e = contextlib.ExitStack()
        es_e.__enter__()
        cur[0] = es_e
        bmask = tl("bmask_s", [8, 528])
        S.dma('sp', bmask[:], bmask_d[:, :], writes=[bmask])
        skv = [tl("skv%d" % i, [128, 1024]) for i in range(2)]
        sqb = tl("sqb", [128, 512])
        sprod = tl("sprod", [128, 512])
        ssc = tl("ssc", [128, 8])
        sp_b = tl("sp_b", [128, 8], BF16)
        svaug = [tl("svaug%d" % i, [128, 528], BF16) for i in range(2)]
        sq8 = tl("sq8", [8, 64])
        sk8 = tl("sk8", [8, 64])
        sv8 = tl("sv8", [8, 64])
        sj8 = tl("sj8", [8, 64])
        ss8 = tl("ss8", [8, 1])
        sdiag = tl("sdiag", [8, 528])
        sres = [tl("sres%d" % i, [8, 66]) for i in range(2)]
        pSa = ptl("pSa", [8, 512])
        pSb = ptl("pSb", [8, 512])
        for i in range(2):
            S.op('dve', lambda e, i=i: e.memset(svaug[i][:], 1.0), writes=[svaug[i]])
        it = 0
        for bb in range(4):
            for t in range(4):
                row = T + 4 * bb + t
                S.dma('sp', sqb[:], bass.AP(tensor=Z.tensor, offset=row * MIXIN, ap=[[0, 128], [1, 512]]),
                      reads=[('Z', NT)], writes=[sqb])
                S.dma('sp', sq8[:], Z[row, 0:512].rearrange("(h e) -> h e", e=64), writes=[sq8])
                S.dma('sp', sk8[:], Z[row, 512:1024].rearrange("(h e) -> h e", e=64), writes=[sk8])
                S.dma('sp', sv8[:], Z[row, 1024:1536].rearrange("(h e) -> h e", e=64), writes=[sv8])
                for br, dil in enumerate((1, 4, 16)):
                    kv = skv[it % 2]
                    va = svaug[it % 2]
                    it += 1
                    start = 2048 + t - dil * 128
                    if dil == 1:
                        ncache = 128 - t
                        S.dma('sp', kv[0:ncache, 0:512], ck[bb, start:2048, :], writes=[kv])
                        S.dma('sp', kv[0:ncache, 512:1024], cv[bb, start:2048, :], writes=[kv])
                        if t > 0:
                            S.dma('sp', kv[ncache:128, :], Z[T + 4 * bb:T + 4 * bb + t, 512:1536], reads=[('Z', NT)], writes=[kv])
                    else:
                        S.dma('sp', kv[:, 0:512], bass.AP(tensor=ck.tensor, offset=(bb * 2048 + start) * 512,
                                                          ap=[[dil * 512, 128], [1, 512]]), writes=[kv])
                        S.dma('sp', kv[:, 512:1024], bass.AP(tensor=cv.tensor, offset=(bb * 2048 + start) * 512,
                                                             ap=[[dil * 512, 128], [1, 512]]), writes=[kv])
                    S.op('act', lambda e, kv=kv, va=va: e.copy(
                        out=va[:].rearrange("p (h e) -> p h e", e=66)[:, :, 0:64],
                        in_=kv[:, 512:1024].rearrange("p (h e) -> p h e", e=64)), reads=[kv], writes=[va])
                    S.op('dve', lambda e, kv=kv: e.tensor_tensor(out=sprod[:], in0=kv[:, 0:512], in1=sqb[:], op=ALU.mult),
                         reads=[kv, sqb], writes=[sprod])
                    S.op('dve', lambda e: e.tensor_reduce(out=ssc[:], in_=sprod[:].rearrange("p (h e) -> p h e", e=64),
                                                          axis=AX.X, op=ALU.add), reads=[sprod], writes=[ssc])
                    S.op('act', lambda e: e.activation(out=sp_b[:], in_=ssc[:], func=AF.Exp, scale=0.125),
                         reads=[ssc], writes=[sp_b])
                    S.op('pe', lambda e, va=va, br=br: e.matmul(pSa[:, 0:264], lhsT=sp_b[:], rhs=va[:, 0:264],
                                                                start=(br == 0), stop=(br == 2)),
                         reads=[sp_b, va], writes=[pSa])
                    S.op('pe', lambda e, va=va, br=br: e.matmul(pSb[:, 0:264], lhsT=sp_b[:], rhs=va[:, 264:528],
                                                                start=(br == 0), stop=(br == 2)),
                         reads=[sp_b, va], writes=[pSb])
                res = sres[(4 * bb + t) % 2]
                S.op('dve', lambda e: e.tensor_tensor(out=sdiag[:, 0:264], in0=pSa[:, 0:264], in1=bmask[:, 0:264], op=ALU.mult),
                     reads=[pSa, bmask], writes=[sdiag])
                S.op('dve', lambda e: e.tensor_tensor(out=sdiag[:, 264:528], in0=pSb[:, 0:264], in1=bmask[:, 264:528], op=ALU.mult),
                     reads=[pSb, bmask], writes=[sdiag])
                S.op('dve', lambda e, res=res: e.tensor_reduce(out=res[:], in_=sdiag[:].rearrange("p (h e) -> p e h", e=66),
                                                               axis=AX.X, op=ALU.add), reads=[sdiag], writes=[res])
                S.op('dve', lambda e: e.tensor_tensor(out=sj8[:], in0=sq8[:], in1=sk8[:], op=ALU.mult),
                     reads=[sq8, sk8], writes=[sj8])
                S.op('dve', lambda e: e.tensor_reduce(out=ss8[:], in_=sj8[:], axis=AX.X, op=ALU.add), reads=[sj8], writes=[ss8])
                S.op('act', lambda e: e.activation(out=ss8[:], in_=ss8[:], func=AF.Exp, scale=0.125), reads=[ss8], writes=[ss8])
                S.op('dve', lambda e: e.tensor_scalar(out=ss8[:], in0=ss8[:], scalar1=3.0, scalar2=None, op0=ALU.mult),
                     reads=[ss8], writes=[ss8])
                S.op('dve', lambda e, res=res: e.scalar_tensor_tensor(out=res[:, 0:64], in0=sv8[:], scalar=ss8[:, 0:1],
                                                                      in1=res[:, 0:64], op0=ALU.mult, op1=ALU.add),
                     reads=[sv8, ss8, res], writes=[res])
                S.op('dve', lambda e, res=res: e.tensor_tensor(out=res[:, 64:65], in0=res[:, 64:65], in1=ss8[:], op=ALU.add),
                     reads=[res, ss8], writes=[res])
                S.dma('pool', OA[0, row, :].rearrange("(h e) -> h e", e=66), res[:], reads=[res], writes=[('OA', 0)])
        es_e.close()
        cur[0] = es
        S.barrier()
        es_f = contextlib.ExitStack()
        es_f.__enter__()
        cur[0] = es_f
        goutc = tl("goutc", [128, 8])
        gcross = tl("gcross", [128, 8])
        S.dma('sp', goutc[:], g_outc[:, :], writes=[goutc])
        S.dma('sp', gcross[:], g_cross[:, :], writes=[gcross])
        wout_b = tl("wout_b", [128, 8 * 1024], BF16)
        wcq_b = tl("wcq_b", [128, 8 * 512], BF16)
        wco_b = tl("wco_b", [128, 4 * 1024], BF16)
        fst = [tl("fst%d" % i, [128, 1024]) for i in range(2)]
        for kc in range(8):
            st = fst[kc % 2]
            S.dma('sp', st[:], w_out[kc * 128:(kc + 1) * 128, :], writes=[st])
            S.op('dve', lambda e, st=st, kc=kc: e.tensor_scalar(out=wout_b[:, kc * 1024:(kc + 1) * 1024], in0=st[:],
                                                                scalar1=goutc[:, kc:kc + 1], scalar2=None, op0=ALU.mult),
                 reads=[st, goutc], writes=[wout_b])
        for kc in range(8):
            st = fst[kc % 2]
            S.dma('sp', st[:, 0:512], w_cq[kc * 128:(kc + 1) * 128, :], writes=[st])
            S.op('dve', lambda e, st=st, kc=kc: e.tensor_scalar(out=wcq_b[:, kc * 512:(kc + 1) * 512], in0=st[:, 0:512],
                                                                scalar1=gcross[:, kc:kc + 1], scalar2=None, op0=ALU.mult),
                 reads=[st, gcross], writes=[wcq_b])
        for kc in range(4):
            st = fst[kc % 2]
            S.dma('sp', st[:], w_co[kc * 128:(kc + 1) * 128, :], writes=[st])
            S.op('dve', lambda e, st=st, kc=kc: e.tensor_copy(out=wco_b[:, kc * 1024:(kc + 1) * 1024], in_=st[:]),
                 reads=[st], writes=[wco_b])
        ones_b = tl("ones_b", [128, 128], BF16)
        S.op('dve', lambda e: e.memset(ones_b[:], 1.0), writes=[ones_b])
        mkT = [tl("mkT%d" % i, [128, 4 * 256], BF16) for i in range(5)]
        mvb = [tl("mvb%d" % i, [128, 2 * 512], BF16) for i in range(5)]
        mkb = tl("mkb", [128, 512], BF16)
        pFT = ptl("pFT", [128, 1024], BF16)
        pTm = pFT
        for g in range(5):
            src_k = o_mk if g == 0 else cmk[g - 1]
            src_v = o_mv if g == 0 else cmv[g - 1]
            for mb in range(2):
                st = fst[mb]
                S.dma('sp', st[:, 0:512], src_k[mb * 128:(mb + 1) * 128, :], writes=[st])
                S.dma('sp', st[:, 512:1024], src_v[mb * 128:(mb + 1) * 128, :], writes=[st])
                S.op('dve', lambda e, st=st: e.tensor_copy(out=mkb[:], in_=st[:, 0:512]), reads=[st], writes=[mkb])
                S.op('dve', lambda e, st=st, g=g, mb=mb: e.tensor_copy(out=mvb[g][:, mb * 512:(mb + 1) * 512], in_=st[:, 512:1024]),
                     reads=[st], writes=[mvb[g]])
                for hh in range(4):
                    S.op('pe', lambda e, hh=hh: e.transpose(out=pTm[:, hh * 128:(hh + 1) * 128], in_=mkb[:, hh * 128:(hh + 1) * 128],
                                                            identity=ident_b[:]), reads=[mkb, 'ident_b'], writes=[pTm])
                S.op('act', lambda e, g=g, mb=mb: e.copy(
                    out=mkT[g][:].rearrange("p (h m) -> p h m", m=256)[:, :, mb * 128:(mb + 1) * 128],
                    in_=pTm[:, 0:512].rearrange("p (h m) -> p h m", m=128)), reads=[pTm], writes=[mkT[g]])
        xa = [tl("xa%d" % i, [128, D]) for i in range(2)]
        oat = [tl("oat%d" % i, [128, 528]) for i in range(3)]
        obt = tl("obt", [128, 512])
        rden = tl("rden", [128, 8])
        oan = tl("oan", [128, 512])
        cat = tl("cat", [128, D], BF16)
        fjunk = tl("fjunk", [128, D])
        fss = tl("fss", [128, 1])
        frs = tl("frs", [128, 1])
        fhb = tl("fhb", [128, D], BF16)
        catT = tl("catT", [128, D], BF16)
        h2T = tl("h2T", [128, D], BF16)
        qcT = tl("qcT", [128, 512], BF16)
        PTx = tl("PTx", [128, 1024], BF16)
        rdx = tl("rdx", [128, 512])
        oTx = tl("oTx", [128, 512], BF16)
        py = [ptl("py%d" % i, [128, 512]) for i in range(2)]
        pq = ptl("pq", [128, 512])
        psx = [ptl("psx%d" % i, [128, 512]) for i in range(2)]
        pox = ptl("pox", [128, 512])
        pdx = ptl("pdx", [128, 512])

        def normT(xtile, outT):
            S.op('act', lambda e: e.activation(out=fjunk[:], in_=xtile[:], func=AF.Square, accum_out=fss[:]),
                 reads=[xtile], writes=[fjunk, fss])
            S.op('dve', lambda e: e.tensor_scalar(out=frs[:], in0=fss[:], scalar1=1.0 / D, scalar2=EPS,
                                                  op0=ALU.mult, op1=ALU.add), reads=[fss], writes=[frs])
            S.op('act', lambda e: e.sqrt(out=frs[:], in_=frs[:]), reads=[frs], writes=[frs])
            S.op('dve', lambda e: e.reciprocal(out=frs[:], in_=frs[:]), reads=[frs], writes=[frs])
            S.op('dve', lambda e: e.tensor_scalar(out=fhb[:], in0=xtile[:], scalar1=frs[:, 0:1], scalar2=None,
                                                  op0=ALU.mult), reads=[xtile, frs], writes=[fhb])
            for kc in range(8):
                S.op('pe', lambda e, kc=kc: e.transpose(out=pFT[:, kc * 128:(kc + 1) * 128],
                                                        in_=fhb[:, kc * 128:(kc + 1) * 128], identity=ident_b[:]),
                     reads=[fhb, 'ident_b'], writes=[pFT])
            S.op('act', lambda e: e.copy(out=outT[:], in_=pFT[:]), reads=[pFT], writes=[outT])

        for i in range(NTT):
            x = xa[i % 2]
            nbr = 3 if i < NT else 1
            if i < NT:
                S.dma('sp', x[:], xp[i * 128:(i + 1) * 128, :], writes=[x])
            else:
                S.op('dve', lambda e, x=x: e.memset(x[:], 0.0), writes=[x])
                S.dma('sp', x[0:16, :], xs[:, :], writes=[x])
                for t3 in (oat[0], obt):
                    S.op('dve', lambda e, t3=t3: e.memset(t3[:], 1.0), writes=[t3])
            nr = 128 if i < NT else 16
            for br in range(nbr):
                S.dma('sp', oat[br][0:nr, :], OA[br, i * 128:i * 128 + nr, :], reads=[('OA', br)], writes=[oat[br]])
            S.dma('sp', obt[0:nr, :], OB[i * 128:i * 128 + nr, :], reads=[('OB', j) for j in range(NTT)], writes=[obt])
            for br in range(1, nbr):
                S.op('dve', lambda e, br=br: e.tensor_tensor(out=oat[0][:], in0=oat[0][:], in1=oat[br][:], op=ALU.add),
                     reads=[oat[0], oat[br]], writes=[oat[0]])
            S.op('dve', lambda e: e.reciprocal(out=rden[:], in_=oat[0][:].rearrange("p (h e) -> p h e", e=66)[:, :, 64]),
                 reads=[oat[0]], writes=[rden])
            for h in range(8):
                S.op('dve', lambda e, h=h: e.tensor_scalar(out=oan[:, h * 64:(h + 1) * 64], in0=oat[0][:, h * 66:h * 66 + 64],
                                                           scalar1=rden[:, h:h + 1], scalar2=None, op0=ALU.mult),
                     reads=[oat[0], rden], writes=[oan])
            S.op('act', lambda e: e.activation(out=fjunk[:, 0:512], in_=oan[:], func=AF.Square, accum_out=fss[:]),
                 reads=[oan], writes=[fjunk, fss])
            S.op('dve', lambda e: e.tensor_scalar(out=frs[:], in0=fss[:], scalar1=1.0 / 512, scalar2=EPS,
                                                  op0=ALU.mult, op1=ALU.add), reads=[fss], writes=[frs])
            S.op('act', lambda e: e.sqrt(out=frs[:], in_=frs[:]), reads=[frs], writes=[frs])
            S.op('dve', lambda e: e.reciprocal(out=frs[:], in_=frs[:]), reads=[frs], writes=[frs])
            S.op('dve', lambda e: e.tensor_scalar(out=cat[:, 0:512], in0=oan[:], scalar1=frs[:, 0:1], scalar2=None,
                                                  op0=ALU.mult), reads=[oan, frs], writes=[cat])
            S.op('dve', lambda e: e.tensor_copy(out=cat[:, 512:1024], in_=obt[:]), reads=[obt], writes=[cat])
            for kc in range(8):
                S.op('pe', lambda e, kc=kc: e.transpose(out=pFT[:, kc * 128:(kc + 1) * 128],
                                                        in_=cat[:, kc * 128:(kc + 1) * 128], identity=ident_b[:]),
                     reads=[cat, 'ident_b'], writes=[pFT])
            S.op('act', lambda e: e.copy(out=catT[:], in_=pFT[:]), reads=[pFT], writes=[catT])
            for half in range(2):
                for kc in range(8):
                    S.op('pe', lambda e, kc=kc, half=half: e.matmul(
                        py[half][:], lhsT=catT[:, kc * 128:(kc + 1) * 128],
                        rhs=wout_b[:, kc * 1024 + half * 512:kc * 1024 + (half + 1) * 512],
                        start=(kc == 0), stop=(kc == 7)), reads=[catT, wout_b], writes=[py[half]])
                S.op('dve', lambda e, half=half, x=x: e.tensor_tensor(out=x[:, half * 512:(half + 1) * 512],
                                                                     in0=x[:, half * 512:(half + 1) * 512], in1=py[half][:],
                                                                     op=ALU.add), reads=[x, py[half]], writes=[x])
            normT(x, h2T)
            for hh in range(4):
                for kc in range(8):
                    S.op('pe', lambda e, kc=kc, hh=hh: e.matmul(
                        pq[:, hh * 128:(hh + 1) * 128], lhsT=wcq_b[:, kc * 512 + hh * 128:kc * 512 + (hh + 1) * 128],
                        rhs=h2T[:, kc * 128:(kc + 1) * 128], start=(kc == 0), stop=(kc == 7)),
                        reads=[wcq_b, h2T], writes=[pq])
            S.op('act', lambda e: e.copy(out=qcT[:], in_=pq[:]), reads=[pq], writes=[qcT])
            groups = [(0, 128, 0)] if i < NT else [(4 * bb, 4, 1 + bb) for bb in range(4)]
            for (c0, ncol, g) in groups:
                for hh in range(4):
                    for mb in range(2):
                        S.op('pe', lambda e, hh=hh, mb=mb, c0=c0, ncol=ncol, g=g: e.matmul(
                            psx[mb][:, hh * 128 + c0:hh * 128 + c0 + ncol],
                            lhsT=mkT[g][:, hh * 256 + mb * 128:hh * 256 + (mb + 1) * 128],
                            rhs=qcT[:, hh * 128 + c0:hh * 128 + c0 + ncol], start=True, stop=True),
                            reads=[mkT[g], qcT], writes=[psx[mb]])
            for mb in range(2):
                S.op('act', lambda e, mb=mb: e.activation(out=PTx[:, mb * 512:(mb + 1) * 512], in_=psx[mb][:], func=AF.Exp,
                                                          scale=float(128 ** -0.5)), reads=[psx[mb]], writes=[PTx])
            for (c0, ncol, g) in groups:
                for hh in range(4):
                    for mb in range(2):
                        S.op('pe', lambda e, hh=hh, mb=mb, c0=c0, ncol=ncol, g=g: e.matmul(
                            pox[:, hh * 128 + c0:hh * 128 + c0 + ncol],
                            lhsT=mvb[g][:, mb * 512 + hh * 128:mb * 512 + (hh + 1) * 128],
                            rhs=PTx[:, mb * 512 + hh * 128 + c0:mb * 512 + hh * 128 + c0 + ncol],
                            start=(mb == 0), stop=(mb == 1)), reads=[mvb[g], PTx], writes=[pox])
                        S.op('pe', lambda e, hh=hh, mb=mb, c0=c0, ncol=ncol: e.matmul(
                            pdx[:, hh * 128 + c0:hh * 128 + c0 + ncol], lhsT=ones_b[:],
                            rhs=PTx[:, mb * 512 + hh * 128 + c0:mb * 512 + hh * 128 + c0 + ncol],
                            start=(mb == 0), stop=(mb == 1)), reads=[ones_b, PTx], writes=[pdx])
            S.op('dve', lambda e: e.reciprocal(out=rdx[:], in_=pdx[:]), reads=[pdx], writes=[rdx])
            S.op('dve', lambda e: e.tensor_tensor(out=oTx[:], in0=pox[:], in1=rdx[:], op=ALU.mult),
                 reads=[pox, rdx], writes=[oTx])
            for half in range(2):
                for hh in range(4):
                    S.op('pe', lambda e, hh=hh, half=half: e.matmul(
                        py[half][:], lhsT=oTx[:, hh * 128:(hh + 1) * 128],
                        rhs=wco_b[:, hh * 1024 + half * 512:hh * 1024 + (half + 1) * 512],
                        start=(hh == 0), stop=(hh == 3)), reads=[oTx, wco_b], writes=[py[half]])
                S.op('dve', lambda e, half=half, x=x: e.tensor_tensor(out=x[:, half * 512:(half + 1) * 512],
                                                                     in0=x[:, half * 512:(half + 1) * 512], in1=py[half][:],
                                                                     op=ALU.add), reads=[x, py[half]], writes=[x])
            S.dma('pool', X2[i * 128:(i + 1) * 128, :], x[:], reads=[x], writes=[('X2', i)])
        es_f.close()
        cur[0] = es
        S.barrier()
        es_g = contextlib.ExitStack()
        es_g.__enter__()
        cur[0] = es_g

        def cap(tile, off, dims):
            return bass.AP(tensor=tile.t, offset=off, ap=dims)

        gffn = tl("gffn", [128, 8])
        gfin = tl("gfin", [128, D])
        iota16 = tl("iota16_s", [128, 16])
        iota128 = tl("iota128_s", [128, 128])
        S.dma('sp', gffn[:], g_ffn[:, :], writes=[gffn])
        S.dma('sp', gfin[:], g_fin[:, :], writes=[gfin])
        S.dma('sp', iota16[:], iota16_d[:, :], writes=[iota16])
        S.dma('sp', iota128[:], iota128_d[:, :], writes=[iota128])
        sc = tl("sc", [128, 2048])
        scw = tl("scw", [128, 2048])
        cand = tl("cand", [128, 2048])
        gffnx = cand
        wpq_b = tl("wpq_b", [128, 8 * 2048], BF16)
        keys_b = tl("keys_b", [128, 2048], BF16)
        gst = [sc, scw]
        S.dma('sp', gffnx[:, 0:1024], g_ffnx[:, :], writes=[gffnx])
        for kc in range(8):
            for hf in range(2):
                st = gst[hf]
                S.dma('sp', st[:, 0:1024], w_pq[kc * 128:(kc + 1) * 128, hf * 1024:(hf + 1) * 1024], writes=[st])
                S.op('dve', lambda e, st=st, kc=kc, hf=hf: e.tensor_scalar(
                    out=wpq_b[:, kc * 2048 + hf * 1024:kc * 2048 + (hf + 1) * 1024], in0=st[:, 0:1024],
                    scalar1=gffn[:, kc:kc + 1], scalar2=None, op0=ALU.mult), reads=[st, gffn], writes=[wpq_b])
        for hf in range(2):
            S.dma('sp', gst[hf][:, 0:1024], keysT[:, hf * 1024:(hf + 1) * 1024], writes=[gst[hf]])
            S.op('dve', lambda e, hf=hf: e.tensor_copy(out=keys_b[:, hf * 1024:(hf + 1) * 1024], in_=gst[hf][:, 0:1024]),
                 reads=[gst[hf]], writes=[keys_b])
        utb = [tl("utb%d" % i, [128, 1024], BF16) for i in range(8)]
        vtb = [tl("vtb%d" % i, [128, 1024], BF16) for i in range(8)]
        for j in range(128):
            su, sv = gst[0], gst[1]
            S.dma('sp', su[:, 0:1024], ut_h[j, :, :], writes=[su])
            S.dma('sp', sv[:, 0:1024], v_h[j, :, :], writes=[sv])
            cu, cv2 = utb[j % 2], vtb[j % 2]
            S.op('dve', lambda e, cu=cu: e.tensor_tensor(out=cu[:], in0=su[:, 0:1024], in1=gffnx[:, 0:1024], op=ALU.mult),
                 reads=[su, gffnx], writes=[cu])
            S.op('act', lambda e, cv2=cv2: e.copy(out=cv2[:], in_=sv[:, 0:1024]), reads=[sv], writes=[cv2])
            S.dma('pool', UTb[j, :, :], cu[:], reads=[cu], writes=[('UTb', j)])
            S.dma('pool', Vb[j, :, :], cv2[:], reads=[cv2], writes=[('Vb', j)])
        xg = [tl("xg%d" % i, [128, D]) for i in range(2)]
        gjunk = tl("gjunk", [128, D])
        gss = tl("gss", [128, 1])
        grs = tl("grs", [128, 1])
        ghb = tl("ghb", [128, D], BF16)
        h3Tb = [tl("h3T%d" % i, [128, D], BF16) for i in range(2)]
        qTb = tl("qTb", [128, 2048], BF16)
        v16 = tl("v16", [128, 256])
        i16 = tl("i16", [128, 256], U32)
        i16f = tl("i16f", [128, 256])
        candw = scw
        c16 = tl("c16", [128, 128])
        ci = tl("ci", [128, 128], U32)
        ca_u = tl("ca_u", [128, 128], U32)
        cb_u = tl("cb_u", [128, 128], U32)
        ca_f = tl("ca_f", [128, 128])
        cb_f = tl("cb_f", [128, 128])
        eq = cand
        IG = tl("IG", [128, 384])
        gsum = tl("gsum", [128, 8])
        IGT = tl("IGT", [128, 384])
        NQ = 8
        Lhb = [tl("Lh%d" % i, [128, NQ * 128], BF16) for i in range(2)]
        Rhb = [tl("Rh%d" % i, [128, NQ * 128], BF16) for i in range(2)]
        Gsbb = [tl("Gsb%d" % i, [128, 16384], BF16) for i in range(2)]
        ga = [tl("ga%d" % i, [128, 128]) for i in range(4)]
        Wb = [tl("Wb%d" % i, [128, 128], BF16) for i in range(4)]
        yo = gjunk
        pGT = ptl("pGT", [128, 1024], BF16)
        pqs = ptl("pqs", [128, 512])
        pG = [ptl("pG%d" % i, [128, 512]) for i in range(2)]
        pAbank = [ps("pAb%d" % i, [128, 512]) for i in range(2)]

        class PSlot:
            def __init__(self, bank, off, k):
                self.bank, self.off, self.k = bank, off, k

            def ap(self):
                return self.bank[:, self.off:self.off + 128]

        pA = [PSlot(pAbank[s_ % 2], 0, 'pAslot%d' % (s_ % 2)) for s_ in range(4)]
        py3 = [ptl("py3_%d" % i, [128, 512]) for i in range(2)]

        def prep(i):
            x = xg[i % 2]
            h3T = h3Tb[i % 2]
            Gsb = Gsbb[i % 2]
            S.dma('sp', x[:], X2[i * 128:(i + 1) * 128, :], reads=[('X2', i)], writes=[x])
            S.op('act', lambda e, x=x: e.activation(out=gjunk[:], in_=x[:], func=AF.Square, accum_out=gss[:]),
                 reads=[x], writes=[gjunk, gss])
            S.op('dve', lambda e: e.tensor_scalar(out=grs[:], in0=gss[:], scalar1=1.0 / D, scalar2=EPS,
                                                  op0=ALU.mult, op1=ALU.add), reads=[gss], writes=[grs])
            S.op('act', lambda e: e.sqrt(out=grs[:], in_=grs[:]), reads=[grs], writes=[grs])
            S.op('dve', lambda e: e.reciprocal(out=grs[:], in_=grs[:]), reads=[grs], writes=[grs])
            S.op('dve', lambda e, x=x: e.tensor_scalar(out=ghb[:], in0=x[:], scalar1=grs[:, 0:1], scalar2=None,
                                                       op0=ALU.mult), reads=[x, grs], writes=[ghb])
            for kc in range(8):
                S.op('pe', lambda e, kc=kc: e.transpose(out=pGT[:, kc * 128:(kc + 1) * 128],
                                                        in_=ghb[:, kc * 128:(kc + 1) * 128], identity=ident_b[:]),
                     reads=[ghb, 'ident_b'], writes=[pGT])
            S.op('act', lambda e: e.copy(out=h3T[:], in_=pGT[:]), reads=[pGT], writes=[h3T])
            for cg in range(4):
                for cc in range(4):
                    c = cg * 4 + cc
                    for kc in range(8):
                        S.op('pe', lambda e, kc=kc, c=c, cc=cc: e.matmul(
                            pqs[:, cc * 128:(cc + 1) * 128], lhsT=wpq_b[:, kc * 2048 + c * 128:kc * 2048 + (c + 1) * 128],
                            rhs=h3T[:, kc * 128:(kc + 1) * 128], start=(kc == 0), stop=(kc == 7)),
                            reads=[wpq_b, h3T], writes=[pqs])
                S.op('act', lambda e, cg=cg: e.copy(out=qTb[:, cg * 512:(cg + 1) * 512], in_=pqs[:]), reads=[pqs], writes=[qTb])
            for cg in range(4):
                for cc in range(4):
                    c = cg * 4 + cc
                    S.op('pe', lambda e, c=c, cc=cc: e.matmul(
                        pqs[:, cc * 128:(cc + 1) * 128], lhsT=qTb[:, c * 128:(c + 1) * 128],
                        rhs=keys_b[:, c * 128:(c + 1) * 128], start=True, stop=True), reads=[qTb, keys_b], writes=[pqs])
                S.op('act', lambda e, cg=cg: e.copy(out=sc[:, cg * 512:(cg + 1) * 512], in_=pqs[:]), reads=[pqs], writes=[sc])
            for c in range(16):
                cs = slice(c * 128, (c + 1) * 128)
                S.op('dve', lambda e, c=c, cs=cs: e.max(out=v16[:, c * 16:c * 16 + 8], in_=sc[:, cs]), reads=[sc], writes=[v16])
                S.op('dve', lambda e, c=c, cs=cs: e.max_index(out=i16[:, c * 16:c * 16 + 8], in_max=v16[:, c * 16:c * 16 + 8],
                                                              in_values=sc[:, cs]), reads=[sc, v16], writes=[i16])
                S.op('dve', lambda e, c=c, cs=cs: e.match_replace(out=scw[:, cs], in_to_replace=v16[:, c * 16:c * 16 + 8],
                                                                  in_values=sc[:, cs], imm_value=-1e30),
                     reads=[sc, v16], writes=[scw])
                S.op('dve', lambda e, c=c, cs=cs: e.max(out=v16[:, c * 16 + 8:c * 16 + 16], in_=scw[:, cs]),
                     reads=[scw], writes=[v16])
                S.op('dve', lambda e, c=c, cs=cs: e.max_index(out=i16[:, c * 16 + 8:c * 16 + 16],
                                                              in_max=v16[:, c * 16 + 8:c * 16 + 16], in_values=scw[:, cs]),
                     reads=[scw, v16], writes=[i16])
            S.op('dve', lambda e: e.tensor_copy(out=i16f[:], in_=i16[:]), reads=[i16], writes=[i16f])
            S.op('dve', lambda e: e.tensor_tensor(
                out=cand[:].rearrange("p (h a b) -> p h a b", h=8, a=16),
                in0=cap(v16, 0, [[256, 128], [32, 8], [1, 16], [0, 16]]),
                in1=cap(v16, 16, [[256, 128], [32, 8], [0, 16], [1, 16]]), op=ALU.add), reads=[v16], writes=[cand])
            for h in range(8):
                cs = slice(h * 256, (h + 1) * 256)
                S.op('dve', lambda e, h=h, cs=cs: e.max(out=c16[:, h * 16:h * 16 + 8], in_=cand[:, cs]), reads=[cand], writes=[c16])
                S.op('dve', lambda e, h=h, cs=cs: e.max_index(out=ci[:, h * 16:h * 16 + 8], in_max=c16[:, h * 16:h * 16 + 8],
                                                              in_values=cand[:, cs]), reads=[cand, c16], writes=[ci])
                S.op('dve', lambda e, h=h, cs=cs: e.match_replace(out=candw[:, cs], in_to_replace=c16[:, h * 16:h * 16 + 8],
                                                                  in_values=cand[:, cs], imm_value=-1e30),
                     reads=[cand, c16], writes=[candw])
                S.op('dve', lambda e, h=h, cs=cs: e.max(out=c16[:, h * 16 + 8:h * 16 + 16], in_=candw[:, cs]),
                     reads=[candw], writes=[c16])
                S.op('dve', lambda e, h=h, cs=cs: e.max_index(out=ci[:, h * 16 + 8:h * 16 + 16],
                                                              in_max=c16[:, h * 16 + 8:h * 16 + 16], in_values=candw[:, cs]),
                     reads=[candw, c16], writes=[ci])
            S.op('dve', lambda e: e.tensor_single_scalar(out=ca_u[:], in_=ci[:], scalar=4, op=ALU.logical_shift_right),
                 reads=[ci], writes=[ca_u])
            S.op('dve', lambda e: e.tensor_single_scalar(out=cb_u[:], in_=ci[:], scalar=15, op=ALU.bitwise_and),
                 reads=[ci], writes=[cb_u])
            S.op('dve', lambda e: e.tensor_copy(out=ca_f[:], in_=ca_u[:]), reads=[ca_u], writes=[ca_f])
            S.op('dve', lambda e: e.tensor_copy(out=cb_f[:], in_=cb_u[:]), reads=[cb_u], writes=[cb_f])
            for which, (src_f, off) in enumerate(((ca_f, 0), (cb_f, 16))):
                S.op('dve', lambda e, src_f=src_f: e.tensor_tensor(
                    out=eq[:].rearrange("p (s a) -> p s a", a=16),
                    in0=cap(src_f, 0, [[128, 128], [1, 128], [0, 16]]),
                    in1=cap(iota16, 0, [[16, 128], [0, 128], [1, 16]]), op=ALU.is_equal),
                    reads=[src_f, iota16], writes=[eq])
                S.op('dve', lambda e, off=off: e.tensor_tensor(
                    out=eq[:].rearrange("p (h k a) -> p h k a", h=8, k=16),
                    in0=eq[:].rearrange("p (h k a) -> p h k a", h=8, k=16),
                    in1=cap(i16f, off, [[256, 128], [32, 8], [0, 16], [1, 16]]), op=ALU.mult),
                    reads=[eq, i16f], writes=[eq])
                S.op('dve', lambda e, which=which: e.tensor_reduce(
                    out=IG[:, which * 128:(which + 1) * 128], in_=eq[:].rearrange("p (s a) -> p s a", a=16),
                    axis=AX.X, op=ALU.add), reads=[eq], writes=[IG])
            S.op('dve', lambda e: e.tensor_tensor(
                out=IG[:, 256:384].rearrange("p (h k) -> p h k", k=16), in0=c16[:].rearrange("p (h k) -> p h k", k=16),
                in1=cap(c16, 0, [[128, 128], [16, 8], [0, 16]]), op=ALU.subtract), reads=[c16], writes=[IG])
            mark()
            S.op('act', lambda e: e.activation(out=IG[:, 256:384], in_=IG[:, 256:384], func=AF.Exp), reads=[IG], writes=[IG])
            S.op('dve', lambda e: e.tensor_reduce(out=gsum[:], in_=IG[:, 256:384].rearrange("p (h k) -> p h k", k=16),
                                                  axis=AX.X, op=ALU.add), reads=[IG], writes=[gsum])
            S.op('dve', lambda e: e.reciprocal(out=gsum[:], in_=gsum[:]), reads=[gsum], writes=[gsum])
            S.op('dve', lambda e: e.tensor_tensor(
                out=IG[:, 256:384].rearrange("p (h k) -> p h k", k=16), in0=IG[:, 256:384].rearrange("p (h k) -> p h k", k=16),
                in1=cap(gsum, 0, [[8, 128], [1, 8], [0, 16]]), op=ALU.mult), reads=[IG, gsum], writes=[IG])
            for w3 in range(3):
                S.op('pe', lambda e, w3=w3: e.transpose(out=pqs[:, w3 * 128:(w3 + 1) * 128], in_=IG[:, w3 * 128:(w3 + 1) * 128],
                                                        identity=ident_f[:]), reads=[IG, 'ident_f'], writes=[pqs])
            S.op('act', lambda e: e.copy(out=IGT[:], in_=pqs[:, 0:384]), reads=[pqs], writes=[IGT])
            def build(hf):
                Lh, Rh = Lhb[hf % 2], Rhb[hf % 2]
                S.op('dve', lambda e, hf=hf: e.tensor_tensor(
                    out=Lh[:].rearrange("p (t i) -> p t i", i=128),
                    in0=cap(iota128, 0, [[128, 128], [0, NQ], [1, 128]]),
                    in1=cap(IGT, hf * NQ, [[384, 128], [1, NQ], [0, 128]]), op=ALU.is_equal),
                    reads=[iota128, IGT], writes=[Lh])
                S.op('dve', lambda e, hf=hf: e.tensor_tensor(
                    out=Rh[:].rearrange("p (t i) -> p t i", i=128),
                    in0=cap(iota128, 0, [[128, 128], [0, NQ], [1, 128]]),
                    in1=cap(IGT, 128 + hf * NQ, [[384, 128], [1, NQ], [0, 128]]), op=ALU.is_equal),
                    reads=[iota128, IGT], writes=[Rh])
                S.op('dve', lambda e, hf=hf: e.tensor_tensor(
                    out=Rh[:].rearrange("p (t i) -> p t i", i=128),
                    in0=Rh[:].rearrange("p (t i) -> p t i", i=128),
                    in1=cap(IGT, 256 + hf * NQ, [[384, 128], [1, NQ], [0, 128]]), op=ALU.mult),
                    reads=[Rh, IGT], writes=[Rh])

            def gmm(hf):
                Lh, Rh = Lhb[hf % 2], Rhb[hf % 2]
                for t4 in range(NQ // 4):
                    pg = pG[t4 % 2]
                    for tt in range(4):
                        tl_ = t4 * 4 + tt
                        S.op('pe', lambda e, pg=pg, tt=tt, tl_=tl_: e.matmul(
                            pg[:, tt * 128:(tt + 1) * 128], lhsT=Lh[:, tl_ * 128:(tl_ + 1) * 128],
                            rhs=Rh[:, tl_ * 128:(tl_ + 1) * 128], start=True, stop=True), reads=[Lh, Rh], writes=[pg])
                    g0 = (hf * NQ + t4 * 4) * 128
                    if t4 % 2 == 0:
                        S.op('act', lambda e, pg=pg, g0=g0: e.copy(out=Gsb[:, g0:g0 + 512], in_=pg[:]), reads=[pg], writes=[Gsb])
                    else:
                        S.op('dve', lambda e, pg=pg, g0=g0: e.tensor_copy(out=Gsb[:, g0:g0 + 512], in_=pg[:]),
                             reads=[pg], writes=[Gsb])

            ng = 128 // NQ
            build(0)
            for hf in range(ng):
                if hf + 1 < ng:
                    build(hf + 1)
                gmm(hf)

        cur_rec = [None]

        def mark():
            if cur_rec[0] is not None:
                cur_rec[0].append(None)

        def record(fn, *args):
            rec = []
            cur_rec[0] = rec
            o_op, o_dma = S.op, S.dma
            S.op = lambda *a, **k: rec.append((o_op, a, k))
            S.dma = lambda *a, **k: rec.append((o_dma, a, k))
            try:
                fn(*args)
            finally:
                S.op, S.dma = o_op, o_dma
                cur_rec[0] = None
            return rec

        def dense(i, nxt):
            x = xg[i % 2]
            h3T = h3Tb[i % 2]
            Gsb = Gsbb[i % 2]
            pos = [0]

            nsplit = nxt.index(None) if None in nxt else len(nxt)

            def pump(upto):
                while pos[0] < min(upto, len(nxt)):
                    if nxt[pos[0]] is not None:
                        f, a, k = nxt[pos[0]]
                        f(*a, **k)
                    pos[0] += 1

            def sched(j):
                if j < 64:
                    return ((j + 1) * nsplit + 63) // 64
                if j < 80:
                    return nsplit
                return nsplit + ((j - 79) * (len(nxt) - nsplit) + 39) // 40
            def stage_u(j):
                j4 = j % 4
                j8 = j % 8
                S.dma('sp', utb[j8][:], UTb[j, :, :], reads=[('UTb', j)], writes=[utb[j8]])
                S.dma('sp', vtb[j8][:], Vb[j, :, :], reads=[('Vb', j)], writes=[vtb[j8]])
                for kc in range(8):
                    S.op('pe', lambda e, kc=kc, j4=j4, j8=j8: e.matmul(
                        pA[j4].ap(), lhsT=utb[j8][:, kc * 128:(kc + 1) * 128], rhs=h3T[:, kc * 128:(kc + 1) * 128],
                        start=(kc == 0), stop=(kc == 7)), reads=[utb[j8], h3T], writes=[pA[j4]])
                S.op('act', lambda e, j4=j4: e.activation(out=ga[j4][:], in_=pA[j4].ap(), func=AF.Gelu),
                     reads=[pA[j4]], writes=[ga[j4]])
                S.op('pool', lambda e, j4=j4, j=j: e.tensor_tensor(
                    out=Wb[j4][:], in0=ga[j4][:], in1=cap(Gsb, j, [[16384, 128], [128, 128]]), op=ALU.mult),
                    reads=[ga[j4], Gsb], writes=[Wb[j4]])

            def stage_v(j):
                j4 = j % 4
                j8 = j % 8
                for half in range(2):
                    S.op('pe', lambda e, half=half, j=j, j4=j4, j8=j8: e.matmul(
                        py3[half][:], lhsT=Wb[j4][:], rhs=vtb[j8][:, half * 512:(half + 1) * 512],
                        start=(j == 0), stop=(j == 127)), reads=[Wb[j4], vtb[j8]], writes=[py3[half]])

            stage_u(0)
            stage_u(1)
            for j in range(128):
                if j + 2 < 128:
                    stage_u(j + 2)
                stage_v(j)
                pump(sched(j))
            pump(len(nxt))
            for half in range(2):
                S.op('dve', lambda e, half=half, x=x: e.tensor_tensor(out=x[:, half * 512:(half + 1) * 512],
                                                                     in0=x[:, half * 512:(half + 1) * 512], in1=py3[half][:],
                                                                     op=ALU.add), reads=[x, py3[half]], writes=[x])
            if debug:
                S.dma('pool', X3[i * 128:(i + 1) * 128, :], x[:], reads=[x])
            S.op('act', lambda e, x=x: e.activation(out=gjunk[:], in_=x[:], func=AF.Square, accum_out=gss[:]),
                 reads=[x], writes=[gjunk, gss])
            S.op('dve', lambda e: e.tensor_scalar(out=grs[:], in0=gss[:], scalar1=1.0 / D, scalar2=EPS,
                                                  op0=ALU.mult, op1=ALU.add), reads=[gss], writes=[grs])
            S.op('act', lambda e: e.sqrt(out=grs[:], in_=grs[:]), reads=[grs], writes=[grs])
            S.op('dve', lambda e: e.reciprocal(out=grs[:], in_=grs[:]), reads=[grs], writes=[grs])
            S.op('dve', lambda e, x=x: e.scalar_tensor_tensor(out=yo[:], in0=x[:], scalar=grs[:, 0:1], in1=gfin[:],
                                                              op0=ALU.mult, op1=ALU.mult), reads=[x, grs, gfin], writes=[yo])
            if i < NT:
                S.dma('pool', y_p[i * 128:(i + 1) * 128, :], yo[:], reads=[yo])
            else:
                S.dma('pool', y_s[:, :], yo[0:16, :], reads=[yo])

        prep(0)
        for i in range(NTT):
            nxt = record(prep, i + 1) if i + 1 < NTT else []
            dense(i, nxt)
        es_g.close()
        cur[0] = es
        S.finish()
    return nc


_PROGRAM = None
_DEBUG_HOOK = None


def kernel(x_prompt, x_sample, cache_swa_k, cache_swa_v, state_hgrn, cache_mem_k, cache_mem_v,
           mem_prompt, norm_mix, w_in, lb_logits, beta_a, gnorm_b, w_out, norm_cross, norm_mem,
           w_cq, w_mk, w_mv, w_co, norm_ffn, w_pq, peer_k1, peer_k2, peer_u, peer_v, norm_final):
    global _PROGRAM
    f = lambda a: np.ascontiguousarray(np.asarray(a, dtype=np.float32))
    if _PROGRAM is None:
        _PROGRAM = build_program()
    nc = _PROGRAM

    def col(g):
        return f(np.asarray(g).reshape(-1, 128).T)

    common = {
        "w_in": f(w_in[0]), "w_mk": f(w_mk[0]), "w_mv": f(w_mv[0]),
        "g_mix": col(norm_mix[0]), "g_mem": col(norm_mem[0]),
        "w_out": f(w_out[0]), "w_cq": f(w_cq[0]), "w_co": f(w_co[0]),
        "g_outc": col(np.concatenate([np.asarray(beta_a[0]).reshape(-1), np.asarray(gnorm_b[0]).reshape(-1)])),
        "g_cross": col(norm_cross[0]),
        "w_pq": f(w_pq[0]), "g_ffn": col(norm_ffn[0]),
        "g_ffnx": f(np.repeat(np.asarray(norm_ffn[0]).reshape(8, 128).T[:, :, None], 128, axis=2).reshape(128, 1024)),
        "keysT": f(np.stack([np.asarray(peer_k1[0]), np.asarray(peer_k2[0])], axis=1).reshape(16, 128, 128)
                   .transpose(2, 0, 1).reshape(128, 2048)),
        "ut_h": f(np.asarray(peer_u[0]).reshape(128, 128, 8, 128).transpose(1, 3, 2, 0).reshape(128, 128, 1024)),
        "v_h": f(np.asarray(peer_v[0]).reshape(128, 128, 1024).transpose(1, 0, 2)),
        "g_fin": f(np.broadcast_to(np.asarray(norm_final)[None, :], (128, D))),
        "iota16": f(np.broadcast_to(np.arange(16)[None, :], (128, 16))),
        "iota128": f(np.broadcast_to(np.arange(128)[None, :], (128, 128))),
        "ident": np.eye(128, dtype=np.float32),
        "lbl0": f(np.broadcast_to(np.asarray(lb_logits)[0][None, :], (128, 512))),
        "lbl1": f(np.broadcast_to(np.asarray(lb_logits)[1][None, :], (128, 512))),
        "triU": np.triu(np.ones((128, 128), np.float32)),
        "tri4": np.tile(np.triu(np.ones((128, 64), np.float32)), (1, 8)),
        "rowmask": (np.arange(128) < 4).astype(np.float32).reshape(128, 1),
        "bmask": np.kron(np.eye(8, dtype=np.float32), np.ones((1, 66), np.float32)),
        "maskC": np.tile(np.triu(np.ones((128, 128), np.float32)), (1, 8)),
        "maskP": np.tile(np.tril(np.ones((128, 128), np.float32)), (1, 8)),
    }
    in_maps = []
    for c in range(NCORES):
        m = dict(common)
        m["xp"] = f(x_prompt[c])
        m["xs"] = f(np.asarray(x_sample[4 * c:4 * c + 4]).reshape(16, D))
        m["memp"] = f(mem_prompt[c])
        m["st_h"] = f(state_hgrn[0, 4 * c:4 * c + 4])
        m["cmk"] = f(np.asarray(cache_mem_k[0, 4 * c:4 * c + 4]).reshape(4, 256, 512))
        m["cmv"] = f(np.asarray(cache_mem_v[0, 4 * c:4 * c + 4]).reshape(4, 256, 512))
        m["ck"] = f(np.asarray(cache_swa_k[0, 4 * c:4 * c + 4]).reshape(4, 2048, 512))
        m["cv"] = f(np.asarray(cache_swa_v[0, 4 * c:4 * c + 4]).reshape(4, 2048, 512))
        in_maps.append(m)
    if _DEBUG_HOOK is not None:
        return _DEBUG_HOOK(in_maps)
    res = run_bass_kernel_spmd(nc, in_maps, core_ids=list(range(NCORES)))
    R = res.results
    y_prompt = np.stack([R[c]["y_p"] for c in range(NCORES)]).reshape(8, T, D)
    y_sample = np.concatenate([R[c]["y_s"].reshape(4, 4, D) for c in range(NCORES)], axis=0)
    p_k = np.stack([R[c]["o_pk"].reshape(2048, 8, 64) for c in range(NCORES)])[None]
    p_v = np.stack([R[c]["o_pv"].reshape(2048, 8, 64) for c in range(NCORES)])[None]
    p_h = np.stack([R[c]["o_ph"] for c in range(NCORES)])[None]
    p_mk = np.stack([R[c]["o_mk"].reshape(256, 4, 128) for c in range(NCORES)])[None]
    p_mv = np.stack([R[c]["o_mv"].reshape(256, 4, 128) for c in range(NCORES)])[None]
    s_k = np.concatenate([R[c]["o_sk"].reshape(4, 4, 8, 64) for c in range(NCORES)], axis=0)[None]
    s_v = np.concatenate([R[c]["o_sv"].reshape(4, 4, 8, 64) for c in range(NCORES)], axis=0)[None]
    s_h = np.concatenate([R[c]["o_sh"] for c in range(NCORES)], axis=0)[None]
    outs = (y_prompt, y_sample, p_k, p_v, p_h, p_mk, p_mv, s_k, s_v, s_h)
    return tuple(np.ascontiguousarray(o.astype(np.float32)) for o in outs)
```
